# Optimizing a Trainium2 kernel written in Bass

```python
import jax, jax.numpy as jnp
from jax import lax
import numpy as np

D_MODEL = 1024
BATCH = 4
SEQ = 8192
DEPTH = 2

CHUNK = 64
MIX_WIDTH = D_MODEL
POOL_WIDTH = MIX_WIDTH // 2
POOL_WINDOWS = (2, 4, 8, 16)
POOL_GROUPS = len(POOL_WINDOWS)
POOL_GW = POOL_WIDTH // POOL_GROUPS
ATT_WIDTH = MIX_WIDTH - POOL_WIDTH
N_HEADS = 8
HEAD_DIM = ATT_WIDTH // N_HEADS
LEFT_CHUNKS = 8
LEFT = LEFT_CHUNKS * CHUNK
BAND = LEFT + CHUNK
MAX_REL = 128
N_REL = 2 * MAX_REL + 1
IN_WIDTH = POOL_WIDTH + 3 * ATT_WIDTH
N_GROUPS = 4
EXPERTS_PER_GROUP = 8
N_EXPERTS = N_GROUPS * EXPERTS_PER_GROUP
TOP_K = 2
D_EXPERT = D_MODEL // 2
EXPERT_BLOCK = 128
EPS = 1e-6
NEG_INF = -1e30

kernel_name = "hybrid_pool_chunkattn_hmoe_adaln"


def rmsnorm(x, g):
    xf = x.astype(jnp.float32)
    y = xf * lax.rsqrt(jnp.mean(xf * xf, axis=-1, keepdims=True) + EPS)
    return (y * g.astype(jnp.float32)).astype(x.dtype)


def pool_mixer(u, w, scale):
    B, S, _ = u.shape
    uf = u.astype(jnp.float32)
    cs0 = jnp.concatenate([jnp.zeros((B, 1, POOL_WIDTH), jnp.float32), jnp.cumsum(uf, axis=1)], axis=1)
    outs = []
    for gi, win in enumerate(POOL_WINDOWS):
        sl = slice(gi * POOL_GW, (gi + 1) * POOL_GW)
        csg = cs0[..., sl]
        lag = jnp.concatenate([jnp.zeros((B, win - 1, POOL_GW), jnp.float32), csg[:, :S + 1 - win]], axis=1)
        cnt = jnp.minimum(jnp.arange(S) + 1, win).astype(jnp.float32)[None, :, None]
        outs.append((csg[:, 1:] - lag) / cnt - uf[..., sl])
    p = jnp.stack(outs, axis=2).astype(u.dtype)
    p = jnp.einsum('bsgc,gcd->bsgd', p, w).reshape(B, S, POOL_WIDTH)
    return p * scale


def chunk_attention(q, k, v, bias):
    B, S, H, Dh = q.shape
    nc = S // CHUNK
    kp = jnp.pad(k, ((0, 0), (LEFT, 0), (0, 0), (0, 0)))
    vp = jnp.pad(v, ((0, 0), (LEFT, 0), (0, 0), (0, 0)))
    kpos = jnp.arange(BAND)
    sm_scale = HEAD_DIM ** -0.5

    def one_chunk(i):
        s0 = i * CHUNK
        qc = lax.dynamic_slice_in_dim(q, s0, CHUNK, axis=1)
        kc = lax.dynamic_slice_in_dim(kp, s0, BAND, axis=1)
        vc = lax.dynamic_slice_in_dim(vp, s0, BAND, axis=1)
        s = jnp.einsum('bqhd,bkhd->bhqk', qc, kc).astype(jnp.float32) * sm_scale + bias[None]
        valid = kpos >= LEFT - s0
        s = jnp.where(valid[None, None, None, :], s, NEG_INF)
        p = jax.nn.softmax(s, axis=-1).astype(v.dtype)
        return jnp.einsum('bhqk,bkhd->bqhd', p, vc)

    o = lax.map(one_chunk, jnp.arange(nc))
    return o.transpose(1, 0, 2, 3, 4).reshape(B, S, H * Dh)


def hier_moe(h, rg_w, rg_b, re_w, re_b, w_gate, w_up, w_down):
    B, S, D = h.shape
    N = B * S
    t = h.reshape(N, D)
    g_prob = jax.nn.softmax((t @ rg_w + rg_b).astype(jnp.float32), axis=-1)
    g_p, g_idx = lax.top_k(g_prob, 1)
    e_logits = (t @ re_w + re_b).astype(jnp.float32).reshape(N, N_GROUPS, EXPERTS_PER_GROUP)
    e_in = jnp.take_along_axis(e_logits, g_idx[:, :, None], axis=1)[:, 0]
    e_top, e_loc = lax.top_k(e_in, TOP_K)
    e_w = jax.nn.softmax(e_top, axis=-1) * g_p
    e_id = g_idx * EXPERTS_PER_GROUP + e_loc

    flat_e = e_id.reshape(-1)
    flat_tok = jnp.repeat(jnp.arange(N, dtype=jnp.int32), TOP_K)
    flat_w = e_w.reshape(-1)
    order = jnp.argsort(flat_e, stable=True)
    se = flat_e[order]
    counts = jnp.bincount(flat_e, length=N_EXPERTS)
    padded = ((counts + EXPERT_BLOCK - 1) // EXPERT_BLOCK) * EXPERT_BLOCK
    pad_end = jnp.cumsum(padded)
    pad_start = pad_end - padded
    start = jnp.cumsum(counts) - counts
    dest = pad_start[se] + jnp.arange(N * TOP_K) - start[se]
    P = N * TOP_K + N_EXPERTS * EXPERT_BLOCK
    nblk = P // EXPERT_BLOCK
    buf_tok = jnp.full((P,), N, jnp.int32).at[dest].set(flat_tok[order])
    buf_w = jnp.zeros((P,), jnp.float32).at[dest].set(flat_w[order])
    blk_e = jnp.minimum(jnp.searchsorted(pad_end, jnp.arange(nblk) * EXPERT_BLOCK, side='right'),
                        N_EXPERTS - 1)
    t_pad = jnp.concatenate([t, jnp.zeros((1, D), t.dtype)], axis=0)
    xb = t_pad[buf_tok].reshape(nblk, EXPERT_BLOCK, D)

    def expert_block(args):
        xblk, e = args
        return (jax.nn.silu(xblk @ w_gate[e]) * (xblk @ w_up[e])) @ w_down[e]

    yb = lax.map(expert_block, (xb, blk_e)).reshape(P, D)
    yb = yb * buf_w[:, None].astype(yb.dtype)
    out = jnp.zeros((N + 1, D), yb.dtype).at[buf_tok].add(yb)[:N]
    return out.reshape(B, S, D)


def setup_inputs(seed: int = 0) -> dict:
    key = jax.random.key(seed)
    ks = jax.random.split(key, 24)
    f32 = jnp.float32
    L, D = DEPTH, D_MODEL
    nrm = lambda k, shape, s: jax.random.normal(k, shape, f32) * s
    return {
        "x": nrm(ks[0], (BATCH, SEQ, D), 1.0),
        "c": nrm(ks[1], (BATCH, D), 1.0),
        "ada_w": nrm(ks[2], (L, D, 6 * D), 0.5 * D ** -0.5),
        "ada_b": nrm(ks[3], (L, 6 * D), 0.02),
        "norm1_g": 1.0 + nrm(ks[4], (L, D), 0.05),
        "norm2_g": 1.0 + nrm(ks[5], (L, D), 0.05),
        "w_in": nrm(ks[6], (L, D, IN_WIDTH), D ** -0.5),
        "pool_w": nrm(ks[7], (L, POOL_GROUPS, POOL_GW, POOL_GW), POOL_GW ** -0.5),
        "pool_scale": 1.0 + nrm(ks[8], (L, POOL_WIDTH), 0.1),
        "q_norm_g": 1.0 + nrm(ks[9], (L, HEAD_DIM), 0.05),
        "k_norm_g": 1.0 + nrm(ks[10], (L, HEAD_DIM), 0.05),
        "rel_bias": nrm(ks[11], (N_HEADS, N_REL), 0.1),
        "w_out": nrm(ks[12], (L, MIX_WIDTH, D), MIX_WIDTH ** -0.5),
        "router_group_w": nrm(ks[13], (L, D, N_GROUPS), D ** -0.5),
        "router_group_b": nrm(ks[14], (L, N_GROUPS), 0.01),
        "router_expert_w": nrm(ks[15], (L, D, N_EXPERTS), D ** -0.5),
        "router_expert_b": nrm(ks[16], (L, N_EXPERTS), 0.01),
        "moe_w_gate": nrm(ks[17], (L, N_EXPERTS, D, D_EXPERT), D ** -0.5),
        "moe_w_up": nrm(ks[18], (L, N_EXPERTS, D, D_EXPERT), D ** -0.5),
        "moe_w_down": nrm(ks[19], (L, N_EXPERTS, D_EXPERT, D), D_EXPERT ** -0.5),
    }


def reference(x, c, ada_w, ada_b, norm1_g, norm2_g, w_in, pool_w, pool_scale, q_norm_g, k_norm_g,
              rel_bias, w_out, router_group_w, router_group_b, router_expert_w, router_expert_b,
              moe_w_gate, moe_w_up, moe_w_down):
    B, S, D = x.shape
    rel = LEFT + jnp.arange(CHUNK)[:, None] - jnp.arange(BAND)[None, :]
    rel_idx = jnp.clip(rel, -MAX_REL, MAX_REL) + MAX_REL
    bias = rel_bias.astype(jnp.float32)[:, rel_idx]
    c_act = jax.nn.silu(c)
    for l in range(DEPTH):
        mod = (c_act @ ada_w[l] + ada_b[l])[:, None, :]
        sh1, sc1, g1, sh2, sc2, g2 = jnp.split(mod, 6, axis=-1)

        h = rmsnorm(x, norm1_g[l]) * (1.0 + sc1) + sh1
        z = h @ w_in[l]
        u = z[..., :POOL_WIDTH]
        q, k, v = jnp.split(z[..., POOL_WIDTH:], 3, axis=-1)
        q = rmsnorm(q.reshape(B, S, N_HEADS, HEAD_DIM), q_norm_g[l])
        k = rmsnorm(k.reshape(B, S, N_HEADS, HEAD_DIM), k_norm_g[l])
        v = v.reshape(B, S, N_HEADS, HEAD_DIM)
        pool_out = pool_mixer(u, pool_w[l], pool_scale[l])
        att_out = chunk_attention(q, k, v, bias)
        mix = jnp.concatenate([pool_out, att_out], axis=-1) @ w_out[l]
        x = x + g1 * mix

        h2 = rmsnorm(x, norm2_g[l]) * (1.0 + sc2) + sh2
        x = x + g2 * hier_moe(h2, router_group_w[l], router_group_b[l], router_expert_w[l],
                              router_expert_b[l], moe_w_gate[l], moe_w_up[l], moe_w_down[l])
    return x
```

```python
from contextlib import ExitStack

import numpy as np
import concourse.bass as bass
import concourse.mybir as mybir
from concourse.bass_utils import run_bass_kernel_spmd

F32 = mybir.dt.float32
BF16 = mybir.dt.bfloat16
I32 = mybir.dt.int32
AF = mybir.ActivationFunctionType
ALU = mybir.AluOpType
AX = mybir.AxisListType

ENGS = ("sp", "act", "dve", "pool", "pe")
PSUM_KEYS = frozenset(["pT", "pZ0", "pZ1", "B3", "B4", "B5", "B6", "B7"])
D = 1024
EPS = 1e-6
NEXP = 32
RT = 8


class Op:
    __slots__ = ("eng", "fn", "dma", "dkey", "deps", "sig", "idx", "tgt", "waits")

    def __init__(self, eng, fn, dma, dkey):
        self.eng = eng
        self.fn = fn
        self.dma = dma
        self.dkey = dkey
        self.deps = []
        self.sig = False
        self.idx = 0
        self.tgt = 0
        self.waits = []


class PB:
    def __init__(self, nc):
        self.nc = nc
        self.ops = []
        self.last_w = {}
        self.readers = {}
        self.dma_cnt = {}

    def add(self, eng, fn, reads=(), writes=(), dma=False, dkey=None):
        if dma and dkey is None:
            dkey = writes[0]
        op = Op(eng, fn, dma, dkey)
        deps = set()
        for k in reads:
            w = self.last_w.get(k)
            if w is not None:
                deps.add(w)
            if k in PSUM_KEYS:
                for r in self.readers.get(k, ()):
                    if r.eng != eng:
                        deps.add(r)
        for k in writes:
            w = self.last_w.get(k)
            if w is not None:
                deps.add(w)
            for r in self.readers.get(k, ()):
                deps.add(r)
        op.deps = list(deps)
        for k in reads:
            self.readers.setdefault(k, []).append(op)
        for k in writes:
            self.last_w[k] = op
            self.readers[k] = []
        if dma:
            self.dma_cnt[dkey] = self.dma_cnt.get(dkey, 0) + 1
            op.tgt = 16 * self.dma_cnt[dkey]
        self.ops.append(op)
        return op

    def barrier(self):
        allkeys = list(set(self.last_w.keys()) | set(self.readers.keys()))
        self.add("sp", lambda e: e.nop(), reads=[], writes=allkeys + ["__bar"])
        for eng in ENGS:
            self.add(eng, lambda e: e.nop(), reads=["__bar"], writes=["__bar_" + eng])
        self.last_w = {k: v for k, v in self.last_w.items() if k.startswith("__bar")}
        self.readers = {k: v for k, v in self.readers.items() if k.startswith("__bar")}

    def emit(self):
        nc = self.nc
        for op in self.ops:
            for d in op.deps:
                if not d.dma:
                    d.sig = True
        cnt = {e: 0 for e in ENGS}
        for op in self.ops:
            if not op.dma and op.sig:
                cnt[op.eng] += 1
                op.idx = cnt[op.eng]
        dkeys = sorted(self.dma_cnt.keys())
        with ExitStack() as st:
            esem = {e: st.enter_context(nc.semaphore("es_" + e)) for e in ENGS}
            dsem = {k: st.enter_context(nc.semaphore("ds%d" % i)) for i, k in enumerate(dkeys)}
            waited = {e: {} for e in ENGS}
            for op in self.ops:
                need = {}
                for d in op.deps:
                    if d.dma:
                        key, val = ("d", d.dkey), d.tgt
                    else:
                        if d.eng == op.eng and op.eng == "pe":
                            continue
                        key, val = ("e", d.eng), d.idx
                    if need.get(key, 0) < val:
                        need[key] = val
                w = waited[op.eng]
                for key, val in need.items():
                    if w.get(key, 0) < val:
                        w[key] = val
                        op.waits.append((dsem[key[1]] if key[0] == "d" else esem[key[1]], val))
            block = st.enter_context(nc.Block())

            def run(engname):
                def body(e):
                    for op in self.ops:
                        if op.eng != engname:
                            continue
                        for (s, v) in op.waits:
                            e.wait_ge(s, v)
                        ins = op.fn(e)
                        if op.dma:
                            ins.then_inc(dsem[op.dkey], 16)
                        elif op.sig:
                            ins.then_inc(esem[engname], 1)
                return body

            block.sync(run("sp"))
            block.scalar(run("act"))
            block.vector(run("dve"))
            block.gpsimd(run("pool"))
            block.tensor(run("pe"))


class Cfg:
    def __init__(self, nkv=4, nfh=0, nm=32, cap=512):
        self.NKV, self.NFH, self.NM, self.CAP = nkv, nfh, nm, cap
        self.NTE = nkv + nfh + nm
        self.NTL = nfh + nm
        self.NST = self.NTE // 4
        self.CT = cap // 128
        self.NTOK = self.NTL * 128
        self.TRASH = 2 * self.NTOK + cap
        self.XSR = self.TRASH + 2 * max(nfh, 1) * 128
        self.NOW = -(-2 * self.NTOK // cap)
        self.NTHR = -(-self.NTOK // cap)
        assert self.NTE % 4 == 0 and nkv % 4 == 0 and nfh % 4 == 0 and cap % 128 == 0


PERL = frozenset(["ada_w", "ada_b", "n1c", "n2c", "w_in", "w_out", "pool_w", "pscale", "gq", "gk", "wr", "br", "wg", "wu", "wd"])
TRC = 4


class _Ctx:
    pass


def build_program(cfgs, debug=False):
    nc = bass.Bass("TRN2", target_bir_lowering=False)
    ctx = _Ctx()
    ctx.nc, ctx.memo, ctx.p, ctx.nl = nc, {}, PB(nc), len(cfgs)
    with ExitStack() as st:
        ctx.st = st
        for li, cfg in enumerate(cfgs):
            _emit_layer(ctx, li, cfg, debug)
        ctx.p.emit()
    return nc


def build_layer(cfg, debug=False):
    return build_program([cfg], debug)


def _emit_layer(ctx, li, cfg, debug=False):
    nc, st, memo = ctx.nc, ctx.st, ctx.memo
    last = li == ctx.nl - 1
    NTE, NTL, NST, NKV, NFH, CT, CAP = cfg.NTE, cfg.NTL, cfg.NST, cfg.NKV, cfg.NFH, cfg.CT, cfg.CAP

    def din(name, shape, dt=F32):
        nm = name + ("_%d" % li if name in PERL else "")
        if nm not in memo:
            memo[nm] = nc.dram_tensor(nm, list(shape), dt, kind="ExternalInput").ap()
        return memo[nm]

    def dscr(name, shape, dt, kind="Internal"):
        if name not in memo:
            memo[name] = nc.dram_tensor(name, list(shape), dt, kind=kind).ap()
        return memo[name]

    xe = din("xe", [NTE * 128, D]) if li == 0 else memo["x1_%d" % (li - 1)]
    cT = din("cT", [128, 8])
    ada_w = din("ada_w", [D, 6 * D])
    ada_b = din("ada_b", [1, 6 * D])
    n1c = din("n1c", [128, 8])
    n2c = din("n2c", [128, 8])
    w_in = din("w_in", [D, 2048])
    w_out = din("w_out", [D, D])
    pool_w = din("pool_w", [128, 4, 128])
    pscale = din("pscale", [128, 4])
    gq = din("gq", [128, 1])
    gk = din("gk", [128, 1])
    btab = din("btab", [128, 8, 5, 128])
    bmask = din("bmask", [128, 5, 128])
    wr = din("wr", [D, 36])
    br = din("br", [128, 36])
    wg = din("wg", [NEXP * D, 512])
    wu = din("wu", [NEXP * D, 512])
    wd = din("wd", [NEXP * 512, D])
    hv = din("hv", [128, 1])
    nhv = din("nhv", [128, 1])
    invc = din("invc", [128, 4, 16])
    tri = din("tri", [128, 128])
    iot = din("iot", [128, 4])
    trashi = din("trashi", [128, 2 * TRC])
    thr = din("thr", [128, 16])
    wv = din("wv", [128, 32])
    ev = din("ev", [128, 32])
    iot8 = din("iot8", [128, 8])
    if last:
        xo = nc.dram_tensor("xo", [NTL * 128, D], F32, kind="ExternalOutput").ap()
    else:
        xo = dscr("x1_%d" % li, [NTL * 128, D], F32)
    xmid = dscr("xmid", [NTL * 128, D], F32, kind="ExternalOutput" if debug else "Internal")
    if debug:
        dbg_logits = nc.dram_tensor("dbg_logits", [128, NTL, 36], F32, kind="ExternalOutput").ap()
        dbg_w = nc.dram_tensor("dbg_w", [128, 2, NTL], F32, kind="ExternalOutput").ap()
        dbg_slot = nc.dram_tensor("dbg_slot", [128, 2, NTL], I32, kind="ExternalOutput").ap()
    xn2s = dscr("xn2s", [NTL * 128, D], BF16)
    xs = dscr("xs", [cfg.XSR, D], BF16)
    ys = dscr("ys", [cfg.XSR, D], F32)

    if True:
        def T(name, shape, dt=F32):
            if name in memo:
                t, shp = memo[name]
                if list(shp) != list(shape):
                    assert len(shp) == len(shape) and shape[1] <= shp[1] and list(shp[2:]) == list(shape[2:]), (name, shp, shape)
                    return t[:, 0:shape[1]]
                return t
            t = st.enter_context(nc.sbuf_tensor(name, list(shape), dt))
            memo[name] = (t, list(shape))
            return t

        def PS(name, shape, dt=F32):
            if name not in memo:
                memo[name] = st.enter_context(nc.psum_tensor(name, list(shape), dt))
            return memo[name]

        ident = T("ident", [128, 128], BF16)
        identf = T("identf", [128, 128])
        ones_bf = T("ones_bf", [128, 128], BF16)
        blk1 = T("blk1", [128, 128], BF16)
        tri_bf = T("tri_bf", [128, 128], BF16)
        onesf = T("onesf", [1, 128])
        epsc = T("epsc", [128, 1])
        eps64 = T("eps64", [128, 1])
        cact = T("cact", [128, 8], BF16)
        ctf = T("ctf", [128, 8])
        n1t = T("n1t", [128, 8])
        n2t = T("n2t", [128, 8])
        modc = T("modc", [128, 4, 8])
        mul1c = T("mul1c", [128, 8])
        mul2c = T("mul2c", [128, 8])
        add1b = T("add1b", [128, 8], BF16)
        g2bc = T("g2bc", [128, D])
        bzc = T("bzc", [128, 12])
        bzv = T("bzv", [128, 512])
        bzvm = T("bzvm", [128, 512])
        pw_bf = T("pw_bf", [128, 4, 128], BF16)
        psc = T("psc", [128, 4])
        gqk = T("gqk", [128, 1])
        gkt = T("gkt", [128, 1])
        BT = T("BT", [128, 8, 5, 128], BF16)
        wr_f = T("wr_f", [128, 8, 36])
        wr_bf = T("wr_bf", [128, 8, 36], BF16)
        wr_raw = T("wr_raw", [128, 8, 36], BF16)
        biasR = T("biasR", [128, 36])
        hvt = T("hvt", [128, 1])
        nhvt = T("nhvt", [128, 1])
        invct = T("invct", [128, 4, 16])
        iott = T("iott", [128, 4])
        trt = T("trt", [128, 2 * TRC])
        thrt = T("thrt", [128, 16])
        wvt = T("wvt", [128, 32])
        evt = T("evt", [128, 32])
        iot8t = T("iot8t", [128, 8])
        ssr = T("ssr", [128, 8])
        rst = T("rst", [128, 8])
        logits = T("logits", [128, NTL, 36])
        w1g = T("w1g", [128, NTL])
        w2g = T("w2g", [128, NTL])
        slot1 = T("slot1", [128, NTL], I32)
        slot2 = T("slot2", [128, NTL], I32)
        widx = T("widx", [128, NEXP, CT], I32)

        AF_WORDS = 14400
        AB_WORDS = 57920
        arf = T("arf", [128, AF_WORDS])
        arb = T("arb", [128, AB_WORDS], BF16)

        class Carver:
            def __init__(self, t, n):
                self.t, self.n, self.off = t, n, 0

            def take(self, *shape):
                n = int(np.prod(shape))
                a = self.t[:, self.off:self.off + n]
                self.off += n
                assert self.off <= self.n, (self.off, self.n)
                if len(shape) == 2:
                    return a.rearrange("p (a b) -> p a b", a=shape[0])
                if len(shape) == 3:
                    return a.rearrange("p (a b c) -> p a b c", a=shape[0], b=shape[1])
                return a

        pT = PS("pT", [128, 1024], BF16)
        pZ = PS("pZ", [128, 2, 512])
        pC = PS("B3", [128, 512])
        pS = PS("pS", [128, 1536])
        pV = PS("B7", [128, 512])

        p = ctx.p
        A = p.add

        def dma(eng, out, in_, reads, writes, dkey=None):
            return A(eng, lambda e: e.dma_start(out=out, in_=in_), reads=reads, writes=writes, dma=True, dkey=dkey)

        A("pool", lambda e: e.memset(identf[:], 0.0), writes=["identf"])
        A("pool", lambda e: e.affine_select(out=identf[:], in_=identf[:], pattern=[[-1, 128]], compare_op=ALU.not_equal,
                                            fill=1.0, base=0, channel_multiplier=1), reads=["identf"], writes=["identf"])
        A("dve", lambda e: e.tensor_copy(out=ident[:], in_=identf[:]), reads=["identf"], writes=["ident"])
        A("pool", lambda e: e.memset(ones_bf[:], 1.0), writes=["ones_bf"])
        A("pool", lambda e: e.memset(blk1[:], 0.0), writes=["blk1"])
        A("pool", lambda e: e.memset(blk1[0:64, 0:64], 1.0), reads=["blk1"], writes=["blk1"])
        A("pool", lambda e: e.memset(blk1[64:128, 64:128], 1.0), reads=["blk1"], writes=["blk1"])
        A("pool", lambda e: e.memset(onesf[:], 1.0), writes=["onesf"])
        A("pool", lambda e: e.memset(epsc[:], EPS), writes=["epsc"])
        A("pool", lambda e: e.memset(eps64[:], 64 * EPS), writes=["eps64"])
        for (dst, src, k) in ((ctf, cT, "ctf"), (n1t, n1c, "n1t"), (n2t, n2c, "n2t"), (psc, pscale, "psc"), (gqk, gq, "gqk"),
                              (gkt, gk, "gkt"), (hvt, hv, "hvt"), (nhvt, nhv, "nhvt"), (invct, invc, "invct"),
                              (iott, iot, "iott"), (trt, trashi, "trt"), (thrt, thr, "thrt"), (wvt, wv, "wvt"), (evt, ev, "evt"), (iot8t, iot8, "iot8t"), (biasR, br, "biasR"), (identf, tri, "identf")):
            dma("sp", dst[:], src, [], [k])
        A("dve", lambda e: e.tensor_copy(out=tri_bf[:], in_=identf[:]), reads=["identf"], writes=["tri_bf"])
        A("dve", lambda e: e.tensor_mul(out=gqk[:], in0=gqk[:], in1=gkt[:]), reads=["gqk", "gkt"], writes=["gqk"])
        dma("pool", pw_bf[:], pool_w, [], ["pw_bf"])
        A("act", lambda e: e.activation(out=cact[:], in_=ctf[:], func=AF.Silu), reads=["ctf"], writes=["cact"])

        cf = Carver(arf, AF_WORDS)
        cb_ = Carver(arb, AB_WORDS)
        g1bc = cf.take(D)
        modrow = cf.take(6 * D)[0:1, :]
        adab = cf.take(6 * D)[0:1, :]
        stage = [cb_.take(8, 1536) for _ in range(2)]
        zf = cf.take(D)
        zb = cb_.take(D)
        A("pool", lambda e: e.memset(zf[:], 0.0), writes=["zf"])
        A("pool", lambda e: e.memset(zb[:], 0.0), writes=["zb"])
        for r0 in range(2 * cfg.NM * 128, cfg.TRASH, 128):
            dma("sp", xs[r0:r0 + 128, :], zb[:], ["zb"], ["xs_z%d" % r0], dkey="zinit")
        for r0 in range(cfg.TRASH, cfg.TRASH + 2 * NFH * 128, 128):
            dma("sp", ys[r0:r0 + 128, :], zf[:], ["zf"], ["ys_z%d" % r0], dkey="zinit")
        dma("sp", adab, ada_b, [], ["adab"])
        for g in range(4):
            sb = stage[g % 2]
            dma("pool", sb[:], ada_w[:, g * 1536:(g + 1) * 1536].rearrange("(k p) n -> p k n", p=128), [], ["stage%d" % (g % 2)])
            for cbk in range(3):
                col = g * 1536 + cbk * 512
                bank = cbk % 2
                for kc in range(8):
                    A("pe", lambda e, sb=sb, kc=kc, cbk=cbk, bank=bank: e.matmul(
                        pZ[0:1, bank, :], lhsT=cact[:, kc:kc + 1], rhs=sb[:, kc, cbk * 512:(cbk + 1) * 512],
                        start=(kc == 0), stop=(kc == 7)), reads=["cact", "stage%d" % (g % 2)], writes=["pZ%d" % bank])
                A("dve", lambda e, col=col, bank=bank: e.tensor_tensor(out=modrow[:, col:col + 512], in0=pZ[0:1, bank, :],
                                                                       in1=adab[:, col:col + 512], op=ALU.add),
                  reads=["pZ%d" % bank, "adab"], writes=["modrow"])
        for vi, base in enumerate((0, D, 3 * D, 4 * D)):
            for kc in range(8):
                A("pe", lambda e, vi=vi, base=base, kc=kc: e.matmul(
                    pC[:, vi * 8 + kc: vi * 8 + kc + 1], lhsT=modrow[:, base + kc * 128: base + (kc + 1) * 128],
                    rhs=onesf[:, 0:1], start=True, stop=True), reads=["modrow", "onesf"], writes=["B3"])
        A("dve", lambda e: e.tensor_copy(out=modc[:], in_=pC[:, 0:32].rearrange("p (a b) -> p a b", a=4)), reads=["B3"], writes=["modc"])
        A("dve", lambda e: e.scalar_tensor_tensor(out=mul1c[:], in0=modc[:, 1, :], scalar=1.0, in1=n1t[:], op0=ALU.add, op1=ALU.mult),
          reads=["modc", "n1t"], writes=["mul1c"])
        A("dve", lambda e: e.scalar_tensor_tensor(out=mul2c[:], in0=modc[:, 3, :], scalar=1.0, in1=n2t[:], op0=ALU.add, op1=ALU.mult),
          reads=["modc", "n2t"], writes=["mul2c"])
        A("dve", lambda e: e.tensor_copy(out=add1b[:], in_=modc[:, 0, :]), reads=["modc"], writes=["add1b"])
        for (dst, base, k) in ((g1bc, 2 * D, "g1bc"), (g2bc, 5 * D, "g2bc")):
            for hb in range(2):
                A("pe", lambda e, base=base, hb=hb: e.matmul(pZ[:, hb, :], lhsT=onesf[:, :], rhs=modrow[:, base + hb * 512: base + (hb + 1) * 512],
                                                            start=True, stop=True), reads=["modrow", "onesf"], writes=["pZ%d" % hb])
                A("dve", lambda e, dst=dst, hb=hb: e.tensor_copy(out=dst[:, hb * 512:(hb + 1) * 512], in_=pZ[:, hb, :]),
                  reads=["pZ%d" % hb], writes=[k])
        p.barrier()

        cf = Carver(arf, AF_WORDS)
        cb_ = Carver(arb, AB_WORDS)
        w_in_bf = cb_.take(8, 2048)
        w_out_bf = cb_.take(8, D)
        g1bc = cf.take(D)
        add2rep = cb_.take(8, 128)
        wst = [cf.take(2, 2048) for _ in range(2)]
        wraw = cb_.take(8, 2048)
        bzrow = cf.take(2048)[0:1, :]
        bzrow_b = cb_.take(512)[0:1, :]
        for pc in range(4):
            sbf = wst[pc % 2]
            dma("sp", sbf[:], w_in[pc * 256:(pc + 1) * 256, :].rearrange("(k p) n -> p k n", p=128), [], ["wst%d" % (pc % 2)])
            for kk in range(2):
                kc = pc * 2 + kk
                A("dve", lambda e, sbf=sbf, kk=kk, kc=kc: e.tensor_scalar(out=w_in_bf[:, kc, :], in0=sbf[:, kk, :], scalar1=mul1c[:, kc:kc + 1],
                                                                       scalar2=None, op0=ALU.mult),
                  reads=["wst%d" % (pc % 2), "mul1c"], writes=["w_in_bf"])
                A("act", lambda e, sbf=sbf, kk=kk, kc=kc: e.activation(out=wraw[:, kc, :], in_=sbf[:, kk, :], func=AF.Copy),
                  reads=["wst%d" % (pc % 2)], writes=["wraw"])
        for cbk in range(4):
            for kc in range(8):
                A("pe", lambda e, cbk=cbk, kc=kc: e.matmul(pZ[0:1, cbk % 2, :], lhsT=add1b[:, kc:kc + 1], rhs=wraw[:, kc, cbk * 512:(cbk + 1) * 512],
                                                          start=(kc == 0), stop=(kc == 7)), reads=["add1b", "wraw"], writes=["pZ%d" % (cbk % 2)])
            A("dve", lambda e, cbk=cbk: e.tensor_copy(out=bzrow[:, cbk * 512:(cbk + 1) * 512], in_=pZ[0:1, cbk % 2, :]),
              reads=["pZ%d" % (cbk % 2)], writes=["bzrow"])
        for oc in range(12):
            A("pe", lambda e, oc=oc: e.matmul(pC[:, oc:oc + 1], lhsT=bzrow[:, oc * 128:(oc + 1) * 128], rhs=onesf[:, 0:1], start=True, stop=True),
              reads=["bzrow", "onesf"], writes=["B3"])
        A("dve", lambda e: e.tensor_copy(out=bzc[:], in_=pC[:, 0:12]), reads=["B3"], writes=["bzc"])
        A("pe", lambda e: e.matmul(pZ[:, 0, :], lhsT=onesf[:, :], rhs=bzrow[:, 1536:2048], start=True, stop=True),
          reads=["bzrow", "onesf"], writes=["pZ0"])
        A("dve", lambda e: e.tensor_copy(out=bzv[:], in_=pZ[:, 0, :]), reads=["pZ0"], writes=["bzv"])
        A("dve", lambda e: e.tensor_scalar(out=bzvm[:], in0=bzv[:], scalar1=hvt[:, 0:1], scalar2=None, op0=ALU.mult),
          reads=["bzv", "hvt"], writes=["bzvm"])
        wost = [wst[0][:, :, 0:D], wst[1][:, :, 0:D]]
        for pc in range(4):
            sbf = wost[pc % 2]
            dma("sp", sbf[:], w_out[pc * 256:(pc + 1) * 256, :].rearrange("(k p) n -> p k n", p=128), [], ["wst%d" % (pc % 2)])
            for kk in range(2):
                kc = pc * 2 + kk
                A("dve", lambda e, sbf=sbf, kk=kk, kc=kc: e.tensor_tensor(out=w_out_bf[:, kc, :], in0=sbf[:, kk, :], in1=g1bc[:], op=ALU.mult),
                  reads=["wst%d" % (pc % 2), "g1bc"], writes=["w_out_bf"])
        btf = cf.take(5, 128)
        pen = cf.take(5, 128)
        mk = cf.take(5, 128)
        dma("sp", mk[:], bmask, [], ["mk"])
        A("dve", lambda e: e.tensor_scalar(out=pen[:], in0=mk[:], scalar1=3750.0, scalar2=-3750.0, op0=ALU.mult, op1=ALU.add),
          reads=["mk"], writes=["pen"])
        for h in range(8):
            dma("sp", btf[:], btab[:, h, :, :], [], ["btf"])
            A("dve", lambda e: e.tensor_tensor(out=btf[:], in0=btf[:], in1=mk[:], op=ALU.mult), reads=["btf", "mk"], writes=["btf"])
            A("dve", lambda e, h=h: e.scalar_tensor_tensor(out=BT[:, h, :, :], in0=btf[:], scalar=0.125, in1=pen[:], op0=ALU.mult, op1=ALU.add),
              reads=["btf", "pen"], writes=["BT"])
        dma("sp", wr_f[:], wr.rearrange("(k p) n -> p k n", p=128), [], ["wr_f"])
        A("dve", lambda e: e.tensor_copy(out=wr_raw[:], in_=wr_f[:]), reads=["wr_f"], writes=["wr_raw"])
        for kc in range(8):
            A("dve", lambda e, kc=kc: e.tensor_scalar(out=wr_bf[:, kc, :], in0=wr_f[:, kc, :], scalar1=mul2c[:, kc:kc + 1], scalar2=None, op0=ALU.mult),
              reads=["wr_f", "mul2c"], writes=["wr_bf"])
            A("dve", lambda e, kc=kc: e.tensor_copy(out=add2rep[:, kc, :], in_=modc[:, 2, kc:kc + 1].to_broadcast([128, 128])),
              reads=["modc"], writes=["add2rep"])
        for kc in range(8):
            A("pe", lambda e, kc=kc: e.matmul(pC[:, 0:36], lhsT=add2rep[:, kc, :], rhs=wr_raw[:, kc, :], start=(kc == 0), stop=(kc == 7)),
              reads=["add2rep", "wr_raw"], writes=["B3"])
        A("dve", lambda e: e.tensor_tensor(out=biasR[:], in0=pC[:, 0:36], in1=biasR[:], op=ALU.add), reads=["B3", "biasR"], writes=["biasR"])
        p.barrier()

        cf = Carver(arf, AF_WORDS)
        cb_ = Carver(arb, AB_WORDS)
        w_in_bf = cb_.take(8, 2048)
        w_out_bf = cb_.take(8, D)
        xin = [cf.take(D) for _ in range(2)]
        xr = [cf.take(D) for _ in range(2)]
        xmd = [cf.take(D) for _ in range(2)]
        qf = [cf.take(512) for _ in range(2)]
        rq = [cf.take(512) for _ in range(2)]
        uT = [[cf.take(528) for _ in range(4)] for _ in range(2)]
        ptmp = [cf.take(528) for _ in range(2)]
        rden = cf.take(2, 4)
        xn = [cb_.take(D) for _ in range(2)]
        hT_ = cb_.take(8, 512)
        hT = [hT_, hT_]
        kT = cb_.take(4, RT * 128)
        Vr = cb_.take(RT, 8 * 65).rearrange("p r (h d) -> p r h d", h=8)
        qTm = [cb_.take(4, 512) for _ in range(2)]
        sq = [cb_.take(512) for _ in range(2)]
        pTt = cb_.take(4, 512)
        mixT_ = cb_.take(8, 512)
        mixT = [mixT_, mixT_]
        PTb = [cb_.take(2, 640) for _ in range(2)]
        att = [cb_.take(512) for _ in range(2)]
        xn2 = [cb_.take(D) for _ in range(2)]
        xn2T = [cb_.take(8, 128) for _ in range(2)]

        for b in range(2):
            for g in range(4):
                A("pool", lambda e, b=b, g=g: e.memset(uT[b][g][:, 0:16], 0.0), writes=["uT%d%d" % (b, g)])
        A("pool", lambda e: e.memset(qTm[0][64:128, :, :], 0.0), writes=["qT"])
        A("pool", lambda e: e.memset(qTm[1][0:64, :, :], 0.0), writes=["qT"])

        SSOFF = (0, 640)
        PVR = ((pS, 1280), (pV, 0), (pV, 256))
        HG = ((0, 1, 2), (3, 4, 5), (6, 7))

        def norm_and_transpose(src, srckey, sl, dstT, dstTkeys, dstcols, xnbuf, xnkey, store_to=None, scale_eng="dve", defer=False):
            A("act", lambda e: e.activation(out=xnbuf[:], in_=src, func=AF.Square, accum_out=ssr[:, sl:sl + 1]),
              reads=[srckey], writes=[xnkey, "ssr%d" % sl])
            A("act", lambda e: e.activation(out=rst[:, sl:sl + 1], in_=ssr[:, sl:sl + 1], func=AF.Ln, scale=1.0 / D, bias=epsc[:]),
              reads=["ssr%d" % sl, "epsc"], writes=["rst%d" % sl])
            A("act", lambda e: e.activation(out=rst[:, sl:sl + 1], in_=rst[:, sl:sl + 1], func=AF.Exp, scale=-0.5),
              reads=["rst%d" % sl], writes=["rst%d" % sl])
            if scale_eng == "dve":
                A("dve", lambda e: e.tensor_scalar(out=xnbuf[:], in0=src, scalar1=rst[:, sl:sl + 1], scalar2=None, op0=ALU.mult),
                  reads=[srckey, "rst%d" % sl], writes=[xnkey])
            else:
                A("act", lambda e: e.activation(out=xnbuf[:], in_=src, func=AF.Copy, scale=rst[:, sl:sl + 1]),
                  reads=[srckey, "rst%d" % sl], writes=[xnkey])
            if store_to is not None:
                dma("sp", store_to, xnbuf[:], [xnkey], ["xn2s"], dkey="st_" + xnkey)

            def part_b():
                for kc in range(8):
                    A("pe", lambda e, kc=kc: e.transpose(out=pT[:, kc * 128:(kc + 1) * 128], in_=xnbuf[:, kc * 128:(kc + 1) * 128], identity=ident[:]),
                      reads=[xnkey, "ident"], writes=["pT"])
                A("dve", lambda e: e.tensor_copy(out=dstT[:, :, dstcols], in_=pT[:].rearrange("p (a b) -> p a b", a=8)),
                  reads=["pT"], writes=dstTkeys)
            if defer:
                return part_b
            part_b()

        def in_chunk(s, oc, ub, slot0, halo_st, full_st):
            bank = oc % 2
            zk = "pZ%d" % bank
            kslots = ["kT%d" % (slot0 + i) for i in range(4)]
            for kc in range(8):
                A("pe", lambda e, kc=kc: e.matmul(pZ[:, bank, :], lhsT=w_in_bf[:, kc, oc * 128:(oc + 1) * 128], rhs=hT_[:, kc, :],
                                                  start=(kc == 0), stop=(kc == 7)), reads=["w_in_bf", "hT"], writes=[zk])
            if oc < 4:
                g = oc
                if halo_st:
                    A("dve", lambda e: e.tensor_scalar(out=uT[ub][g][:, 16:528], in0=pZ[:, bank, :], scalar1=bzc[:, oc:oc + 1],
                                                       scalar2=hvt[:, 0:1], op0=ALU.add, op1=ALU.mult),
                      reads=[zk, "bzc", "hvt"], writes=["uT%d%d" % (ub, g)])
                else:
                    A("dve", lambda e: e.tensor_scalar(out=uT[ub][g][:, 16:528], in0=pZ[:, bank, :], scalar1=bzc[:, oc:oc + 1],
                                                       scalar2=None, op0=ALU.add),
                      reads=[zk, "bzc"], writes=["uT%d%d" % (ub, g)])
                return
            isq = oc < 8
            c = (oc - 4) % 4
            tb = oc % 2
            A("dve", lambda e: e.tensor_scalar(out=qf[tb][:], in0=pZ[:, bank, :], scalar1=bzc[:, oc:oc + 1], scalar2=None, op0=ALU.add),
              reads=[zk, "bzc"], writes=["qf%d" % tb])
            A("act", lambda e: e.activation(out=sq[tb][:], in_=qf[tb][:], func=AF.Square),
              reads=["qf%d" % tb], writes=["sq%d" % tb])
            A("pe", lambda e: e.matmul(pC[:], lhsT=blk1[:], rhs=sq[tb][:], start=True, stop=True), reads=["blk1", "sq%d" % tb], writes=["B3"])
            A("act", lambda e: e.activation(out=rq[tb][:], in_=pC[:], func=AF.Ln, bias=eps64[:]), reads=["B3", "eps64"], writes=["rq%d" % tb])
            A("act", lambda e: e.activation(out=rq[tb][:], in_=rq[tb][:], func=AF.Exp, scale=-0.5), reads=["rq%d" % tb], writes=["rq%d" % tb])
            if isq:
                A("dve", lambda e: e.tensor_tensor(out=qTm[0][0:64, c, :], in0=qf[tb][0:64, :], in1=rq[tb][0:64, :], op=ALU.mult),
                  reads=["qf%d" % tb, "rq%d" % tb], writes=["qT"])
                A("dve", lambda e: e.tensor_tensor(out=qTm[1][64:128, c, :], in0=qf[tb][64:128, :], in1=rq[tb][64:128, :], op=ALU.mult),
                  reads=["qf%d" % tb, "rq%d" % tb], writes=["qT"])
            else:
                A("dve", lambda e: e.scalar_tensor_tensor(out=kT[:, c, slot0 * 128:(slot0 + 4) * 128], in0=qf[tb][:], scalar=gqk[:, 0:1],
                                                          in1=rq[tb][:], op0=ALU.mult, op1=ALU.mult),
                  reads=["qf%d" % tb, "rq%d" % tb, "gqk"], writes=kslots)

        def v_tile(s, i, halo_st):
            te = 4 * s + i
            sl = te % RT
            bank = i % 2
            for kc in range(8):
                A("pe", lambda e, kc=kc: e.matmul(pZ[:, bank, :], lhsT=hT_[:, kc, i * 128:(i + 1) * 128], rhs=w_in_bf[:, kc, 1536:2048],
                                                  start=(kc == 0), stop=(kc == 7)), reads=["w_in_bf", "hT"], writes=["pZ%d" % bank])
            zv = pZ[:, bank, :].rearrange("p (h d) -> p h d", h=8)
            if halo_st:
                A("dve", lambda e: e.scalar_tensor_tensor(out=Vr[:, sl, :, 0:64], in0=zv, scalar=hvt[:, 0:1],
                                                          in1=bzvm[:].rearrange("p (h d) -> p h d", h=8), op0=ALU.mult, op1=ALU.add),
                  reads=["pZ%d" % bank, "hvt", "bzvm"], writes=["V%d" % sl])
                A("pool", lambda e: e.tensor_copy(out=Vr[:, sl, :, 64:65], in_=hvt[:, 0:1].unsqueeze(1).to_broadcast([128, 8, 1])),
                  reads=["hvt"], writes=["V%d" % sl])
            else:
                A("dve", lambda e: e.tensor_tensor(out=Vr[:, sl, :, 0:64], in0=zv, in1=bzv[:].rearrange("p (h d) -> p h d", h=8), op=ALU.add),
                  reads=["pZ%d" % bank, "bzv"], writes=["V%d" % sl])
                A("pool", lambda e: e.memset(Vr[:, sl, :, 64:65], 1.0), writes=["V%d" % sl])

        def pool_group(g, ub, first_main):
            U = uT[ub][g]
            uk = "uT%d%d" % (ub, g)
            cur, curk = U, uk
            sh = 1
            for stp in range(g + 1):
                dstb = ptmp[stp % 2]
                dk = "ptmp%d" % (stp % 2)
                lo = 2 * sh - 1
                A("pool", lambda e, cur=cur, dstb=dstb, lo=lo, sh=sh: e.tensor_tensor(out=dstb[:, lo:528], in0=cur[:, lo:528], in1=cur[:, lo - sh:528 - sh], op=ALU.add),
                  reads=[curk], writes=[dk])
                cur, curk = dstb, dk
                sh *= 2
            w = 2 ** (g + 1)
            fin = cur
            A("dve", lambda e: e.scalar_tensor_tensor(out=pTt[:, g, :], in0=fin[:, 16:528], scalar=1.0 / w, in1=U[:, 16:528],
                                                      op0=ALU.mult, op1=ALU.subtract),
              reads=[curk, uk], writes=["pTt%d" % g])
            if first_main:
                A("pool", lambda e: e.tensor_tensor(out=fin[:, 0:16], in0=fin[:, 16:32], in1=invct[:, g, :], op=ALU.mult),
                  reads=[curk, "invct"], writes=[curk])
                A("pool", lambda e: e.tensor_tensor(out=pTt[:, g, 0:16], in0=fin[:, 0:16], in1=U[:, 16:32], op=ALU.subtract),
                  reads=[curk, uk], writes=["pTt%d" % g])
            A("pe", lambda e: e.matmul(pC[:], lhsT=pw_bf[:, g, :], rhs=pTt[:, g, :], start=True, stop=True), reads=["pw_bf", "pTt%d" % g], writes=["B3"])
            A("act", lambda e: e.activation(out=mixT_[:, g, :], in_=pC[:], func=AF.Copy, scale=psc[:, g:g + 1]),
              reads=["B3", "psc"], writes=["mixT"])

        def attn_pair(te, i, pr, ab):
            c = pr
            pb2 = pr % 2
            PTp = PTb[pb2]
            ptk = "PT%d" % pb2
            for hh in range(2):
                pb = 64 * hh
                h = 2 * pr + hh
                for t in range(4):
                    ksl = (te - 4 + t) % RT
                    A("pe", lambda e, t=t, ksl=ksl, pb=pb, hh=hh: e.matmul(
                        pS[:, hh * 512 + t * 128: hh * 512 + (t + 1) * 128], lhsT=kT[:, c, ksl * 128:(ksl + 1) * 128],
                        rhs=qTm[hh][:, c, i * 128:(i + 1) * 128], start=True, stop=False),
                      reads=["kT%d" % ksl, "qT"], writes=["B%d" % (4 + hh)])
                    A("pe", lambda e, t=t, h=h, hh=hh: e.matmul(pS[:, hh * 512 + t * 128: hh * 512 + (t + 1) * 128], lhsT=BT[:, h, t, :], rhs=ident[:],
                                                                start=False, stop=True), reads=["BT", "ident"], writes=["B%d" % (4 + hh)])
            ksl4 = te % RT
            for hh in range(2):
                pb = 64 * hh
                h = 2 * pr + hh
                A("pe", lambda e, pb=pb, hh=hh: e.matmul(
                    pS[:, 1024 + hh * 128: 1024 + (hh + 1) * 128], lhsT=kT[:, c, ksl4 * 128:(ksl4 + 1) * 128],
                    rhs=qTm[hh][:, c, i * 128:(i + 1) * 128], start=True, stop=False),
                  reads=["kT%d" % ksl4, "qT"], writes=["B6"])
                A("pe", lambda e, h=h, hh=hh: e.matmul(pS[:, 1024 + hh * 128: 1024 + (hh + 1) * 128], lhsT=BT[:, h, 4, :], rhs=ident[:],
                                                       start=False, stop=True), reads=["BT", "ident"], writes=["B6"])
            for hh in range(2):
                A("act", lambda e, hh=hh: e.activation(out=PTp[:, hh, 0:512], in_=pS[:, hh * 512:(hh + 1) * 512], func=AF.Exp, scale=8.0),
                  reads=["B%d" % (4 + hh)], writes=[ptk])
            A("act", lambda e: e.activation(out=PTp[:, :, 512:640], in_=pS[:, 1024:1280].rearrange("p (a b) -> p a b", a=2), func=AF.Exp, scale=8.0),
              reads=["B6"], writes=[ptk])
            for hh in range(2):
                h = 2 * pr + hh
                pvt, pvk = (pV, "B7") if h < 4 else (pC, "B3")
                co = (h % 4) * 65
                for t in range(5):
                    ksl = (te - 4 + t) % RT
                    A("pe", lambda e, t=t, ksl=ksl, hh=hh, h=h, pvt=pvt, co=co: e.matmul(
                        pvt[:, co: co + 65], lhsT=PTp[:, hh, t * 128:(t + 1) * 128], rhs=Vr[:, ksl, h, :],
                        start=(t == 0), stop=(t == 4)), reads=[ptk, "V%d" % ksl], writes=[pvk])

        def attn_norm(hgi, ab):
            pvt, pvk = (pV, "B7") if hgi == 0 else (pC, "B3")
            pvv = pvt[:, 0:260].rearrange("p (h d) -> p h d", h=4)
            A("dve", lambda e: e.tensor_scalar(out=rden[:, hgi, :].unsqueeze(2), in0=pvv[:, :, 64:65], scalar1=1e-30, scalar2=None, op0=ALU.add),
              reads=[pvk], writes=["rden%d" % hgi])
            A("dve", lambda e: e.reciprocal(out=rden[:, hgi, :], in_=rden[:, hgi, :]),
              reads=["rden%d" % hgi], writes=["rden%d" % hgi])
            A("dve", lambda e: e.tensor_tensor(
                out=att[ab][:, hgi * 256:(hgi + 1) * 256].rearrange("p (h d) -> p h d", h=4), in0=pvv[:, :, 0:64],
                in1=rden[:, hgi, :].unsqueeze(2).to_broadcast([128, 4, 64]), op=ALU.mult),
              reads=[pvk, "rden%d" % hgi], writes=["att%d" % ab])

        def attention_tile(s, i):
            te = 4 * s + i
            ab = te % 2
            for pr in range(4):
                attn_pair(te, i, pr, ab)
                if pr % 2 == 1:
                    attn_norm(pr // 2, ab)

        def post_attention(s, i):
            te = 4 * s + i
            tl = te - NKV
            ab = te % 2
            for c in range(4):
                A("pe", lambda e, c=c: e.transpose(out=pT[:, c * 128:(c + 1) * 128], in_=att[ab][:, c * 128:(c + 1) * 128], identity=ident[:]),
                  reads=["att%d" % ab, "ident"], writes=["pT"])
            A("dve", lambda e: e.tensor_copy(out=mixT_[:, 4:8, i * 128:(i + 1) * 128], in_=pT[:, 0:512].rearrange("p (a b) -> p a b", a=4)),
              reads=["pT"], writes=["mixT"])
            for cbk in range(2):
                for kc in range(8):
                    A("pe", lambda e, cbk=cbk, kc=kc: e.matmul(pZ[:, cbk, :], lhsT=mixT_[:, kc, i * 128:(i + 1) * 128], rhs=w_out_bf[:, kc, cbk * 512:(cbk + 1) * 512],
                                                               start=(kc == 0), stop=(kc == 7)), reads=["mixT", "w_out_bf"], writes=["pZ%d" % cbk])
            rb = te % 2
            dma("sp", xr[rb][:], xe[te * 128:(te + 1) * 128, :], [], ["xr%d" % rb])
            A("dve", lambda e: e.tensor_tensor(out=xmd[rb][:], in0=pZ[:].rearrange("p a b -> p (a b)"), in1=xr[rb][:], op=ALU.add),
              reads=["pZ0", "pZ1", "xr%d" % rb], writes=["xmd%d" % rb])
            dma("sp", xmid[tl * 128:(tl + 1) * 128, :], xmd[rb][:], ["xmd%d" % rb], ["xmid"], dkey="st_xmd%d" % rb)

            def norm2_a():
                pb_ = norm_and_transpose(xmd[rb][:], "xmd%d" % rb, te % 8, xn2T[rb], ["xn2T%d" % rb], slice(0, 128), xn2[rb], "xn2%d" % rb,
                                         store_to=xn2s[tl * 128:(tl + 1) * 128, :], scale_eng="act", defer=True)

                def part_b():
                    pb_()
                    for kc in range(8):
                        A("pe", lambda e, kc=kc: e.matmul(pC[:, 0:36], lhsT=xn2T[rb][:, kc, :], rhs=wr_bf[:, kc, :], start=(kc == 0), stop=(kc == 7)),
                          reads=["xn2T%d" % rb, "wr_bf"], writes=["B3"])
                    A("dve", lambda e: e.tensor_tensor(out=logits[:, tl, :], in0=pC[:, 0:36], in1=biasR[:], op=ALU.add),
                      reads=["B3", "biasR"], writes=["logits"])
                return part_b
            return norm2_a

        def tail_copy(ub, g):
            A("pool", lambda e: e.tensor_copy(out=uT[ub][g][:, 0:16], in_=uT[1 - ub][g][:, 512:528]),
              reads=["uT%d%d" % (1 - ub, g)], writes=["uT%d%d" % (ub, g)])

        def norm_tile(s, i, defer=False):
            te = 4 * s + i
            xi = te % 2
            dma("sp", xin[xi][:], xe[te * 128:(te + 1) * 128, :], [], ["xin%d" % xi])
            return norm_and_transpose(xin[xi][:], "xin%d" % xi, te % 8, hT_, ["hT"], slice(i * 128, (i + 1) * 128),
                                      xn[te % 2], "xn%d" % (te % 2), defer=defer)

        q_norm2 = []
        q_b = []

        def do_st(s):
            halo_st = (4 * s) < NKV + NFH
            full_st = (4 * s) >= NKV
            first_main = (4 * s) == NKV + NFH
            if s == 0:
                for i in range(4):
                    norm_tile(0, i)
            ub = s % 2
            if s > 0:
                for g in range(4):
                    tail_copy(ub, g)
            slot0 = (4 * s) % RT
            for oc in range(12):
                if 4 <= oc < 8 and not full_st:
                    continue
                in_chunk(s, oc, ub, slot0, halo_st, full_st)
            for i in range(4):
                v_tile(s, i, halo_st)
            if full_st:
                for g in range(4):
                    pool_group(g, ub, first_main)
            for i in range(4):
                nb = norm_tile(s + 1, i, defer=True) if s + 1 < NST else None
                if full_st:
                    attention_tile(s, i)
                    if q_norm2:
                        q_b.append(q_norm2.pop(0)())
                    if len(q_b) > 1:
                        q_b.pop(0)()
                if nb is not None:
                    nb()
                if full_st:
                    q_norm2.append(post_attention(s, i))
            if s == NST - 1:
                while q_norm2:
                    q_b.append(q_norm2.pop(0)())
                while q_b:
                    q_b.pop(0)()

        for s in range(NST):
            do_st(s)
        p.barrier()

        cf = Carver(arf, AF_WORDS)
        cb_ = Carver(arb, AB_WORDS)
        NL = NTL
        R1 = cf.take(NL, 32)
        R2 = cf.take(NL, 32)
        R3 = cf.take(NL, 32)
        R4 = cf.take(NL, 32)
        sm = [cf.take(NL) for _ in range(6)]
        cntb = [cf.take(32) for _ in range(2)]
        startb = cf.take(32)
        widf = cf.take(NEXP, CT)
        ybuf = [cf.take(D) for _ in range(2)]
        sgb = [cf.take(CAP) for _ in range(2)]
        xmb = [cf.take(D) for _ in range(2)]
        y1b = [cf.take(D) for _ in range(2)]
        y2b_ = cf.take(D)
        y2b = [y2b_, y2b_]
        Abf = cb_.take(NL, 32)
        xtl = [cb_.take(D) for _ in range(2)]
        xw = [cb_.take(CT, D) for _ in range(2)]
        xsT = cb_.take(8, CAP)
        actT = cb_.take(4, CAP)
        Wg = [cb_.take(8, 512) for _ in range(2)]
        Wu = [cb_.take(8, 512) for _ in range(2)]
        Wd = [cb_.take(4, D) for _ in range(2)]

        WGK = [["Wg%d_%d" % (b, kc) for kc in range(8)] for b in range(2)]
        WUK = [["Wu%d_%d" % (b, kc) for kc in range(8)] for b in range(2)]
        WDK = [["Wd%d_%d" % (b, jc) for jc in range(4)] for b in range(2)]

        def load_w(e_):
            b = e_ % 2
            dma("pool", Wg[b][:], wg[e_ * D:(e_ + 1) * D, :].rearrange("(k p) n -> p k n", p=128), [], WGK[b], dkey="Wg%d" % b)
            dma("pool", Wu[b][:], wu[e_ * D:(e_ + 1) * D, :].rearrange("(k p) n -> p k n", p=128), [], WUK[b], dkey="Wu%d" % b)
            dma("pool", Wd[b][:], wd[e_ * 512:(e_ + 1) * 512, :].rearrange("(k p) n -> p k n", p=128), [], WDK[b], dkey="Wd%d" % b)

        load_w(0)
        load_w(1)

        gl = logits[:, :, 0:4]
        el = logits[:, :, 4:36]
        V = lambda e: e
        gmax, gsum, m1, m2, dd, ee = sm
        gone = R1[:, :, 0:4]
        A("dve", lambda e: e.reduce_max(out=gmax[:], in_=gl, axis=AX.X), reads=["logits"], writes=["gmax"])
        A("dve", lambda e: e.tensor_tensor(out=gone, in0=gl, in1=gmax[:].unsqueeze(2).to_broadcast([128, NL, 4]), op=ALU.is_equal),
          reads=["logits", "gmax"], writes=["R1"])
        gex = R2[:, :, 0:4]
        A("dve", lambda e: e.tensor_tensor(out=gex, in0=gl, in1=gmax[:].unsqueeze(2).to_broadcast([128, NL, 4]), op=ALU.subtract),
          reads=["logits", "gmax"], writes=["R2"])
        A("act", lambda e: e.activation(out=gex, in_=gex, func=AF.Exp), reads=["R2"], writes=["R2"])
        A("dve", lambda e: e.reduce_sum(out=gsum[:], in_=gex, axis=AX.X), reads=["R2"], writes=["gsum"])
        A("dve", lambda e: e.reciprocal(out=gsum[:], in_=gsum[:]), reads=["gsum"], writes=["gsum"])
        BIG = 1.0e4
        A("dve", lambda e: e.tensor_scalar(out=gone, in0=gone, scalar1=BIG, scalar2=-BIG, op0=ALU.mult, op1=ALU.add), reads=["R1"], writes=["R1"])
        em = R3
        A("dve", lambda e: e.tensor_tensor(out=em[:].rearrange("p n (g j) -> p n g j", g=4), in0=el.rearrange("p n (g j) -> p n g j", g=4),
                                           in1=gone.unsqueeze(3).to_broadcast([128, NL, 4, 8]), op=ALU.add), reads=["logits", "R1"], writes=["R3"])
        A("dve", lambda e: e.reduce_max(out=m1[:], in_=em[:], axis=AX.X), reads=["R3"], writes=["m1"])
        oh1 = R1
        A("dve", lambda e: e.tensor_tensor(out=oh1[:], in0=em[:], in1=m1[:].unsqueeze(2).to_broadcast([128, NL, 32]), op=ALU.is_equal),
          reads=["R3", "m1"], writes=["R1"])
        em2 = R2
        A("dve", lambda e: e.scalar_tensor_tensor(out=em2[:], in0=oh1[:], scalar=-BIG, in1=em[:], op0=ALU.mult, op1=ALU.add),
          reads=["R1", "R3"], writes=["R2"])
        A("dve", lambda e: e.reduce_max(out=m2[:], in_=em2[:], axis=AX.X), reads=["R2"], writes=["m2"])
        oh2 = R3
        A("dve", lambda e: e.tensor_tensor(out=oh2[:], in0=em2[:], in1=m2[:].unsqueeze(2).to_broadcast([128, NL, 32]), op=ALU.is_equal),
          reads=["R2", "m2"], writes=["R3"])
        A("dve", lambda e: e.tensor_tensor(out=dd[:], in0=m2[:], in1=m1[:], op=ALU.subtract), reads=["m1", "m2"], writes=["dd"])
        A("act", lambda e: e.activation(out=ee[:], in_=dd[:], func=AF.Exp), reads=["dd"], writes=["ee"])
        A("dve", lambda e: e.tensor_scalar(out=dd[:], in0=ee[:], scalar1=1.0, scalar2=None, op0=ALU.add), reads=["ee"], writes=["dd"])
        A("dve", lambda e: e.reciprocal(out=dd[:], in_=dd[:]), reads=["dd"], writes=["dd"])
        A("dve", lambda e: e.tensor_tensor(out=ee[:], in0=ee[:], in1=dd[:], op=ALU.mult), reads=["ee", "dd"], writes=["ee"])
        A("dve", lambda e: e.tensor_tensor(out=w1g[:], in0=dd[:], in1=gsum[:], op=ALU.mult), reads=["dd", "gsum"], writes=["w1g"])
        A("dve", lambda e: e.tensor_tensor(out=w2g[:], in0=ee[:], in1=gsum[:], op=ALU.mult), reads=["ee", "gsum"], writes=["w2g"])
        Asum = R2
        A("dve", lambda e: e.tensor_tensor(out=Asum[:], in0=oh1[:], in1=oh2[:], op=ALU.add), reads=["R1", "R3"], writes=["R2"])
        if NFH > 0:
            A("dve", lambda e: e.tensor_scalar(out=Asum[:, 0:NFH, :], in0=Asum[:, 0:NFH, :], scalar1=hvt[:, 0:1], scalar2=None, op0=ALU.mult),
              reads=["R2", "hvt"], writes=["R2"])
        A("dve", lambda e: e.tensor_copy(out=Abf[:], in_=Asum[:]), reads=["R2"], writes=["Abf"])
        Af = Abf[:].rearrange("p n e -> p (n e)")
        ncol = NL * 32
        banks = [(pZ[:, 0, :], "pZ0"), (pZ[:, 1, :], "pZ1"), (pC[:], "B3"), (pV[:], "B7")]
        assert ncol <= 1536
        Rk = R4[:].rearrange("p n e -> p (n e)")
        Tt = R2[:].rearrange("p n e -> p (n e)")
        for (lhs, lk, dst, dk) in ((tri_bf, "tri_bf", Rk, "R4"), (ones_bf, "ones_bf", Tt, "R2")):
            for c0 in range(0, ncol, 512):
                cw = min(512, ncol - c0)
                A("pe", lambda e, lhs=lhs, c0=c0, cw=cw: e.matmul(pS[:, c0:c0 + cw], lhsT=lhs[:], rhs=Af[:, c0:c0 + cw], start=True, stop=True),
                  reads=[lk, "Abf"], writes=["B4", "B5", "B6"])
            A("dve", lambda e, dst=dst: e.tensor_copy(out=dst, in_=pS[:, 0:ncol]), reads=["B4", "B5", "B6"], writes=[dk])
        A("dve", lambda e: e.memset(cntb[0][:], 0.0), writes=["cnt"])
        for n in range(NL):
            if n > 0:
                A("dve", lambda e, n=n: e.tensor_tensor(out=R4[:, n, :], in0=R4[:, n, :], in1=cntb[0][:], op=ALU.add), reads=["R4", "cnt"], writes=["R4"])
            A("dve", lambda e, n=n: e.tensor_tensor(out=cntb[0][:], in0=cntb[0][:], in1=R2[:, n, :], op=ALU.add), reads=["R2", "cnt"], writes=["cnt"])
        A("dve", lambda e: e.memset(startb[:], 0.0), writes=["startb"])
        for j in range(1, 32):
            A("dve", lambda e, j=j: e.tensor_tensor(out=startb[:, j:j + 1], in0=startb[:, j - 1:j], in1=cntb[0][:, j - 1:j], op=ALU.add),
              reads=["startb", "cnt"], writes=["startb"])
        A("dve", lambda e: e.tensor_tensor(out=R4[:], in0=R4[:], in1=startb[:].unsqueeze(1).to_broadcast([128, NL, 32]), op=ALU.add),
          reads=["R4", "startb"], writes=["R4"])
        for ki, (oh, ohk, sl_i, slk, tmpk) in enumerate(((oh1, "R1", slot1, "slot1", "gmax"), (oh2, "R3", slot2, "slot2", "m1"))):
            tmp = gmax if tmpk == "gmax" else m1
            A("dve", lambda e, oh=oh: e.tensor_tensor(out=oh[:], in0=oh[:], in1=R4[:], op=ALU.mult), reads=[ohk, "R4"], writes=[ohk])
            A("dve", lambda e, oh=oh, tmp=tmp: e.reduce_sum(out=tmp[:], in_=oh[:], axis=AX.X), reads=[ohk], writes=[tmpk])
            if NFH > 0:
                A("dve", lambda e, tmp=tmp: e.tensor_scalar(out=tmp[:, 0:NFH], in0=tmp[:, 0:NFH], scalar1=hvt[:, 0:1], scalar2=None, op0=ALU.mult),
                  reads=[tmpk, "hvt"], writes=[tmpk])
                A("dve", lambda e, tmp=tmp, ki=ki: e.scalar_tensor_tensor(out=tmp[:, 0:NFH], in0=trt[:, ki * TRC: ki * TRC + NFH], scalar=nhvt[:, 0:1], in1=tmp[:, 0:NFH],
                                                                 op0=ALU.mult, op1=ALU.add), reads=[tmpk, "trt", "nhvt"], writes=[tmpk])
            A("dve", lambda e, tmp=tmp, sl_i=sl_i: e.tensor_copy(out=sl_i[:], in_=tmp[:]), reads=[tmpk], writes=[slk])
        A("dve", lambda e: e.tensor_tensor(out=widf[:], in0=startb[:].unsqueeze(2).to_broadcast([128, NEXP, CT]),
                                           in1=iott[:, 0:CT].unsqueeze(1).to_broadcast([128, NEXP, CT]), op=ALU.add),
          reads=["startb", "iott"], writes=["widf"])
        A("dve", lambda e: e.tensor_copy(out=widx[:], in_=widf[:]), reads=["widf"], writes=["widx"])

        if debug:
            dma("sp", dbg_logits, logits[:], ["logits"], ["dbg_logits"])
            dma("sp", dbg_w[:, 0, :], w1g[:], ["w1g"], ["dbg_w1"])
            dma("sp", dbg_w[:, 1, :], w2g[:], ["w2g"], ["dbg_w2"])
            dma("sp", dbg_slot[:, 0, :], slot1[:], ["slot1"], ["dbg_s1"])
            dma("sp", dbg_slot[:, 1, :], slot2[:], ["slot2"], ["dbg_s2"])
        xskeys = []
        for tl in range(NTL):
            b = tl % 2
            dma("sp", xtl[b][:], xn2s[tl * 128:(tl + 1) * 128, :], ["xn2s"], ["xtl%d" % b])
            for k_, (sl_i, slk) in enumerate(((slot1, "slot1"), (slot2, "slot2"))):
                key = "xs_%d_%d" % (tl, k_)
                xskeys.append(key)
                A("pool", lambda e, sl_i=sl_i, tl=tl, b=b: e.indirect_dma_start(
                    out=xs[:, :], out_offset=bass.IndirectOffsetOnAxis(ap=sl_i[:, tl:tl + 1], axis=0), in_=xtl[b][:], in_offset=None),
                  reads=["xtl%d" % b, slk], writes=[key], dma=True, dkey="sc_xtl%d" % b)

        NOW, NTHR = cfg.NOW, cfg.NTHR
        BIGI = 1.0e6
        cnt_ = cntb[0]
        assert NOW <= NL and NTHR <= NL
        gtm = R2[:, 0:NTHR, :].rearrange("p t e -> p (t e)").rearrange("p (e t) -> p e t", e=32)
        nov = cf.take(32)
        cum = cf.take(32)
        indw = R1[:, 0:NOW, :]
        tmpw = R3[:, 0:NOW, :]
        limv = cf.take(32)
        jbase = cf.take(32)
        wsc = [cf.take(NOW) for _ in range(5)]
        gidf = cf.take(NOW, CT)
        yidf = cf.take(NOW, CT)
        mskf = cf.take(NOW, CT)
        wgidf = cf.take(NOW, 8)
        wdidf = cf.take(NOW, 4)
        gidx = T("gidx", [128, NOW, CT], I32)
        yidx = T("yidx", [128, NOW, CT], I32)
        wgidx = T("wgidx", [128, NOW, 8], I32)
        wdidx = T("wdidx", [128, NOW, 4], I32)
        DV = lambda fn, r, w: A("dve", fn, reads=r, writes=w)
        DV(lambda e: e.tensor_tensor(out=gtm[:], in0=cnt_[:].unsqueeze(2).to_broadcast([128, 32, NTHR]),
                                     in1=thrt[:, 0:NTHR].unsqueeze(1).to_broadcast([128, 32, NTHR]), op=ALU.is_gt), ["cnt", "thrt"], ["R2"])
        DV(lambda e: e.reduce_sum(out=nov[:], in_=gtm[:], axis=AX.X), ["R2"], ["nov"])
        DV(lambda e: e.memset(cum[:], 0.0), [], ["cum"])
        for j in range(1, 32):
            DV(lambda e, j=j: e.tensor_tensor(out=cum[:, j:j + 1], in0=cum[:, j - 1:j], in1=nov[:, j - 1:j], op=ALU.add), ["cum", "nov"], ["cum"])
        wvb = wvt[:, 0:NOW].unsqueeze(2).to_broadcast([128, NOW, 32])
        DV(lambda e: e.tensor_tensor(out=indw[:], in0=cum[:].unsqueeze(1).to_broadcast([128, NOW, 32]), in1=wvb, op=ALU.is_le), ["cum", "wvt"], ["R1"])
        DV(lambda e: e.tensor_tensor(out=limv[:], in0=cum[:], in1=nov[:], op=ALU.add), ["cum", "nov"], ["limv"])
        DV(lambda e: e.tensor_tensor(out=tmpw[:], in0=limv[:].unsqueeze(1).to_broadcast([128, NOW, 32]), in1=wvb, op=ALU.is_gt), ["limv", "wvt"], ["R3"])
        DV(lambda e: e.tensor_tensor(out=indw[:], in0=indw[:], in1=tmpw[:], op=ALU.mult), ["R1", "R3"], ["R1"])
        vld, ew, ow, lw, tw = wsc
        DV(lambda e: e.reduce_sum(out=vld[:], in_=indw[:], axis=AX.X), ["R1"], ["vld"])
        DV(lambda e: e.tensor_tensor(out=tmpw[:], in0=indw[:], in1=evt[:].unsqueeze(1).to_broadcast([128, NOW, 32]), op=ALU.mult), ["R1", "evt"], ["R3"])
        DV(lambda e: e.reduce_sum(out=ew[:], in_=tmpw[:], axis=AX.X), ["R3"], ["ew"])
        DV(lambda e: e.tensor_scalar(out=jbase[:], in0=cum[:], scalar1=-float(CAP), scalar2=float(CAP), op0=ALU.mult, op1=ALU.add), ["cum"], ["jbase"])
        DV(lambda e: e.tensor_tensor(out=jbase[:], in0=jbase[:], in1=startb[:], op=ALU.add), ["jbase", "startb"], ["jbase"])
        DV(lambda e: e.tensor_tensor(out=tmpw[:], in0=indw[:], in1=jbase[:].unsqueeze(1).to_broadcast([128, NOW, 32]), op=ALU.mult), ["R1", "jbase"], ["R3"])
        DV(lambda e: e.reduce_sum(out=ow[:], in_=tmpw[:], axis=AX.X), ["R3"], ["ow"])
        DV(lambda e: e.scalar_tensor_tensor(out=ow[:], in0=wvt[:, 0:NOW], scalar=float(CAP), in1=ow[:], op0=ALU.mult, op1=ALU.add), ["ow", "wvt"], ["ow"])
        DV(lambda e: e.tensor_tensor(out=ow[:], in0=ow[:], in1=vld[:], op=ALU.mult), ["ow", "vld"], ["ow"])
        DV(lambda e: e.tensor_tensor(out=limv[:], in0=startb[:], in1=cnt_[:], op=ALU.add), ["startb", "cnt"], ["limv"])
        DV(lambda e: e.tensor_tensor(out=tmpw[:], in0=indw[:], in1=limv[:].unsqueeze(1).to_broadcast([128, NOW, 32]), op=ALU.mult), ["R1", "limv"], ["R3"])
        DV(lambda e: e.reduce_sum(out=lw[:], in_=tmpw[:], axis=AX.X), ["R3"], ["lw"])
        DV(lambda e: e.tensor_scalar(out=tw[:], in0=vld[:], scalar1=-BIGI, scalar2=BIGI, op0=ALU.mult, op1=ALU.add), ["vld"], ["tw"])
        DV(lambda e: e.tensor_tensor(out=gidf[:], in0=ow[:].unsqueeze(2).to_broadcast([128, NOW, CT]),
                                     in1=iott[:, 0:CT].unsqueeze(1).to_broadcast([128, NOW, CT]), op=ALU.add), ["ow", "iott"], ["gidf"])
        DV(lambda e: e.tensor_tensor(out=mskf[:], in0=gidf[:], in1=lw[:].unsqueeze(2).to_broadcast([128, NOW, CT]), op=ALU.is_lt), ["gidf", "lw"], ["mskf"])
        DV(lambda e: e.scalar_tensor_tensor(out=yidf[:], in0=gidf[:], scalar=-BIGI, in1=mskf[:], op0=ALU.add, op1=ALU.mult), ["gidf", "mskf"], ["yidf"])
        DV(lambda e: e.tensor_scalar(out=yidf[:], in0=yidf[:], scalar1=BIGI, scalar2=None, op0=ALU.add), ["yidf"], ["yidf"])
        DV(lambda e: e.tensor_tensor(out=gidf[:], in0=gidf[:], in1=tw[:].unsqueeze(2).to_broadcast([128, NOW, CT]), op=ALU.add), ["gidf", "tw"], ["gidf"])
        DV(lambda e: e.tensor_copy(out=gidx[:], in_=gidf[:]), ["gidf"], ["gidx"])
        DV(lambda e: e.tensor_copy(out=yidx[:], in_=yidf[:]), ["yidf"], ["yidx"])
        DV(lambda e: e.scalar_tensor_tensor(out=ew[:], in0=ew[:], scalar=1024.0, in1=tw[:], op0=ALU.mult, op1=ALU.add), ["ew", "tw"], ["ew"])
        DV(lambda e: e.tensor_tensor(out=wgidf[:], in0=ew[:].unsqueeze(2).to_broadcast([128, NOW, 8]),
                                     in1=iot8t[:].unsqueeze(1).to_broadcast([128, NOW, 8]), op=ALU.add), ["ew", "iot8t"], ["wgidf"])
        DV(lambda e: e.tensor_copy(out=wgidx[:], in_=wgidf[:]), ["wgidf"], ["wgidx"])
        DV(lambda e: e.scalar_tensor_tensor(out=ew[:], in0=ew[:], scalar=0.5, in1=tw[:], op0=ALU.mult, op1=ALU.add), ["ew", "tw"], ["ew"])
        DV(lambda e: e.tensor_tensor(out=wdidf[:], in0=ew[:].unsqueeze(2).to_broadcast([128, NOW, 4]),
                                     in1=iot8t[:, 0:4].unsqueeze(1).to_broadcast([128, NOW, 4]), op=ALU.add), ["ew", "iot8t"], ["wdidf"])
        DV(lambda e: e.tensor_copy(out=wdidx[:], in_=wdidf[:]), ["wdidf"], ["wdidx"])

        gbanks = [(pZ[:, 0, :], "pZ0"), (pZ[:, 1, :], "pZ1"), (pC[:], "B3"), (pV[:], "B7")]
        dbanks = [(pS[:, 512:1024], "B5"), (pS[:, 1024:1536], "B6")]
        pT2 = pS[:, 0:512].bitcast(BF16)
        tbanks = [(pT, "pT"), (pT2, "B4")]
        cnts = {"gi": 0, "di": 0, "ti": 0}
        NJOB = NEXP + NOW
        wg2, wu2, wd2 = wg, wu, wd

        bregs = memo.setdefault("__bregs", {})

        def breg(e, val):
            if val not in bregs:
                r = e.alloc_register("bc%d" % val)
                e.reg_mov(r, val)
                bregs[val] = r
            return bregs[val]

        def job_rows(k, j, for_y):
            if k < NEXP:
                return widx[:, k, j:j + 1]
            return (yidx if for_y else gidx)[:, k - NEXP, j:j + 1]

        def job_load_w(k):
            b = k % 2
            if k < NEXP:
                load_w(k)
                return
            w = k - NEXP
            og, ou, od = [], [], []
            for kc in range(8):
                og.append(A("pool", lambda e, kc=kc: e.indirect_dma_start(out=Wg[b][:, kc, :], out_offset=None, in_=wg2,
                                                                          in_offset=bass.IndirectOffsetOnAxis(ap=wgidx[:, w, kc:kc + 1], axis=0),
                                                                          bounds_check=breg(e, NEXP * 1024 - 1), oob_is_err=False),
                            reads=["wgidx"], writes=[WGK[b][kc]], dma=True, dkey="Wg%d" % b))
                ou.append(A("pool", lambda e, kc=kc: e.indirect_dma_start(out=Wu[b][:, kc, :], out_offset=None, in_=wu2,
                                                                          in_offset=bass.IndirectOffsetOnAxis(ap=wgidx[:, w, kc:kc + 1], axis=0),
                                                                          bounds_check=breg(e, NEXP * 1024 - 1), oob_is_err=False),
                            reads=["wgidx"], writes=[WUK[b][kc]], dma=True, dkey="Wu%d" % b))
            for jc in range(4):
                od.append(A("pool", lambda e, jc=jc: e.indirect_dma_start(out=Wd[b][:, jc, :], out_offset=None, in_=wd2,
                                                                          in_offset=bass.IndirectOffsetOnAxis(ap=wdidx[:, w, jc:jc + 1], axis=0),
                                                                          bounds_check=breg(e, NEXP * 512 - 1), oob_is_err=False),
                            reads=["wdidx"], writes=[WDK[b][jc]], dma=True, dkey="Wd%d" % b))
            for grp in (og, ou, od):
                for o_ in grp:
                    o_.tgt = grp[-1].tgt

        def job_gather(k):
            b = k % 2
            for j in range(CT):
                rows = job_rows(k, j, False)
                if k < NEXP:
                    A("pool", lambda e, j=j, rows=rows: e.indirect_dma_start(
                        out=xw[b][:, j, :], out_offset=None, in_=xs[:, :], in_offset=bass.IndirectOffsetOnAxis(ap=rows, axis=0)),
                      reads=xskeys + ["widx"], writes=["xw%d_%d" % (b, j)], dma=True)
                else:
                    A("pool", lambda e, j=j, rows=rows: e.indirect_dma_start(
                        out=xw[b][:, j, :], out_offset=None, in_=xs[:, :], in_offset=bass.IndirectOffsetOnAxis(ap=rows, axis=0),
                        bounds_check=breg(e, cfg.XSR - 1), oob_is_err=False),
                      reads=xskeys + ["gidx"], writes=["xw%d_%d" % (b, j)], dma=True)

        def job_compute(k):
            b = k % 2
            for kc in range(8):
                (tps, tk_) = tbanks[cnts["ti"] % 2]
                cnts["ti"] += 1
                for j in range(CT):
                    A("pe", lambda e, kc=kc, j=j, tps=tps: e.transpose(out=tps[:, j * 128:(j + 1) * 128],
                                                                       in_=xw[b][:, j, kc * 128:(kc + 1) * 128], identity=ident[:]),
                      reads=["xw%d_%d" % (b, j), "ident"], writes=[tk_])
                A("act", lambda e, kc=kc, tps=tps: e.activation(out=xsT[:, kc, :], in_=tps[:, 0:CAP], func=AF.Identity,
                                                                scale=mul2c[:, kc:kc + 1], bias=modc[:, 2, kc:kc + 1]),
                  reads=[tk_, "mul2c", "modc"], writes=["xsT%d" % kc])
            for jc in range(4):
                (gps, gk_) = gbanks[cnts["gi"] % 4]
                (ups, uk_) = gbanks[(cnts["gi"] + 1) % 4]
                cnts["gi"] += 2
                for kc in range(8):
                    A("pe", lambda e, gps=gps, kc=kc, jc=jc: e.matmul(gps[:, 0:CAP], lhsT=Wg[b][:, kc, jc * 128:(jc + 1) * 128], rhs=xsT[:, kc, :],
                                                                     start=(kc == 0), stop=(kc == 7)), reads=WGK[b] + ["xsT%d" % kc], writes=[gk_])
                for kc in range(8):
                    A("pe", lambda e, ups=ups, kc=kc, jc=jc: e.matmul(ups[:, 0:CAP], lhsT=Wu[b][:, kc, jc * 128:(jc + 1) * 128], rhs=xsT[:, kc, :],
                                                                     start=(kc == 0), stop=(kc == 7)), reads=WUK[b] + ["xsT%d" % kc], writes=[uk_])
                sb_ = jc % 2
                A("act", lambda e, gps=gps, sb_=sb_: e.activation(out=sgb[sb_][:], in_=gps[:, 0:CAP], func=AF.Silu), reads=[gk_], writes=["sg%d" % sb_])
                A("dve", lambda e, ups=ups, sb_=sb_, jc=jc: e.tensor_tensor(out=actT[:, jc, :], in0=ups[:, 0:CAP], in1=sgb[sb_][:], op=ALU.mult),
                  reads=[uk_, "sg%d" % sb_], writes=["actT"])
            for j in range(CT):
                yb = (k * CT + j) % 2
                for cbk in range(2):
                    (dps, dk_) = dbanks[cnts["di"] % 2]
                    cnts["di"] += 1
                    for jc in range(4):
                        A("pe", lambda e, dps=dps, jc=jc, j=j, cbk=cbk: e.matmul(dps, lhsT=actT[:, jc, j * 128:(j + 1) * 128], rhs=Wd[b][:, jc, cbk * 512:(cbk + 1) * 512],
                                                                                start=(jc == 0), stop=(jc == 3)), reads=["actT"] + WDK[b], writes=[dk_])
                    A("dve", lambda e, dps=dps, cbk=cbk, yb=yb: e.tensor_tensor(out=ybuf[yb][:, cbk * 512:(cbk + 1) * 512], in0=dps, in1=g2bc[:, cbk * 512:(cbk + 1) * 512], op=ALU.mult),
                      reads=[dk_, "g2bc"], writes=["ybuf%d" % yb])
                rows = job_rows(k, j, True)
                if k < NEXP:
                    A("pool", lambda e, rows=rows, yb=yb: e.indirect_dma_start(
                        out=ys[:, :], out_offset=bass.IndirectOffsetOnAxis(ap=rows, axis=0), in_=ybuf[yb][:], in_offset=None),
                      reads=["ybuf%d" % yb, "widx"], writes=["ys"], dma=True, dkey="sc_ybuf%d" % yb)
                else:
                    A("pool", lambda e, rows=rows, yb=yb: e.indirect_dma_start(
                        out=ys[:, :], out_offset=bass.IndirectOffsetOnAxis(ap=rows, axis=0), in_=ybuf[yb][:], in_offset=None,
                        bounds_check=breg(e, cfg.XSR - 1), oob_is_err=False),
                      reads=["ybuf%d" % yb, "yidx"], writes=["ys"], dma=True, dkey="sc_ybuf%d" % yb)

        job_gather(0)
        for k in range(NJOB):
            if k + 1 < NJOB:
                job_gather(k + 1)
            job_compute(k)
            if k + 2 < NJOB:
                job_load_w(k + 2)

        for tl in range(NTL):
            b = tl % 2
            dma("sp", xmb[b][:], xmid[tl * 128:(tl + 1) * 128, :], ["xmid"], ["xmb%d" % b])
            A("pool", lambda e, tl=tl, b=b: e.indirect_dma_start(out=y1b[b][:], out_offset=None, in_=ys[:, :],
                                                                 in_offset=bass.IndirectOffsetOnAxis(ap=slot1[:, tl:tl + 1], axis=0)),
              reads=["ys", "slot1"], writes=["y1b%d" % b], dma=True)
            A("pool", lambda e, tl=tl, b=b: e.indirect_dma_start(out=y2b[b][:], out_offset=None, in_=ys[:, :],
                                                                 in_offset=bass.IndirectOffsetOnAxis(ap=slot2[:, tl:tl + 1], axis=0)),
              reads=["ys", "slot2"], writes=["y2b"], dma=True)
            A("dve", lambda e, tl=tl, b=b: e.scalar_tensor_tensor(out=xmb[b][:], in0=y1b[b][:], scalar=w1g[:, tl:tl + 1], in1=xmb[b][:], op0=ALU.mult, op1=ALU.add),
              reads=["y1b%d" % b, "w1g", "xmb%d" % b], writes=["xmb%d" % b])
            A("dve", lambda e, tl=tl, b=b: e.scalar_tensor_tensor(out=xmb[b][:], in0=y2b[b][:], scalar=w2g[:, tl:tl + 1], in1=xmb[b][:], op0=ALU.mult, op1=ALU.add),
              reads=["y2b", "w2g", "xmb%d" % b], writes=["xmb%d" % b])
            dma("sp", xo[tl * 128:(tl + 1) * 128, :], xmb[b][:], ["xmb%d" % b], ["xo"], dkey="st_xmb%d" % b)
        p.barrier()


def _colform(v):
    return np.ascontiguousarray(v.reshape(-1, 128).T).astype(np.float32)


def _const_tables(cfg):
    q = np.arange(128)[:, None]
    tabs_idx = np.zeros((128, 5, 128), np.int64)
    mask = np.zeros((128, 5, 128), np.float32)
    for t in range(5):
        k = np.arange(128)[None, :]
        rel = 128 * (4 - t) + q - k
        tabs_idx[:, t, :] = np.clip(rel, -128, 128) + 128
        qc = q // 64
        kc = 2 * (t - 4) + k // 64
        ok = (kc <= qc) & (kc >= qc - 8)
        mask[:, t, :] = ok
    tri = (np.arange(128)[:, None] < np.arange(128)[None, :]).astype(np.float32)
    iot = (np.arange(128)[:, None] + 128 * np.arange(4)[None, :]).astype(np.float32)
    trash = (cfg.TRASH + np.arange(128)[:, None] + 128 * np.arange(max(cfg.NFH, 1))[None, :]).astype(np.float32)
    return tabs_idx, mask, tri, iot, trash


def layer_inputs(cfg, l, xe, cb, first_half, P, li=0):
    tabs_idx, mask, tri, iot, trash = _const_tables(cfg)
    btab = np.ascontiguousarray(P["rel_bias"][:, tabs_idx].transpose(1, 0, 2, 3)).astype(np.float32)
    invc = np.zeros((128, 4, 16), np.float32)
    for g, w in enumerate((2, 4, 8, 16)):
        cnt = np.minimum(np.arange(16) + 1, w) if first_half else np.full(16, w)
        invc[:, g, :] = (1.0 / cnt.astype(np.float64)).astype(np.float32)[None, :]
    hvv = 0.0 if first_half else 1.0
    trash = (cfg.TRASH + np.arange(128)[:, None] + 128 * np.arange(TRC)[None, :]).astype(np.float32)
    trash = np.concatenate([trash, trash + cfg.NFH * 128], axis=1)
    m = {
        "xe": np.ascontiguousarray(xe, dtype=np.float32),
        "cT": _colform(cb),
        "ada_w": P["ada_w"][l], "ada_b": P["ada_b"][l][None, :],
        "n1c": _colform(P["norm1_g"][l]), "n2c": _colform(P["norm2_g"][l]),
        "w_in": P["w_in"][l], "w_out": P["w_out"][l],
        "pool_w": np.ascontiguousarray(P["pool_w"][l].transpose(1, 0, 2)),
        "pscale": _colform(P["pool_scale"][l]),
        "gq": np.ascontiguousarray(np.tile(P["q_norm_g"][l], 2)[:, None]), "gk": np.ascontiguousarray(np.tile(P["k_norm_g"][l], 2)[:, None]),
        "btab": btab, "bmask": mask,
        "wr": np.ascontiguousarray(np.concatenate([P["router_group_w"][l], P["router_expert_w"][l]], axis=1)),
        "br": np.ascontiguousarray(np.tile(np.concatenate([P["router_group_b"][l], P["router_expert_b"][l]])[None, :], (128, 1))),
        "wg": P["moe_w_gate"][l].reshape(NEXP * D, 512), "wu": P["moe_w_up"][l].reshape(NEXP * D, 512), "wd": P["moe_w_down"][l].reshape(NEXP * 512, D),
        "hv": np.full((128, 1), hvv, np.float32), "nhv": np.full((128, 1), 1.0 - hvv, np.float32),
        "invc": invc, "tri": tri, "iot": iot, "trashi": trash,
        "thr": np.tile((cfg.CAP * (np.arange(16) + 1)).astype(np.float32)[None, :], (128, 1)),
        "wv": np.tile(np.arange(32, dtype=np.float32)[None, :], (128, 1)),
        "ev": np.tile(np.arange(32, dtype=np.float32)[None, :], (128, 1)),
        "iot8": (np.arange(128)[:, None] + 128 * np.arange(8)[None, :]).astype(np.float32),
    }
    return {(k + "_%d" % li if k in PERL else k): v for k, v in m.items()}


_NC_CACHE = {}


def kernel(**inputs):
    P = {k: np.asarray(v) for k, v in inputs.items()}
    x = P["x"]
    B, S, _ = x.shape
    cfg0 = Cfg(nkv=4, nfh=4, nm=32, cap=512)
    cfg1 = Cfg(nkv=4, nfh=0, nm=32, cap=512)
    if "nc" not in _NC_CACHE:
        _NC_CACHE["nc"] = build_program([cfg0, cfg1])
    nc = _NC_CACHE["nc"]
    half = S // 2
    in_maps = []
    for c in range(8):
        b, hf = c // 2, c % 2
        main = x[b, hf * half:(hf + 1) * half]
        halo = np.zeros((1024, D), np.float32) if hf == 0 else x[b, half - 1024:half]
        xe = np.concatenate([halo, main], axis=0)
        m = layer_inputs(cfg0, 0, xe, P["c"][b], hf == 0, P, li=0)
        m1 = layer_inputs(cfg1, 1, xe[:128], P["c"][b], hf == 0, P, li=1)
        m.update({k: v for k, v in m1.items() if k.endswith("_1")})
        in_maps.append(m)
    res = run_bass_kernel_spmd(nc, in_maps, core_ids=list(range(8)))
    out = np.empty_like(x)
    for c in range(8):
        b, hf = c // 2, c % 2
        out[b, hf * half:(hf + 1) * half] = res.results[c]["xo"]
    return out
```

```python
from contextlib import ExitStack

import numpy as np
import concourse.bass as bass
import concourse.mybir as mybir
from concourse.bass_utils import run_bass_kernel_spmd

F32 = mybir.dt.float32
BF16 = mybir.dt.bfloat16
I32 = mybir.dt.int32
AF = mybir.ActivationFunctionType
ALU = mybir.AluOpType
AX = mybir.AxisListType

ENGS = ("sp", "act", "dve", "pool", "pe")
PSUM_KEYS = frozenset(["pT", "pZ0", "pZ1", "B3", "B4", "B5", "B6", "B7"])
D = 1024
EPS = 1e-6
NEXP = 32
RT = 8


class Op:
    __slots__ = ("eng", "fn", "dma", "dkey", "deps", "sig", "idx", "tgt", "waits")

    def __init__(self, eng, fn, dma, dkey):
        self.eng = eng
        self.fn = fn
        self.dma = dma
        self.dkey = dkey
        self.deps = []
        self.sig = False
        self.idx = 0
        self.tgt = 0
        self.waits = []


class PB:
    def __init__(self, nc):
        self.nc = nc
        self.ops = []
        self.last_w = {}
        self.readers = {}
        self.dma_cnt = {}

    def add(self, eng, fn, reads=(), writes=(), dma=False, dkey=None):
        if dma and dkey is None:
            dkey = writes[0]
        op = Op(eng, fn, dma, dkey)
        deps = set()
        for k in reads:
            w = self.last_w.get(k)
            if w is not None:
                deps.add(w)
            if k in PSUM_KEYS:
                for r in self.readers.get(k, ()):
                    if r.eng != eng:
                        deps.add(r)
        for k in writes:
            w = self.last_w.get(k)
            if w is not None:
                deps.add(w)
            for r in self.readers.get(k, ()):
                deps.add(r)
        op.deps = list(deps)
        for k in reads:
            self.readers.setdefault(k, []).append(op)
        for k in writes:
            self.last_w[k] = op
            self.readers[k] = []
        if dma:
            self.dma_cnt[dkey] = self.dma_cnt.get(dkey, 0) + 1
            op.tgt = 16 * self.dma_cnt[dkey]
        self.ops.append(op)
        return op

    def barrier(self):
        allkeys = list(set(self.last_w.keys()) | set(self.readers.keys()))
        self.add("sp", lambda e: e.nop(), reads=[], writes=allkeys + ["__bar"])
        for eng in ENGS:
            self.add(eng, lambda e: e.nop(), reads=["__bar"], writes=["__bar_" + eng])
        self.last_w = {k: v for k, v in self.last_w.items() if k.startswith("__bar")}
        self.readers = {k: v for k, v in self.readers.items() if k.startswith("__bar")}

    def emit(self):
        nc = self.nc
        for op in self.ops:
            for d in op.deps:
                if not d.dma:
                    d.sig = True
        cnt = {e: 0 for e in ENGS}
        for op in self.ops:
            if not op.dma and op.sig:
                cnt[op.eng] += 1
                op.idx = cnt[op.eng]
        dkeys = sorted(self.dma_cnt.keys())
        with ExitStack() as st:
            esem = {e: st.enter_context(nc.semaphore("es_" + e)) for e in ENGS}
            dsem = {k: st.enter_context(nc.semaphore("ds%d" % i)) for i, k in enumerate(dkeys)}
            waited = {e: {} for e in ENGS}
            for op in self.ops:
                need = {}
                for d in op.deps:
                    if d.dma:
                        key, val = ("d", d.dkey), d.tgt
                    else:
                        if d.eng == op.eng and op.eng == "pe":
                            continue
                        key, val = ("e", d.eng), d.idx
                    if need.get(key, 0) < val:
                        need[key] = val
                w = waited[op.eng]
                for key, val in need.items():
                    if w.get(key, 0) < val:
                        w[key] = val
                        op.waits.append((dsem[key[1]] if key[0] == "d" else esem[key[1]], val))
            block = st.enter_context(nc.Block())

            def run(engname):
                def body(e):
                    for op in self.ops:
                        if op.eng != engname:
                            continue
                        for (s, v) in op.waits:
                            e.wait_ge(s, v)
                        ins = op.fn(e)
                        if op.dma:
                            ins.then_inc(dsem[op.dkey], 16)
                        elif op.sig:
                            ins.then_inc(esem[engname], 1)
                return body

            block.sync(run("sp"))
            block.scalar(run("act"))
            block.vector(run("dve"))
            block.gpsimd(run("pool"))
            block.tensor(run("pe"))


class Cfg:
    def __init__(self, nkv=4, nfh=0, nm=32, cap=512):
        self.NKV, self.NFH, self.NM, self.CAP = nkv, nfh, nm, cap
        self.NTE = nkv + nfh + nm
        self.NTL = nfh + nm
        self.NST = self.NTE // 4
        self.CT = cap // 128
        self.NTOK = self.NTL * 128
        self.TRASH = 2 * self.NTOK + cap
        self.XSR = self.TRASH + 2 * max(nfh, 1) * 128
        self.NOW = -(-2 * self.NTOK // cap)
        self.NTHR = -(-self.NTOK // cap)
        assert self.NTE % 4 == 0 and nkv % 4 == 0 and nfh % 4 == 0 and cap % 128 == 0


PERL = frozenset(["ada_w", "ada_b", "n1c", "n2c", "w_in", "w_out", "pool_w", "pscale", "gq", "gk", "wr", "br", "wg", "wu", "wd"])
TRC = 4


class _Ctx:
    pass


def build_program(cfgs, debug=False):
    nc = bass.Bass("TRN2", target_bir_lowering=False)
    ctx = _Ctx()
    ctx.nc, ctx.memo, ctx.p, ctx.nl = nc, {}, PB(nc), len(cfgs)
    with ExitStack() as st:
        ctx.st = st
        for li, cfg in enumerate(cfgs):
            _emit_layer(ctx, li, cfg, debug)
        ctx.p.emit()
    return nc


def build_layer(cfg, debug=False):
    return build_program([cfg], debug)


def _emit_layer(ctx, li, cfg, debug=False):
    nc, st, memo = ctx.nc, ctx.st, ctx.memo
    last = li == ctx.nl - 1
    NTE, NTL, NST, NKV, NFH, CT, CAP = cfg.NTE, cfg.NTL, cfg.NST, cfg.NKV, cfg.NFH, cfg.CT, cfg.CAP

    def din(name, shape, dt=F32):
        nm = name + ("_%d" % li if name in PERL else "")
        if nm not in memo:
            memo[nm] = nc.dram_tensor(nm, list(shape), dt, kind="ExternalInput").ap()
        return memo[nm]

    def dscr(name, shape, dt, kind="Internal"):
        if name not in memo:
            memo[name] = nc.dram_tensor(name, list(shape), dt, kind=kind).ap()
        return memo[name]

    xe = din("xe", [NTE * 128, D]) if li == 0 else memo["x1_%d" % (li - 1)]
    cT = din("cT", [128, 8])
    ada_w = din("ada_w", [D, 6 * D])
    ada_b = din("ada_b", [1, 6 * D])
    n1c = din("n1c", [128, 8])
    n2c = din("n2c", [128, 8])
    w_in = din("w_in", [D, 2048])
    w_out = din("w_out", [D, D])
    pool_w = din("pool_w", [128, 4, 128])
    pscale = din("pscale", [128, 4])
    gq = din("gq", [128, 1])
    gk = din("gk", [128, 1])
    btab = din("btab", [128, 8, 5, 128])
    bmask = din("bmask", [128, 5, 128])
    wr = din("wr", [D, 36])
    br = din("br", [128, 36])
    wg = din("wg", [NEXP * D, 512])
    wu = din("wu", [NEXP * D, 512])
    wd = din("wd", [NEXP * 512, D])
    hv = din("hv", [128, 1])
    nhv = din("nhv", [128, 1])
    invc = din("invc", [128, 4, 16])
    tri = din("tri", [128, 128])
    iot = din("iot", [128, 4])
    trashi = din("trashi", [128, 2 * TRC])
    thr = din("thr", [128, 16])
    wv = din("wv", [128, 32])
    ev = din("ev", [128, 32])
    iot8 = din("iot8", [128, 8])
    if last:
        xo = nc.dram_tensor("xo", [NTL * 128, D], F32, kind="ExternalOutput").ap()
    else:
        xo = dscr("x1_%d" % li, [NTL * 128, D], F32)
    xmid = dscr("xmid", [NTL * 128, D], F32, kind="ExternalOutput" if debug else "Internal")
    if debug:
        dbg_logits = nc.dram_tensor("dbg_logits", [128, NTL, 36], F32, kind="ExternalOutput").ap()
        dbg_w = nc.dram_tensor("dbg_w", [128, 2, NTL], F32, kind="ExternalOutput").ap()
        dbg_slot = nc.dram_tensor("dbg_slot", [128, 2, NTL], I32, kind="ExternalOutput").ap()
    xn2s = dscr("xn2s", [NTL * 128, D], BF16)
    xs = dscr("xs", [cfg.XSR, D], BF16)
    ys = dscr("ys", [cfg.XSR, D], F32)

    if True:
        def T(name, shape, dt=F32):
            if name in memo:
                t, shp = memo[name]
                if list(shp) != list(shape):
                    assert len(shp) == len(shape) and shape[1] <= shp[1] and list(shp[2:]) == list(shape[2:]), (name, shp, shape)
                    return t[:, 0:shape[1]]
                return t
            t = st.enter_context(nc.sbuf_tensor(name, list(shape), dt))
            memo[name] = (t, list(shape))
            return t

        def PS(name, shape, dt=F32):
            if name not in memo:
                memo[name] = st.enter_context(nc.psum_tensor(name, list(shape), dt))
            return memo[name]

        ident = T("ident", [128, 128], BF16)
        identf = T("identf", [128, 128])
        ones_bf = T("ones_bf", [128, 128], BF16)
        blk1 = T("blk1", [128, 128], BF16)
        tri_bf = T("tri_bf", [128, 128], BF16)
        onesf = T("onesf", [1, 128])
        epsc = T("epsc", [128, 1])
        eps64 = T("eps64", [128, 1])
        cact = T("cact", [128, 8], BF16)
        ctf = T("ctf", [128, 8])
        n1t = T("n1t", [128, 8])
        n2t = T("n2t", [128, 8])
        modc = T("modc", [128, 4, 8])
        mul1c = T("mul1c", [128, 8])
        mul2c = T("mul2c", [128, 8])
        add1b = T("add1b", [128, 8], BF16)
        g2bc = T("g2bc", [128, D])
        bzc = T("bzc", [128, 12])
        bzv = T("bzv", [128, 512])
        bzvm = T("bzvm", [128, 512])
        pw_bf = T("pw_bf", [128, 4, 128], BF16)
        psc = T("psc", [128, 4])
        gqk = T("gqk", [128, 1])
        gkt = T("gkt", [128, 1])
        BT = T("BT", [128, 8, 5, 128], BF16)
        wr_f = T("wr_f", [128, 8, 36])
        wr_bf = T("wr_bf", [128, 8, 36], BF16)
        wr_raw = T("wr_raw", [128, 8, 36], BF16)
        biasR = T("biasR", [128, 36])
        hvt = T("hvt", [128, 1])
        nhvt = T("nhvt", [128, 1])
        invct = T("invct", [128, 4, 16])
        iott = T("iott", [128, 4])
        trt = T("trt", [128, 2 * TRC])
        thrt = T("thrt", [128, 16])
        wvt = T("wvt", [128, 32])
        evt = T("evt", [128, 32])
        iot8t = T("iot8t", [128, 8])
        ssr = T("ssr", [128, 8])
        rst = T("rst", [128, 8])
        logits = T("logits", [128, NTL, 36])
        w1g = T("w1g", [128, NTL])
        w2g = T("w2g", [128, NTL])
        slot1 = T("slot1", [128, NTL], I32)
        slot2 = T("slot2", [128, NTL], I32)
        widx = T("widx", [128, NEXP, CT], I32)

        AF_WORDS = 14400
        AB_WORDS = 57920
        arf = T("arf", [128, AF_WORDS])
        arb = T("arb", [128, AB_WORDS], BF16)

        class Carver:
            def __init__(self, t, n):
                self.t, self.n, self.off = t, n, 0

            def take(self, *shape):
                n = int(np.prod(shape))
                a = self.t[:, self.off:self.off + n]
                self.off += n
                assert self.off <= self.n, (self.off, self.n)
                if len(shape) == 2:
                    return a.rearrange("p (a b) -> p a b", a=shape[0])
                if len(shape) == 3:
                    return a.rearrange("p (a b c) -> p a b c", a=shape[0], b=shape[1])
                return a

        pT = PS("pT", [128, 1024], BF16)
        pZ = PS("pZ", [128, 2, 512])
        pC = PS("B3", [128, 512])
        pS = PS("pS", [128, 1536])
        pV = PS("B7", [128, 512])

        p = ctx.p
        A = p.add

        def dma(eng, out, in_, reads, writes, dkey=None):
            return A(eng, lambda e: e.dma_start(out=out, in_=in_), reads=reads, writes=writes, dma=True, dkey=dkey)

        A("pool", lambda e: e.memset(identf[:], 0.0), writes=["identf"])
        A("pool", lambda e: e.affine_select(out=identf[:], in_=identf[:], pattern=[[-1, 128]], compare_op=ALU.not_equal,
                                            fill=1.0, base=0, channel_multiplier=1), reads=["identf"], writes=["identf"])
        A("dve", lambda e: e.tensor_copy(out=ident[:], in_=identf[:]), reads=["identf"], writes=["ident"])
        A("pool", lambda e: e.memset(ones_bf[:], 1.0), writes=["ones_bf"])
        A("pool", lambda e: e.memset(blk1[:], 0.0), writes=["blk1"])
        A("pool", lambda e: e.memset(blk1[0:64, 0:64], 1.0), reads=["blk1"], writes=["blk1"])
        A("pool", lambda e: e.memset(blk1[64:128, 64:128], 1.0), reads=["blk1"], writes=["blk1"])
        A("pool", lambda e: e.memset(onesf[:], 1.0), writes=["onesf"])
        A("pool", lambda e: e.memset(epsc[:], EPS), writes=["epsc"])
        A("pool", lambda e: e.memset(eps64[:], 64 * EPS), writes=["eps64"])
        for (dst, src, k) in ((ctf, cT, "ctf"), (n1t, n1c, "n1t"), (n2t, n2c, "n2t"), (psc, pscale, "psc"), (gqk, gq, "gqk"),
                              (gkt, gk, "gkt"), (hvt, hv, "hvt"), (nhvt, nhv, "nhvt"), (invct, invc, "invct"),
                              (iott, iot, "iott"), (trt, trashi, "trt"), (thrt, thr, "thrt"), (wvt, wv, "wvt"), (evt, ev, "evt"), (iot8t, iot8, "iot8t"), (biasR, br, "biasR"), (identf, tri, "identf")):
            dma("sp", dst[:], src, [], [k])
        A("dve", lambda e: e.tensor_copy(out=tri_bf[:], in_=identf[:]), reads=["identf"], writes=["tri_bf"])
        A("dve", lambda e: e.tensor_mul(out=gqk[:], in0=gqk[:], in1=gkt[:]), reads=["gqk", "gkt"], writes=["gqk"])
        dma("pool", pw_bf[:], pool_w, [], ["pw_bf"])
        A("act", lambda e: e.activation(out=cact[:], in_=ctf[:], func=AF.Silu), reads=["ctf"], writes=["cact"])

        cf = Carver(arf, AF_WORDS)
        cb_ = Carver(arb, AB_WORDS)
        g1bc = cf.take(D)
        modrow = cf.take(6 * D)[0:1, :]
        adab = cf.take(6 * D)[0:1, :]
        stage = [cb_.take(8, 1536) for _ in range(2)]
        zf = cf.take(D)
        zb = cb_.take(D)
        A("pool", lambda e: e.memset(zf[:], 0.0), writes=["zf"])
        A("pool", lambda e: e.memset(zb[:], 0.0), writes=["zb"])
        for r0 in range(2 * cfg.NM * 128, cfg.TRASH, 128):
            dma("sp", xs[r0:r0 + 128, :], zb[:], ["zb"], ["xs_z%d" % r0], dkey="zinit")
        for r0 in range(cfg.TRASH, cfg.TRASH + 2 * NFH * 128, 128):
            dma("sp", ys[r0:r0 + 128, :], zf[:], ["zf"], ["ys_z%d" % r0], dkey="zinit")
        dma("sp", adab, ada_b, [], ["adab"])
        for g in range(4):
            sb = stage[g % 2]
            dma("pool", sb[:], ada_w[:, g * 1536:(g + 1) * 1536].rearrange("(k p) n -> p k n", p=128), [], ["stage%d" % (g % 2)])
            for cbk in range(3):
                col = g * 1536 + cbk * 512
                bank = cbk % 2
                for kc in range(8):
                    A("pe", lambda e, sb=sb, kc=kc, cbk=cbk, bank=bank: e.matmul(
                        pZ[0:1, bank, :], lhsT=cact[:, kc:kc + 1], rhs=sb[:, kc, cbk * 512:(cbk + 1) * 512],
                        start=(kc == 0), stop=(kc == 7)), reads=["cact", "stage%d" % (g % 2)], writes=["pZ%d" % bank])
                A("dve", lambda e, col=col, bank=bank: e.tensor_tensor(out=modrow[:, col:col + 512], in0=pZ[0:1, bank, :],
                                                                       in1=adab[:, col:col + 512], op=ALU.add),
                  reads=["pZ%d" % bank, "adab"], writes=["modrow"])
        for vi, base in enumerate((0, D, 3 * D, 4 * D)):
            for kc in range(8):
                A("pe", lambda e, vi=vi, base=base, kc=kc: e.matmul(
                    pC[:, vi * 8 + kc: vi * 8 + kc + 1], lhsT=modrow[:, base + kc * 128: base + (kc + 1) * 128],
                    rhs=onesf[:, 0:1], start=True, stop=True), reads=["modrow", "onesf"], writes=["B3"])
        A("dve", lambda e: e.tensor_copy(out=modc[:], in_=pC[:, 0:32].rearrange("p (a b) -> p a b", a=4)), reads=["B3"], writes=["modc"])
        A("dve", lambda e: e.scalar_tensor_tensor(out=mul1c[:], in0=modc[:, 1, :], scalar=1.0, in1=n1t[:], op0=ALU.add, op1=ALU.mult),
          reads=["modc", "n1t"], writes=["mul1c"])
        A("dve", lambda e: e.scalar_tensor_tensor(out=mul2c[:], in0=modc[:, 3, :], scalar=1.0, in1=n2t[:], op0=ALU.add, op1=ALU.mult),
          reads=["modc", "n2t"], writes=["mul2c"])
        A("dve", lambda e: e.tensor_copy(out=add1b[:], in_=modc[:, 0, :]), reads=["modc"], writes=["add1b"])
        for (dst, base, k) in ((g1bc, 2 * D, "g1bc"), (g2bc, 5 * D, "g2bc")):
            for hb in range(2):
                A("pe", lambda e, base=base, hb=hb: e.matmul(pZ[:, hb, :], lhsT=onesf[:, :], rhs=modrow[:, base + hb * 512: base + (hb + 1) * 512],
                                                            start=True, stop=True), reads=["modrow", "onesf"], writes=["pZ%d" % hb])
                A("dve", lambda e, dst=dst, hb=hb: e.tensor_copy(out=dst[:, hb * 512:(hb + 1) * 512], in_=pZ[:, hb, :]),
                  reads=["pZ%d" % hb], writes=[k])
        p.barrier()

        cf = Carver(arf, AF_WORDS)
        cb_ = Carver(arb, AB_WORDS)
        w_in_bf = cb_.take(8, 2048)
        w_out_bf = cb_.take(8, D)
        g1bc = cf.take(D)
        add2rep = cb_.take(8, 128)
        wst = [cf.take(2, 2048) for _ in range(2)]
        wraw = cb_.take(8, 2048)
        bzrow = cf.take(2048)[0:1, :]
        bzrow_b = cb_.take(512)[0:1, :]
        for pc in range(4):
            sbf = wst[pc % 2]
            dma("sp", sbf[:], w_in[pc * 256:(pc + 1) * 256, :].rearrange("(k p) n -> p k n", p=128), [], ["wst%d" % (pc % 2)])
            for kk in range(2):
                kc = pc * 2 + kk
                A("dve", lambda e, sbf=sbf, kk=kk, kc=kc: e.tensor_scalar(out=w_in_bf[:, kc, :], in0=sbf[:, kk, :], scalar1=mul1c[:, kc:kc + 1],
                                                                       scalar2=None, op0=ALU.mult),
                  reads=["wst%d" % (pc % 2), "mul1c"], writes=["w_in_bf"])
                A("act", lambda e, sbf=sbf, kk=kk, kc=kc: e.activation(out=wraw[:, kc, :], in_=sbf[:, kk, :], func=AF.Copy),
                  reads=["wst%d" % (pc % 2)], writes=["wraw"])
        for cbk in range(4):
            for kc in range(8):
                A("pe", lambda e, cbk=cbk, kc=kc: e.matmul(pZ[0:1, cbk % 2, :], lhsT=add1b[:, kc:kc + 1], rhs=wraw[:, kc, cbk * 512:(cbk + 1) * 512],
                                                          start=(kc == 0), stop=(kc == 7)), reads=["add1b", "wraw"], writes=["pZ%d" % (cbk % 2)])
            A("dve", lambda e, cbk=cbk: e.tensor_copy(out=bzrow[:, cbk * 512:(cbk + 1) * 512], in_=pZ[0:1, cbk % 2, :]),
              reads=["pZ%d" % (cbk % 2)], writes=["bzrow"])
        for oc in range(12):
            A("pe", lambda e, oc=oc: e.matmul(pC[:, oc:oc + 1], lhsT=bzrow[:, oc * 128:(oc + 1) * 128], rhs=onesf[:, 0:1], start=True, stop=True),
              reads=["bzrow", "onesf"], writes=["B3"])
        A("dve", lambda e: e.tensor_copy(out=bzc[:], in_=pC[:, 0:12]), reads=["B3"], writes=["bzc"])
        A("pe", lambda e: e.matmul(pZ[:, 0, :], lhsT=onesf[:, :], rhs=bzrow[:, 1536:2048], start=True, stop=True),
          reads=["bzrow", "onesf"], writes=["pZ0"])
        A("dve", lambda e: e.tensor_copy(out=bzv[:], in_=pZ[:, 0, :]), reads=["pZ0"], writes=["bzv"])
        A("dve", lambda e: e.tensor_scalar(out=bzvm[:], in0=bzv[:], scalar1=hvt[:, 0:1], scalar2=None, op0=ALU.mult),
          reads=["bzv", "hvt"], writes=["bzvm"])
        wost = [wst[0][:, :, 0:D], wst[1][:, :, 0:D]]
        for pc in range(4):
            sbf = wost[pc % 2]
            dma("sp", sbf[:], w_out[pc * 256:(pc + 1) * 256, :].rearrange("(k p) n -> p k n", p=128), [], ["wst%d" % (pc % 2)])
            for kk in range(2):
                kc = pc * 2 + kk
                A("dve", lambda e, sbf=sbf, kk=kk, kc=kc: e.tensor_tensor(out=w_out_bf[:, kc, :], in0=sbf[:, kk, :], in1=g1bc[:], op=ALU.mult),
                  reads=["wst%d" % (pc % 2), "g1bc"], writes=["w_out_bf"])
        btf = cf.take(5, 128)
        pen = cf.take(5, 128)
        mk = cf.take(5, 128)
        dma("sp", mk[:], bmask, [], ["mk"])
        A("dve", lambda e: e.tensor_scalar(out=pen[:], in0=mk[:], scalar1=3750.0, scalar2=-3750.0, op0=ALU.mult, op1=ALU.add),
          reads=["mk"], writes=["pen"])
        for h in range(8):
            dma("sp", btf[:], btab[:, h, :, :], [], ["btf"])
            A("dve", lambda e: e.tensor_tensor(out=btf[:], in0=btf[:], in1=mk[:], op=ALU.mult), reads=["btf", "mk"], writes=["btf"])
            A("dve", lambda e, h=h: e.scalar_tensor_tensor(out=BT[:, h, :, :], in0=btf[:], scalar=0.125, in1=pen[:], op0=ALU.mult, op1=ALU.add),
              reads=["btf", "pen"], writes=["BT"])
        dma("sp", wr_f[:], wr.rearrange("(k p) n -> p k n", p=128), [], ["wr_f"])
        A("dve", lambda e: e.tensor_copy(out=wr_raw[:], in_=wr_f[:]), reads=["wr_f"], writes=["wr_raw"])
        for kc in range(8):
            A("dve", lambda e, kc=kc: e.tensor_scalar(out=wr_bf[:, kc, :], in0=wr_f[:, kc, :], scalar1=mul2c[:, kc:kc + 1], scalar2=None, op0=ALU.mult),
              reads=["wr_f", "mul2c"], writes=["wr_bf"])
            A("dve", lambda e, kc=kc: e.tensor_copy(out=add2rep[:, kc, :], in_=modc[:, 2, kc:kc + 1].to_broadcast([128, 128])),
              reads=["modc"], writes=["add2rep"])
        for kc in range(8):
            A("pe", lambda e, kc=kc: e.matmul(pC[:, 0:36], lhsT=add2rep[:, kc, :], rhs=wr_raw[:, kc, :], start=(kc == 0), stop=(kc == 7)),
              reads=["add2rep", "wr_raw"], writes=["B3"])
        A("dve", lambda e: e.tensor_tensor(out=biasR[:], in0=pC[:, 0:36], in1=biasR[:], op=ALU.add), reads=["B3", "biasR"], writes=["biasR"])
        p.barrier()

        cf = Carver(arf, AF_WORDS)
        cb_ = Carver(arb, AB_WORDS)
        w_in_bf = cb_.take(8, 2048)
        w_out_bf = cb_.take(8, D)
        xin = [cf.take(D) for _ in range(2)]
        xr = [cf.take(D) for _ in range(2)]
        xmd = [cf.take(D) for _ in range(2)]
        qf = [cf.take(512) for _ in range(2)]
        rq = [cf.take(512) for _ in range(2)]
        uT = [[cf.take(528) for _ in range(4)] for _ in range(2)]
        ptmp = [cf.take(528) for _ in range(2)]
        rden = cf.take(2, 4)
        xn = [cb_.take(D) for _ in range(2)]
        hT_ = cb_.take(8, 512)
        hT = [hT_, hT_]
        kT = cb_.take(4, RT * 128)
        Vr = cb_.take(RT, 8 * 65).rearrange("p r (h d) -> p r h d", h=8)
        qTm = [cb_.take(4, 512) for _ in range(2)]
        sq = [cb_.take(512) for _ in range(2)]
        pTt = cb_.take(4, 512)
        mixT_ = cb_.take(8, 512)
        mixT = [mixT_, mixT_]
        PTb = [cb_.take(2, 640) for _ in range(2)]
        att = [cb_.take(512) for _ in range(2)]
        xn2 = [cb_.take(D) for _ in range(2)]
        xn2T = [cb_.take(8, 128) for _ in range(2)]

        for b in range(2):
            for g in range(4):
                A("pool", lambda e, b=b, g=g: e.memset(uT[b][g][:, 0:16], 0.0), writes=["uT%d%d" % (b, g)])
        A("pool", lambda e: e.memset(qTm[0][64:128, :, :], 0.0), writes=["qT"])
        A("pool", lambda e: e.memset(qTm[1][0:64, :, :], 0.0), writes=["qT"])

        SSOFF = (0, 640)
        PVR = ((pS, 1280), (pV, 0), (pV, 256))
        HG = ((0, 1, 2), (3, 4, 5), (6, 7))

        def norm_and_transpose(src, srckey, sl, dstT, dstTkeys, dstcols, xnbuf, xnkey, store_to=None, scale_eng="dve", defer=False):
            A("act", lambda e: e.activation(out=xnbuf[:], in_=src, func=AF.Square, accum_out=ssr[:, sl:sl + 1]),
              reads=[srckey], writes=[xnkey, "ssr%d" % sl])
            A("act", lambda e: e.activation(out=rst[:, sl:sl + 1], in_=ssr[:, sl:sl + 1], func=AF.Ln, scale=1.0 / D, bias=epsc[:]),
              reads=["ssr%d" % sl, "epsc"], writes=["rst%d" % sl])
            A("act", lambda e: e.activation(out=rst[:, sl:sl + 1], in_=rst[:, sl:sl + 1], func=AF.Exp, scale=-0.5),
              reads=["rst%d" % sl], writes=["rst%d" % sl])
            if scale_eng == "dve":
                A("dve", lambda e: e.tensor_scalar(out=xnbuf[:], in0=src, scalar1=rst[:, sl:sl + 1], scalar2=None, op0=ALU.mult),
                  reads=[srckey, "rst%d" % sl], writes=[xnkey])
            else:
                A("act", lambda e: e.activation(out=xnbuf[:], in_=src, func=AF.Copy, scale=rst[:, sl:sl + 1]),
                  reads=[srckey, "rst%d" % sl], writes=[xnkey])
            if store_to is not None:
                dma("pool", store_to, xnbuf[:], [xnkey], ["xn2s"], dkey="st_" + xnkey)

            def part_b():
                for kc in range(8):
                    A("pe", lambda e, kc=kc: e.transpose(out=pT[:, kc * 128:(kc + 1) * 128], in_=xnbuf[:, kc * 128:(kc + 1) * 128], identity=ident[:]),
                      reads=[xnkey, "ident"], writes=["pT"])
                A("dve", lambda e: e.tensor_copy(out=dstT[:, :, dstcols], in_=pT[:].rearrange("p (a b) -> p a b", a=8)),
                  reads=["pT"], writes=dstTkeys)
            if defer:
                return part_b
            part_b()

        ZB = [(pZ[:, 0, :], "pZ0"), (pZ[:, 1, :], "pZ1"), (pS[:, 0:512], "B4"), (pS[:, 512:1024], "B5"), (pS[:, 1024:1536], "B6")]
        SB = [(pC, "B3"), (pV, "B7")]
        zcnt = {"z": 0, "s": 0}

        def in_chunk(s, oc, ub, slot0, halo_st, full_st):
            zps, zk = ZB[zcnt["z"] % 5]
            zcnt["z"] += 1
            kslots = ["kT%d" % (slot0 + i) for i in range(4)]
            for kc in range(8):
                A("pe", lambda e, kc=kc: e.matmul(zps, lhsT=w_in_bf[:, kc, oc * 128:(oc + 1) * 128], rhs=hT_[:, kc, :],
                                                  start=(kc == 0), stop=(kc == 7)), reads=["w_in_bf", "hT"], writes=[zk])
            if oc < 4:
                g = oc
                if halo_st:
                    A("dve", lambda e: e.tensor_scalar(out=uT[ub][g][:, 16:528], in0=zps, scalar1=bzc[:, oc:oc + 1],
                                                       scalar2=hvt[:, 0:1], op0=ALU.add, op1=ALU.mult),
                      reads=[zk, "bzc", "hvt"], writes=["uT%d%d" % (ub, g)])
                else:
                    A("dve", lambda e: e.tensor_scalar(out=uT[ub][g][:, 16:528], in0=zps, scalar1=bzc[:, oc:oc + 1],
                                                       scalar2=None, op0=ALU.add),
                      reads=[zk, "bzc"], writes=["uT%d%d" % (ub, g)])
                return None
            isq = oc < 8
            c = (oc - 4) % 4
            tb = oc % 2
            A("dve", lambda e: e.tensor_scalar(out=qf[tb][:], in0=zps, scalar1=bzc[:, oc:oc + 1], scalar2=None, op0=ALU.add),
              reads=[zk, "bzc"], writes=["qf%d" % tb])
            A("act", lambda e: e.activation(out=sq[tb][:], in_=qf[tb][:], func=AF.Square),
              reads=["qf%d" % tb], writes=["sq%d" % tb])
            sps, sk = SB[zcnt["s"] % 2]
            zcnt["s"] += 1

            def part2():
                A("pe", lambda e: e.matmul(sps[:], lhsT=blk1[:], rhs=sq[tb][:], start=True, stop=True), reads=["blk1", "sq%d" % tb], writes=[sk])
                A("act", lambda e: e.activation(out=rq[tb][:], in_=sps[:], func=AF.Ln, bias=eps64[:]), reads=[sk, "eps64"], writes=["rq%d" % tb])
                A("act", lambda e: e.activation(out=rq[tb][:], in_=rq[tb][:], func=AF.Exp, scale=-0.5), reads=["rq%d" % tb], writes=["rq%d" % tb])
                if isq:
                    A("dve", lambda e: e.tensor_tensor(out=qTm[0][0:64, c, :], in0=qf[tb][0:64, :], in1=rq[tb][0:64, :], op=ALU.mult),
                      reads=["qf%d" % tb, "rq%d" % tb], writes=["qT"])
                    A("dve", lambda e: e.tensor_tensor(out=qTm[1][64:128, c, :], in0=qf[tb][64:128, :], in1=rq[tb][64:128, :], op=ALU.mult),
                      reads=["qf%d" % tb, "rq%d" % tb], writes=["qT"])
                else:
                    A("dve", lambda e: e.scalar_tensor_tensor(out=kT[:, c, slot0 * 128:(slot0 + 4) * 128], in0=qf[tb][:], scalar=gqk[:, 0:1],
                                                              in1=rq[tb][:], op0=ALU.mult, op1=ALU.mult),
                      reads=["qf%d" % tb, "rq%d" % tb, "gqk"], writes=kslots)
            return part2

        def v_tile(s, i, halo_st):
            te = 4 * s + i
            sl = te % RT
            zps, zk = ZB[zcnt["z"] % 5]
            zcnt["z"] += 1
            for kc in range(8):
                A("pe", lambda e, kc=kc: e.matmul(zps, lhsT=hT_[:, kc, i * 128:(i + 1) * 128], rhs=w_in_bf[:, kc, 1536:2048],
                                                  start=(kc == 0), stop=(kc == 7)), reads=["w_in_bf", "hT"], writes=[zk])
            zv = zps.rearrange("p (h d) -> p h d", h=8)
            if halo_st:
                A("dve", lambda e: e.scalar_tensor_tensor(out=Vr[:, sl, :, 0:64], in0=zv, scalar=hvt[:, 0:1],
                                                          in1=bzvm[:].rearrange("p (h d) -> p h d", h=8), op0=ALU.mult, op1=ALU.add),
                  reads=[zk, "hvt", "bzvm"], writes=["V%d" % sl])
                A("pool", lambda e: e.tensor_copy(out=Vr[:, sl, :, 64:65], in_=hvt[:, 0:1].unsqueeze(1).to_broadcast([128, 8, 1])),
                  reads=["hvt"], writes=["V%d" % sl])
            else:
                A("dve", lambda e: e.tensor_tensor(out=Vr[:, sl, :, 0:64], in0=zv, in1=bzv[:].rearrange("p (h d) -> p h d", h=8), op=ALU.add),
                  reads=[zk, "bzv"], writes=["V%d" % sl])
                A("pool", lambda e: e.memset(Vr[:, sl, :, 64:65], 1.0), writes=["V%d" % sl])

        def pool_group(g, ub, first_main):
            U = uT[ub][g]
            uk = "uT%d%d" % (ub, g)
            cur, curk = U, uk
            sh = 1
            for stp in range(g + 1):
                dstb = ptmp[stp % 2]
                dk = "ptmp%d" % (stp % 2)
                lo = 2 * sh - 1
                A("pool", lambda e, cur=cur, dstb=dstb, lo=lo, sh=sh: e.tensor_tensor(out=dstb[:, lo:528], in0=cur[:, lo:528], in1=cur[:, lo - sh:528 - sh], op=ALU.add),
                  reads=[curk], writes=[dk])
                cur, curk = dstb, dk
                sh *= 2
            w = 2 ** (g + 1)
            fin = cur
            A("dve", lambda e: e.scalar_tensor_tensor(out=pTt[:, g, :], in0=fin[:, 16:528], scalar=1.0 / w, in1=U[:, 16:528],
                                                      op0=ALU.mult, op1=ALU.subtract),
              reads=[curk, uk], writes=["pTt%d" % g])
            if first_main:
                A("pool", lambda e: e.tensor_tensor(out=fin[:, 0:16], in0=fin[:, 16:32], in1=invct[:, g, :], op=ALU.mult),
                  reads=[curk, "invct"], writes=[curk])
                A("pool", lambda e: e.tensor_tensor(out=pTt[:, g, 0:16], in0=fin[:, 0:16], in1=U[:, 16:32], op=ALU.subtract),
                  reads=[curk, uk], writes=["pTt%d" % g])
            A("pe", lambda e: e.matmul(pC[:], lhsT=pw_bf[:, g, :], rhs=pTt[:, g, :], start=True, stop=True), reads=["pw_bf", "pTt%d" % g], writes=["B3"])
            A("act", lambda e: e.activation(out=mixT_[:, g, :], in_=pC[:], func=AF.Copy, scale=psc[:, g:g + 1]),
              reads=["B3", "psc"], writes=["mixT"])

        def attn_pair(te, i, pr, ab):
            c = pr
            pb2 = pr % 2
            PTp = PTb[pb2]
            ptk = "PT%d" % pb2
            for hh in range(2):
                pb = 64 * hh
                h = 2 * pr + hh
                for t in range(4):
                    ksl = (te - 4 + t) % RT
                    A("pe", lambda e, t=t, ksl=ksl, pb=pb, hh=hh: e.matmul(
                        pS[:, hh * 512 + t * 128: hh * 512 + (t + 1) * 128], lhsT=kT[:, c, ksl * 128:(ksl + 1) * 128],
                        rhs=qTm[hh][:, c, i * 128:(i + 1) * 128], start=True, stop=False),
                      reads=["kT%d" % ksl, "qT"], writes=["B%d" % (4 + hh)])
                    A("pe", lambda e, t=t, h=h, hh=hh: e.matmul(pS[:, hh * 512 + t * 128: hh * 512 + (t + 1) * 128], lhsT=BT[:, h, t, :], rhs=ident[:],
                                                                start=False, stop=True), reads=["BT", "ident"], writes=["B%d" % (4 + hh)])
            ksl4 = te % RT
            for hh in range(2):
                pb = 64 * hh
                h = 2 * pr + hh
                A("pe", lambda e, pb=pb, hh=hh: e.matmul(
                    pS[:, 1024 + hh * 128: 1024 + (hh + 1) * 128], lhsT=kT[:, c, ksl4 * 128:(ksl4 + 1) * 128],
                    rhs=qTm[hh][:, c, i * 128:(i + 1) * 128], start=True, stop=False),
                  reads=["kT%d" % ksl4, "qT"], writes=["B6"])
                A("pe", lambda e, h=h, hh=hh: e.matmul(pS[:, 1024 + hh * 128: 1024 + (hh + 1) * 128], lhsT=BT[:, h, 4, :], rhs=ident[:],
                                                       start=False, stop=True), reads=["BT", "ident"], writes=["B6"])
            for hh in range(2):
                A("act", lambda e, hh=hh: e.activation(out=PTp[:, hh, 0:512], in_=pS[:, hh * 512:(hh + 1) * 512], func=AF.Exp, scale=8.0),
                  reads=["B%d" % (4 + hh)], writes=[ptk])
            A("act", lambda e: e.activation(out=PTp[:, :, 512:640], in_=pS[:, 1024:1280].rearrange("p (a b) -> p a b", a=2), func=AF.Exp, scale=8.0),
              reads=["B6"], writes=[ptk])
            for hh in range(2):
                h = 2 * pr + hh
                pvt, pvk = (pV, "B7") if h < 4 else (pC, "B3")
                co = (h % 4) * 65
                for t in range(5):
                    ksl = (te - 4 + t) % RT
                    A("pe", lambda e, t=t, ksl=ksl, hh=hh, h=h, pvt=pvt, co=co: e.matmul(
                        pvt[:, co: co + 65], lhsT=PTp[:, hh, t * 128:(t + 1) * 128], rhs=Vr[:, ksl, h, :],
                        start=(t == 0), stop=(t == 4)), reads=[ptk, "V%d" % ksl], writes=[pvk])

        def attn_norm(hgi, ab):
            pvt, pvk = (pV, "B7") if hgi == 0 else (pC, "B3")
            pvv = pvt[:, 0:260].rearrange("p (h d) -> p h d", h=4)
            A("dve", lambda e: e.tensor_scalar(out=rden[:, hgi, :].unsqueeze(2), in0=pvv[:, :, 64:65], scalar1=1e-30, scalar2=None, op0=ALU.add),
              reads=[pvk], writes=["rden%d" % hgi])
            A("dve", lambda e: e.reciprocal(out=rden[:, hgi, :], in_=rden[:, hgi, :]),
              reads=["rden%d" % hgi], writes=["rden%d" % hgi])
            A("dve", lambda e: e.tensor_tensor(
                out=att[ab][:, hgi * 256:(hgi + 1) * 256].rearrange("p (h d) -> p h d", h=4), in0=pvv[:, :, 0:64],
                in1=rden[:, hgi, :].unsqueeze(2).to_broadcast([128, 4, 64]), op=ALU.mult),
              reads=[pvk, "rden%d" % hgi], writes=["att%d" % ab])

        def attention_tile(s, i):
            te = 4 * s + i
            ab = te % 2
            for pr in range(4):
                attn_pair(te, i, pr, ab)
                if pr % 2 == 1:
                    attn_norm(pr // 2, ab)

        def post_attention(s, i):
            te = 4 * s + i
            tl = te - NKV
            ab = te % 2
            for c in range(4):
                A("pe", lambda e, c=c: e.transpose(out=pT[:, c * 128:(c + 1) * 128], in_=att[ab][:, c * 128:(c + 1) * 128], identity=ident[:]),
                  reads=["att%d" % ab, "ident"], writes=["pT"])
            A("dve", lambda e: e.tensor_copy(out=mixT_[:, 4:8, i * 128:(i + 1) * 128], in_=pT[:, 0:512].rearrange("p (a b) -> p a b", a=4)),
              reads=["pT"], writes=["mixT"])
            for cbk in range(2):
                for kc in range(8):
                    A("pe", lambda e, cbk=cbk, kc=kc: e.matmul(pZ[:, cbk, :], lhsT=mixT_[:, kc, i * 128:(i + 1) * 128], rhs=w_out_bf[:, kc, cbk * 512:(cbk + 1) * 512],
                                                               start=(kc == 0), stop=(kc == 7)), reads=["mixT", "w_out_bf"], writes=["pZ%d" % cbk])
            rb = te % 2
            dma("sp", xr[rb][:], xe[te * 128:(te + 1) * 128, :], [], ["xr%d" % rb])
            A("dve", lambda e: e.tensor_tensor(out=xmd[rb][:], in0=pZ[:].rearrange("p a b -> p (a b)"), in1=xr[rb][:], op=ALU.add),
              reads=["pZ0", "pZ1", "xr%d" % rb], writes=["xmd%d" % rb])
            dma("pool", xmid[tl * 128:(tl + 1) * 128, :], xmd[rb][:], ["xmd%d" % rb], ["xmid"], dkey="st_xmd%d" % rb)

            def norm2_a():
                pb_ = norm_and_transpose(xmd[rb][:], "xmd%d" % rb, te % 8, xn2T[rb], ["xn2T%d" % rb], slice(0, 128), xn2[rb], "xn2%d" % rb,
                                         store_to=xn2s[tl * 128:(tl + 1) * 128, :], scale_eng="act", defer=True)

                def part_b():
                    pb_()
                    for kc in range(8):
                        A("pe", lambda e, kc=kc: e.matmul(pC[:, 0:36], lhsT=xn2T[rb][:, kc, :], rhs=wr_bf[:, kc, :], start=(kc == 0), stop=(kc == 7)),
                          reads=["xn2T%d" % rb, "wr_bf"], writes=["B3"])
                    A("dve", lambda e: e.tensor_tensor(out=logits[:, tl, :], in0=pC[:, 0:36], in1=biasR[:], op=ALU.add),
                      reads=["B3", "biasR"], writes=["logits"])
                return part_b
            return norm2_a

        def tail_copy(ub, g):
            A("pool", lambda e: e.tensor_copy(out=uT[ub][g][:, 0:16], in_=uT[1 - ub][g][:, 512:528]),
              reads=["uT%d%d" % (1 - ub, g)], writes=["uT%d%d" % (ub, g)])

        def norm_tile(s, i, defer=False):
            te = 4 * s + i
            xi = te % 2
            dma("sp", xin[xi][:], xe[te * 128:(te + 1) * 128, :], [], ["xin%d" % xi])
            return norm_and_transpose(xin[xi][:], "xin%d" % xi, te % 8, hT_, ["hT"], slice(i * 128, (i + 1) * 128),
                                      xn[te % 2], "xn%d" % (te % 2), defer=defer)

        q_norm2 = []
        q_b = []

        def do_st(s):
            halo_st = (4 * s) < NKV + NFH
            full_st = (4 * s) >= NKV
            first_main = (4 * s) == NKV + NFH
            if s == 0:
                for i in range(4):
                    norm_tile(0, i)
            ub = s % 2
            if s > 0:
                for g in range(4):
                    tail_copy(ub, g)
            slot0 = (4 * s) % RT
            pend2 = None
            for oc in range(12):
                if 4 <= oc < 8 and not full_st:
                    continue
                p2 = in_chunk(s, oc, ub, slot0, halo_st, full_st)
                if pend2 is not None:
                    pend2()
                pend2 = p2
            v_tile(s, 0, halo_st)
            if pend2 is not None:
                pend2()
            for i in range(1, 4):
                v_tile(s, i, halo_st)
            if full_st:
                for g in range(4):
                    pool_group(g, ub, first_main)
            for i in range(4):
                nb = norm_tile(s + 1, i, defer=True) if s + 1 < NST else None
                if full_st:
                    attention_tile(s, i)
                    if q_norm2:
                        q_b.append(q_norm2.pop(0)())
                    if len(q_b) > 1:
                        q_b.pop(0)()
                if nb is not None:
                    nb()
                if full_st:
                    q_norm2.append(post_attention(s, i))
            if s == NST - 1:
                while q_norm2:
                    q_b.append(q_norm2.pop(0)())
                while q_b:
                    q_b.pop(0)()

        for s in range(NST):
            do_st(s)
        p.barrier()

        cf = Carver(arf, AF_WORDS)
        cb_ = Carver(arb, AB_WORDS)
        NL = NTL
        R1 = cf.take(NL, 32)
        R2 = cf.take(NL, 32)
        R3 = cf.take(NL, 32)
        R4 = cf.take(NL, 32)
        sm = [cf.take(NL) for _ in range(6)]
        cntb = [cf.take(32) for _ in range(2)]
        startb = cf.take(32)
        widf = cf.take(NEXP, CT)
        ybuf = [cf.take(D) for _ in range(2)]
        sgb = [cf.take(CAP) for _ in range(2)]
        xmb = [cf.take(D) for _ in range(2)]
        y1b = [cf.take(D) for _ in range(2)]
        y2b_ = cf.take(D)
        y2b = [y2b_, y2b_]
        Abf = cb_.take(NL, 32)
        xtl = [cb_.take(D) for _ in range(2)]
        xw = [cb_.take(CT, D) for _ in range(2)]
        xsT = cb_.take(8, CAP)
        actT = cb_.take(4, CAP)
        Wg = [cb_.take(8, 512) for _ in range(2)]
        Wu = [cb_.take(8, 512) for _ in range(2)]
        Wd = [cb_.take(4, D) for _ in range(2)]

        WGK = [["Wg%d_%d" % (b, kc) for kc in range(8)] for b in range(2)]
        WUK = [["Wu%d_%d" % (b, kc) for kc in range(8)] for b in range(2)]
        WDK = [["Wd%d_%d" % (b, jc) for jc in range(4)] for b in range(2)]

        def load_w(e_):
            b = e_ % 2
            dma("pool", Wg[b][:], wg[e_ * D:(e_ + 1) * D, :].rearrange("(k p) n -> p k n", p=128), [], WGK[b], dkey="Wg%d" % b)
            dma("pool", Wu[b][:], wu[e_ * D:(e_ + 1) * D, :].rearrange("(k p) n -> p k n", p=128), [], WUK[b], dkey="Wu%d" % b)
            dma("pool", Wd[b][:], wd[e_ * 512:(e_ + 1) * 512, :].rearrange("(k p) n -> p k n", p=128), [], WDK[b], dkey="Wd%d" % b)

        load_w(0)
        load_w(1)

        gl = logits[:, :, 0:4]
        el = logits[:, :, 4:36]
        V = lambda e: e
        gmax, gsum, m1, m2, dd, ee = sm
        gone = R1[:, :, 0:4]
        A("dve", lambda e: e.reduce_max(out=gmax[:], in_=gl, axis=AX.X), reads=["logits"], writes=["gmax"])
        A("dve", lambda e: e.tensor_tensor(out=gone, in0=gl, in1=gmax[:].unsqueeze(2).to_broadcast([128, NL, 4]), op=ALU.is_equal),
          reads=["logits", "gmax"], writes=["R1"])
        gex = R2[:, :, 0:4]
        A("dve", lambda e: e.tensor_tensor(out=gex, in0=gl, in1=gmax[:].unsqueeze(2).to_broadcast([128, NL, 4]), op=ALU.subtract),
          reads=["logits", "gmax"], writes=["R2"])
        A("act", lambda e: e.activation(out=gex, in_=gex, func=AF.Exp), reads=["R2"], writes=["R2"])
        A("dve", lambda e: e.reduce_sum(out=gsum[:], in_=gex, axis=AX.X), reads=["R2"], writes=["gsum"])
        A("dve", lambda e: e.reciprocal(out=gsum[:], in_=gsum[:]), reads=["gsum"], writes=["gsum"])
        BIG = 1.0e4
        A("dve", lambda e: e.tensor_scalar(out=gone, in0=gone, scalar1=BIG, scalar2=-BIG, op0=ALU.mult, op1=ALU.add), reads=["R1"], writes=["R1"])
        em = R3
        A("dve", lambda e: e.tensor_tensor(out=em[:].rearrange("p n (g j) -> p n g j", g=4), in0=el.rearrange("p n (g j) -> p n g j", g=4),
                                           in1=gone.unsqueeze(3).to_broadcast([128, NL, 4, 8]), op=ALU.add), reads=["logits", "R1"], writes=["R3"])
        A("dve", lambda e: e.reduce_max(out=m1[:], in_=em[:], axis=AX.X), reads=["R3"], writes=["m1"])
        oh1 = R1
        A("dve", lambda e: e.tensor_tensor(out=oh1[:], in0=em[:], in1=m1[:].unsqueeze(2).to_broadcast([128, NL, 32]), op=ALU.is_equal),
          reads=["R3", "m1"], writes=["R1"])
        em2 = R2
        A("dve", lambda e: e.scalar_tensor_tensor(out=em2[:], in0=oh1[:], scalar=-BIG, in1=em[:], op0=ALU.mult, op1=ALU.add),
          reads=["R1", "R3"], writes=["R2"])
        A("dve", lambda e: e.reduce_max(out=m2[:], in_=em2[:], axis=AX.X), reads=["R2"], writes=["m2"])
        oh2 = R3
        A("dve", lambda e: e.tensor_tensor(out=oh2[:], in0=em2[:], in1=m2[:].unsqueeze(2).to_broadcast([128, NL, 32]), op=ALU.is_equal),
          reads=["R2", "m2"], writes=["R3"])
        A("dve", lambda e: e.tensor_tensor(out=dd[:], in0=m2[:], in1=m1[:], op=ALU.subtract), reads=["m1", "m2"], writes=["dd"])
        A("act", lambda e: e.activation(out=ee[:], in_=dd[:], func=AF.Exp), reads=["dd"], writes=["ee"])
        A("dve", lambda e: e.tensor_scalar(out=dd[:], in0=ee[:], scalar1=1.0, scalar2=None, op0=ALU.add), reads=["ee"], writes=["dd"])
        A("dve", lambda e: e.reciprocal(out=dd[:], in_=dd[:]), reads=["dd"], writes=["dd"])
        A("dve", lambda e: e.tensor_tensor(out=ee[:], in0=ee[:], in1=dd[:], op=ALU.mult), reads=["ee", "dd"], writes=["ee"])
        A("dve", lambda e: e.tensor_tensor(out=w1g[:], in0=dd[:], in1=gsum[:], op=ALU.mult), reads=["dd", "gsum"], writes=["w1g"])
        A("dve", lambda e: e.tensor_tensor(out=w2g[:], in0=ee[:], in1=gsum[:], op=ALU.mult), reads=["ee", "gsum"], writes=["w2g"])
        Asum = R2
        A("dve", lambda e: e.tensor_tensor(out=Asum[:], in0=oh1[:], in1=oh2[:], op=ALU.add), reads=["R1", "R3"], writes=["R2"])
        if NFH > 0:
            A("dve", lambda e: e.tensor_scalar(out=Asum[:, 0:NFH, :], in0=Asum[:, 0:NFH, :], scalar1=hvt[:, 0:1], scalar2=None, op0=ALU.mult),
              reads=["R2", "hvt"], writes=["R2"])
        A("dve", lambda e: e.tensor_copy(out=Abf[:], in_=Asum[:]), reads=["R2"], writes=["Abf"])
        Af = Abf[:].rearrange("p n e -> p (n e)")
        ncol = NL * 32
        banks = [(pZ[:, 0, :], "pZ0"), (pZ[:, 1, :], "pZ1"), (pC[:], "B3"), (pV[:], "B7")]
        assert ncol <= 1536
        Rk = R4[:].rearrange("p n e -> p (n e)")
        Tt = R2[:].rearrange("p n e -> p (n e)")
        for (lhs, lk, dst, dk) in ((tri_bf, "tri_bf", Rk, "R4"), (ones_bf, "ones_bf", Tt, "R2")):
            for c0 in range(0, ncol, 512):
                cw = min(512, ncol - c0)
                A("pe", lambda e, lhs=lhs, c0=c0, cw=cw: e.matmul(pS[:, c0:c0 + cw], lhsT=lhs[:], rhs=Af[:, c0:c0 + cw], start=True, stop=True),
                  reads=[lk, "Abf"], writes=["B4", "B5", "B6"])
            A("dve", lambda e, dst=dst: e.tensor_copy(out=dst, in_=pS[:, 0:ncol]), reads=["B4", "B5", "B6"], writes=[dk])
        A("dve", lambda e: e.memset(cntb[0][:], 0.0), writes=["cnt"])
        for n in range(NL):
            if n > 0:
                A("dve", lambda e, n=n: e.tensor_tensor(out=R4[:, n, :], in0=R4[:, n, :], in1=cntb[0][:], op=ALU.add), reads=["R4", "cnt"], writes=["R4"])
            A("dve", lambda e, n=n: e.tensor_tensor(out=cntb[0][:], in0=cntb[0][:], in1=R2[:, n, :], op=ALU.add), reads=["R2", "cnt"], writes=["cnt"])
        A("dve", lambda e: e.memset(startb[:], 0.0), writes=["startb"])
        for j in range(1, 32):
            A("dve", lambda e, j=j: e.tensor_tensor(out=startb[:, j:j + 1], in0=startb[:, j - 1:j], in1=cntb[0][:, j - 1:j], op=ALU.add),
              reads=["startb", "cnt"], writes=["startb"])
        A("dve", lambda e: e.tensor_tensor(out=R4[:], in0=R4[:], in1=startb[:].unsqueeze(1).to_broadcast([128, NL, 32]), op=ALU.add),
          reads=["R4", "startb"], writes=["R4"])
        for ki, (oh, ohk, sl_i, slk, tmpk) in enumerate(((oh1, "R1", slot1, "slot1", "gmax"), (oh2, "R3", slot2, "slot2", "m1"))):
            tmp = gmax if tmpk == "gmax" else m1
            A("dve", lambda e, oh=oh: e.tensor_tensor(out=oh[:], in0=oh[:], in1=R4[:], op=ALU.mult), reads=[ohk, "R4"], writes=[ohk])
            A("dve", lambda e, oh=oh, tmp=tmp: e.reduce_sum(out=tmp[:], in_=oh[:], axis=AX.X), reads=[ohk], writes=[tmpk])
            if NFH > 0:
                A("dve", lambda e, tmp=tmp: e.tensor_scalar(out=tmp[:, 0:NFH], in0=tmp[:, 0:NFH], scalar1=hvt[:, 0:1], scalar2=None, op0=ALU.mult),
                  reads=[tmpk, "hvt"], writes=[tmpk])
                A("dve", lambda e, tmp=tmp, ki=ki: e.scalar_tensor_tensor(out=tmp[:, 0:NFH], in0=trt[:, ki * TRC: ki * TRC + NFH], scalar=nhvt[:, 0:1], in1=tmp[:, 0:NFH],
                                                                 op0=ALU.mult, op1=ALU.add), reads=[tmpk, "trt", "nhvt"], writes=[tmpk])
            A("dve", lambda e, tmp=tmp, sl_i=sl_i: e.tensor_copy(out=sl_i[:], in_=tmp[:]), reads=[tmpk], writes=[slk])
        A("dve", lambda e: e.tensor_tensor(out=widf[:], in0=startb[:].unsqueeze(2).to_broadcast([128, NEXP, CT]),
                                           in1=iott[:, 0:CT].unsqueeze(1).to_broadcast([128, NEXP, CT]), op=ALU.add),
          reads=["startb", "iott"], writes=["widf"])
        A("dve", lambda e: e.tensor_copy(out=widx[:], in_=widf[:]), reads=["widf"], writes=["widx"])

        if debug:
            dma("sp", dbg_logits, logits[:], ["logits"], ["dbg_logits"])
            dma("sp", dbg_w[:, 0, :], w1g[:], ["w1g"], ["dbg_w1"])
            dma("sp", dbg_w[:, 1, :], w2g[:], ["w2g"], ["dbg_w2"])
            dma("sp", dbg_slot[:, 0, :], slot1[:], ["slot1"], ["dbg_s1"])
            dma("sp", dbg_slot[:, 1, :], slot2[:], ["slot2"], ["dbg_s2"])
        xskeys = []
        for tl in range(NTL):
            b = tl % 2
            dma("sp", xtl[b][:], xn2s[tl * 128:(tl + 1) * 128, :], ["xn2s"], ["xtl%d" % b])
            for k_, (sl_i, slk) in enumerate(((slot1, "slot1"), (slot2, "slot2"))):
                key = "xs_%d_%d" % (tl, k_)
                xskeys.append(key)
                A("pool", lambda e, sl_i=sl_i, tl=tl, b=b: e.indirect_dma_start(
                    out=xs[:, :], out_offset=bass.IndirectOffsetOnAxis(ap=sl_i[:, tl:tl + 1], axis=0), in_=xtl[b][:], in_offset=None),
                  reads=["xtl%d" % b, slk], writes=[key], dma=True, dkey="sc_xtl%d" % b)

        NOW, NTHR = cfg.NOW, cfg.NTHR
        BIGI = 1.0e6
        cnt_ = cntb[0]
        assert NOW <= NL and NTHR <= NL
        gtm = R2[:, 0:NTHR, :].rearrange("p t e -> p (t e)").rearrange("p (e t) -> p e t", e=32)
        nov = cf.take(32)
        cum = cf.take(32)
        indw = R1[:, 0:NOW, :]
        tmpw = R3[:, 0:NOW, :]
        limv = cf.take(32)
        jbase = cf.take(32)
        wsc = [cf.take(NOW) for _ in range(5)]
        gidf = cf.take(NOW, CT)
        yidf = cf.take(NOW, CT)
        mskf = cf.take(NOW, CT)
        wgidf = cf.take(NOW, 8)
        wdidf = cf.take(NOW, 4)
        gidx = T("gidx", [128, NOW, CT], I32)
        yidx = T("yidx", [128, NOW, CT], I32)
        wgidx = T("wgidx", [128, NOW, 8], I32)
        wdidx = T("wdidx", [128, NOW, 4], I32)
        DV = lambda fn, r, w: A("dve", fn, reads=r, writes=w)
        DV(lambda e: e.tensor_tensor(out=gtm[:], in0=cnt_[:].unsqueeze(2).to_broadcast([128, 32, NTHR]),
                                     in1=thrt[:, 0:NTHR].unsqueeze(1).to_broadcast([128, 32, NTHR]), op=ALU.is_gt), ["cnt", "thrt"], ["R2"])
        DV(lambda e: e.reduce_sum(out=nov[:], in_=gtm[:], axis=AX.X), ["R2"], ["nov"])
        DV(lambda e: e.memset(cum[:], 0.0), [], ["cum"])
        for j in range(1, 32):
            DV(lambda e, j=j: e.tensor_tensor(out=cum[:, j:j + 1], in0=cum[:, j - 1:j], in1=nov[:, j - 1:j], op=ALU.add), ["cum", "nov"], ["cum"])
        wvb = wvt[:, 0:NOW].unsqueeze(2).to_broadcast([128, NOW, 32])
        DV(lambda e: e.tensor_tensor(out=indw[:], in0=cum[:].unsqueeze(1).to_broadcast([128, NOW, 32]), in1=wvb, op=ALU.is_le), ["cum", "wvt"], ["R1"])
        DV(lambda e: e.tensor_tensor(out=limv[:], in0=cum[:], in1=nov[:], op=ALU.add), ["cum", "nov"], ["limv"])
        DV(lambda e: e.tensor_tensor(out=tmpw[:], in0=limv[:].unsqueeze(1).to_broadcast([128, NOW, 32]), in1=wvb, op=ALU.is_gt), ["limv", "wvt"], ["R3"])
        DV(lambda e: e.tensor_tensor(out=indw[:], in0=indw[:], in1=tmpw[:], op=ALU.mult), ["R1", "R3"], ["R1"])
        vld, ew, ow, lw, tw = wsc
        DV(lambda e: e.reduce_sum(out=vld[:], in_=indw[:], axis=AX.X), ["R1"], ["vld"])
        DV(lambda e: e.tensor_tensor(out=tmpw[:], in0=indw[:], in1=evt[:].unsqueeze(1).to_broadcast([128, NOW, 32]), op=ALU.mult), ["R1", "evt"], ["R3"])
        DV(lambda e: e.reduce_sum(out=ew[:], in_=tmpw[:], axis=AX.X), ["R3"], ["ew"])
        DV(lambda e: e.tensor_scalar(out=jbase[:], in0=cum[:], scalar1=-float(CAP), scalar2=float(CAP), op0=ALU.mult, op1=ALU.add), ["cum"], ["jbase"])
        DV(lambda e: e.tensor_tensor(out=jbase[:], in0=jbase[:], in1=startb[:], op=ALU.add), ["jbase", "startb"], ["jbase"])
        DV(lambda e: e.tensor_tensor(out=tmpw[:], in0=indw[:], in1=jbase[:].unsqueeze(1).to_broadcast([128, NOW, 32]), op=ALU.mult), ["R1", "jbase"], ["R3"])
        DV(lambda e: e.reduce_sum(out=ow[:], in_=tmpw[:], axis=AX.X), ["R3"], ["ow"])
        DV(lambda e: e.scalar_tensor_tensor(out=ow[:], in0=wvt[:, 0:NOW], scalar=float(CAP), in1=ow[:], op0=ALU.mult, op1=ALU.add), ["ow", "wvt"], ["ow"])
        DV(lambda e: e.tensor_tensor(out=ow[:], in0=ow[:], in1=vld[:], op=ALU.mult), ["ow", "vld"], ["ow"])
        DV(lambda e: e.tensor_tensor(out=limv[:], in0=startb[:], in1=cnt_[:], op=ALU.add), ["startb", "cnt"], ["limv"])
        DV(lambda e: e.tensor_tensor(out=tmpw[:], in0=indw[:], in1=limv[:].unsqueeze(1).to_broadcast([128, NOW, 32]), op=ALU.mult), ["R1", "limv"], ["R3"])
        DV(lambda e: e.reduce_sum(out=lw[:], in_=tmpw[:], axis=AX.X), ["R3"], ["lw"])
        DV(lambda e: e.tensor_scalar(out=tw[:], in0=vld[:], scalar1=-BIGI, scalar2=BIGI, op0=ALU.mult, op1=ALU.add), ["vld"], ["tw"])
        DV(lambda e: e.tensor_tensor(out=gidf[:], in0=ow[:].unsqueeze(2).to_broadcast([128, NOW, CT]),
                                     in1=iott[:, 0:CT].unsqueeze(1).to_broadcast([128, NOW, CT]), op=ALU.add), ["ow", "iott"], ["gidf"])
        DV(lambda e: e.tensor_tensor(out=mskf[:], in0=gidf[:], in1=lw[:].unsqueeze(2).to_broadcast([128, NOW, CT]), op=ALU.is_lt), ["gidf", "lw"], ["mskf"])
        DV(lambda e: e.scalar_tensor_tensor(out=yidf[:], in0=gidf[:], scalar=-BIGI, in1=mskf[:], op0=ALU.add, op1=ALU.mult), ["gidf", "mskf"], ["yidf"])
        DV(lambda e: e.tensor_scalar(out=yidf[:], in0=yidf[:], scalar1=BIGI, scalar2=None, op0=ALU.add), ["yidf"], ["yidf"])
        DV(lambda e: e.tensor_tensor(out=gidf[:], in0=gidf[:], in1=tw[:].unsqueeze(2).to_broadcast([128, NOW, CT]), op=ALU.add), ["gidf", "tw"], ["gidf"])
        DV(lambda e: e.tensor_copy(out=gidx[:], in_=gidf[:]), ["gidf"], ["gidx"])
        DV(lambda e: e.tensor_copy(out=yidx[:], in_=yidf[:]), ["yidf"], ["yidx"])
        DV(lambda e: e.scalar_tensor_tensor(out=ew[:], in0=ew[:], scalar=1024.0, in1=tw[:], op0=ALU.mult, op1=ALU.add), ["ew", "tw"], ["ew"])
        DV(lambda e: e.tensor_tensor(out=wgidf[:], in0=ew[:].unsqueeze(2).to_broadcast([128, NOW, 8]),
                                     in1=iot8t[:].unsqueeze(1).to_broadcast([128, NOW, 8]), op=ALU.add), ["ew", "iot8t"], ["wgidf"])
        DV(lambda e: e.tensor_copy(out=wgidx[:], in_=wgidf[:]), ["wgidf"], ["wgidx"])
        DV(lambda e: e.scalar_tensor_tensor(out=ew[:], in0=ew[:], scalar=0.5, in1=tw[:], op0=ALU.mult, op1=ALU.add), ["ew", "tw"], ["ew"])
        DV(lambda e: e.tensor_tensor(out=wdidf[:], in0=ew[:].unsqueeze(2).to_broadcast([128, NOW, 4]),
                                     in1=iot8t[:, 0:4].unsqueeze(1).to_broadcast([128, NOW, 4]), op=ALU.add), ["ew", "iot8t"], ["wdidf"])
        DV(lambda e: e.tensor_copy(out=wdidx[:], in_=wdidf[:]), ["wdidf"], ["wdidx"])

        gbanks = [(pZ[:, 0, :], "pZ0"), (pZ[:, 1, :], "pZ1"), (pC[:], "B3"), (pV[:], "B7")]
        dbanks = [(pS[:, 512:1024], "B5"), (pS[:, 1024:1536], "B6")]
        pT2 = pS[:, 0:512].bitcast(BF16)
        tbanks = [(pT, "pT"), (pT2, "B4")]
        cnts = {"gi": 0, "di": 0, "ti": 0}
        NJOB = NEXP + NOW
        wg2, wu2, wd2 = wg, wu, wd

        bregs = memo.setdefault("__bregs", {})

        def breg(e, val):
            if val not in bregs:
                r = e.alloc_register("bc%d" % val)
                e.reg_mov(r, val)
                bregs[val] = r
            return bregs[val]

        def job_rows(k, j, for_y):
            if k < NEXP:
                return widx[:, k, j:j + 1]
            return (yidx if for_y else gidx)[:, k - NEXP, j:j + 1]

        def job_load_w(k):
            b = k % 2
            if k < NEXP:
                load_w(k)
                return
            w = k - NEXP
            og, ou, od = [], [], []
            for kc in range(8):
                og.append(A("pool", lambda e, kc=kc: e.indirect_dma_start(out=Wg[b][:, kc, :], out_offset=None, in_=wg2,
                                                                          in_offset=bass.IndirectOffsetOnAxis(ap=wgidx[:, w, kc:kc + 1], axis=0),
                                                                          bounds_check=breg(e, NEXP * 1024 - 1), oob_is_err=False),
                            reads=["wgidx"], writes=[WGK[b][kc]], dma=True, dkey="Wg%d" % b))
                ou.append(A("pool", lambda e, kc=kc: e.indirect_dma_start(out=Wu[b][:, kc, :], out_offset=None, in_=wu2,
                                                                          in_offset=bass.IndirectOffsetOnAxis(ap=wgidx[:, w, kc:kc + 1], axis=0),
                                                                          bounds_check=breg(e, NEXP * 1024 - 1), oob_is_err=False),
                            reads=["wgidx"], writes=[WUK[b][kc]], dma=True, dkey="Wu%d" % b))
            for jc in range(4):
                od.append(A("pool", lambda e, jc=jc: e.indirect_dma_start(out=Wd[b][:, jc, :], out_offset=None, in_=wd2,
                                                                          in_offset=bass.IndirectOffsetOnAxis(ap=wdidx[:, w, jc:jc + 1], axis=0),
                                                                          bounds_check=breg(e, NEXP * 512 - 1), oob_is_err=False),
                            reads=["wdidx"], writes=[WDK[b][jc]], dma=True, dkey="Wd%d" % b))
            for grp in (og, ou, od):
                for o_ in grp:
                    o_.tgt = grp[-1].tgt

        def job_gather(k):
            b = k % 2
            for j in range(CT):
                rows = job_rows(k, j, False)
                if k < NEXP:
                    A("pool", lambda e, j=j, rows=rows: e.indirect_dma_start(
                        out=xw[b][:, j, :], out_offset=None, in_=xs[:, :], in_offset=bass.IndirectOffsetOnAxis(ap=rows, axis=0)),
                      reads=xskeys + ["widx"], writes=["xw%d_%d" % (b, j)], dma=True)
                else:
                    A("pool", lambda e, j=j, rows=rows: e.indirect_dma_start(
                        out=xw[b][:, j, :], out_offset=None, in_=xs[:, :], in_offset=bass.IndirectOffsetOnAxis(ap=rows, axis=0),
                        bounds_check=breg(e, cfg.XSR - 1), oob_is_err=False),
                      reads=xskeys + ["gidx"], writes=["xw%d_%d" % (b, j)], dma=True)

        def job_compute(k):
            b = k % 2
            for kc in range(8):
                (tps, tk_) = tbanks[cnts["ti"] % 2]
                cnts["ti"] += 1
                for j in range(CT):
                    A("pe", lambda e, kc=kc, j=j, tps=tps: e.transpose(out=tps[:, j * 128:(j + 1) * 128],
                                                                       in_=xw[b][:, j, kc * 128:(kc + 1) * 128], identity=ident[:]),
                      reads=["xw%d_%d" % (b, j), "ident"], writes=[tk_])
                A("act", lambda e, kc=kc, tps=tps: e.activation(out=xsT[:, kc, :], in_=tps[:, 0:CAP], func=AF.Identity,
                                                                scale=mul2c[:, kc:kc + 1], bias=modc[:, 2, kc:kc + 1]),
                  reads=[tk_, "mul2c", "modc"], writes=["xsT%d" % kc])
            for jc in range(4):
                (gps, gk_) = gbanks[cnts["gi"] % 4]
                (ups, uk_) = gbanks[(cnts["gi"] + 1) % 4]
                cnts["gi"] += 2
                for kc in range(8):
                    A("pe", lambda e, gps=gps, kc=kc, jc=jc: e.matmul(gps[:, 0:CAP], lhsT=Wg[b][:, kc, jc * 128:(jc + 1) * 128], rhs=xsT[:, kc, :],
                                                                     start=(kc == 0), stop=(kc == 7)), reads=WGK[b] + ["xsT%d" % kc], writes=[gk_])
                for kc in range(8):
                    A("pe", lambda e, ups=ups, kc=kc, jc=jc: e.matmul(ups[:, 0:CAP], lhsT=Wu[b][:, kc, jc * 128:(jc + 1) * 128], rhs=xsT[:, kc, :],
                                                                     start=(kc == 0), stop=(kc == 7)), reads=WUK[b] + ["xsT%d" % kc], writes=[uk_])
                sb_ = jc % 2
                A("act", lambda e, gps=gps, sb_=sb_: e.activation(out=sgb[sb_][:], in_=gps[:, 0:CAP], func=AF.Silu), reads=[gk_], writes=["sg%d" % sb_])
                A("dve", lambda e, ups=ups, sb_=sb_, jc=jc: e.tensor_tensor(out=actT[:, jc, :], in0=ups[:, 0:CAP], in1=sgb[sb_][:], op=ALU.mult),
                  reads=[uk_, "sg%d" % sb_], writes=["actT"])
            for j in range(CT):
                yb = (k * CT + j) % 2
                for cbk in range(2):
                    (dps, dk_) = dbanks[cnts["di"] % 2]
                    cnts["di"] += 1
                    for jc in range(4):
                        A("pe", lambda e, dps=dps, jc=jc, j=j, cbk=cbk: e.matmul(dps, lhsT=actT[:, jc, j * 128:(j + 1) * 128], rhs=Wd[b][:, jc, cbk * 512:(cbk + 1) * 512],
                                                                                start=(jc == 0), stop=(jc == 3)), reads=["actT"] + WDK[b], writes=[dk_])
                    A("dve", lambda e, dps=dps, cbk=cbk, yb=yb: e.tensor_tensor(out=ybuf[yb][:, cbk * 512:(cbk + 1) * 512], in0=dps, in1=g2bc[:, cbk * 512:(cbk + 1) * 512], op=ALU.mult),
                      reads=[dk_, "g2bc"], writes=["ybuf%d" % yb])
                rows = job_rows(k, j, True)
                if k < NEXP:
                    A("pool", lambda e, rows=rows, yb=yb: e.indirect_dma_start(
                        out=ys[:, :], out_offset=bass.IndirectOffsetOnAxis(ap=rows, axis=0), in_=ybuf[yb][:], in_offset=None),
                      reads=["ybuf%d" % yb, "widx"], writes=["ys"], dma=True, dkey="sc_ybuf%d" % yb)
                else:
                    A("pool", lambda e, rows=rows, yb=yb: e.indirect_dma_start(
                        out=ys[:, :], out_offset=bass.IndirectOffsetOnAxis(ap=rows, axis=0), in_=ybuf[yb][:], in_offset=None,
                        bounds_check=breg(e, cfg.XSR - 1), oob_is_err=False),
                      reads=["ybuf%d" % yb, "yidx"], writes=["ys"], dma=True, dkey="sc_ybuf%d" % yb)

        job_gather(0)
        for k in range(NJOB):
            if k + 1 < NJOB:
                job_gather(k + 1)
            job_compute(k)
            if k + 2 < NJOB:
                job_load_w(k + 2)

        for tl in range(NTL):
            b = tl % 2
            dma("sp", xmb[b][:], xmid[tl * 128:(tl + 1) * 128, :], ["xmid"], ["xmb%d" % b])
            A("pool", lambda e, tl=tl, b=b: e.indirect_dma_start(out=y1b[b][:], out_offset=None, in_=ys[:, :],
                                                                 in_offset=bass.IndirectOffsetOnAxis(ap=slot1[:, tl:tl + 1], axis=0)),
              reads=["ys", "slot1"], writes=["y1b%d" % b], dma=True)
            A("pool", lambda e, tl=tl, b=b: e.indirect_dma_start(out=y2b[b][:], out_offset=None, in_=ys[:, :],
                                                                 in_offset=bass.IndirectOffsetOnAxis(ap=slot2[:, tl:tl + 1], axis=0)),
              reads=["ys", "slot2"], writes=["y2b"], dma=True)
            A("dve", lambda e, tl=tl, b=b: e.scalar_tensor_tensor(out=xmb[b][:], in0=y1b[b][:], scalar=w1g[:, tl:tl + 1], in1=xmb[b][:], op0=ALU.mult, op1=ALU.add),
              reads=["y1b%d" % b, "w1g", "xmb%d" % b], writes=["xmb%d" % b])
            A("dve", lambda e, tl=tl, b=b: e.scalar_tensor_tensor(out=xmb[b][:], in0=y2b[b][:], scalar=w2g[:, tl:tl + 1], in1=xmb[b][:], op0=ALU.mult, op1=ALU.add),
              reads=["y2b", "w2g", "xmb%d" % b], writes=["xmb%d" % b])
            dma("sp", xo[tl * 128:(tl + 1) * 128, :], xmb[b][:], ["xmb%d" % b], ["xo"], dkey="st_xmb%d" % b)
        p.barrier()


def _colform(v):
    return np.ascontiguousarray(v.reshape(-1, 128).T).astype(np.float32)


def _const_tables(cfg):
    q = np.arange(128)[:, None]
    tabs_idx = np.zeros((128, 5, 128), np.int64)
    mask = np.zeros((128, 5, 128), np.float32)
    for t in range(5):
        k = np.arange(128)[None, :]
        rel = 128 * (4 - t) + q - k
        tabs_idx[:, t, :] = np.clip(rel, -128, 128) + 128
        qc = q // 64
        kc = 2 * (t - 4) + k // 64
        ok = (kc <= qc) & (kc >= qc - 8)
        mask[:, t, :] = ok
    tri = (np.arange(128)[:, None] < np.arange(128)[None, :]).astype(np.float32)
    iot = (np.arange(128)[:, None] + 128 * np.arange(4)[None, :]).astype(np.float32)
    trash = (cfg.TRASH + np.arange(128)[:, None] + 128 * np.arange(max(cfg.NFH, 1))[None, :]).astype(np.float32)
    return tabs_idx, mask, tri, iot, trash


def layer_inputs(cfg, l, xe, cb, first_half, P, li=0):
    tabs_idx, mask, tri, iot, trash = _const_tables(cfg)
    btab = np.ascontiguousarray(P["rel_bias"][:, tabs_idx].transpose(1, 0, 2, 3)).astype(np.float32)
    invc = np.zeros((128, 4, 16), np.float32)
    for g, w in enumerate((2, 4, 8, 16)):
        cnt = np.minimum(np.arange(16) + 1, w) if first_half else np.full(16, w)
        invc[:, g, :] = (1.0 / cnt.astype(np.float64)).astype(np.float32)[None, :]
    hvv = 0.0 if first_half else 1.0
    trash = (cfg.TRASH + np.arange(128)[:, None] + 128 * np.arange(TRC)[None, :]).astype(np.float32)
    trash = np.concatenate([trash, trash + cfg.NFH * 128], axis=1)
    m = {
        "xe": np.ascontiguousarray(xe, dtype=np.float32),
        "cT": _colform(cb),
        "ada_w": P["ada_w"][l], "ada_b": P["ada_b"][l][None, :],
        "n1c": _colform(P["norm1_g"][l]), "n2c": _colform(P["norm2_g"][l]),
        "w_in": P["w_in"][l], "w_out": P["w_out"][l],
        "pool_w": np.ascontiguousarray(P["pool_w"][l].transpose(1, 0, 2)),
        "pscale": _colform(P["pool_scale"][l]),
        "gq": np.ascontiguousarray(np.tile(P["q_norm_g"][l], 2)[:, None]), "gk": np.ascontiguousarray(np.tile(P["k_norm_g"][l], 2)[:, None]),
        "btab": btab, "bmask": mask,
        "wr": np.ascontiguousarray(np.concatenate([P["router_group_w"][l], P["router_expert_w"][l]], axis=1)),
        "br": np.ascontiguousarray(np.tile(np.concatenate([P["router_group_b"][l], P["router_expert_b"][l]])[None, :], (128, 1))),
        "wg": P["moe_w_gate"][l].reshape(NEXP * D, 512), "wu": P["moe_w_up"][l].reshape(NEXP * D, 512), "wd": P["moe_w_down"][l].reshape(NEXP * 512, D),
        "hv": np.full((128, 1), hvv, np.float32), "nhv": np.full((128, 1), 1.0 - hvv, np.float32),
        "invc": invc, "tri": tri, "iot": iot, "trashi": trash,
        "thr": np.tile((cfg.CAP * (np.arange(16) + 1)).astype(np.float32)[None, :], (128, 1)),
        "wv": np.tile(np.arange(32, dtype=np.float32)[None, :], (128, 1)),
        "ev": np.tile(np.arange(32, dtype=np.float32)[None, :], (128, 1)),
        "iot8": (np.arange(128)[:, None] + 128 * np.arange(8)[None, :]).astype(np.float32),
    }
    return {(k + "_%d" % li if k in PERL else k): v for k, v in m.items()}


_NC_CACHE = {}


def kernel(**inputs):
    P = {k: np.asarray(v) for k, v in inputs.items()}
    x = P["x"]
    B, S, _ = x.shape
    cfg0 = Cfg(nkv=4, nfh=4, nm=32, cap=512)
    cfg1 = Cfg(nkv=4, nfh=0, nm=32, cap=512)
    if "nc" not in _NC_CACHE:
        _NC_CACHE["nc"] = build_program([cfg0, cfg1])
    nc = _NC_CACHE["nc"]
    half = S // 2
    in_maps = []
    for c in range(8):
        b, hf = c // 2, c % 2
        main = x[b, hf * half:(hf + 1) * half]
        halo = np.zeros((1024, D), np.float32) if hf == 0 else x[b, half - 1024:half]
        xe = np.concatenate([halo, main], axis=0)
        m = layer_inputs(cfg0, 0, xe, P["c"][b], hf == 0, P, li=0)
        m1 = layer_inputs(cfg1, 1, xe[:128], P["c"][b], hf == 0, P, li=1)
        m.update({k: v for k, v in m1.items() if k.endswith("_1")})
        in_maps.append(m)
    res = run_bass_kernel_spmd(nc, in_maps, core_ids=list(range(8)))
    out = np.empty_like(x)
    for c in range(8):
        b, hf = c // 2, c % 2
        out[b, hf * half:(hf + 1) * half] = res.results[c]["xo"]
    return out
```

```python
from contextlib import ExitStack

import numpy as np
import concourse.bass as bass
import concourse.mybir as mybir
from concourse.bass_utils import run_bass_kernel_spmd

F32 = mybir.dt.float32
BF16 = mybir.dt.bfloat16
I32 = mybir.dt.int32
AF = mybir.ActivationFunctionType
ALU = mybir.AluOpType
AX = mybir.AxisListType

ENGS = ("sp", "act", "dve", "pool", "pe")
PSUM_KEYS = frozenset(["pT", "pZ0", "pZ1", "B3", "B4", "B5", "B6", "B7"])
D = 1024
EPS = 1e-6
NEXP = 32
RT = 8


class Op:
    __slots__ = ("eng", "fn", "dma", "dkey", "deps", "sig", "idx", "tgt", "waits")

    def __init__(self, eng, fn, dma, dkey):
        self.eng = eng
        self.fn = fn
        self.dma = dma
        self.dkey = dkey
        self.deps = []
        self.sig = False
        self.idx = 0
        self.tgt = 0
        self.waits = []


class PB:
    def __init__(self, nc):
        self.nc = nc
        self.ops = []
        self.last_w = {}
        self.readers = {}
        self.dma_cnt = {}

    def add(self, eng, fn, reads=(), writes=(), dma=False, dkey=None):
        if dma and dkey is None:
            dkey = writes[0]
        op = Op(eng, fn, dma, dkey)
        deps = set()
        for k in reads:
            w = self.last_w.get(k)
            if w is not None:
                deps.add(w)
            if k in PSUM_KEYS:
                for r in self.readers.get(k, ()):
                    if r.eng != eng:
                        deps.add(r)
        for k in writes:
            w = self.last_w.get(k)
            if w is not None:
                deps.add(w)
            for r in self.readers.get(k, ()):
                deps.add(r)
        op.deps = list(deps)
        for k in reads:
            self.readers.setdefault(k, []).append(op)
        for k in writes:
            self.last_w[k] = op
            self.readers[k] = []
        if dma:
            self.dma_cnt[dkey] = self.dma_cnt.get(dkey, 0) + 1
            op.tgt = 16 * self.dma_cnt[dkey]
        self.ops.append(op)
        return op

    def barrier(self):
        allkeys = list(set(self.last_w.keys()) | set(self.readers.keys()))
        self.add("sp", lambda e: e.nop(), reads=[], writes=allkeys + ["__bar"])
        for eng in ENGS:
            self.add(eng, lambda e: e.nop(), reads=["__bar"], writes=["__bar_" + eng])
        self.last_w = {k: v for k, v in self.last_w.items() if k.startswith("__bar")}
        self.readers = {k: v for k, v in self.readers.items() if k.startswith("__bar")}

    def emit(self):
        nc = self.nc
        for op in self.ops:
            for d in op.deps:
                if not d.dma:
                    d.sig = True
        cnt = {e: 0 for e in ENGS}
        for op in self.ops:
            if not op.dma and op.sig:
                cnt[op.eng] += 1
                op.idx = cnt[op.eng]
        dkeys = sorted(self.dma_cnt.keys())
        with ExitStack() as st:
            esem = {e: st.enter_context(nc.semaphore("es_" + e)) for e in ENGS}
            dsem = {k: st.enter_context(nc.semaphore("ds%d" % i)) for i, k in enumerate(dkeys)}
            waited = {e: {} for e in ENGS}
            for op in self.ops:
                need = {}
                for d in op.deps:
                    if d.dma:
                        key, val = ("d", d.dkey), d.tgt
                    else:
                        if d.eng == op.eng and op.eng == "pe":
                            continue
                        key, val = ("e", d.eng), d.idx
                    if need.get(key, 0) < val:
                        need[key] = val
                w = waited[op.eng]
                for key, val in need.items():
                    if w.get(key, 0) < val:
                        w[key] = val
                        op.waits.append((dsem[key[1]] if key[0] == "d" else esem[key[1]], val))
            block = st.enter_context(nc.Block())

            def run(engname):
                def body(e):
                    for op in self.ops:
                        if op.eng != engname:
                            continue
                        for (s, v) in op.waits:
                            e.wait_ge(s, v)
                        ins = op.fn(e)
                        if op.dma:
                            ins.then_inc(dsem[op.dkey], 16)
                        elif op.sig:
                            ins.then_inc(esem[engname], 1)
                return body

            block.sync(run("sp"))
            block.scalar(run("act"))
            block.vector(run("dve"))
            block.gpsimd(run("pool"))
            block.tensor(run("pe"))


class Cfg:
    def __init__(self, nkv=4, nfh=0, nm=32, cap=512):
        self.NKV, self.NFH, self.NM, self.CAP = nkv, nfh, nm, cap
        self.NTE = nkv + nfh + nm
        self.NTL = nfh + nm
        self.NST = self.NTE // 4
        self.CT = cap // 128
        self.NTOK = self.NTL * 128
        self.TRASH = 2 * self.NTOK + cap
        self.XSR = self.TRASH + 2 * max(nfh, 1) * 128
        self.NOW = -(-2 * self.NTOK // cap)
        self.NTHR = -(-self.NTOK // cap)
        assert self.NTE % 4 == 0 and nkv % 4 == 0 and nfh % 4 == 0 and cap % 128 == 0


PERL = frozenset(["ada_w", "ada_b", "n1c", "n2c", "w_in", "w_out", "pool_w", "pscale", "gq", "gk", "wr", "br", "wg", "wu", "wd"])
TRC = 4


class _Ctx:
    pass


def build_program(cfgs, debug=False):
    nc = bass.Bass("TRN2", target_bir_lowering=False)
    ctx = _Ctx()
    ctx.nc, ctx.memo, ctx.p, ctx.nl = nc, {}, PB(nc), len(cfgs)
    with ExitStack() as st:
        ctx.st = st
        for li, cfg in enumerate(cfgs):
            _emit_layer(ctx, li, cfg, debug)
        ctx.p.emit()
    return nc


def build_layer(cfg, debug=False):
    return build_program([cfg], debug)


def _emit_layer(ctx, li, cfg, debug=False):
    nc, st, memo = ctx.nc, ctx.st, ctx.memo
    last = li == ctx.nl - 1
    NTE, NTL, NST, NKV, NFH, CT, CAP = cfg.NTE, cfg.NTL, cfg.NST, cfg.NKV, cfg.NFH, cfg.CT, cfg.CAP

    def din(name, shape, dt=F32):
        nm = name + ("_%d" % li if name in PERL else "")
        if nm not in memo:
            memo[nm] = nc.dram_tensor(nm, list(shape), dt, kind="ExternalInput").ap()
        return memo[nm]

    def dscr(name, shape, dt, kind="Internal"):
        if name not in memo:
            memo[name] = nc.dram_tensor(name, list(shape), dt, kind=kind).ap()
        return memo[name]

    xe = din("xe", [NTE * 128, D]) if li == 0 else memo["x1_%d" % (li - 1)]
    cT = din("cT", [128, 8])
    ada_w = din("ada_w", [D, 6 * D])
    ada_b = din("ada_b", [1, 6 * D])
    n1c = din("n1c", [128, 8])
    n2c = din("n2c", [128, 8])
    w_in = din("w_in", [D, 2048])
    w_out = din("w_out", [D, D])
    pool_w = din("pool_w", [128, 4, 128])
    pscale = din("pscale", [128, 4])
    gq = din("gq", [128, 1])
    gk = din("gk", [128, 1])
    btab = din("btab", [128, 8, 5, 128])
    bmask = din("bmask", [128, 5, 128])
    wr = din("wr", [D, 36])
    br = din("br", [128, 36])
    wg = din("wg", [NEXP * D, 512])
    wu = din("wu", [NEXP * D, 512])
    wd = din("wd", [NEXP * 512, D])
    hv = din("hv", [128, 1])
    nhv = din("nhv", [128, 1])
    invc = din("invc", [128, 4, 16])
    tri = din("tri", [128, 128])
    iot = din("iot", [128, 4])
    trashi = din("trashi", [128, 2 * TRC])
    thr = din("thr", [128, 16])
    wv = din("wv", [128, 32])
    ev = din("ev", [128, 32])
    iot8 = din("iot8", [128, 8])
    if last:
        xo = nc.dram_tensor("xo", [NTL * 128, D], F32, kind="ExternalOutput").ap()
    else:
        xo = dscr("x1_%d" % li, [NTL * 128, D], F32)
    xmid = dscr("xmid", [NTL * 128, D], F32, kind="ExternalOutput" if debug else "Internal")
    if debug:
        dbg_logits = nc.dram_tensor("dbg_logits", [128, NTL, 36], F32, kind="ExternalOutput").ap()
        dbg_w = nc.dram_tensor("dbg_w", [128, 2, NTL], F32, kind="ExternalOutput").ap()
        dbg_slot = nc.dram_tensor("dbg_slot", [128, 2, NTL], I32, kind="ExternalOutput").ap()
    xn2s = dscr("xn2s", [NTL * 128, D], BF16)
    xs = dscr("xs", [cfg.XSR, D], BF16)
    ys = dscr("ys", [cfg.XSR, D], F32)

    if True:
        def T(name, shape, dt=F32):
            if name in memo:
                t, shp = memo[name]
                if list(shp) != list(shape):
                    assert len(shp) == len(shape) and shape[1] <= shp[1] and list(shp[2:]) == list(shape[2:]), (name, shp, shape)
                    return t[:, 0:shape[1]]
                return t
            t = st.enter_context(nc.sbuf_tensor(name, list(shape), dt))
            memo[name] = (t, list(shape))
            return t

        def PS(name, shape, dt=F32):
            if name not in memo:
                memo[name] = st.enter_context(nc.psum_tensor(name, list(shape), dt))
            return memo[name]

        ident = T("ident", [128, 128], BF16)
        identf = T("identf", [128, 128])
        ones_bf = T("ones_bf", [128, 128], BF16)
        blk1 = T("blk1", [128, 128], BF16)
        tri_bf = T("tri_bf", [128, 128], BF16)
        onesf = T("onesf", [1, 128])
        epsc = T("epsc", [128, 1])
        eps64 = T("eps64", [128, 1])
        cact = T("cact", [128, 8], BF16)
        ctf = T("ctf", [128, 8])
        n1t = T("n1t", [128, 8])
        n2t = T("n2t", [128, 8])
        modc = T("modc", [128, 4, 8])
        mul1c = T("mul1c", [128, 8])
        mul2c = T("mul2c", [128, 8])
        add1b = T("add1b", [128, 8], BF16)
        g2bc = T("g2bc", [128, D])
        bzc = T("bzc", [128, 12])
        bzv = T("bzv", [128, 512])
        bzvm = T("bzvm", [128, 512])
        pw_bf = T("pw_bf", [128, 4, 128], BF16)
        psc = T("psc", [128, 4])
        gqk = T("gqk", [128, 1])
        gkt = T("gkt", [128, 1])
        BT = T("BT", [128, 8, 5, 128], BF16)
        wr_f = T("wr_f", [128, 8, 36])
        wr_bf = T("wr_bf", [128, 8, 36], BF16)
        wr_raw = T("wr_raw", [128, 8, 36], BF16)
        biasR = T("biasR", [128, 36])
        hvt = T("hvt", [128, 1])
        nhvt = T("nhvt", [128, 1])
        invct = T("invct", [128, 4, 16])
        iott = T("iott", [128, 4])
        trt = T("trt", [128, 2 * TRC])
        thrt = T("thrt", [128, 16])
        wvt = T("wvt", [128, 32])
        evt = T("evt", [128, 32])
        iot8t = T("iot8t", [128, 8])
        ssr = T("ssr", [128, 8])
        rst = T("rst", [128, 8])
        logits = T("logits", [128, NTL, 36])
        w1g = T("w1g", [128, NTL])
        w2g = T("w2g", [128, NTL])
        slot1 = T("slot1", [128, NTL], I32)
        slot2 = T("slot2", [128, NTL], I32)
        widx = T("widx", [128, NEXP, CT], I32)

        AF_WORDS = 15104
        AB_WORDS = 58432
        arf = T("arf", [128, AF_WORDS])
        arb = T("arb", [128, AB_WORDS], BF16)

        class Carver:
            def __init__(self, t, n):
                self.t, self.n, self.off = t, n, 0

            def take(self, *shape):
                n = int(np.prod(shape))
                a = self.t[:, self.off:self.off + n]
                self.off += n
                assert self.off <= self.n, (self.off, self.n)
                if len(shape) == 2:
                    return a.rearrange("p (a b) -> p a b", a=shape[0])
                if len(shape) == 3:
                    return a.rearrange("p (a b c) -> p a b c", a=shape[0], b=shape[1])
                return a

        pT = PS("pT", [128, 1024], BF16)
        pZ = PS("pZ", [128, 2, 512])
        pC = PS("B3", [128, 512])
        pS = PS("pS", [128, 1536])
        pV = PS("B7", [128, 512])

        p = ctx.p
        A = p.add

        def dma(eng, out, in_, reads, writes, dkey=None):
            return A(eng, lambda e: e.dma_start(out=out, in_=in_), reads=reads, writes=writes, dma=True, dkey=dkey)

        A("pool", lambda e: e.memset(identf[:], 0.0), writes=["identf"])
        A("pool", lambda e: e.affine_select(out=identf[:], in_=identf[:], pattern=[[-1, 128]], compare_op=ALU.not_equal,
                                            fill=1.0, base=0, channel_multiplier=1), reads=["identf"], writes=["identf"])
        A("dve", lambda e: e.tensor_copy(out=ident[:], in_=identf[:]), reads=["identf"], writes=["ident"])
        A("pool", lambda e: e.memset(ones_bf[:], 1.0), writes=["ones_bf"])
        A("pool", lambda e: e.memset(blk1[:], 0.0), writes=["blk1"])
        A("pool", lambda e: e.memset(blk1[0:64, 0:64], 1.0), reads=["blk1"], writes=["blk1"])
        A("pool", lambda e: e.memset(blk1[64:128, 64:128], 1.0), reads=["blk1"], writes=["blk1"])
        A("pool", lambda e: e.memset(onesf[:], 1.0), writes=["onesf"])
        A("pool", lambda e: e.memset(epsc[:], EPS), writes=["epsc"])
        A("pool", lambda e: e.memset(eps64[:], 64 * EPS), writes=["eps64"])
        for (dst, src, k) in ((ctf, cT, "ctf"), (n1t, n1c, "n1t"), (n2t, n2c, "n2t"), (psc, pscale, "psc"), (gqk, gq, "gqk"),
                              (gkt, gk, "gkt"), (hvt, hv, "hvt"), (nhvt, nhv, "nhvt"), (invct, invc, "invct"),
                              (iott, iot, "iott"), (trt, trashi, "trt"), (thrt, thr, "thrt"), (wvt, wv, "wvt"), (evt, ev, "evt"), (iot8t, iot8, "iot8t"), (biasR, br, "biasR"), (identf, tri, "identf")):
            dma("sp", dst[:], src, [], [k])
        A("dve", lambda e: e.tensor_copy(out=tri_bf[:], in_=identf[:]), reads=["identf"], writes=["tri_bf"])
        A("dve", lambda e: e.tensor_mul(out=gqk[:], in0=gqk[:], in1=gkt[:]), reads=["gqk", "gkt"], writes=["gqk"])
        dma("pool", pw_bf[:], pool_w, [], ["pw_bf"])
        A("act", lambda e: e.activation(out=cact[:], in_=ctf[:], func=AF.Silu), reads=["ctf"], writes=["cact"])

        cf = Carver(arf, AF_WORDS)
        cb_ = Carver(arb, AB_WORDS)
        g1bc = cf.take(D)
        modrow = cf.take(6 * D)[0:1, :]
        adab = cf.take(6 * D)[0:1, :]
        stage = [cb_.take(8, 1536) for _ in range(2)]
        zf = cf.take(D)
        zb = cb_.take(D)
        A("pool", lambda e: e.memset(zf[:], 0.0), writes=["zf"])
        A("pool", lambda e: e.memset(zb[:], 0.0), writes=["zb"])
        for r0 in range(2 * cfg.NM * 128, cfg.TRASH, 128):
            dma("sp", xs[r0:r0 + 128, :], zb[:], ["zb"], ["xs_z%d" % r0], dkey="zinit")
        for r0 in range(cfg.TRASH, cfg.TRASH + 2 * NFH * 128, 128):
            dma("sp", ys[r0:r0 + 128, :], zf[:], ["zf"], ["ys_z%d" % r0], dkey="zinit")
        dma("sp", adab, ada_b, [], ["adab"])
        for g in range(4):
            sb = stage[g % 2]
            dma("pool", sb[:], ada_w[:, g * 1536:(g + 1) * 1536].rearrange("(k p) n -> p k n", p=128), [], ["stage%d" % (g % 2)])
            for cbk in range(3):
                col = g * 1536 + cbk * 512
                bank = cbk % 2
                for kc in range(8):
                    A("pe", lambda e, sb=sb, kc=kc, cbk=cbk, bank=bank: e.matmul(
                        pZ[0:1, bank, :], lhsT=cact[:, kc:kc + 1], rhs=sb[:, kc, cbk * 512:(cbk + 1) * 512],
                        start=(kc == 0), stop=(kc == 7)), reads=["cact", "stage%d" % (g % 2)], writes=["pZ%d" % bank])
                A("dve", lambda e, col=col, bank=bank: e.tensor_tensor(out=modrow[:, col:col + 512], in0=pZ[0:1, bank, :],
                                                                       in1=adab[:, col:col + 512], op=ALU.add),
                  reads=["pZ%d" % bank, "adab"], writes=["modrow"])
        for vi, base in enumerate((0, D, 3 * D, 4 * D)):
            for kc in range(8):
                A("pe", lambda e, vi=vi, base=base, kc=kc: e.matmul(
                    pC[:, vi * 8 + kc: vi * 8 + kc + 1], lhsT=modrow[:, base + kc * 128: base + (kc + 1) * 128],
                    rhs=onesf[:, 0:1], start=True, stop=True), reads=["modrow", "onesf"], writes=["B3"])
        A("dve", lambda e: e.tensor_copy(out=modc[:], in_=pC[:, 0:32].rearrange("p (a b) -> p a b", a=4)), reads=["B3"], writes=["modc"])
        A("dve", lambda e: e.scalar_tensor_tensor(out=mul1c[:], in0=modc[:, 1, :], scalar=1.0, in1=n1t[:], op0=ALU.add, op1=ALU.mult),
          reads=["modc", "n1t"], writes=["mul1c"])
        A("dve", lambda e: e.scalar_tensor_tensor(out=mul2c[:], in0=modc[:, 3, :], scalar=1.0, in1=n2t[:], op0=ALU.add, op1=ALU.mult),
          reads=["modc", "n2t"], writes=["mul2c"])
        A("dve", lambda e: e.tensor_copy(out=add1b[:], in_=modc[:, 0, :]), reads=["modc"], writes=["add1b"])
        for (dst, base, k) in ((g1bc, 2 * D, "g1bc"), (g2bc, 5 * D, "g2bc")):
            for hb in range(2):
                A("pe", lambda e, base=base, hb=hb: e.matmul(pZ[:, hb, :], lhsT=onesf[:, :], rhs=modrow[:, base + hb * 512: base + (hb + 1) * 512],
                                                            start=True, stop=True), reads=["modrow", "onesf"], writes=["pZ%d" % hb])
                A("dve", lambda e, dst=dst, hb=hb: e.tensor_copy(out=dst[:, hb * 512:(hb + 1) * 512], in_=pZ[:, hb, :]),
                  reads=["pZ%d" % hb], writes=[k])
        p.barrier()

        cf = Carver(arf, AF_WORDS)
        cb_ = Carver(arb, AB_WORDS)
        w_in_bf = cb_.take(8, 2048)
        w_out_bf = cb_.take(8, D)
        g1bc = cf.take(D)
        add2rep = cb_.take(8, 128)
        wst = [cf.take(2, 2048) for _ in range(2)]
        wraw = cb_.take(8, 2048)
        bzrow = cf.take(2048)[0:1, :]
        bzrow_b = cb_.take(512)[0:1, :]
        for pc in range(4):
            sbf = wst[pc % 2]
            dma("sp", sbf[:], w_in[pc * 256:(pc + 1) * 256, :].rearrange("(k p) n -> p k n", p=128), [], ["wst%d" % (pc % 2)])
            for kk in range(2):
                kc = pc * 2 + kk
                A("dve", lambda e, sbf=sbf, kk=kk, kc=kc: e.tensor_scalar(out=w_in_bf[:, kc, :], in0=sbf[:, kk, :], scalar1=mul1c[:, kc:kc + 1],
                                                                       scalar2=None, op0=ALU.mult),
                  reads=["wst%d" % (pc % 2), "mul1c"], writes=["w_in_bf"])
                A("act", lambda e, sbf=sbf, kk=kk, kc=kc: e.activation(out=wraw[:, kc, :], in_=sbf[:, kk, :], func=AF.Copy),
                  reads=["wst%d" % (pc % 2)], writes=["wraw"])
        for cbk in range(4):
            for kc in range(8):
                A("pe", lambda e, cbk=cbk, kc=kc: e.matmul(pZ[0:1, cbk % 2, :], lhsT=add1b[:, kc:kc + 1], rhs=wraw[:, kc, cbk * 512:(cbk + 1) * 512],
                                                          start=(kc == 0), stop=(kc == 7)), reads=["add1b", "wraw"], writes=["pZ%d" % (cbk % 2)])
            A("dve", lambda e, cbk=cbk: e.tensor_copy(out=bzrow[:, cbk * 512:(cbk + 1) * 512], in_=pZ[0:1, cbk % 2, :]),
              reads=["pZ%d" % (cbk % 2)], writes=["bzrow"])
        for oc in range(12):
            A("pe", lambda e, oc=oc: e.matmul(pC[:, oc:oc + 1], lhsT=bzrow[:, oc * 128:(oc + 1) * 128], rhs=onesf[:, 0:1], start=True, stop=True),
              reads=["bzrow", "onesf"], writes=["B3"])
        A("dve", lambda e: e.tensor_copy(out=bzc[:], in_=pC[:, 0:12]), reads=["B3"], writes=["bzc"])
        A("pe", lambda e: e.matmul(pZ[:, 0, :], lhsT=onesf[:, :], rhs=bzrow[:, 1536:2048], start=True, stop=True),
          reads=["bzrow", "onesf"], writes=["pZ0"])
        A("dve", lambda e: e.tensor_copy(out=bzv[:], in_=pZ[:, 0, :]), reads=["pZ0"], writes=["bzv"])
        A("dve", lambda e: e.tensor_scalar(out=bzvm[:], in0=bzv[:], scalar1=hvt[:, 0:1], scalar2=None, op0=ALU.mult),
          reads=["bzv", "hvt"], writes=["bzvm"])
        wost = [wst[0][:, :, 0:D], wst[1][:, :, 0:D]]
        for pc in range(4):
            sbf = wost[pc % 2]
            dma("sp", sbf[:], w_out[pc * 256:(pc + 1) * 256, :].rearrange("(k p) n -> p k n", p=128), [], ["wst%d" % (pc % 2)])
            for kk in range(2):
                kc = pc * 2 + kk
                A("dve", lambda e, sbf=sbf, kk=kk, kc=kc: e.tensor_tensor(out=w_out_bf[:, kc, :], in0=sbf[:, kk, :], in1=g1bc[:], op=ALU.mult),
                  reads=["wst%d" % (pc % 2), "g1bc"], writes=["w_out_bf"])
        btf = cf.take(5, 128)
        pen = cf.take(5, 128)
        mk = cf.take(5, 128)
        dma("sp", mk[:], bmask, [], ["mk"])
        A("dve", lambda e: e.tensor_scalar(out=pen[:], in0=mk[:], scalar1=3750.0, scalar2=-3750.0, op0=ALU.mult, op1=ALU.add),
          reads=["mk"], writes=["pen"])
        for h in range(8):
            dma("sp", btf[:], btab[:, h, :, :], [], ["btf"])
            A("dve", lambda e: e.tensor_tensor(out=btf[:], in0=btf[:], in1=mk[:], op=ALU.mult), reads=["btf", "mk"], writes=["btf"])
            A("dve", lambda e, h=h: e.scalar_tensor_tensor(out=BT[:, h, :, :], in0=btf[:], scalar=0.125, in1=pen[:], op0=ALU.mult, op1=ALU.add),
              reads=["btf", "pen"], writes=["BT"])
        dma("sp", wr_f[:], wr.rearrange("(k p) n -> p k n", p=128), [], ["wr_f"])
        A("dve", lambda e: e.tensor_copy(out=wr_raw[:], in_=wr_f[:]), reads=["wr_f"], writes=["wr_raw"])
        for kc in range(8):
            A("dve", lambda e, kc=kc: e.tensor_scalar(out=wr_bf[:, kc, :], in0=wr_f[:, kc, :], scalar1=mul2c[:, kc:kc + 1], scalar2=None, op0=ALU.mult),
              reads=["wr_f", "mul2c"], writes=["wr_bf"])
            A("dve", lambda e, kc=kc: e.tensor_copy(out=add2rep[:, kc, :], in_=modc[:, 2, kc:kc + 1].to_broadcast([128, 128])),
              reads=["modc"], writes=["add2rep"])
        for kc in range(8):
            A("pe", lambda e, kc=kc: e.matmul(pC[:, 0:36], lhsT=add2rep[:, kc, :], rhs=wr_raw[:, kc, :], start=(kc == 0), stop=(kc == 7)),
              reads=["add2rep", "wr_raw"], writes=["B3"])
        A("dve", lambda e: e.tensor_tensor(out=biasR[:], in0=pC[:, 0:36], in1=biasR[:], op=ALU.add), reads=["B3", "biasR"], writes=["biasR"])
        p.barrier()

        cf = Carver(arf, AF_WORDS)
        cb_ = Carver(arb, AB_WORDS)
        w_in_bf = cb_.take(8, 2048)
        w_out_bf = cb_.take(8, D)
        xin = [cf.take(D) for _ in range(2)]
        xr = [cf.take(D) for _ in range(2)]
        xmd = [cf.take(D) for _ in range(2)]
        qf = [cf.take(512) for _ in range(3)]
        rq = [cf.take(512) for _ in range(3)]
        uT = [[cf.take(528) for _ in range(4)] for _ in range(2)]
        ptmp = [cf.take(528) for _ in range(2)]
        rden = cf.take(2, 4)
        xn = [cb_.take(D) for _ in range(2)]
        hT_ = cb_.take(8, 512)
        hT = [hT_, hT_]
        kT = cb_.take(4, RT * 128)
        Vr = cb_.take(RT, 8 * 65).rearrange("p r (h d) -> p r h d", h=8)
        qTm = [cb_.take(4, 512) for _ in range(2)]
        sq = [cb_.take(512) for _ in range(3)]
        pTt = cb_.take(4, 512)
        mixT_ = cb_.take(8, 512)
        mixT = [mixT_, mixT_]
        PTb = [cb_.take(2, 640) for _ in range(2)]
        att = [cb_.take(512) for _ in range(2)]
        xn2 = [cb_.take(D) for _ in range(2)]
        xn2T = [cb_.take(8, 128) for _ in range(2)]

        for b in range(2):
            for g in range(4):
                A("pool", lambda e, b=b, g=g: e.memset(uT[b][g][:, 0:16], 0.0), writes=["uT%d%d" % (b, g)])
        A("pool", lambda e: e.memset(qTm[0][64:128, :, :], 0.0), writes=["qT"])
        A("pool", lambda e: e.memset(qTm[1][0:64, :, :], 0.0), writes=["qT"])

        SSOFF = (0, 640)
        PVR = ((pS, 1280), (pV, 0), (pV, 256))
        HG = ((0, 1, 2), (3, 4, 5), (6, 7))

        def norm_and_transpose(src, srckey, sl, dstT, dstTkeys, dstcols, xnbuf, xnkey, store_to=None, scale_eng="dve", defer=False):
            A("act", lambda e: e.activation(out=xnbuf[:], in_=src, func=AF.Square, accum_out=ssr[:, sl:sl + 1]),
              reads=[srckey], writes=[xnkey, "ssr%d" % sl])
            A("act", lambda e: e.activation(out=rst[:, sl:sl + 1], in_=ssr[:, sl:sl + 1], func=AF.Ln, scale=1.0 / D, bias=epsc[:]),
              reads=["ssr%d" % sl, "epsc"], writes=["rst%d" % sl])
            A("act", lambda e: e.activation(out=rst[:, sl:sl + 1], in_=rst[:, sl:sl + 1], func=AF.Exp, scale=-0.5),
              reads=["rst%d" % sl], writes=["rst%d" % sl])
            if scale_eng == "dve":
                A("dve", lambda e: e.tensor_scalar(out=xnbuf[:], in0=src, scalar1=rst[:, sl:sl + 1], scalar2=None, op0=ALU.mult),
                  reads=[srckey, "rst%d" % sl], writes=[xnkey])
            else:
                A("act", lambda e: e.activation(out=xnbuf[:], in_=src, func=AF.Copy, scale=rst[:, sl:sl + 1]),
                  reads=[srckey, "rst%d" % sl], writes=[xnkey])
            if store_to is not None:
                dma("pool", store_to, xnbuf[:], [xnkey], ["xn2s"], dkey="st_" + xnkey)

            def part_b():
                for kc in range(8):
                    A("pe", lambda e, kc=kc: e.transpose(out=pT[:, kc * 128:(kc + 1) * 128], in_=xnbuf[:, kc * 128:(kc + 1) * 128], identity=ident[:]),
                      reads=[xnkey, "ident"], writes=["pT"])
                A("dve", lambda e: e.tensor_copy(out=dstT[:, :, dstcols], in_=pT[:].rearrange("p (a b) -> p a b", a=8)),
                  reads=["pT"], writes=dstTkeys)
            if defer:
                return part_b
            part_b()

        ZB = [(pZ[:, 0, :], "pZ0"), (pZ[:, 1, :], "pZ1"), (pS[:, 0:512], "B4"), (pS[:, 512:1024], "B5"), (pS[:, 1024:1536], "B6")]
        SB = [(pC, "B3"), (pV, "B7")]
        zcnt = {"z": 0, "s": 0}

        def in_chunk(s, oc, ub, slot0, halo_st, full_st):
            zps, zk = ZB[zcnt["z"] % 5]
            zcnt["z"] += 1
            kslots = ["kT%d" % (slot0 + i) for i in range(4)]
            for kc in range(8):
                A("pe", lambda e, kc=kc: e.matmul(zps, lhsT=w_in_bf[:, kc, oc * 128:(oc + 1) * 128], rhs=hT_[:, kc, :],
                                                  start=(kc == 0), stop=(kc == 7)), reads=["w_in_bf", "hT"], writes=[zk])
            if oc < 4:
                g = oc
                if halo_st:
                    A("dve", lambda e: e.tensor_scalar(out=uT[ub][g][:, 16:528], in0=zps, scalar1=bzc[:, oc:oc + 1],
                                                       scalar2=hvt[:, 0:1], op0=ALU.add, op1=ALU.mult),
                      reads=[zk, "bzc", "hvt"], writes=["uT%d%d" % (ub, g)])
                else:
                    A("dve", lambda e: e.tensor_scalar(out=uT[ub][g][:, 16:528], in0=zps, scalar1=bzc[:, oc:oc + 1],
                                                       scalar2=None, op0=ALU.add),
                      reads=[zk, "bzc"], writes=["uT%d%d" % (ub, g)])
                return None
            isq = oc < 8
            c = (oc - 4) % 4
            tb = oc % 3
            A("dve", lambda e: e.tensor_scalar(out=qf[tb][:], in0=zps, scalar1=bzc[:, oc:oc + 1], scalar2=None, op0=ALU.add),
              reads=[zk, "bzc"], writes=["qf%d" % tb])
            A("act", lambda e: e.activation(out=sq[tb][:], in_=qf[tb][:], func=AF.Square),
              reads=["qf%d" % tb], writes=["sq%d" % tb])
            sps, sk = SB[zcnt["s"] % 2]
            zcnt["s"] += 1

            def part2():
                A("pe", lambda e: e.matmul(sps[:], lhsT=blk1[:], rhs=sq[tb][:], start=True, stop=True), reads=["blk1", "sq%d" % tb], writes=[sk])
                A("act", lambda e: e.activation(out=rq[tb][:], in_=sps[:], func=AF.Ln, bias=eps64[:]), reads=[sk, "eps64"], writes=["rq%d" % tb])
                A("act", lambda e: e.activation(out=rq[tb][:], in_=rq[tb][:], func=AF.Exp, scale=-0.5), reads=["rq%d" % tb], writes=["rq%d" % tb])
                if isq:
                    A("dve", lambda e: e.tensor_tensor(out=qTm[0][0:64, c, :], in0=qf[tb][0:64, :], in1=rq[tb][0:64, :], op=ALU.mult),
                      reads=["qf%d" % tb, "rq%d" % tb], writes=["qT"])
                    A("dve", lambda e: e.tensor_tensor(out=qTm[1][64:128, c, :], in0=qf[tb][64:128, :], in1=rq[tb][64:128, :], op=ALU.mult),
                      reads=["qf%d" % tb, "rq%d" % tb], writes=["qT"])
                else:
                    A("dve", lambda e: e.scalar_tensor_tensor(out=kT[:, c, slot0 * 128:(slot0 + 4) * 128], in0=qf[tb][:], scalar=gqk[:, 0:1],
                                                              in1=rq[tb][:], op0=ALU.mult, op1=ALU.mult),
                      reads=["qf%d" % tb, "rq%d" % tb, "gqk"], writes=kslots)
            return part2

        def v_tile(s, i, halo_st):
            te = 4 * s + i
            sl = te % RT
            zps, zk = ZB[zcnt["z"] % 5]
            zcnt["z"] += 1
            for kc in range(8):
                A("pe", lambda e, kc=kc: e.matmul(zps, lhsT=hT_[:, kc, i * 128:(i + 1) * 128], rhs=w_in_bf[:, kc, 1536:2048],
                                                  start=(kc == 0), stop=(kc == 7)), reads=["w_in_bf", "hT"], writes=[zk])
            zv = zps.rearrange("p (h d) -> p h d", h=8)
            if halo_st:
                A("dve", lambda e: e.scalar_tensor_tensor(out=Vr[:, sl, :, 0:64], in0=zv, scalar=hvt[:, 0:1],
                                                          in1=bzvm[:].rearrange("p (h d) -> p h d", h=8), op0=ALU.mult, op1=ALU.add),
                  reads=[zk, "hvt", "bzvm"], writes=["V%d" % sl])
                A("pool", lambda e: e.tensor_copy(out=Vr[:, sl, :, 64:65], in_=hvt[:, 0:1].unsqueeze(1).to_broadcast([128, 8, 1])),
                  reads=["hvt"], writes=["V%d" % sl])
            else:
                A("dve", lambda e: e.tensor_tensor(out=Vr[:, sl, :, 0:64], in0=zv, in1=bzv[:].rearrange("p (h d) -> p h d", h=8), op=ALU.add),
                  reads=[zk, "bzv"], writes=["V%d" % sl])
                A("pool", lambda e: e.memset(Vr[:, sl, :, 64:65], 1.0), writes=["V%d" % sl])

        def pool_group(g, ub, first_main):
            U = uT[ub][g]
            uk = "uT%d%d" % (ub, g)
            cur, curk = U, uk
            sh = 1
            for stp in range(g + 1):
                dstb = ptmp[stp % 2]
                dk = "ptmp%d" % (stp % 2)
                lo = 2 * sh - 1
                A("pool", lambda e, cur=cur, dstb=dstb, lo=lo, sh=sh: e.tensor_tensor(out=dstb[:, lo:528], in0=cur[:, lo:528], in1=cur[:, lo - sh:528 - sh], op=ALU.add),
                  reads=[curk], writes=[dk])
                cur, curk = dstb, dk
                sh *= 2
            w = 2 ** (g + 1)
            fin = cur
            A("dve", lambda e: e.scalar_tensor_tensor(out=pTt[:, g, :], in0=fin[:, 16:528], scalar=1.0 / w, in1=U[:, 16:528],
                                                      op0=ALU.mult, op1=ALU.subtract),
              reads=[curk, uk], writes=["pTt%d" % g])
            if first_main:
                A("pool", lambda e: e.tensor_tensor(out=fin[:, 0:16], in0=fin[:, 16:32], in1=invct[:, g, :], op=ALU.mult),
                  reads=[curk, "invct"], writes=[curk])
                A("pool", lambda e: e.tensor_tensor(out=pTt[:, g, 0:16], in0=fin[:, 0:16], in1=U[:, 16:32], op=ALU.subtract),
                  reads=[curk, uk], writes=["pTt%d" % g])
            def part_b():
                sps, sk = SB[g % 2]
                A("pe", lambda e: e.matmul(sps[:], lhsT=pw_bf[:, g, :], rhs=pTt[:, g, :], start=True, stop=True), reads=["pw_bf", "pTt%d" % g], writes=[sk])
                A("act", lambda e: e.activation(out=mixT_[:, g, :], in_=sps[:], func=AF.Copy, scale=psc[:, g:g + 1]),
                  reads=[sk, "psc"], writes=["mixT"])
            return part_b

        def attn_pair(te, i, pr, ab):
            c = pr
            pb2 = pr % 2
            PTp = PTb[pb2]
            ptk = "PT%d" % pb2
            for hh in range(2):
                pb = 64 * hh
                h = 2 * pr + hh
                for t in range(4):
                    ksl = (te - 4 + t) % RT
                    A("pe", lambda e, t=t, ksl=ksl, pb=pb, hh=hh: e.matmul(
                        pS[:, hh * 512 + t * 128: hh * 512 + (t + 1) * 128], lhsT=kT[:, c, ksl * 128:(ksl + 1) * 128],
                        rhs=qTm[hh][:, c, i * 128:(i + 1) * 128], start=True, stop=False),
                      reads=["kT%d" % ksl, "qT"], writes=["B%d" % (4 + hh)])
                    A("pe", lambda e, t=t, h=h, hh=hh: e.matmul(pS[:, hh * 512 + t * 128: hh * 512 + (t + 1) * 128], lhsT=BT[:, h, t, :], rhs=ident[:],
                                                                start=False, stop=True), reads=["BT", "ident"], writes=["B%d" % (4 + hh)])
            ksl4 = te % RT
            for hh in range(2):
                pb = 64 * hh
                h = 2 * pr + hh
                A("pe", lambda e, pb=pb, hh=hh: e.matmul(
                    pS[:, 1024 + hh * 128: 1024 + (hh + 1) * 128], lhsT=kT[:, c, ksl4 * 128:(ksl4 + 1) * 128],
                    rhs=qTm[hh][:, c, i * 128:(i + 1) * 128], start=True, stop=False),
                  reads=["kT%d" % ksl4, "qT"], writes=["B6"])
                A("pe", lambda e, h=h, hh=hh: e.matmul(pS[:, 1024 + hh * 128: 1024 + (hh + 1) * 128], lhsT=BT[:, h, 4, :], rhs=ident[:],
                                                       start=False, stop=True), reads=["BT", "ident"], writes=["B6"])
            for hh in range(2):
                A("act", lambda e, hh=hh: e.activation(out=PTp[:, hh, 0:512], in_=pS[:, hh * 512:(hh + 1) * 512], func=AF.Exp, scale=8.0),
                  reads=["B%d" % (4 + hh)], writes=[ptk])
            A("act", lambda e: e.activation(out=PTp[:, :, 512:640], in_=pS[:, 1024:1280].rearrange("p (a b) -> p a b", a=2), func=AF.Exp, scale=8.0),
              reads=["B6"], writes=[ptk])
            for hh in range(2):
                h = 2 * pr + hh
                pvt, pvk = (pV, "B7") if h < 4 else (pC, "B3")
                co = (h % 4) * 65
                for t in range(5):
                    ksl = (te - 4 + t) % RT
                    A("pe", lambda e, t=t, ksl=ksl, hh=hh, h=h, pvt=pvt, co=co: e.matmul(
                        pvt[:, co: co + 65], lhsT=PTp[:, hh, t * 128:(t + 1) * 128], rhs=Vr[:, ksl, h, :],
                        start=(t == 0), stop=(t == 4)), reads=[ptk, "V%d" % ksl], writes=[pvk])

        def attn_norm(hgi, ab):
            pvt, pvk = (pV, "B7") if hgi == 0 else (pC, "B3")
            pvv = pvt[:, 0:260].rearrange("p (h d) -> p h d", h=4)
            A("dve", lambda e: e.tensor_scalar(out=rden[:, hgi, :].unsqueeze(2), in0=pvv[:, :, 64:65], scalar1=1e-30, scalar2=None, op0=ALU.add),
              reads=[pvk], writes=["rden%d" % hgi])
            A("dve", lambda e: e.reciprocal(out=rden[:, hgi, :], in_=rden[:, hgi, :]),
              reads=["rden%d" % hgi], writes=["rden%d" % hgi])
            A("dve", lambda e: e.tensor_tensor(
                out=att[ab][:, hgi * 256:(hgi + 1) * 256].rearrange("p (h d) -> p h d", h=4), in0=pvv[:, :, 0:64],
                in1=rden[:, hgi, :].unsqueeze(2).to_broadcast([128, 4, 64]), op=ALU.mult),
              reads=[pvk, "rden%d" % hgi], writes=["att%d" % ab])

        def attention_tile(s, i):
            te = 4 * s + i
            ab = te % 2
            for pr in range(4):
                attn_pair(te, i, pr, ab)
                if pr % 2 == 1:
                    attn_norm(pr // 2, ab)

        def post_attention(s, i):
            te = 4 * s + i
            tl = te - NKV
            ab = te % 2
            for c in range(4):
                A("pe", lambda e, c=c: e.transpose(out=pT[:, c * 128:(c + 1) * 128], in_=att[ab][:, c * 128:(c + 1) * 128], identity=ident[:]),
                  reads=["att%d" % ab, "ident"], writes=["pT"])
            A("dve", lambda e: e.tensor_copy(out=mixT_[:, 4:8, i * 128:(i + 1) * 128], in_=pT[:, 0:512].rearrange("p (a b) -> p a b", a=4)),
              reads=["pT"], writes=["mixT"])
            for cbk in range(2):
                for kc in range(8):
                    A("pe", lambda e, cbk=cbk, kc=kc: e.matmul(pZ[:, cbk, :], lhsT=mixT_[:, kc, i * 128:(i + 1) * 128], rhs=w_out_bf[:, kc, cbk * 512:(cbk + 1) * 512],
                                                               start=(kc == 0), stop=(kc == 7)), reads=["mixT", "w_out_bf"], writes=["pZ%d" % cbk])
            rb = te % 2
            dma("sp", xr[rb][:], xe[te * 128:(te + 1) * 128, :], [], ["xr%d" % rb])
            A("dve", lambda e: e.tensor_tensor(out=xmd[rb][:], in0=pZ[:].rearrange("p a b -> p (a b)"), in1=xr[rb][:], op=ALU.add),
              reads=["pZ0", "pZ1", "xr%d" % rb], writes=["xmd%d" % rb])
            dma("pool", xmid[tl * 128:(tl + 1) * 128, :], xmd[rb][:], ["xmd%d" % rb], ["xmid"], dkey="st_xmd%d" % rb)

            def norm2_a():
                pb_ = norm_and_transpose(xmd[rb][:], "xmd%d" % rb, te % 8, xn2T[rb], ["xn2T%d" % rb], slice(0, 128), xn2[rb], "xn2%d" % rb,
                                         store_to=xn2s[tl * 128:(tl + 1) * 128, :], scale_eng="act", defer=True)

                def part_b():
                    pb_()
                    for kc in range(8):
                        A("pe", lambda e, kc=kc: e.matmul(pC[:, 0:36], lhsT=xn2T[rb][:, kc, :], rhs=wr_bf[:, kc, :], start=(kc == 0), stop=(kc == 7)),
                          reads=["xn2T%d" % rb, "wr_bf"], writes=["B3"])
                    A("dve", lambda e: e.tensor_tensor(out=logits[:, tl, :], in0=pC[:, 0:36], in1=biasR[:], op=ALU.add),
                      reads=["B3", "biasR"], writes=["logits"])
                return part_b
            return norm2_a

        def tail_copy(ub, g):
            A("pool", lambda e: e.tensor_copy(out=uT[ub][g][:, 0:16], in_=uT[1 - ub][g][:, 512:528]),
              reads=["uT%d%d" % (1 - ub, g)], writes=["uT%d%d" % (ub, g)])

        def norm_tile(s, i, defer=False):
            te = 4 * s + i
            xi = te % 2
            dma("sp", xin[xi][:], xe[te * 128:(te + 1) * 128, :], [], ["xin%d" % xi])
            return norm_and_transpose(xin[xi][:], "xin%d" % xi, te % 8, hT_, ["hT"], slice(i * 128, (i + 1) * 128),
                                      xn[te % 2], "xn%d" % (te % 2), defer=defer)

        q_norm2 = []
        q_b = []

        def do_st(s):
            halo_st = (4 * s) < NKV + NFH
            full_st = (4 * s) >= NKV
            first_main = (4 * s) == NKV + NFH
            if s == 0:
                for i in range(4):
                    norm_tile(0, i)
            ub = s % 2
            if s > 0:
                for g in range(4):
                    tail_copy(ub, g)
            slot0 = (4 * s) % RT
            pend2 = []
            pool_b = []
            for oc in range(12):
                if 4 <= oc < 8 and not full_st:
                    continue
                p2 = in_chunk(s, oc, ub, slot0, halo_st, full_st)
                if oc == 3 and full_st:
                    for g in range(4):
                        pool_b.append(pool_group(g, ub, first_main))
                if len(pend2) > 1:
                    pend2.pop(0)()
                if p2 is not None:
                    pend2.append(p2)
            v_tile(s, 0, halo_st)
            if pend2:
                pend2.pop(0)()
            v_tile(s, 1, halo_st)
            if pend2:
                pend2.pop(0)()
            for i in range(2, 4):
                v_tile(s, i, halo_st)
            for i in range(4):
                nb = norm_tile(s + 1, i, defer=True) if s + 1 < NST else None
                if full_st:
                    if i == 0:
                        for pb2 in pool_b:
                            pb2()
                    attention_tile(s, i)
                    if q_norm2:
                        q_b.append(q_norm2.pop(0)())
                    if len(q_b) > 1:
                        q_b.pop(0)()
                if nb is not None:
                    nb()
                if full_st:
                    q_norm2.append(post_attention(s, i))
            if s == NST - 1:
                while q_norm2:
                    q_b.append(q_norm2.pop(0)())
                while q_b:
                    q_b.pop(0)()

        for s in range(NST):
            do_st(s)
        p.barrier()

        cf = Carver(arf, AF_WORDS)
        cb_ = Carver(arb, AB_WORDS)
        NL = NTL
        R1 = cf.take(NL, 32)
        R2 = cf.take(NL, 32)
        R3 = cf.take(NL, 32)
        R4 = cf.take(NL, 32)
        sm = [cf.take(NL) for _ in range(6)]
        cntb = [cf.take(32) for _ in range(2)]
        startb = cf.take(32)
        widf = cf.take(NEXP, CT)
        ybuf = [cf.take(D) for _ in range(2)]
        sgb = [cf.take(CAP) for _ in range(2)]
        xmb = [cf.take(D) for _ in range(2)]
        y1b = [cf.take(D) for _ in range(2)]
        y2b_ = cf.take(D)
        y2b = [y2b_, y2b_]
        Abf = cb_.take(NL, 32)
        xtl = [cb_.take(D) for _ in range(2)]
        xw = [cb_.take(CT, D) for _ in range(2)]
        xsT = cb_.take(8, CAP)
        actT = cb_.take(4, CAP)
        Wg = [cb_.take(8, 512) for _ in range(2)]
        Wu = [cb_.take(8, 512) for _ in range(2)]
        Wd = [cb_.take(4, D) for _ in range(2)]

        WGK = [["Wg%d_%d" % (b, kc) for kc in range(8)] for b in range(2)]
        WUK = [["Wu%d_%d" % (b, kc) for kc in range(8)] for b in range(2)]
        WDK = [["Wd%d_%d" % (b, jc) for jc in range(4)] for b in range(2)]

        def load_w(e_):
            b = e_ % 2
            dma("pool", Wg[b][:], wg[e_ * D:(e_ + 1) * D, :].rearrange("(k p) n -> p k n", p=128), [], WGK[b], dkey="Wg%d" % b)
            dma("pool", Wu[b][:], wu[e_ * D:(e_ + 1) * D, :].rearrange("(k p) n -> p k n", p=128), [], WUK[b], dkey="Wu%d" % b)
            dma("pool", Wd[b][:], wd[e_ * 512:(e_ + 1) * 512, :].rearrange("(k p) n -> p k n", p=128), [], WDK[b], dkey="Wd%d" % b)

        load_w(0)
        load_w(1)

        gl = logits[:, :, 0:4]
        el = logits[:, :, 4:36]
        V = lambda e: e
        gmax, gsum, m1, m2, dd, ee = sm
        gone = R1[:, :, 0:4]
        A("dve", lambda e: e.reduce_max(out=gmax[:], in_=gl, axis=AX.X), reads=["logits"], writes=["gmax"])
        A("dve", lambda e: e.tensor_tensor(out=gone, in0=gl, in1=gmax[:].unsqueeze(2).to_broadcast([128, NL, 4]), op=ALU.is_equal),
          reads=["logits", "gmax"], writes=["R1"])
        gex = R2[:, :, 0:4]
        A("dve", lambda e: e.tensor_tensor(out=gex, in0=gl, in1=gmax[:].unsqueeze(2).to_broadcast([128, NL, 4]), op=ALU.subtract),
          reads=["logits", "gmax"], writes=["R2"])
        A("act", lambda e: e.activation(out=gex, in_=gex, func=AF.Exp), reads=["R2"], writes=["R2"])
        A("dve", lambda e: e.reduce_sum(out=gsum[:], in_=gex, axis=AX.X), reads=["R2"], writes=["gsum"])
        A("dve", lambda e: e.reciprocal(out=gsum[:], in_=gsum[:]), reads=["gsum"], writes=["gsum"])
        BIG = 1.0e4
        A("dve", lambda e: e.tensor_scalar(out=gone, in0=gone, scalar1=BIG, scalar2=-BIG, op0=ALU.mult, op1=ALU.add), reads=["R1"], writes=["R1"])
        em = R3
        A("dve", lambda e: e.tensor_tensor(out=em[:].rearrange("p n (g j) -> p n g j", g=4), in0=el.rearrange("p n (g j) -> p n g j", g=4),
                                           in1=gone.unsqueeze(3).to_broadcast([128, NL, 4, 8]), op=ALU.add), reads=["logits", "R1"], writes=["R3"])
        A("dve", lambda e: e.reduce_max(out=m1[:], in_=em[:], axis=AX.X), reads=["R3"], writes=["m1"])
        oh1 = R1
        A("dve", lambda e: e.tensor_tensor(out=oh1[:], in0=em[:], in1=m1[:].unsqueeze(2).to_broadcast([128, NL, 32]), op=ALU.is_equal),
          reads=["R3", "m1"], writes=["R1"])
        em2 = R2
        A("dve", lambda e: e.scalar_tensor_tensor(out=em2[:], in0=oh1[:], scalar=-BIG, in1=em[:], op0=ALU.mult, op1=ALU.add),
          reads=["R1", "R3"], writes=["R2"])
        A("dve", lambda e: e.reduce_max(out=m2[:], in_=em2[:], axis=AX.X), reads=["R2"], writes=["m2"])
        oh2 = R3
        A("dve", lambda e: e.tensor_tensor(out=oh2[:], in0=em2[:], in1=m2[:].unsqueeze(2).to_broadcast([128, NL, 32]), op=ALU.is_equal),
          reads=["R2", "m2"], writes=["R3"])
        A("dve", lambda e: e.tensor_tensor(out=dd[:], in0=m2[:], in1=m1[:], op=ALU.subtract), reads=["m1", "m2"], writes=["dd"])
        A("act", lambda e: e.activation(out=ee[:], in_=dd[:], func=AF.Exp), reads=["dd"], writes=["ee"])
        A("dve", lambda e: e.tensor_scalar(out=dd[:], in0=ee[:], scalar1=1.0, scalar2=None, op0=ALU.add), reads=["ee"], writes=["dd"])
        A("dve", lambda e: e.reciprocal(out=dd[:], in_=dd[:]), reads=["dd"], writes=["dd"])
        A("dve", lambda e: e.tensor_tensor(out=ee[:], in0=ee[:], in1=dd[:], op=ALU.mult), reads=["ee", "dd"], writes=["ee"])
        A("dve", lambda e: e.tensor_tensor(out=w1g[:], in0=dd[:], in1=gsum[:], op=ALU.mult), reads=["dd", "gsum"], writes=["w1g"])
        A("dve", lambda e: e.tensor_tensor(out=w2g[:], in0=ee[:], in1=gsum[:], op=ALU.mult), reads=["ee", "gsum"], writes=["w2g"])
        Asum = R2
        A("dve", lambda e: e.tensor_tensor(out=Asum[:], in0=oh1[:], in1=oh2[:], op=ALU.add), reads=["R1", "R3"], writes=["R2"])
        if NFH > 0:
            A("dve", lambda e: e.tensor_scalar(out=Asum[:, 0:NFH, :], in0=Asum[:, 0:NFH, :], scalar1=hvt[:, 0:1], scalar2=None, op0=ALU.mult),
              reads=["R2", "hvt"], writes=["R2"])
        A("dve", lambda e: e.tensor_copy(out=Abf[:], in_=Asum[:]), reads=["R2"], writes=["Abf"])
        Af = Abf[:].rearrange("p n e -> p (n e)")
        ncol = NL * 32
        banks = [(pZ[:, 0, :], "pZ0"), (pZ[:, 1, :], "pZ1"), (pC[:], "B3"), (pV[:], "B7")]
        assert ncol <= 1536
        Rk = R4[:].rearrange("p n e -> p (n e)")
        Tt = R2[:].rearrange("p n e -> p (n e)")
        for (lhs, lk, dst, dk) in ((tri_bf, "tri_bf", Rk, "R4"), (ones_bf, "ones_bf", Tt, "R2")):
            for c0 in range(0, ncol, 512):
                cw = min(512, ncol - c0)
                A("pe", lambda e, lhs=lhs, c0=c0, cw=cw: e.matmul(pS[:, c0:c0 + cw], lhsT=lhs[:], rhs=Af[:, c0:c0 + cw], start=True, stop=True),
                  reads=[lk, "Abf"], writes=["B4", "B5", "B6"])
            A("dve", lambda e, dst=dst: e.tensor_copy(out=dst, in_=pS[:, 0:ncol]), reads=["B4", "B5", "B6"], writes=[dk])
        A("dve", lambda e: e.memset(cntb[0][:], 0.0), writes=["cnt"])
        for n in range(NL):
            if n > 0:
                A("dve", lambda e, n=n: e.tensor_tensor(out=R4[:, n, :], in0=R4[:, n, :], in1=cntb[0][:], op=ALU.add), reads=["R4", "cnt"], writes=["R4"])
            A("dve", lambda e, n=n: e.tensor_tensor(out=cntb[0][:], in0=cntb[0][:], in1=R2[:, n, :], op=ALU.add), reads=["R2", "cnt"], writes=["cnt"])
        A("dve", lambda e: e.memset(startb[:], 0.0), writes=["startb"])
        for j in range(1, 32):
            A("dve", lambda e, j=j: e.tensor_tensor(out=startb[:, j:j + 1], in0=startb[:, j - 1:j], in1=cntb[0][:, j - 1:j], op=ALU.add),
              reads=["startb", "cnt"], writes=["startb"])
        A("dve", lambda e: e.tensor_tensor(out=R4[:], in0=R4[:], in1=startb[:].unsqueeze(1).to_broadcast([128, NL, 32]), op=ALU.add),
          reads=["R4", "startb"], writes=["R4"])
        for ki, (oh, ohk, sl_i, slk, tmpk) in enumerate(((oh1, "R1", slot1, "slot1", "gmax"), (oh2, "R3", slot2, "slot2", "m1"))):
            tmp = gmax if tmpk == "gmax" else m1
            A("dve", lambda e, oh=oh: e.tensor_tensor(out=oh[:], in0=oh[:], in1=R4[:], op=ALU.mult), reads=[ohk, "R4"], writes=[ohk])
            A("dve", lambda e, oh=oh, tmp=tmp: e.reduce_sum(out=tmp[:], in_=oh[:], axis=AX.X), reads=[ohk], writes=[tmpk])
            if NFH > 0:
                A("dve", lambda e, tmp=tmp: e.tensor_scalar(out=tmp[:, 0:NFH], in0=tmp[:, 0:NFH], scalar1=hvt[:, 0:1], scalar2=None, op0=ALU.mult),
                  reads=[tmpk, "hvt"], writes=[tmpk])
                A("dve", lambda e, tmp=tmp, ki=ki: e.scalar_tensor_tensor(out=tmp[:, 0:NFH], in0=trt[:, ki * TRC: ki * TRC + NFH], scalar=nhvt[:, 0:1], in1=tmp[:, 0:NFH],
                                                                 op0=ALU.mult, op1=ALU.add), reads=[tmpk, "trt", "nhvt"], writes=[tmpk])
            A("dve", lambda e, tmp=tmp, sl_i=sl_i: e.tensor_copy(out=sl_i[:], in_=tmp[:]), reads=[tmpk], writes=[slk])
        A("dve", lambda e: e.tensor_tensor(out=widf[:], in0=startb[:].unsqueeze(2).to_broadcast([128, NEXP, CT]),
                                           in1=iott[:, 0:CT].unsqueeze(1).to_broadcast([128, NEXP, CT]), op=ALU.add),
          reads=["startb", "iott"], writes=["widf"])
        A("dve", lambda e: e.tensor_copy(out=widx[:], in_=widf[:]), reads=["widf"], writes=["widx"])

        if debug:
            dma("sp", dbg_logits, logits[:], ["logits"], ["dbg_logits"])
            dma("sp", dbg_w[:, 0, :], w1g[:], ["w1g"], ["dbg_w1"])
            dma("sp", dbg_w[:, 1, :], w2g[:], ["w2g"], ["dbg_w2"])
            dma("sp", dbg_slot[:, 0, :], slot1[:], ["slot1"], ["dbg_s1"])
            dma("sp", dbg_slot[:, 1, :], slot2[:], ["slot2"], ["dbg_s2"])
        xskeys = []
        for tl in range(NTL):
            b = tl % 2
            dma("sp", xtl[b][:], xn2s[tl * 128:(tl + 1) * 128, :], ["xn2s"], ["xtl%d" % b])
            for k_, (sl_i, slk) in enumerate(((slot1, "slot1"), (slot2, "slot2"))):
                key = "xs_%d_%d" % (tl, k_)
                xskeys.append(key)
                A("pool", lambda e, sl_i=sl_i, tl=tl, b=b: e.indirect_dma_start(
                    out=xs[:, :], out_offset=bass.IndirectOffsetOnAxis(ap=sl_i[:, tl:tl + 1], axis=0), in_=xtl[b][:], in_offset=None),
                  reads=["xtl%d" % b, slk], writes=[key], dma=True, dkey="sc_xtl%d" % b)

        NOW, NTHR = cfg.NOW, cfg.NTHR
        BIGI = 1.0e6
        cnt_ = cntb[0]
        assert NOW <= NL and NTHR <= NL
        gtm = R2[:, 0:NTHR, :].rearrange("p t e -> p (t e)").rearrange("p (e t) -> p e t", e=32)
        nov = cf.take(32)
        cum = cf.take(32)
        indw = R1[:, 0:NOW, :]
        tmpw = R3[:, 0:NOW, :]
        limv = cf.take(32)
        jbase = cf.take(32)
        wsc = [cf.take(NOW) for _ in range(5)]
        gidf = cf.take(NOW, CT)
        yidf = cf.take(NOW, CT)
        mskf = cf.take(NOW, CT)
        wgidf = cf.take(NOW, 8)
        wdidf = cf.take(NOW, 4)
        gidx = T("gidx", [128, NOW, CT], I32)
        yidx = T("yidx", [128, NOW, CT], I32)
        wgidx = T("wgidx", [128, NOW, 8], I32)
        wdidx = T("wdidx", [128, NOW, 4], I32)
        DV = lambda fn, r, w: A("dve", fn, reads=r, writes=w)
        DV(lambda e: e.tensor_tensor(out=gtm[:], in0=cnt_[:].unsqueeze(2).to_broadcast([128, 32, NTHR]),
                                     in1=thrt[:, 0:NTHR].unsqueeze(1).to_broadcast([128, 32, NTHR]), op=ALU.is_gt), ["cnt", "thrt"], ["R2"])
        DV(lambda e: e.reduce_sum(out=nov[:], in_=gtm[:], axis=AX.X), ["R2"], ["nov"])
        DV(lambda e: e.memset(cum[:], 0.0), [], ["cum"])
        for j in range(1, 32):
            DV(lambda e, j=j: e.tensor_tensor(out=cum[:, j:j + 1], in0=cum[:, j - 1:j], in1=nov[:, j - 1:j], op=ALU.add), ["cum", "nov"], ["cum"])
        wvb = wvt[:, 0:NOW].unsqueeze(2).to_broadcast([128, NOW, 32])
        DV(lambda e: e.tensor_tensor(out=indw[:], in0=cum[:].unsqueeze(1).to_broadcast([128, NOW, 32]), in1=wvb, op=ALU.is_le), ["cum", "wvt"], ["R1"])
        DV(lambda e: e.tensor_tensor(out=limv[:], in0=cum[:], in1=nov[:], op=ALU.add), ["cum", "nov"], ["limv"])
        DV(lambda e: e.tensor_tensor(out=tmpw[:], in0=limv[:].unsqueeze(1).to_broadcast([128, NOW, 32]), in1=wvb, op=ALU.is_gt), ["limv", "wvt"], ["R3"])
        DV(lambda e: e.tensor_tensor(out=indw[:], in0=indw[:], in1=tmpw[:], op=ALU.mult), ["R1", "R3"], ["R1"])
        vld, ew, ow, lw, tw = wsc
        DV(lambda e: e.reduce_sum(out=vld[:], in_=indw[:], axis=AX.X), ["R1"], ["vld"])
        DV(lambda e: e.tensor_tensor(out=tmpw[:], in0=indw[:], in1=evt[:].unsqueeze(1).to_broadcast([128, NOW, 32]), op=ALU.mult), ["R1", "evt"], ["R3"])
        DV(lambda e: e.reduce_sum(out=ew[:], in_=tmpw[:], axis=AX.X), ["R3"], ["ew"])
        DV(lambda e: e.tensor_scalar(out=jbase[:], in0=cum[:], scalar1=-float(CAP), scalar2=float(CAP), op0=ALU.mult, op1=ALU.add), ["cum"], ["jbase"])
        DV(lambda e: e.tensor_tensor(out=jbase[:], in0=jbase[:], in1=startb[:], op=ALU.add), ["jbase", "startb"], ["jbase"])
        DV(lambda e: e.tensor_tensor(out=tmpw[:], in0=indw[:], in1=jbase[:].unsqueeze(1).to_broadcast([128, NOW, 32]), op=ALU.mult), ["R1", "jbase"], ["R3"])
        DV(lambda e: e.reduce_sum(out=ow[:], in_=tmpw[:], axis=AX.X), ["R3"], ["ow"])
        DV(lambda e: e.scalar_tensor_tensor(out=ow[:], in0=wvt[:, 0:NOW], scalar=float(CAP), in1=ow[:], op0=ALU.mult, op1=ALU.add), ["ow", "wvt"], ["ow"])
        DV(lambda e: e.tensor_tensor(out=ow[:], in0=ow[:], in1=vld[:], op=ALU.mult), ["ow", "vld"], ["ow"])
        DV(lambda e: e.tensor_tensor(out=limv[:], in0=startb[:], in1=cnt_[:], op=ALU.add), ["startb", "cnt"], ["limv"])
        DV(lambda e: e.tensor_tensor(out=tmpw[:], in0=indw[:], in1=limv[:].unsqueeze(1).to_broadcast([128, NOW, 32]), op=ALU.mult), ["R1", "limv"], ["R3"])
        DV(lambda e: e.reduce_sum(out=lw[:], in_=tmpw[:], axis=AX.X), ["R3"], ["lw"])
        DV(lambda e: e.tensor_scalar(out=tw[:], in0=vld[:], scalar1=-BIGI, scalar2=BIGI, op0=ALU.mult, op1=ALU.add), ["vld"], ["tw"])
        DV(lambda e: e.tensor_tensor(out=gidf[:], in0=ow[:].unsqueeze(2).to_broadcast([128, NOW, CT]),
                                     in1=iott[:, 0:CT].unsqueeze(1).to_broadcast([128, NOW, CT]), op=ALU.add), ["ow", "iott"], ["gidf"])
        DV(lambda e: e.tensor_tensor(out=mskf[:], in0=gidf[:], in1=lw[:].unsqueeze(2).to_broadcast([128, NOW, CT]), op=ALU.is_lt), ["gidf", "lw"], ["mskf"])
        DV(lambda e: e.scalar_tensor_tensor(out=yidf[:], in0=gidf[:], scalar=-BIGI, in1=mskf[:], op0=ALU.add, op1=ALU.mult), ["gidf", "mskf"], ["yidf"])
        DV(lambda e: e.tensor_scalar(out=yidf[:], in0=yidf[:], scalar1=BIGI, scalar2=None, op0=ALU.add), ["yidf"], ["yidf"])
        DV(lambda e: e.tensor_tensor(out=gidf[:], in0=gidf[:], in1=tw[:].unsqueeze(2).to_broadcast([128, NOW, CT]), op=ALU.add), ["gidf", "tw"], ["gidf"])
        DV(lambda e: e.tensor_copy(out=gidx[:], in_=gidf[:]), ["gidf"], ["gidx"])
        DV(lambda e: e.tensor_copy(out=yidx[:], in_=yidf[:]), ["yidf"], ["yidx"])
        DV(lambda e: e.scalar_tensor_tensor(out=ew[:], in0=ew[:], scalar=1024.0, in1=tw[:], op0=ALU.mult, op1=ALU.add), ["ew", "tw"], ["ew"])
        DV(lambda e: e.tensor_tensor(out=wgidf[:], in0=ew[:].unsqueeze(2).to_broadcast([128, NOW, 8]),
                                     in1=iot8t[:].unsqueeze(1).to_broadcast([128, NOW, 8]), op=ALU.add), ["ew", "iot8t"], ["wgidf"])
        DV(lambda e: e.tensor_copy(out=wgidx[:], in_=wgidf[:]), ["wgidf"], ["wgidx"])
        DV(lambda e: e.scalar_tensor_tensor(out=ew[:], in0=ew[:], scalar=0.5, in1=tw[:], op0=ALU.mult, op1=ALU.add), ["ew", "tw"], ["ew"])
        DV(lambda e: e.tensor_tensor(out=wdidf[:], in0=ew[:].unsqueeze(2).to_broadcast([128, NOW, 4]),
                                     in1=iot8t[:, 0:4].unsqueeze(1).to_broadcast([128, NOW, 4]), op=ALU.add), ["ew", "iot8t"], ["wdidf"])
        DV(lambda e: e.tensor_copy(out=wdidx[:], in_=wdidf[:]), ["wdidf"], ["wdidx"])

        gbanks = [(pZ[:, 0, :], "pZ0"), (pZ[:, 1, :], "pZ1"), (pC[:], "B3"), (pV[:], "B7")]
        dbanks = [(pS[:, 512:1024], "B5"), (pS[:, 1024:1536], "B6")]
        pT2 = pS[:, 0:512].bitcast(BF16)
        tbanks = [(pT, "pT"), (pT2, "B4")]
        cnts = {"gi": 0, "di": 0, "ti": 0}
        NJOB = NEXP + NOW
        wg2, wu2, wd2 = wg, wu, wd

        bregs = memo.setdefault("__bregs", {})

        def breg(e, val):
            if val not in bregs:
                r = e.alloc_register("bc%d" % val)
                e.reg_mov(r, val)
                bregs[val] = r
            return bregs[val]

        def job_rows(k, j, for_y):
            if k < NEXP:
                return widx[:, k, j:j + 1]
            return (yidx if for_y else gidx)[:, k - NEXP, j:j + 1]

        def job_load_w(k):
            b = k % 2
            if k < NEXP:
                load_w(k)
                return
            w = k - NEXP
            og, ou, od = [], [], []
            for kc in range(8):
                og.append(A("pool", lambda e, kc=kc: e.indirect_dma_start(out=Wg[b][:, kc, :], out_offset=None, in_=wg2,
                                                                          in_offset=bass.IndirectOffsetOnAxis(ap=wgidx[:, w, kc:kc + 1], axis=0),
                                                                          bounds_check=breg(e, NEXP * 1024 - 1), oob_is_err=False),
                            reads=["wgidx"], writes=[WGK[b][kc]], dma=True, dkey="Wg%d" % b))
                ou.append(A("pool", lambda e, kc=kc: e.indirect_dma_start(out=Wu[b][:, kc, :], out_offset=None, in_=wu2,
                                                                          in_offset=bass.IndirectOffsetOnAxis(ap=wgidx[:, w, kc:kc + 1], axis=0),
                                                                          bounds_check=breg(e, NEXP * 1024 - 1), oob_is_err=False),
                            reads=["wgidx"], writes=[WUK[b][kc]], dma=True, dkey="Wu%d" % b))
            for jc in range(4):
                od.append(A("pool", lambda e, jc=jc: e.indirect_dma_start(out=Wd[b][:, jc, :], out_offset=None, in_=wd2,
                                                                          in_offset=bass.IndirectOffsetOnAxis(ap=wdidx[:, w, jc:jc + 1], axis=0),
                                                                          bounds_check=breg(e, NEXP * 512 - 1), oob_is_err=False),
                            reads=["wdidx"], writes=[WDK[b][jc]], dma=True, dkey="Wd%d" % b))
            for grp in (og, ou, od):
                for o_ in grp:
                    o_.tgt = grp[-1].tgt

        def job_gather(k):
            b = k % 2
            for j in range(CT):
                rows = job_rows(k, j, False)
                if k < NEXP:
                    A("pool", lambda e, j=j, rows=rows: e.indirect_dma_start(
                        out=xw[b][:, j, :], out_offset=None, in_=xs[:, :], in_offset=bass.IndirectOffsetOnAxis(ap=rows, axis=0)),
                      reads=xskeys + ["widx"], writes=["xw%d_%d" % (b, j)], dma=True)
                else:
                    A("pool", lambda e, j=j, rows=rows: e.indirect_dma_start(
                        out=xw[b][:, j, :], out_offset=None, in_=xs[:, :], in_offset=bass.IndirectOffsetOnAxis(ap=rows, axis=0),
                        bounds_check=breg(e, cfg.XSR - 1), oob_is_err=False),
                      reads=xskeys + ["gidx"], writes=["xw%d_%d" % (b, j)], dma=True)

        def job_compute(k):
            b = k % 2
            for kc in range(8):
                (tps, tk_) = tbanks[cnts["ti"] % 2]
                cnts["ti"] += 1
                for j in range(CT):
                    A("pe", lambda e, kc=kc, j=j, tps=tps: e.transpose(out=tps[:, j * 128:(j + 1) * 128],
                                                                       in_=xw[b][:, j, kc * 128:(kc + 1) * 128], identity=ident[:]),
                      reads=["xw%d_%d" % (b, j), "ident"], writes=[tk_])
                A("act", lambda e, kc=kc, tps=tps: e.activation(out=xsT[:, kc, :], in_=tps[:, 0:CAP], func=AF.Identity,
                                                                scale=mul2c[:, kc:kc + 1], bias=modc[:, 2, kc:kc + 1]),
                  reads=[tk_, "mul2c", "modc"], writes=["xsT%d" % kc])
            for jc in range(4):
                (gps, gk_) = gbanks[cnts["gi"] % 4]
                (ups, uk_) = gbanks[(cnts["gi"] + 1) % 4]
                cnts["gi"] += 2
                for kc in range(8):
                    A("pe", lambda e, gps=gps, kc=kc, jc=jc: e.matmul(gps[:, 0:CAP], lhsT=Wg[b][:, kc, jc * 128:(jc + 1) * 128], rhs=xsT[:, kc, :],
                                                                     start=(kc == 0), stop=(kc == 7)), reads=WGK[b] + ["xsT%d" % kc], writes=[gk_])
                for kc in range(8):
                    A("pe", lambda e, ups=ups, kc=kc, jc=jc: e.matmul(ups[:, 0:CAP], lhsT=Wu[b][:, kc, jc * 128:(jc + 1) * 128], rhs=xsT[:, kc, :],
                                                                     start=(kc == 0), stop=(kc == 7)), reads=WUK[b] + ["xsT%d" % kc], writes=[uk_])
                sb_ = jc % 2
                A("act", lambda e, gps=gps, sb_=sb_: e.activation(out=sgb[sb_][:], in_=gps[:, 0:CAP], func=AF.Silu), reads=[gk_], writes=["sg%d" % sb_])
                A("dve", lambda e, ups=ups, sb_=sb_, jc=jc: e.tensor_tensor(out=actT[:, jc, :], in0=ups[:, 0:CAP], in1=sgb[sb_][:], op=ALU.mult),
                  reads=[uk_, "sg%d" % sb_], writes=["actT"])
            for j in range(CT):
                yb = (k * CT + j) % 2
                for cbk in range(2):
                    (dps, dk_) = dbanks[cnts["di"] % 2]
                    cnts["di"] += 1
                    for jc in range(4):
                        A("pe", lambda e, dps=dps, jc=jc, j=j, cbk=cbk: e.matmul(dps, lhsT=actT[:, jc, j * 128:(j + 1) * 128], rhs=Wd[b][:, jc, cbk * 512:(cbk + 1) * 512],
                                                                                start=(jc == 0), stop=(jc == 3)), reads=["actT"] + WDK[b], writes=[dk_])
                    A("dve", lambda e, dps=dps, cbk=cbk, yb=yb: e.tensor_tensor(out=ybuf[yb][:, cbk * 512:(cbk + 1) * 512], in0=dps, in1=g2bc[:, cbk * 512:(cbk + 1) * 512], op=ALU.mult),
                      reads=[dk_, "g2bc"], writes=["ybuf%d" % yb])
                rows = job_rows(k, j, True)
                if k < NEXP:
                    A("pool", lambda e, rows=rows, yb=yb: e.indirect_dma_start(
                        out=ys[:, :], out_offset=bass.IndirectOffsetOnAxis(ap=rows, axis=0), in_=ybuf[yb][:], in_offset=None),
                      reads=["ybuf%d" % yb, "widx"], writes=["ys"], dma=True, dkey="sc_ybuf%d" % yb)
                else:
                    A("pool", lambda e, rows=rows, yb=yb: e.indirect_dma_start(
                        out=ys[:, :], out_offset=bass.IndirectOffsetOnAxis(ap=rows, axis=0), in_=ybuf[yb][:], in_offset=None,
                        bounds_check=breg(e, cfg.XSR - 1), oob_is_err=False),
                      reads=["ybuf%d" % yb, "yidx"], writes=["ys"], dma=True, dkey="sc_ybuf%d" % yb)

        job_gather(0)
        for k in range(NJOB):
            if k + 1 < NJOB:
                job_gather(k + 1)
            job_compute(k)
            if k + 2 < NJOB:
                job_load_w(k + 2)

        for tl in range(NTL):
            b = tl % 2
            dma("sp", xmb[b][:], xmid[tl * 128:(tl + 1) * 128, :], ["xmid"], ["xmb%d" % b])
            A("pool", lambda e, tl=tl, b=b: e.indirect_dma_start(out=y1b[b][:], out_offset=None, in_=ys[:, :],
                                                                 in_offset=bass.IndirectOffsetOnAxis(ap=slot1[:, tl:tl + 1], axis=0)),
              reads=["ys", "slot1"], writes=["y1b%d" % b], dma=True)
            A("pool", lambda e, tl=tl, b=b: e.indirect_dma_start(out=y2b[b][:], out_offset=None, in_=ys[:, :],
                                                                 in_offset=bass.IndirectOffsetOnAxis(ap=slot2[:, tl:tl + 1], axis=0)),
              reads=["ys", "slot2"], writes=["y2b"], dma=True)
            A("dve", lambda e, tl=tl, b=b: e.scalar_tensor_tensor(out=xmb[b][:], in0=y1b[b][:], scalar=w1g[:, tl:tl + 1], in1=xmb[b][:], op0=ALU.mult, op1=ALU.add),
              reads=["y1b%d" % b, "w1g", "xmb%d" % b], writes=["xmb%d" % b])
            A("dve", lambda e, tl=tl, b=b: e.scalar_tensor_tensor(out=xmb[b][:], in0=y2b[b][:], scalar=w2g[:, tl:tl + 1], in1=xmb[b][:], op0=ALU.mult, op1=ALU.add),
              reads=["y2b", "w2g", "xmb%d" % b], writes=["xmb%d" % b])
            dma("sp", xo[tl * 128:(tl + 1) * 128, :], xmb[b][:], ["xmb%d" % b], ["xo"], dkey="st_xmb%d" % b)
        p.barrier()


def _colform(v):
    return np.ascontiguousarray(v.reshape(-1, 128).T).astype(np.float32)


def _const_tables(cfg):
    q = np.arange(128)[:, None]
    tabs_idx = np.zeros((128, 5, 128), np.int64)
    mask = np.zeros((128, 5, 128), np.float32)
    for t in range(5):
        k = np.arange(128)[None, :]
        rel = 128 * (4 - t) + q - k
        tabs_idx[:, t, :] = np.clip(rel, -128, 128) + 128
        qc = q // 64
        kc = 2 * (t - 4) + k // 64
        ok = (kc <= qc) & (kc >= qc - 8)
        mask[:, t, :] = ok
    tri = (np.arange(128)[:, None] < np.arange(128)[None, :]).astype(np.float32)
    iot = (np.arange(128)[:, None] + 128 * np.arange(4)[None, :]).astype(np.float32)
    trash = (cfg.TRASH + np.arange(128)[:, None] + 128 * np.arange(max(cfg.NFH, 1))[None, :]).astype(np.float32)
    return tabs_idx, mask, tri, iot, trash


def layer_inputs(cfg, l, xe, cb, first_half, P, li=0):
    tabs_idx, mask, tri, iot, trash = _const_tables(cfg)
    btab = np.ascontiguousarray(P["rel_bias"][:, tabs_idx].transpose(1, 0, 2, 3)).astype(np.float32)
    invc = np.zeros((128, 4, 16), np.float32)
    for g, w in enumerate((2, 4, 8, 16)):
        cnt = np.minimum(np.arange(16) + 1, w) if first_half else np.full(16, w)
        invc[:, g, :] = (1.0 / cnt.astype(np.float64)).astype(np.float32)[None, :]
    hvv = 0.0 if first_half else 1.0
    trash = (cfg.TRASH + np.arange(128)[:, None] + 128 * np.arange(TRC)[None, :]).astype(np.float32)
    trash = np.concatenate([trash, trash + cfg.NFH * 128], axis=1)
    m = {
        "xe": np.ascontiguousarray(xe, dtype=np.float32),
        "cT": _colform(cb),
        "ada_w": P["ada_w"][l], "ada_b": P["ada_b"][l][None, :],
        "n1c": _colform(P["norm1_g"][l]), "n2c": _colform(P["norm2_g"][l]),
        "w_in": P["w_in"][l], "w_out": P["w_out"][l],
        "pool_w": np.ascontiguousarray(P["pool_w"][l].transpose(1, 0, 2)),
        "pscale": _colform(P["pool_scale"][l]),
        "gq": np.ascontiguousarray(np.tile(P["q_norm_g"][l], 2)[:, None]), "gk": np.ascontiguousarray(np.tile(P["k_norm_g"][l], 2)[:, None]),
        "btab": btab, "bmask": mask,
        "wr": np.ascontiguousarray(np.concatenate([P["router_group_w"][l], P["router_expert_w"][l]], axis=1)),
        "br": np.ascontiguousarray(np.tile(np.concatenate([P["router_group_b"][l], P["router_expert_b"][l]])[None, :], (128, 1))),
        "wg": P["moe_w_gate"][l].reshape(NEXP * D, 512), "wu": P["moe_w_up"][l].reshape(NEXP * D, 512), "wd": P["moe_w_down"][l].reshape(NEXP * 512, D),
        "hv": np.full((128, 1), hvv, np.float32), "nhv": np.full((128, 1), 1.0 - hvv, np.float32),
        "invc": invc, "tri": tri, "iot": iot, "trashi": trash,
        "thr": np.tile((cfg.CAP * (np.arange(16) + 1)).astype(np.float32)[None, :], (128, 1)),
        "wv": np.tile(np.arange(32, dtype=np.float32)[None, :], (128, 1)),
        "ev": np.tile(np.arange(32, dtype=np.float32)[None, :], (128, 1)),
        "iot8": (np.arange(128)[:, None] + 128 * np.arange(8)[None, :]).astype(np.float32),
    }
    return {(k + "_%d" % li if k in PERL else k): v for k, v in m.items()}


_NC_CACHE = {}


def kernel(**inputs):
    P = {k: np.asarray(v) for k, v in inputs.items()}
    x = P["x"]
    B, S, _ = x.shape
    cfg0 = Cfg(nkv=4, nfh=4, nm=32, cap=512)
    cfg1 = Cfg(nkv=4, nfh=0, nm=32, cap=512)
    if "nc" not in _NC_CACHE:
        _NC_CACHE["nc"] = build_program([cfg0, cfg1])
    nc = _NC_CACHE["nc"]
    half = S // 2
    in_maps = []
    for c in range(8):
        b, hf = c // 2, c % 2
        main = x[b, hf * half:(hf + 1) * half]
        halo = np.zeros((1024, D), np.float32) if hf == 0 else x[b, half - 1024:half]
        xe = np.concatenate([halo, main], axis=0)
        m = layer_inputs(cfg0, 0, xe, P["c"][b], hf == 0, P, li=0)
        m1 = layer_inputs(cfg1, 1, xe[:128], P["c"][b], hf == 0, P, li=1)
        m.update({k: v for k, v in m1.items() if k.endswith("_1")})
        in_maps.append(m)
    res = run_bass_kernel_spmd(nc, in_maps, core_ids=list(range(8)))
    out = np.empty_like(x)
    for c in range(8):
        b, hf = c // 2, c % 2
        out[b, hf * half:(hf + 1) * half] = res.results[c]["xo"]
    return out
```

```python
from contextlib import ExitStack

import numpy as np
import concourse.bass as bass
import concourse.mybir as mybir
from concourse.bass_utils import run_bass_kernel_spmd

F32 = mybir.dt.float32
BF16 = mybir.dt.bfloat16
I32 = mybir.dt.int32
AF = mybir.ActivationFunctionType
ALU = mybir.AluOpType
AX = mybir.AxisListType

ENGS = ("sp", "act", "dve", "pool", "pe")
PSUM_KEYS = frozenset(["pT", "pZ0", "pZ1", "B3", "B4", "B5", "B6", "B7"])
D = 1024
EPS = 1e-6
NEXP = 32
RT = 8


class Op:
    __slots__ = ("eng", "fn", "dma", "dkey", "deps", "sig", "idx", "tgt", "waits")

    def __init__(self, eng, fn, dma, dkey):
        self.eng = eng
        self.fn = fn
        self.dma = dma
        self.dkey = dkey
        self.deps = []
        self.sig = False
        self.idx = 0
        self.tgt = 0
        self.waits = []


class PB:
    def __init__(self, nc):
        self.nc = nc
        self.ops = []
        self.last_w = {}
        self.readers = {}
        self.dma_cnt = {}

    def add(self, eng, fn, reads=(), writes=(), dma=False, dkey=None):
        if dma and dkey is None:
            dkey = writes[0]
        op = Op(eng, fn, dma, dkey)
        deps = set()
        for k in reads:
            w = self.last_w.get(k)
            if w is not None:
                deps.add(w)
            if k in PSUM_KEYS:
                for r in self.readers.get(k, ()):
                    if r.eng != eng:
                        deps.add(r)
        for k in writes:
            w = self.last_w.get(k)
            if w is not None:
                deps.add(w)
            for r in self.readers.get(k, ()):
                deps.add(r)
        op.deps = list(deps)
        for k in reads:
            self.readers.setdefault(k, []).append(op)
        for k in writes:
            self.last_w[k] = op
            self.readers[k] = []
        if dma:
            self.dma_cnt[dkey] = self.dma_cnt.get(dkey, 0) + 1
            op.tgt = 16 * self.dma_cnt[dkey]
        self.ops.append(op)
        return op

    def barrier(self):
        allkeys = list(set(self.last_w.keys()) | set(self.readers.keys()))
        self.add("sp", lambda e: e.nop(), reads=[], writes=allkeys + ["__bar"])
        for eng in ENGS:
            self.add(eng, lambda e: e.nop(), reads=["__bar"], writes=["__bar_" + eng])
        self.last_w = {k: v for k, v in self.last_w.items() if k.startswith("__bar")}
        self.readers = {k: v for k, v in self.readers.items() if k.startswith("__bar")}

    def emit(self):
        nc = self.nc
        for op in self.ops:
            for d in op.deps:
                if not d.dma:
                    d.sig = True
        cnt = {e: 0 for e in ENGS}
        for op in self.ops:
            if not op.dma and op.sig:
                cnt[op.eng] += 1
                op.idx = cnt[op.eng]
        dkeys = sorted(self.dma_cnt.keys())
        with ExitStack() as st:
            esem = {e: st.enter_context(nc.semaphore("es_" + e)) for e in ENGS}
            dsem = {k: st.enter_context(nc.semaphore("ds%d" % i)) for i, k in enumerate(dkeys)}
            waited = {e: {} for e in ENGS}
            for op in self.ops:
                need = {}
                for d in op.deps:
                    if d.dma:
                        key, val = ("d", d.dkey), d.tgt
                    else:
                        if d.eng == op.eng and op.eng == "pe":
                            continue
                        key, val = ("e", d.eng), d.idx
                    if need.get(key, 0) < val:
                        need[key] = val
                w = waited[op.eng]
                for key, val in need.items():
                    if w.get(key, 0) < val:
                        w[key] = val
                        op.waits.append((dsem[key[1]] if key[0] == "d" else esem[key[1]], val))
            block = st.enter_context(nc.Block())

            def run(engname):
                def body(e):
                    for op in self.ops:
                        if op.eng != engname:
                            continue
                        for (s, v) in op.waits:
                            e.wait_ge(s, v)
                        ins = op.fn(e)
                        if op.dma:
                            ins.then_inc(dsem[op.dkey], 16)
                        elif op.sig:
                            ins.then_inc(esem[engname], 1)
                return body

            block.sync(run("sp"))
            block.scalar(run("act"))
            block.vector(run("dve"))
            block.gpsimd(run("pool"))
            block.tensor(run("pe"))


class Cfg:
    def __init__(self, nkv=4, nfh=0, nm=32, cap=512):
        self.NKV, self.NFH, self.NM, self.CAP = nkv, nfh, nm, cap
        self.NTE = nkv + nfh + nm
        self.NTL = nfh + nm
        self.NST = self.NTE // 4
        self.CT = cap // 128
        self.NTOK = self.NTL * 128
        self.TRASH = 2 * self.NTOK + cap
        self.XSR = self.TRASH + 2 * max(nfh, 1) * 128
        self.NOW = -(-2 * self.NTOK // cap)
        self.NTHR = -(-self.NTOK // cap)
        assert self.NTE % 4 == 0 and nkv % 4 == 0 and nfh % 4 == 0 and cap % 128 == 0


PERL = frozenset(["ada_w", "ada_b", "n1c", "n2c", "w_in", "w_out", "pool_w", "pscale", "gq", "gk", "wr", "br", "wg", "wu", "wd"])
TRC = 4


class _Ctx:
    pass


def build_program(cfgs, debug=False):
    nc = bass.Bass("TRN2", target_bir_lowering=False)
    ctx = _Ctx()
    ctx.nc, ctx.memo, ctx.p, ctx.nl = nc, {}, PB(nc), len(cfgs)
    with ExitStack() as st:
        ctx.st = st
        for li, cfg in enumerate(cfgs):
            _emit_layer(ctx, li, cfg, debug)
        ctx.p.emit()
    return nc


def build_layer(cfg, debug=False):
    return build_program([cfg], debug)


def _emit_layer(ctx, li, cfg, debug=False):
    nc, st, memo = ctx.nc, ctx.st, ctx.memo
    last = li == ctx.nl - 1
    NTE, NTL, NST, NKV, NFH, CT, CAP = cfg.NTE, cfg.NTL, cfg.NST, cfg.NKV, cfg.NFH, cfg.CT, cfg.CAP

    def din(name, shape, dt=F32):
        nm = name + ("_%d" % li if name in PERL else "")
        if nm not in memo:
            memo[nm] = nc.dram_tensor(nm, list(shape), dt, kind="ExternalInput").ap()
        return memo[nm]

    def dscr(name, shape, dt, kind="Internal"):
        if name not in memo:
            memo[name] = nc.dram_tensor(name, list(shape), dt, kind=kind).ap()
        return memo[name]

    xe = din("xe", [NTE * 128, D]) if li == 0 else memo["x1_%d" % (li - 1)]
    cT = din("cT", [128, 8])
    ada_w = din("ada_w", [D, 6 * D])
    ada_b = din("ada_b", [1, 6 * D])
    n1c = din("n1c", [128, 8])
    n2c = din("n2c", [128, 8])
    w_in = din("w_in", [D, 2048])
    w_out = din("w_out", [D, D])
    pool_w = din("pool_w", [128, 4, 128])
    pscale = din("pscale", [128, 4])
    gq = din("gq", [128, 1])
    gk = din("gk", [128, 1])
    btab = din("btab", [128, 8, 5, 128])
    bmask = din("bmask", [128, 5, 128])
    wr = din("wr", [D, 36])
    br = din("br", [128, 36])
    wg = din("wg", [NEXP * D, 512])
    wu = din("wu", [NEXP * D, 512])
    wd = din("wd", [NEXP * 512, D])
    hv = din("hv", [128, 1])
    nhv = din("nhv", [128, 1])
    invc = din("invc", [128, 4, 16])
    tri = din("tri", [128, 128])
    iot = din("iot", [128, 4])
    trashi = din("trashi", [128, 2 * TRC])
    thr = din("thr", [128, 16])
    wv = din("wv", [128, 32])
    ev = din("ev", [128, 32])
    iot8 = din("iot8", [128, 8])
    if last:
        xo = nc.dram_tensor("xo", [NTL * 128, D], F32, kind="ExternalOutput").ap()
    else:
        xo = dscr("x1_%d" % li, [NTL * 128, D], F32)
    xmid = dscr("xmid", [NTL * 128, D], F32, kind="ExternalOutput" if debug else "Internal")
    if debug:
        dbg_logits = nc.dram_tensor("dbg_logits", [128, NTL, 36], F32, kind="ExternalOutput").ap()
        dbg_w = nc.dram_tensor("dbg_w", [128, 2, NTL], F32, kind="ExternalOutput").ap()
        dbg_slot = nc.dram_tensor("dbg_slot", [128, 2, NTL], I32, kind="ExternalOutput").ap()
    xn2s = dscr("xn2s", [NTL * 128, D], BF16)
    xs = dscr("xs", [cfg.XSR, D], BF16)
    ys = dscr("ys", [cfg.XSR, D], F32)

    if True:
        def T(name, shape, dt=F32):
            if name in memo:
                t, shp = memo[name]
                if list(shp) != list(shape):
                    assert len(shp) == len(shape) and shape[1] <= shp[1] and list(shp[2:]) == list(shape[2:]), (name, shp, shape)
                    return t[:, 0:shape[1]]
                return t
            t = st.enter_context(nc.sbuf_tensor(name, list(shape), dt))
            memo[name] = (t, list(shape))
            return t

        def PS(name, shape, dt=F32):
            if name not in memo:
                memo[name] = st.enter_context(nc.psum_tensor(name, list(shape), dt))
            return memo[name]

        ident = T("ident", [128, 128], BF16)
        identf = T("identf", [128, 128])
        ones_bf = T("ones_bf", [128, 128], BF16)
        blk1 = T("blk1", [128, 128], BF16)
        tri_bf = T("tri_bf", [128, 128], BF16)
        onesf = T("onesf", [1, 128])
        epsc = T("epsc", [128, 1])
        eps64 = T("eps64", [128, 1])
        cact = T("cact", [128, 8], BF16)
        ctf = T("ctf", [128, 8])
        n1t = T("n1t", [128, 8])
        n2t = T("n2t", [128, 8])
        modc = T("modc", [128, 4, 8])
        mul1c = T("mul1c", [128, 8])
        mul2c = T("mul2c", [128, 8])
        add1b = T("add1b", [128, 8], BF16)
        g2bc = T("g2bc", [128, D])
        bzc = T("bzc", [128, 12])
        bzv = T("bzv", [128, 512])
        bzvm = T("bzvm", [128, 512])
        pw_bf = T("pw_bf", [128, 4, 128], BF16)
        psc = T("psc", [128, 4])
        gqk = T("gqk", [128, 1])
        gkt = T("gkt", [128, 1])
        BT = T("BT", [128, 8, 5, 128], BF16)
        wr_f = T("wr_f", [128, 8, 36])
        wr_bf = T("wr_bf", [128, 8, 36], BF16)
        wr_raw = T("wr_raw", [128, 8, 36], BF16)
        biasR = T("biasR", [128, 36])
        hvt = T("hvt", [128, 1])
        nhvt = T("nhvt", [128, 1])
        invct = T("invct", [128, 4, 16])
        iott = T("iott", [128, 4])
        trt = T("trt", [128, 2 * TRC])
        thrt = T("thrt", [128, 16])
        wvt = T("wvt", [128, 32])
        evt = T("evt", [128, 32])
        iot8t = T("iot8t", [128, 8])
        ssr = T("ssr", [128, 8])
        rst = T("rst", [128, 8])
        logits = T("logits", [128, NTL, 36])
        w1g = T("w1g", [128, NTL])
        w2g = T("w2g", [128, NTL])
        slot1 = T("slot1", [128, NTL], I32)
        slot2 = T("slot2", [128, NTL], I32)
        widx = T("widx", [128, NEXP, CT], I32)

        AF_WORDS = 15104
        AB_WORDS = 58432
        arf = T("arf", [128, AF_WORDS])
        arb = T("arb", [128, AB_WORDS], BF16)

        class Carver:
            def __init__(self, t, n):
                self.t, self.n, self.off = t, n, 0

            def take(self, *shape):
                n = int(np.prod(shape))
                a = self.t[:, self.off:self.off + n]
                self.off += n
                assert self.off <= self.n, (self.off, self.n)
                if len(shape) == 2:
                    return a.rearrange("p (a b) -> p a b", a=shape[0])
                if len(shape) == 3:
                    return a.rearrange("p (a b c) -> p a b c", a=shape[0], b=shape[1])
                return a

        pT = PS("pT", [128, 1024], BF16)
        pZ = PS("pZ", [128, 2, 512])
        pC = PS("B3", [128, 512])
        pS = PS("pS", [128, 1536])
        pV = PS("B7", [128, 512])

        p = ctx.p
        A = p.add

        def dma(eng, out, in_, reads, writes, dkey=None):
            return A(eng, lambda e: e.dma_start(out=out, in_=in_), reads=reads, writes=writes, dma=True, dkey=dkey)

        A("pool", lambda e: e.memset(identf[:], 0.0), writes=["identf"])
        A("pool", lambda e: e.affine_select(out=identf[:], in_=identf[:], pattern=[[-1, 128]], compare_op=ALU.not_equal,
                                            fill=1.0, base=0, channel_multiplier=1), reads=["identf"], writes=["identf"])
        A("dve", lambda e: e.tensor_copy(out=ident[:], in_=identf[:]), reads=["identf"], writes=["ident"])
        A("pool", lambda e: e.memset(ones_bf[:], 1.0), writes=["ones_bf"])
        A("pool", lambda e: e.memset(blk1[:], 0.0), writes=["blk1"])
        A("pool", lambda e: e.memset(blk1[0:64, 0:64], 1.0), reads=["blk1"], writes=["blk1"])
        A("pool", lambda e: e.memset(blk1[64:128, 64:128], 1.0), reads=["blk1"], writes=["blk1"])
        A("pool", lambda e: e.memset(onesf[:], 1.0), writes=["onesf"])
        A("pool", lambda e: e.memset(epsc[:], EPS), writes=["epsc"])
        A("pool", lambda e: e.memset(eps64[:], 64 * EPS), writes=["eps64"])
        for (dst, src, k) in ((ctf, cT, "ctf"), (n1t, n1c, "n1t"), (n2t, n2c, "n2t"), (psc, pscale, "psc"), (gqk, gq, "gqk"),
                              (gkt, gk, "gkt"), (hvt, hv, "hvt"), (nhvt, nhv, "nhvt"), (invct, invc, "invct"),
                              (iott, iot, "iott"), (trt, trashi, "trt"), (thrt, thr, "thrt"), (wvt, wv, "wvt"), (evt, ev, "evt"), (iot8t, iot8, "iot8t"), (biasR, br, "biasR"), (identf, tri, "identf")):
            dma("sp", dst[:], src, [], [k])
        A("dve", lambda e: e.tensor_copy(out=tri_bf[:], in_=identf[:]), reads=["identf"], writes=["tri_bf"])
        A("dve", lambda e: e.tensor_mul(out=gqk[:], in0=gqk[:], in1=gkt[:]), reads=["gqk", "gkt"], writes=["gqk"])
        dma("pool", pw_bf[:], pool_w, [], ["pw_bf"])
        A("act", lambda e: e.activation(out=cact[:], in_=ctf[:], func=AF.Silu), reads=["ctf"], writes=["cact"])

        cf = Carver(arf, AF_WORDS)
        cb_ = Carver(arb, AB_WORDS)
        g1bc = cf.take(D)
        modrow = cf.take(6 * D)[0:1, :]
        adab = cf.take(6 * D)[0:1, :]
        stage = [cb_.take(8, 1536) for _ in range(2)]
        zf = cf.take(D)
        zb = cb_.take(D)
        A("pool", lambda e: e.memset(zf[:], 0.0), writes=["zf"])
        A("pool", lambda e: e.memset(zb[:], 0.0), writes=["zb"])
        for r0 in range(2 * cfg.NM * 128, cfg.TRASH, 128):
            dma("sp", xs[r0:r0 + 128, :], zb[:], ["zb"], ["xs_z%d" % r0], dkey="zinit")
        for r0 in range(cfg.TRASH, cfg.TRASH + 2 * NFH * 128, 128):
            dma("sp", ys[r0:r0 + 128, :], zf[:], ["zf"], ["ys_z%d" % r0], dkey="zinit")
        dma("sp", adab, ada_b, [], ["adab"])
        for g in range(4):
            sb = stage[g % 2]
            dma("pool", sb[:], ada_w[:, g * 1536:(g + 1) * 1536].rearrange("(k p) n -> p k n", p=128), [], ["stage%d" % (g % 2)])
            for cbk in range(3):
                col = g * 1536 + cbk * 512
                bank = cbk % 2
                for kc in range(8):
                    A("pe", lambda e, sb=sb, kc=kc, cbk=cbk, bank=bank: e.matmul(
                        pZ[0:1, bank, :], lhsT=cact[:, kc:kc + 1], rhs=sb[:, kc, cbk * 512:(cbk + 1) * 512],
                        start=(kc == 0), stop=(kc == 7)), reads=["cact", "stage%d" % (g % 2)], writes=["pZ%d" % bank])
                A("dve", lambda e, col=col, bank=bank: e.tensor_tensor(out=modrow[:, col:col + 512], in0=pZ[0:1, bank, :],
                                                                       in1=adab[:, col:col + 512], op=ALU.add),
                  reads=["pZ%d" % bank, "adab"], writes=["modrow"])
        for vi, base in enumerate((0, D, 3 * D, 4 * D)):
            for kc in range(8):
                A("pe", lambda e, vi=vi, base=base, kc=kc: e.matmul(
                    pC[:, vi * 8 + kc: vi * 8 + kc + 1], lhsT=modrow[:, base + kc * 128: base + (kc + 1) * 128],
                    rhs=onesf[:, 0:1], start=True, stop=True), reads=["modrow", "onesf"], writes=["B3"])
        A("dve", lambda e: e.tensor_copy(out=modc[:], in_=pC[:, 0:32].rearrange("p (a b) -> p a b", a=4)), reads=["B3"], writes=["modc"])
        A("dve", lambda e: e.scalar_tensor_tensor(out=mul1c[:], in0=modc[:, 1, :], scalar=1.0, in1=n1t[:], op0=ALU.add, op1=ALU.mult),
          reads=["modc", "n1t"], writes=["mul1c"])
        A("dve", lambda e: e.scalar_tensor_tensor(out=mul2c[:], in0=modc[:, 3, :], scalar=1.0, in1=n2t[:], op0=ALU.add, op1=ALU.mult),
          reads=["modc", "n2t"], writes=["mul2c"])
        A("dve", lambda e: e.tensor_copy(out=add1b[:], in_=modc[:, 0, :]), reads=["modc"], writes=["add1b"])
        for (dst, base, k) in ((g1bc, 2 * D, "g1bc"), (g2bc, 5 * D, "g2bc")):
            for hb in range(2):
                A("pe", lambda e, base=base, hb=hb: e.matmul(pZ[:, hb, :], lhsT=onesf[:, :], rhs=modrow[:, base + hb * 512: base + (hb + 1) * 512],
                                                            start=True, stop=True), reads=["modrow", "onesf"], writes=["pZ%d" % hb])
                A("dve", lambda e, dst=dst, hb=hb: e.tensor_copy(out=dst[:, hb * 512:(hb + 1) * 512], in_=pZ[:, hb, :]),
                  reads=["pZ%d" % hb], writes=[k])
        p.barrier()

        cf = Carver(arf, AF_WORDS)
        cb_ = Carver(arb, AB_WORDS)
        w_in_bf = cb_.take(8, 2048)
        w_out_bf = cb_.take(8, D)
        g1bc = cf.take(D)
        add2rep = cb_.take(8, 128)
        wst = [cf.take(2, 2048) for _ in range(2)]
        wraw = cb_.take(8, 2048)
        bzrow = cf.take(2048)[0:1, :]
        bzrow_b = cb_.take(512)[0:1, :]
        for pc in range(4):
            sbf = wst[pc % 2]
            dma("sp", sbf[:], w_in[pc * 256:(pc + 1) * 256, :].rearrange("(k p) n -> p k n", p=128), [], ["wst%d" % (pc % 2)])
            for kk in range(2):
                kc = pc * 2 + kk
                A("dve", lambda e, sbf=sbf, kk=kk, kc=kc: e.tensor_scalar(out=w_in_bf[:, kc, :], in0=sbf[:, kk, :], scalar1=mul1c[:, kc:kc + 1],
                                                                       scalar2=None, op0=ALU.mult),
                  reads=["wst%d" % (pc % 2), "mul1c"], writes=["w_in_bf"])
                A("act", lambda e, sbf=sbf, kk=kk, kc=kc: e.activation(out=wraw[:, kc, :], in_=sbf[:, kk, :], func=AF.Copy),
                  reads=["wst%d" % (pc % 2)], writes=["wraw"])
        for cbk in range(4):
            for kc in range(8):
                A("pe", lambda e, cbk=cbk, kc=kc: e.matmul(pZ[0:1, cbk % 2, :], lhsT=add1b[:, kc:kc + 1], rhs=wraw[:, kc, cbk * 512:(cbk + 1) * 512],
                                                          start=(kc == 0), stop=(kc == 7)), reads=["add1b", "wraw"], writes=["pZ%d" % (cbk % 2)])
            A("dve", lambda e, cbk=cbk: e.tensor_copy(out=bzrow[:, cbk * 512:(cbk + 1) * 512], in_=pZ[0:1, cbk % 2, :]),
              reads=["pZ%d" % (cbk % 2)], writes=["bzrow"])
        for oc in range(12):
            A("pe", lambda e, oc=oc: e.matmul(pC[:, oc:oc + 1], lhsT=bzrow[:, oc * 128:(oc + 1) * 128], rhs=onesf[:, 0:1], start=True, stop=True),
              reads=["bzrow", "onesf"], writes=["B3"])
        A("dve", lambda e: e.tensor_copy(out=bzc[:], in_=pC[:, 0:12]), reads=["B3"], writes=["bzc"])
        A("pe", lambda e: e.matmul(pZ[:, 0, :], lhsT=onesf[:, :], rhs=bzrow[:, 1536:2048], start=True, stop=True),
          reads=["bzrow", "onesf"], writes=["pZ0"])
        A("dve", lambda e: e.tensor_copy(out=bzv[:], in_=pZ[:, 0, :]), reads=["pZ0"], writes=["bzv"])
        A("dve", lambda e: e.tensor_scalar(out=bzvm[:], in0=bzv[:], scalar1=hvt[:, 0:1], scalar2=None, op0=ALU.mult),
          reads=["bzv", "hvt"], writes=["bzvm"])
        wost = [wst[0][:, :, 0:D], wst[1][:, :, 0:D]]
        for pc in range(4):
            sbf = wost[pc % 2]
            dma("sp", sbf[:], w_out[pc * 256:(pc + 1) * 256, :].rearrange("(k p) n -> p k n", p=128), [], ["wst%d" % (pc % 2)])
            for kk in range(2):
                kc = pc * 2 + kk
                A("dve", lambda e, sbf=sbf, kk=kk, kc=kc: e.tensor_tensor(out=w_out_bf[:, kc, :], in0=sbf[:, kk, :], in1=g1bc[:], op=ALU.mult),
                  reads=["wst%d" % (pc % 2), "g1bc"], writes=["w_out_bf"])
        btf = cf.take(5, 128)
        pen = cf.take(5, 128)
        mk = cf.take(5, 128)
        dma("sp", mk[:], bmask, [], ["mk"])
        A("dve", lambda e: e.tensor_scalar(out=pen[:], in0=mk[:], scalar1=3750.0, scalar2=-3750.0, op0=ALU.mult, op1=ALU.add),
          reads=["mk"], writes=["pen"])
        for h in range(8):
            dma("sp", btf[:], btab[:, h, :, :], [], ["btf"])
            A("dve", lambda e: e.tensor_tensor(out=btf[:], in0=btf[:], in1=mk[:], op=ALU.mult), reads=["btf", "mk"], writes=["btf"])
            A("dve", lambda e, h=h: e.scalar_tensor_tensor(out=BT[:, h, :, :], in0=btf[:], scalar=0.125, in1=pen[:], op0=ALU.mult, op1=ALU.add),
              reads=["btf", "pen"], writes=["BT"])
        dma("sp", wr_f[:], wr.rearrange("(k p) n -> p k n", p=128), [], ["wr_f"])
        A("dve", lambda e: e.tensor_copy(out=wr_raw[:], in_=wr_f[:]), reads=["wr_f"], writes=["wr_raw"])
        for kc in range(8):
            A("dve", lambda e, kc=kc: e.tensor_scalar(out=wr_bf[:, kc, :], in0=wr_f[:, kc, :], scalar1=mul2c[:, kc:kc + 1], scalar2=None, op0=ALU.mult),
              reads=["wr_f", "mul2c"], writes=["wr_bf"])
            A("dve", lambda e, kc=kc: e.tensor_copy(out=add2rep[:, kc, :], in_=modc[:, 2, kc:kc + 1].to_broadcast([128, 128])),
              reads=["modc"], writes=["add2rep"])
        for kc in range(8):
            A("pe", lambda e, kc=kc: e.matmul(pC[:, 0:36], lhsT=add2rep[:, kc, :], rhs=wr_raw[:, kc, :], start=(kc == 0), stop=(kc == 7)),
              reads=["add2rep", "wr_raw"], writes=["B3"])
        A("dve", lambda e: e.tensor_tensor(out=biasR[:], in0=pC[:, 0:36], in1=biasR[:], op=ALU.add), reads=["B3", "biasR"], writes=["biasR"])
        p.barrier()

        cf = Carver(arf, AF_WORDS)
        cb_ = Carver(arb, AB_WORDS)
        w_in_bf = cb_.take(8, 2048)
        w_out_bf = cb_.take(8, D)
        xin = [cf.take(D) for _ in range(2)]
        xr = [cf.take(D) for _ in range(2)]
        xmd = [cf.take(D) for _ in range(2)]
        qf = [cf.take(512) for _ in range(3)]
        rq = [cf.take(512) for _ in range(3)]
        uT = [[cf.take(528) for _ in range(4)] for _ in range(2)]
        ptmp = [cf.take(528) for _ in range(2)]
        rden = cf.take(2, 4)
        xn = [cb_.take(D) for _ in range(2)]
        hT_ = cb_.take(8, 512)
        hT = [hT_, hT_]
        kT = cb_.take(4, RT * 128)
        Vr = cb_.take(RT, 8 * 65).rearrange("p r (h d) -> p r h d", h=8)
        qTm = [cb_.take(4, 512) for _ in range(2)]
        sq = [cb_.take(512) for _ in range(3)]
        pTt = cb_.take(4, 512)
        mixT_ = cb_.take(8, 512)
        mixT = [mixT_, mixT_]
        PTb = [cb_.take(2, 640) for _ in range(2)]
        att = [cb_.take(512) for _ in range(2)]
        xn2 = [cb_.take(D) for _ in range(2)]
        xn2T = [cb_.take(8, 128) for _ in range(2)]

        for b in range(2):
            for g in range(4):
                A("pool", lambda e, b=b, g=g: e.memset(uT[b][g][:, 0:16], 0.0), writes=["uT%d%d" % (b, g)])
        A("pool", lambda e: e.memset(qTm[0][64:128, :, :], 0.0), writes=["qT"])
        A("pool", lambda e: e.memset(qTm[1][0:64, :, :], 0.0), writes=["qT"])

        SSOFF = (0, 640)
        PVR = ((pS, 1280), (pV, 0), (pV, 256))
        HG = ((0, 1, 2), (3, 4, 5), (6, 7))

        def norm_and_transpose(src, srckey, sl, dstT, dstTkeys, dstcols, xnbuf, xnkey, store_to=None, scale_eng="dve", defer=False):
            A("act", lambda e: e.activation(out=xnbuf[:], in_=src, func=AF.Square, accum_out=ssr[:, sl:sl + 1]),
              reads=[srckey], writes=[xnkey, "ssr%d" % sl])
            A("act", lambda e: e.activation(out=rst[:, sl:sl + 1], in_=ssr[:, sl:sl + 1], func=AF.Ln, scale=1.0 / D, bias=epsc[:]),
              reads=["ssr%d" % sl, "epsc"], writes=["rst%d" % sl])
            A("act", lambda e: e.activation(out=rst[:, sl:sl + 1], in_=rst[:, sl:sl + 1], func=AF.Exp, scale=-0.5),
              reads=["rst%d" % sl], writes=["rst%d" % sl])
            if scale_eng == "dve":
                A("dve", lambda e: e.tensor_scalar(out=xnbuf[:], in0=src, scalar1=rst[:, sl:sl + 1], scalar2=None, op0=ALU.mult),
                  reads=[srckey, "rst%d" % sl], writes=[xnkey])
            else:
                A("act", lambda e: e.activation(out=xnbuf[:], in_=src, func=AF.Copy, scale=rst[:, sl:sl + 1]),
                  reads=[srckey, "rst%d" % sl], writes=[xnkey])
            if store_to is not None:
                dma("pool", store_to, xnbuf[:], [xnkey], ["xn2s"], dkey="st_" + xnkey)

            def part_b():
                for kc in range(8):
                    A("pe", lambda e, kc=kc: e.transpose(out=pT[:, kc * 128:(kc + 1) * 128], in_=xnbuf[:, kc * 128:(kc + 1) * 128], identity=ident[:]),
                      reads=[xnkey, "ident"], writes=["pT"])
                A("dve", lambda e: e.tensor_copy(out=dstT[:, :, dstcols], in_=pT[:].rearrange("p (a b) -> p a b", a=8)),
                  reads=["pT"], writes=dstTkeys)
            if defer:
                return part_b
            part_b()

        ZB = [(pZ[:, 0, :], "pZ0"), (pZ[:, 1, :], "pZ1"), (pS[:, 0:512], "B4"), (pS[:, 512:1024], "B5"), (pS[:, 1024:1536], "B6")]
        SB = [(pC, "B3"), (pV, "B7")]
        zcnt = {"z": 0, "s": 0}

        def in_chunk(s, oc, ub, slot0, halo_st, full_st):
            zps, zk = ZB[zcnt["z"] % 5]
            zcnt["z"] += 1
            kslots = ["kT%d" % (slot0 + i) for i in range(4)]
            for kc in range(8):
                A("pe", lambda e, kc=kc: e.matmul(zps, lhsT=w_in_bf[:, kc, oc * 128:(oc + 1) * 128], rhs=hT_[:, kc, :],
                                                  start=(kc == 0), stop=(kc == 7)), reads=["w_in_bf", "hT"], writes=[zk])
            if oc < 4:
                g = oc
                if halo_st:
                    A("dve", lambda e: e.tensor_scalar(out=uT[ub][g][:, 16:528], in0=zps, scalar1=bzc[:, oc:oc + 1],
                                                       scalar2=hvt[:, 0:1], op0=ALU.add, op1=ALU.mult),
                      reads=[zk, "bzc", "hvt"], writes=["uT%d%d" % (ub, g)])
                else:
                    A("dve", lambda e: e.tensor_scalar(out=uT[ub][g][:, 16:528], in0=zps, scalar1=bzc[:, oc:oc + 1],
                                                       scalar2=None, op0=ALU.add),
                      reads=[zk, "bzc"], writes=["uT%d%d" % (ub, g)])
                return None
            isq = oc < 8
            c = (oc - 4) % 4
            tb = oc % 3
            A("dve", lambda e: e.tensor_scalar(out=qf[tb][:], in0=zps, scalar1=bzc[:, oc:oc + 1], scalar2=None, op0=ALU.add),
              reads=[zk, "bzc"], writes=["qf%d" % tb])
            A("act", lambda e: e.activation(out=sq[tb][:], in_=qf[tb][:], func=AF.Square),
              reads=["qf%d" % tb], writes=["sq%d" % tb])
            sps, sk = SB[zcnt["s"] % 2]
            zcnt["s"] += 1

            def part2():
                A("pe", lambda e: e.matmul(sps[:], lhsT=blk1[:], rhs=sq[tb][:], start=True, stop=True), reads=["blk1", "sq%d" % tb], writes=[sk])
                A("act", lambda e: e.activation(out=rq[tb][:], in_=sps[:], func=AF.Ln, bias=eps64[:]), reads=[sk, "eps64"], writes=["rq%d" % tb])
                A("act", lambda e: e.activation(out=rq[tb][:], in_=rq[tb][:], func=AF.Exp, scale=-0.5), reads=["rq%d" % tb], writes=["rq%d" % tb])
                if isq:
                    A("dve", lambda e: e.tensor_tensor(out=qTm[0][0:64, c, :], in0=qf[tb][0:64, :], in1=rq[tb][0:64, :], op=ALU.mult),
                      reads=["qf%d" % tb, "rq%d" % tb], writes=["qT"])
                    A("dve", lambda e: e.tensor_tensor(out=qTm[1][64:128, c, :], in0=qf[tb][64:128, :], in1=rq[tb][64:128, :], op=ALU.mult),
                      reads=["qf%d" % tb, "rq%d" % tb], writes=["qT"])
                else:
                    A("dve", lambda e: e.scalar_tensor_tensor(out=kT[:, c, slot0 * 128:(slot0 + 4) * 128], in0=qf[tb][:], scalar=gqk[:, 0:1],
                                                              in1=rq[tb][:], op0=ALU.mult, op1=ALU.mult),
                      reads=["qf%d" % tb, "rq%d" % tb, "gqk"], writes=kslots)
            return part2

        def v_tile(s, i, halo_st):
            te = 4 * s + i
            sl = te % RT
            zps, zk = ZB[zcnt["z"] % 5]
            zcnt["z"] += 1
            for kc in range(8):
                A("pe", lambda e, kc=kc: e.matmul(zps, lhsT=hT_[:, kc, i * 128:(i + 1) * 128], rhs=w_in_bf[:, kc, 1536:2048],
                                                  start=(kc == 0), stop=(kc == 7)), reads=["w_in_bf", "hT"], writes=[zk])
            zv = zps.rearrange("p (h d) -> p h d", h=8)
            if halo_st:
                A("dve", lambda e: e.scalar_tensor_tensor(out=Vr[:, sl, :, 0:64], in0=zv, scalar=hvt[:, 0:1],
                                                          in1=bzvm[:].rearrange("p (h d) -> p h d", h=8), op0=ALU.mult, op1=ALU.add),
                  reads=[zk, "hvt", "bzvm"], writes=["V%d" % sl])
                A("pool", lambda e: e.tensor_copy(out=Vr[:, sl, :, 64:65], in_=hvt[:, 0:1].unsqueeze(1).to_broadcast([128, 8, 1])),
                  reads=["hvt"], writes=["V%d" % sl])
            else:
                A("dve", lambda e: e.tensor_tensor(out=Vr[:, sl, :, 0:64], in0=zv, in1=bzv[:].rearrange("p (h d) -> p h d", h=8), op=ALU.add),
                  reads=[zk, "bzv"], writes=["V%d" % sl])
                A("pool", lambda e: e.memset(Vr[:, sl, :, 64:65], 1.0), writes=["V%d" % sl])

        def pool_group(g, ub, first_main):
            U = uT[ub][g]
            uk = "uT%d%d" % (ub, g)
            cur, curk = U, uk
            sh = 1
            for stp in range(g + 1):
                dstb = ptmp[stp % 2]
                dk = "ptmp%d" % (stp % 2)
                lo = 2 * sh - 1
                A("pool", lambda e, cur=cur, dstb=dstb, lo=lo, sh=sh: e.tensor_tensor(out=dstb[:, lo:528], in0=cur[:, lo:528], in1=cur[:, lo - sh:528 - sh], op=ALU.add),
                  reads=[curk], writes=[dk])
                cur, curk = dstb, dk
                sh *= 2
            w = 2 ** (g + 1)
            fin = cur
            A("dve", lambda e: e.scalar_tensor_tensor(out=pTt[:, g, :], in0=fin[:, 16:528], scalar=1.0 / w, in1=U[:, 16:528],
                                                      op0=ALU.mult, op1=ALU.subtract),
              reads=[curk, uk], writes=["pTt%d" % g])
            if first_main:
                A("pool", lambda e: e.tensor_tensor(out=fin[:, 0:16], in0=fin[:, 16:32], in1=invct[:, g, :], op=ALU.mult),
                  reads=[curk, "invct"], writes=[curk])
                A("pool", lambda e: e.tensor_tensor(out=pTt[:, g, 0:16], in0=fin[:, 0:16], in1=U[:, 16:32], op=ALU.subtract),
                  reads=[curk, uk], writes=["pTt%d" % g])
            def part_b():
                sps, sk = SB[g % 2]
                A("pe", lambda e: e.matmul(sps[:], lhsT=pw_bf[:, g, :], rhs=pTt[:, g, :], start=True, stop=True), reads=["pw_bf", "pTt%d" % g], writes=[sk])
                A("act", lambda e: e.activation(out=mixT_[:, g, :], in_=sps[:], func=AF.Copy, scale=psc[:, g:g + 1]),
                  reads=[sk, "psc"], writes=["mixT"])
            return part_b

        def attn_pair(te, i, pr, ab):
            c = pr
            pb2 = pr % 2
            PTp = PTb[pb2]
            ptk = "PT%d" % pb2
            for hh in range(2):
                pb = 64 * hh
                h = 2 * pr + hh
                for t in range(4):
                    ksl = (te - 4 + t) % RT
                    A("pe", lambda e, t=t, ksl=ksl, pb=pb, hh=hh: e.matmul(
                        pS[:, hh * 512 + t * 128: hh * 512 + (t + 1) * 128], lhsT=kT[:, c, ksl * 128:(ksl + 1) * 128],
                        rhs=qTm[hh][:, c, i * 128:(i + 1) * 128], start=True, stop=False),
                      reads=["kT%d" % ksl, "qT"], writes=["B%d" % (4 + hh)])
                    A("pe", lambda e, t=t, h=h, hh=hh: e.matmul(pS[:, hh * 512 + t * 128: hh * 512 + (t + 1) * 128], lhsT=BT[:, h, t, :], rhs=ident[:],
                                                                start=False, stop=True), reads=["BT", "ident"], writes=["B%d" % (4 + hh)])
            ksl4 = te % RT
            for hh in range(2):
                pb = 64 * hh
                h = 2 * pr + hh
                A("pe", lambda e, pb=pb, hh=hh: e.matmul(
                    pS[:, 1024 + hh * 128: 1024 + (hh + 1) * 128], lhsT=kT[:, c, ksl4 * 128:(ksl4 + 1) * 128],
                    rhs=qTm[hh][:, c, i * 128:(i + 1) * 128], start=True, stop=False),
                  reads=["kT%d" % ksl4, "qT"], writes=["B6"])
                A("pe", lambda e, h=h, hh=hh: e.matmul(pS[:, 1024 + hh * 128: 1024 + (hh + 1) * 128], lhsT=BT[:, h, 4, :], rhs=ident[:],
                                                       start=False, stop=True), reads=["BT", "ident"], writes=["B6"])
            for hh in range(2):
                A("act", lambda e, hh=hh: e.activation(out=PTp[:, hh, 0:512], in_=pS[:, hh * 512:(hh + 1) * 512], func=AF.Exp, scale=8.0),
                  reads=["B%d" % (4 + hh)], writes=[ptk])
            A("act", lambda e: e.activation(out=PTp[:, :, 512:640], in_=pS[:, 1024:1280].rearrange("p (a b) -> p a b", a=2), func=AF.Exp, scale=8.0),
              reads=["B6"], writes=[ptk])
            for hh in range(2):
                h = 2 * pr + hh
                pvt, pvk = (pV, "B7") if h < 4 else (pC, "B3")
                co = (h % 4) * 65
                for t in range(5):
                    ksl = (te - 4 + t) % RT
                    A("pe", lambda e, t=t, ksl=ksl, hh=hh, h=h, pvt=pvt, co=co: e.matmul(
                        pvt[:, co: co + 65], lhsT=PTp[:, hh, t * 128:(t + 1) * 128], rhs=Vr[:, ksl, h, :],
                        start=(t == 0), stop=(t == 4)), reads=[ptk, "V%d" % ksl], writes=[pvk])

        def attn_norm(hgi, ab):
            pvt, pvk = (pV, "B7") if hgi == 0 else (pC, "B3")
            pvv = pvt[:, 0:260].rearrange("p (h d) -> p h d", h=4)
            A("dve", lambda e: e.tensor_scalar(out=rden[:, hgi, :].unsqueeze(2), in0=pvv[:, :, 64:65], scalar1=1e-30, scalar2=None, op0=ALU.add),
              reads=[pvk], writes=["rden%d" % hgi])
            A("dve", lambda e: e.reciprocal(out=rden[:, hgi, :], in_=rden[:, hgi, :]),
              reads=["rden%d" % hgi], writes=["rden%d" % hgi])
            A("dve", lambda e: e.tensor_tensor(
                out=att[ab][:, hgi * 256:(hgi + 1) * 256].rearrange("p (h d) -> p h d", h=4), in0=pvv[:, :, 0:64],
                in1=rden[:, hgi, :].unsqueeze(2).to_broadcast([128, 4, 64]), op=ALU.mult),
              reads=[pvk, "rden%d" % hgi], writes=["att%d" % ab])

        def attention_tile(s, i):
            te = 4 * s + i
            ab = te % 2
            for pr in range(4):
                attn_pair(te, i, pr, ab)
                if pr % 2 == 1:
                    attn_norm(pr // 2, ab)

        def post_attention(s, i):
            te = 4 * s + i
            tl = te - NKV
            ab = te % 2
            for c in range(4):
                A("pe", lambda e, c=c: e.transpose(out=pT[:, c * 128:(c + 1) * 128], in_=att[ab][:, c * 128:(c + 1) * 128], identity=ident[:]),
                  reads=["att%d" % ab, "ident"], writes=["pT"])
            A("dve", lambda e: e.tensor_copy(out=mixT_[:, 4:8, i * 128:(i + 1) * 128], in_=pT[:, 0:512].rearrange("p (a b) -> p a b", a=4)),
              reads=["pT"], writes=["mixT"])
            for cbk in range(2):
                for kc in range(8):
                    A("pe", lambda e, cbk=cbk, kc=kc: e.matmul(pZ[:, cbk, :], lhsT=mixT_[:, kc, i * 128:(i + 1) * 128], rhs=w_out_bf[:, kc, cbk * 512:(cbk + 1) * 512],
                                                               start=(kc == 0), stop=(kc == 7)), reads=["mixT", "w_out_bf"], writes=["pZ%d" % cbk])
            rb = te % 2
            dma("sp", xr[rb][:], xe[te * 128:(te + 1) * 128, :], [], ["xr%d" % rb])
            A("dve", lambda e: e.tensor_tensor(out=xmd[rb][:], in0=pZ[:].rearrange("p a b -> p (a b)"), in1=xr[rb][:], op=ALU.add),
              reads=["pZ0", "pZ1", "xr%d" % rb], writes=["xmd%d" % rb])
            dma("pool", xmid[tl * 128:(tl + 1) * 128, :], xmd[rb][:], ["xmd%d" % rb], ["xmid"], dkey="st_xmd%d" % rb)

            def norm2_a():
                pb_ = norm_and_transpose(xmd[rb][:], "xmd%d" % rb, te % 8, xn2T[rb], ["xn2T%d" % rb], slice(0, 128), xn2[rb], "xn2%d" % rb,
                                         store_to=xn2s[tl * 128:(tl + 1) * 128, :], scale_eng="act", defer=True)

                def part_b():
                    pb_()
                    for kc in range(8):
                        A("pe", lambda e, kc=kc: e.matmul(pC[:, 0:36], lhsT=xn2T[rb][:, kc, :], rhs=wr_bf[:, kc, :], start=(kc == 0), stop=(kc == 7)),
                          reads=["xn2T%d" % rb, "wr_bf"], writes=["B3"])
                    A("dve", lambda e: e.tensor_tensor(out=logits[:, tl, :], in0=pC[:, 0:36], in1=biasR[:], op=ALU.add),
                      reads=["B3", "biasR"], writes=["logits"])
                return part_b
            return norm2_a

        def tail_copy(ub, g):
            A("pool", lambda e: e.tensor_copy(out=uT[ub][g][:, 0:16], in_=uT[1 - ub][g][:, 512:528]),
              reads=["uT%d%d" % (1 - ub, g)], writes=["uT%d%d" % (ub, g)])

        def norm_tile(s, i, defer=False):
            te = 4 * s + i
            xi = te % 2
            dma("sp", xin[xi][:], xe[te * 128:(te + 1) * 128, :], [], ["xin%d" % xi])
            return norm_and_transpose(xin[xi][:], "xin%d" % xi, te % 8, hT_, ["hT"], slice(i * 128, (i + 1) * 128),
                                      xn[te % 2], "xn%d" % (te % 2), defer=defer)

        q_norm2 = []
        q_b = []

        def do_st(s):
            halo_st = (4 * s) < NKV + NFH
            full_st = (4 * s) >= NKV
            first_main = (4 * s) == NKV + NFH
            if s == 0:
                for i in range(4):
                    norm_tile(0, i)
            ub = s % 2
            if s > 0:
                for g in range(4):
                    tail_copy(ub, g)
            slot0 = (4 * s) % RT
            pend2 = []
            pool_b = []
            for oc in range(12):
                if 4 <= oc < 8 and not full_st:
                    continue
                p2 = in_chunk(s, oc, ub, slot0, halo_st, full_st)
                if oc == 3 and full_st:
                    for g in range(4):
                        pool_b.append(pool_group(g, ub, first_main))
                if len(pend2) > 1:
                    pend2.pop(0)()
                if p2 is not None:
                    pend2.append(p2)
            v_tile(s, 0, halo_st)
            if pend2:
                pend2.pop(0)()
            v_tile(s, 1, halo_st)
            if pend2:
                pend2.pop(0)()
            for i in range(2, 4):
                v_tile(s, i, halo_st)
            for i in range(4):
                nb = norm_tile(s + 1, i, defer=True) if s + 1 < NST else None
                if full_st:
                    if i == 0:
                        for pb2 in pool_b:
                            pb2()
                    attention_tile(s, i)
                    if q_norm2:
                        q_b.append(q_norm2.pop(0)())
                    if len(q_b) > 1:
                        q_b.pop(0)()
                if nb is not None:
                    nb()
                if full_st:
                    q_norm2.append(post_attention(s, i))
            if s == NST - 1:
                while q_norm2:
                    q_b.append(q_norm2.pop(0)())
                while q_b:
                    q_b.pop(0)()

        for s in range(NST):
            do_st(s)
        p.barrier()

        cf = Carver(arf, AF_WORDS)
        cb_ = Carver(arb, AB_WORDS)
        NL = NTL
        R1 = cf.take(NL, 32)
        R2 = cf.take(NL, 32)
        R3 = cf.take(NL, 32)
        R4 = cf.take(NL, 32)
        sm = [cf.take(NL) for _ in range(6)]
        cntb = [cf.take(32) for _ in range(2)]
        startb = cf.take(32)
        widf = cf.take(NEXP, CT)
        ybuf = [cf.take(D) for _ in range(2)]
        sgb = [cf.take(CAP) for _ in range(2)]
        xmb = [cf.take(D) for _ in range(2)]
        y1b = [cf.take(D) for _ in range(2)]
        y2b_ = cf.take(D)
        y2b = [y2b_, y2b_]
        Abf = cb_.take(NL, 32)
        xtl = [cb_.take(D) for _ in range(4)]
        xw = [cb_.take(CT, D) for _ in range(2)]
        xsT = cb_.take(8, CAP)
        actT = cb_.take(4, CAP)
        Wg = [cb_.take(8, 512) for _ in range(2)]
        Wu = [cb_.take(8, 512) for _ in range(2)]
        Wd = [cb_.take(4, D) for _ in range(2)]

        WGK = [["Wg%d_%d" % (b, kc) for kc in range(8)] for b in range(2)]
        WUK = [["Wu%d_%d" % (b, kc) for kc in range(8)] for b in range(2)]
        WDK = [["Wd%d_%d" % (b, jc) for jc in range(4)] for b in range(2)]

        def load_w(e_):
            b = e_ % 2
            dma("pool", Wg[b][:], wg[e_ * D:(e_ + 1) * D, :].rearrange("(k p) n -> p k n", p=128), [], WGK[b], dkey="Wg%d" % b)
            dma("pool", Wu[b][:], wu[e_ * D:(e_ + 1) * D, :].rearrange("(k p) n -> p k n", p=128), [], WUK[b], dkey="Wu%d" % b)
            dma("pool", Wd[b][:], wd[e_ * 512:(e_ + 1) * 512, :].rearrange("(k p) n -> p k n", p=128), [], WDK[b], dkey="Wd%d" % b)

        load_w(0)
        load_w(1)

        gl = logits[:, :, 0:4]
        el = logits[:, :, 4:36]
        V = lambda e: e
        gmax, gsum, m1, m2, dd, ee = sm
        gone = R1[:, :, 0:4]
        A("dve", lambda e: e.reduce_max(out=gmax[:], in_=gl, axis=AX.X), reads=["logits"], writes=["gmax"])
        A("dve", lambda e: e.tensor_tensor(out=gone, in0=gl, in1=gmax[:].unsqueeze(2).to_broadcast([128, NL, 4]), op=ALU.is_equal),
          reads=["logits", "gmax"], writes=["R1"])
        gex = R2[:, :, 0:4]
        A("dve", lambda e: e.tensor_tensor(out=gex, in0=gl, in1=gmax[:].unsqueeze(2).to_broadcast([128, NL, 4]), op=ALU.subtract),
          reads=["logits", "gmax"], writes=["R2"])
        A("act", lambda e: e.activation(out=gex, in_=gex, func=AF.Exp), reads=["R2"], writes=["R2"])
        A("dve", lambda e: e.reduce_sum(out=gsum[:], in_=gex, axis=AX.X), reads=["R2"], writes=["gsum"])
        A("dve", lambda e: e.reciprocal(out=gsum[:], in_=gsum[:]), reads=["gsum"], writes=["gsum"])
        BIG = 1.0e4
        A("dve", lambda e: e.tensor_scalar(out=gone, in0=gone, scalar1=BIG, scalar2=-BIG, op0=ALU.mult, op1=ALU.add), reads=["R1"], writes=["R1"])
        em = R3
        A("dve", lambda e: e.tensor_tensor(out=em[:].rearrange("p n (g j) -> p n g j", g=4), in0=el.rearrange("p n (g j) -> p n g j", g=4),
                                           in1=gone.unsqueeze(3).to_broadcast([128, NL, 4, 8]), op=ALU.add), reads=["logits", "R1"], writes=["R3"])
        A("dve", lambda e: e.reduce_max(out=m1[:], in_=em[:], axis=AX.X), reads=["R3"], writes=["m1"])
        oh1 = R1
        A("dve", lambda e: e.tensor_tensor(out=oh1[:], in0=em[:], in1=m1[:].unsqueeze(2).to_broadcast([128, NL, 32]), op=ALU.is_equal),
          reads=["R3", "m1"], writes=["R1"])
        em2 = R2
        A("dve", lambda e: e.scalar_tensor_tensor(out=em2[:], in0=oh1[:], scalar=-BIG, in1=em[:], op0=ALU.mult, op1=ALU.add),
          reads=["R1", "R3"], writes=["R2"])
        A("dve", lambda e: e.reduce_max(out=m2[:], in_=em2[:], axis=AX.X), reads=["R2"], writes=["m2"])
        oh2 = R3
        A("dve", lambda e: e.tensor_tensor(out=oh2[:], in0=em2[:], in1=m2[:].unsqueeze(2).to_broadcast([128, NL, 32]), op=ALU.is_equal),
          reads=["R2", "m2"], writes=["R3"])
        A("dve", lambda e: e.tensor_tensor(out=dd[:], in0=m2[:], in1=m1[:], op=ALU.subtract), reads=["m1", "m2"], writes=["dd"])
        A("act", lambda e: e.activation(out=ee[:], in_=dd[:], func=AF.Exp), reads=["dd"], writes=["ee"])
        A("dve", lambda e: e.tensor_scalar(out=dd[:], in0=ee[:], scalar1=1.0, scalar2=None, op0=ALU.add), reads=["ee"], writes=["dd"])
        A("dve", lambda e: e.reciprocal(out=dd[:], in_=dd[:]), reads=["dd"], writes=["dd"])
        A("dve", lambda e: e.tensor_tensor(out=ee[:], in0=ee[:], in1=dd[:], op=ALU.mult), reads=["ee", "dd"], writes=["ee"])
        A("dve", lambda e: e.tensor_tensor(out=w1g[:], in0=dd[:], in1=gsum[:], op=ALU.mult), reads=["dd", "gsum"], writes=["w1g"])
        A("dve", lambda e: e.tensor_tensor(out=w2g[:], in0=ee[:], in1=gsum[:], op=ALU.mult), reads=["ee", "gsum"], writes=["w2g"])
        Asum = R2
        A("dve", lambda e: e.tensor_tensor(out=Asum[:], in0=oh1[:], in1=oh2[:], op=ALU.add), reads=["R1", "R3"], writes=["R2"])
        if NFH > 0:
            A("dve", lambda e: e.tensor_scalar(out=Asum[:, 0:NFH, :], in0=Asum[:, 0:NFH, :], scalar1=hvt[:, 0:1], scalar2=None, op0=ALU.mult),
              reads=["R2", "hvt"], writes=["R2"])
        A("dve", lambda e: e.tensor_copy(out=Abf[:], in_=Asum[:]), reads=["R2"], writes=["Abf"])
        Af = Abf[:].rearrange("p n e -> p (n e)")
        ncol = NL * 32
        banks = [(pZ[:, 0, :], "pZ0"), (pZ[:, 1, :], "pZ1"), (pC[:], "B3"), (pV[:], "B7")]
        assert ncol <= 1536
        Rk = R4[:].rearrange("p n e -> p (n e)")
        Tt = R2[:].rearrange("p n e -> p (n e)")
        for (lhs, lk, dst, dk) in ((tri_bf, "tri_bf", Rk, "R4"), (ones_bf, "ones_bf", Tt, "R2")):
            for c0 in range(0, ncol, 512):
                cw = min(512, ncol - c0)
                A("pe", lambda e, lhs=lhs, c0=c0, cw=cw: e.matmul(pS[:, c0:c0 + cw], lhsT=lhs[:], rhs=Af[:, c0:c0 + cw], start=True, stop=True),
                  reads=[lk, "Abf"], writes=["B4", "B5", "B6"])
            A("dve", lambda e, dst=dst: e.tensor_copy(out=dst, in_=pS[:, 0:ncol]), reads=["B4", "B5", "B6"], writes=[dk])
        A("dve", lambda e: e.memset(cntb[0][:], 0.0), writes=["cnt"])
        for n in range(NL):
            if n > 0:
                A("dve", lambda e, n=n: e.tensor_tensor(out=R4[:, n, :], in0=R4[:, n, :], in1=cntb[0][:], op=ALU.add), reads=["R4", "cnt"], writes=["R4"])
            A("dve", lambda e, n=n: e.tensor_tensor(out=cntb[0][:], in0=cntb[0][:], in1=R2[:, n, :], op=ALU.add), reads=["R2", "cnt"], writes=["cnt"])
        A("dve", lambda e: e.memset(startb[:], 0.0), writes=["startb"])
        for j in range(1, 32):
            A("dve", lambda e, j=j: e.tensor_tensor(out=startb[:, j:j + 1], in0=startb[:, j - 1:j], in1=cntb[0][:, j - 1:j], op=ALU.add),
              reads=["startb", "cnt"], writes=["startb"])
        A("dve", lambda e: e.tensor_tensor(out=R4[:], in0=R4[:], in1=startb[:].unsqueeze(1).to_broadcast([128, NL, 32]), op=ALU.add),
          reads=["R4", "startb"], writes=["R4"])
        for ki, (oh, ohk, sl_i, slk, tmpk) in enumerate(((oh1, "R1", slot1, "slot1", "gmax"), (oh2, "R3", slot2, "slot2", "m1"))):
            tmp = gmax if tmpk == "gmax" else m1
            A("dve", lambda e, oh=oh: e.tensor_tensor(out=oh[:], in0=oh[:], in1=R4[:], op=ALU.mult), reads=[ohk, "R4"], writes=[ohk])
            A("dve", lambda e, oh=oh, tmp=tmp: e.reduce_sum(out=tmp[:], in_=oh[:], axis=AX.X), reads=[ohk], writes=[tmpk])
            if NFH > 0:
                A("dve", lambda e, tmp=tmp: e.tensor_scalar(out=tmp[:, 0:NFH], in0=tmp[:, 0:NFH], scalar1=hvt[:, 0:1], scalar2=None, op0=ALU.mult),
                  reads=[tmpk, "hvt"], writes=[tmpk])
                A("dve", lambda e, tmp=tmp, ki=ki: e.scalar_tensor_tensor(out=tmp[:, 0:NFH], in0=trt[:, ki * TRC: ki * TRC + NFH], scalar=nhvt[:, 0:1], in1=tmp[:, 0:NFH],
                                                                 op0=ALU.mult, op1=ALU.add), reads=[tmpk, "trt", "nhvt"], writes=[tmpk])
            A("dve", lambda e, tmp=tmp, sl_i=sl_i: e.tensor_copy(out=sl_i[:], in_=tmp[:]), reads=[tmpk], writes=[slk])
        A("dve", lambda e: e.tensor_tensor(out=widf[:], in0=startb[:].unsqueeze(2).to_broadcast([128, NEXP, CT]),
                                           in1=iott[:, 0:CT].unsqueeze(1).to_broadcast([128, NEXP, CT]), op=ALU.add),
          reads=["startb", "iott"], writes=["widf"])
        A("dve", lambda e: e.tensor_copy(out=widx[:], in_=widf[:]), reads=["widf"], writes=["widx"])

        if debug:
            dma("sp", dbg_logits, logits[:], ["logits"], ["dbg_logits"])
            dma("sp", dbg_w[:, 0, :], w1g[:], ["w1g"], ["dbg_w1"])
            dma("sp", dbg_w[:, 1, :], w2g[:], ["w2g"], ["dbg_w2"])
            dma("sp", dbg_slot[:, 0, :], slot1[:], ["slot1"], ["dbg_s1"])
            dma("sp", dbg_slot[:, 1, :], slot2[:], ["slot2"], ["dbg_s2"])
        xskeys = []
        for tl in range(NTL):
            b = tl % 4
            dma("sp", xtl[b][:], xn2s[tl * 128:(tl + 1) * 128, :], ["xn2s"], ["xtl%d" % b])
            for k_, (sl_i, slk) in enumerate(((slot1, "slot1"), (slot2, "slot2"))):
                key = "xs_%d_%d" % (tl, k_)
                xskeys.append(key)
                A("pool", lambda e, sl_i=sl_i, tl=tl, b=b: e.indirect_dma_start(
                    out=xs[:, :], out_offset=bass.IndirectOffsetOnAxis(ap=sl_i[:, tl:tl + 1], axis=0), in_=xtl[b][:], in_offset=None),
                  reads=["xtl%d" % b, slk], writes=[key], dma=True, dkey="sc_xtl%d" % b)

        NOW, NTHR = cfg.NOW, cfg.NTHR
        BIGI = 1.0e6
        cnt_ = cntb[0]
        assert NOW <= NL and NTHR <= NL
        gtm = R2[:, 0:NTHR, :].rearrange("p t e -> p (t e)").rearrange("p (e t) -> p e t", e=32)
        nov = cf.take(32)
        cum = cf.take(32)
        indw = R1[:, 0:NOW, :]
        tmpw = R3[:, 0:NOW, :]
        limv = cf.take(32)
        jbase = cf.take(32)
        wsc = [cf.take(NOW) for _ in range(5)]
        gidf = cf.take(NOW, CT)
        yidf = cf.take(NOW, CT)
        mskf = cf.take(NOW, CT)
        wgidf = cf.take(NOW, 8)
        wdidf = cf.take(NOW, 4)
        gidx = T("gidx", [128, NOW, CT], I32)
        yidx = T("yidx", [128, NOW, CT], I32)
        wgidx = T("wgidx", [128, NOW, 8], I32)
        wdidx = T("wdidx", [128, NOW, 4], I32)
        DV = lambda fn, r, w: A("dve", fn, reads=r, writes=w)
        DV(lambda e: e.tensor_tensor(out=gtm[:], in0=cnt_[:].unsqueeze(2).to_broadcast([128, 32, NTHR]),
                                     in1=thrt[:, 0:NTHR].unsqueeze(1).to_broadcast([128, 32, NTHR]), op=ALU.is_gt), ["cnt", "thrt"], ["R2"])
        DV(lambda e: e.reduce_sum(out=nov[:], in_=gtm[:], axis=AX.X), ["R2"], ["nov"])
        DV(lambda e: e.memset(cum[:], 0.0), [], ["cum"])
        for j in range(1, 32):
            DV(lambda e, j=j: e.tensor_tensor(out=cum[:, j:j + 1], in0=cum[:, j - 1:j], in1=nov[:, j - 1:j], op=ALU.add), ["cum", "nov"], ["cum"])
        wvb = wvt[:, 0:NOW].unsqueeze(2).to_broadcast([128, NOW, 32])
        DV(lambda e: e.tensor_tensor(out=indw[:], in0=cum[:].unsqueeze(1).to_broadcast([128, NOW, 32]), in1=wvb, op=ALU.is_le), ["cum", "wvt"], ["R1"])
        DV(lambda e: e.tensor_tensor(out=limv[:], in0=cum[:], in1=nov[:], op=ALU.add), ["cum", "nov"], ["limv"])
        DV(lambda e: e.tensor_tensor(out=tmpw[:], in0=limv[:].unsqueeze(1).to_broadcast([128, NOW, 32]), in1=wvb, op=ALU.is_gt), ["limv", "wvt"], ["R3"])
        DV(lambda e: e.tensor_tensor(out=indw[:], in0=indw[:], in1=tmpw[:], op=ALU.mult), ["R1", "R3"], ["R1"])
        vld, ew, ow, lw, tw = wsc
        DV(lambda e: e.reduce_sum(out=vld[:], in_=indw[:], axis=AX.X), ["R1"], ["vld"])
        DV(lambda e: e.tensor_tensor(out=tmpw[:], in0=indw[:], in1=evt[:].unsqueeze(1).to_broadcast([128, NOW, 32]), op=ALU.mult), ["R1", "evt"], ["R3"])
        DV(lambda e: e.reduce_sum(out=ew[:], in_=tmpw[:], axis=AX.X), ["R3"], ["ew"])
        DV(lambda e: e.tensor_scalar(out=jbase[:], in0=cum[:], scalar1=-float(CAP), scalar2=float(CAP), op0=ALU.mult, op1=ALU.add), ["cum"], ["jbase"])
        DV(lambda e: e.tensor_tensor(out=jbase[:], in0=jbase[:], in1=startb[:], op=ALU.add), ["jbase", "startb"], ["jbase"])
        DV(lambda e: e.tensor_tensor(out=tmpw[:], in0=indw[:], in1=jbase[:].unsqueeze(1).to_broadcast([128, NOW, 32]), op=ALU.mult), ["R1", "jbase"], ["R3"])
        DV(lambda e: e.reduce_sum(out=ow[:], in_=tmpw[:], axis=AX.X), ["R3"], ["ow"])
        DV(lambda e: e.scalar_tensor_tensor(out=ow[:], in0=wvt[:, 0:NOW], scalar=float(CAP), in1=ow[:], op0=ALU.mult, op1=ALU.add), ["ow", "wvt"], ["ow"])
        DV(lambda e: e.tensor_tensor(out=ow[:], in0=ow[:], in1=vld[:], op=ALU.mult), ["ow", "vld"], ["ow"])
        DV(lambda e: e.tensor_tensor(out=limv[:], in0=startb[:], in1=cnt_[:], op=ALU.add), ["startb", "cnt"], ["limv"])
        DV(lambda e: e.tensor_tensor(out=tmpw[:], in0=indw[:], in1=limv[:].unsqueeze(1).to_broadcast([128, NOW, 32]), op=ALU.mult), ["R1", "limv"], ["R3"])
        DV(lambda e: e.reduce_sum(out=lw[:], in_=tmpw[:], axis=AX.X), ["R3"], ["lw"])
        DV(lambda e: e.tensor_scalar(out=tw[:], in0=vld[:], scalar1=-BIGI, scalar2=BIGI, op0=ALU.mult, op1=ALU.add), ["vld"], ["tw"])
        DV(lambda e: e.tensor_tensor(out=gidf[:], in0=ow[:].unsqueeze(2).to_broadcast([128, NOW, CT]),
                                     in1=iott[:, 0:CT].unsqueeze(1).to_broadcast([128, NOW, CT]), op=ALU.add), ["ow", "iott"], ["gidf"])
        DV(lambda e: e.tensor_tensor(out=mskf[:], in0=gidf[:], in1=lw[:].unsqueeze(2).to_broadcast([128, NOW, CT]), op=ALU.is_lt), ["gidf", "lw"], ["mskf"])
        DV(lambda e: e.scalar_tensor_tensor(out=yidf[:], in0=gidf[:], scalar=-BIGI, in1=mskf[:], op0=ALU.add, op1=ALU.mult), ["gidf", "mskf"], ["yidf"])
        DV(lambda e: e.tensor_scalar(out=yidf[:], in0=yidf[:], scalar1=BIGI, scalar2=None, op0=ALU.add), ["yidf"], ["yidf"])
        DV(lambda e: e.tensor_tensor(out=gidf[:], in0=gidf[:], in1=tw[:].unsqueeze(2).to_broadcast([128, NOW, CT]), op=ALU.add), ["gidf", "tw"], ["gidf"])
        DV(lambda e: e.tensor_copy(out=gidx[:], in_=gidf[:]), ["gidf"], ["gidx"])
        DV(lambda e: e.tensor_copy(out=yidx[:], in_=yidf[:]), ["yidf"], ["yidx"])
        DV(lambda e: e.scalar_tensor_tensor(out=ew[:], in0=ew[:], scalar=1024.0, in1=tw[:], op0=ALU.mult, op1=ALU.add), ["ew", "tw"], ["ew"])
        DV(lambda e: e.tensor_tensor(out=wgidf[:], in0=ew[:].unsqueeze(2).to_broadcast([128, NOW, 8]),
                                     in1=iot8t[:].unsqueeze(1).to_broadcast([128, NOW, 8]), op=ALU.add), ["ew", "iot8t"], ["wgidf"])
        DV(lambda e: e.tensor_copy(out=wgidx[:], in_=wgidf[:]), ["wgidf"], ["wgidx"])
        DV(lambda e: e.scalar_tensor_tensor(out=ew[:], in0=ew[:], scalar=0.5, in1=tw[:], op0=ALU.mult, op1=ALU.add), ["ew", "tw"], ["ew"])
        DV(lambda e: e.tensor_tensor(out=wdidf[:], in0=ew[:].unsqueeze(2).to_broadcast([128, NOW, 4]),
                                     in1=iot8t[:, 0:4].unsqueeze(1).to_broadcast([128, NOW, 4]), op=ALU.add), ["ew", "iot8t"], ["wdidf"])
        DV(lambda e: e.tensor_copy(out=wdidx[:], in_=wdidf[:]), ["wdidf"], ["wdidx"])

        gbanks = [(pZ[:, 0, :], "pZ0"), (pZ[:, 1, :], "pZ1"), (pC[:], "B3"), (pV[:], "B7")]
        dbanks = [(pS[:, 512:1024], "B5"), (pS[:, 1024:1536], "B6")]
        pT2 = pS[:, 0:512].bitcast(BF16)
        tbanks = [(pT, "pT"), (pT2, "B4")]
        cnts = {"gi": 0, "di": 0, "ti": 0}
        NJOB = NEXP + NOW
        wg2, wu2, wd2 = wg, wu, wd

        bregs = memo.setdefault("__bregs", {})

        def breg(e, val):
            if val not in bregs:
                r = e.alloc_register("bc%d" % val)
                e.reg_mov(r, val)
                bregs[val] = r
            return bregs[val]

        def job_rows(k, j, for_y):
            if k < NEXP:
                return widx[:, k, j:j + 1]
            return (yidx if for_y else gidx)[:, k - NEXP, j:j + 1]

        def job_load_w(k):
            b = k % 2
            if k < NEXP:
                load_w(k)
                return
            w = k - NEXP
            og, ou, od = [], [], []
            for kc in range(8):
                og.append(A("pool", lambda e, kc=kc: e.indirect_dma_start(out=Wg[b][:, kc, :], out_offset=None, in_=wg2,
                                                                          in_offset=bass.IndirectOffsetOnAxis(ap=wgidx[:, w, kc:kc + 1], axis=0),
                                                                          bounds_check=breg(e, NEXP * 1024 - 1), oob_is_err=False),
                            reads=["wgidx"], writes=[WGK[b][kc]], dma=True, dkey="Wg%d" % b))
                ou.append(A("pool", lambda e, kc=kc: e.indirect_dma_start(out=Wu[b][:, kc, :], out_offset=None, in_=wu2,
                                                                          in_offset=bass.IndirectOffsetOnAxis(ap=wgidx[:, w, kc:kc + 1], axis=0),
                                                                          bounds_check=breg(e, NEXP * 1024 - 1), oob_is_err=False),
                            reads=["wgidx"], writes=[WUK[b][kc]], dma=True, dkey="Wu%d" % b))
            for jc in range(4):
                od.append(A("pool", lambda e, jc=jc: e.indirect_dma_start(out=Wd[b][:, jc, :], out_offset=None, in_=wd2,
                                                                          in_offset=bass.IndirectOffsetOnAxis(ap=wdidx[:, w, jc:jc + 1], axis=0),
                                                                          bounds_check=breg(e, NEXP * 512 - 1), oob_is_err=False),
                            reads=["wdidx"], writes=[WDK[b][jc]], dma=True, dkey="Wd%d" % b))
            for grp in (og, ou, od):
                for o_ in grp:
                    o_.tgt = grp[-1].tgt

        def job_gather(k):
            b = k % 2
            for j in range(CT):
                rows = job_rows(k, j, False)
                if k < NEXP:
                    A("pool", lambda e, j=j, rows=rows: e.indirect_dma_start(
                        out=xw[b][:, j, :], out_offset=None, in_=xs[:, :], in_offset=bass.IndirectOffsetOnAxis(ap=rows, axis=0)),
                      reads=xskeys + ["widx"], writes=["xw%d_%d" % (b, j)], dma=True)
                else:
                    A("pool", lambda e, j=j, rows=rows: e.indirect_dma_start(
                        out=xw[b][:, j, :], out_offset=None, in_=xs[:, :], in_offset=bass.IndirectOffsetOnAxis(ap=rows, axis=0),
                        bounds_check=breg(e, cfg.XSR - 1), oob_is_err=False),
                      reads=xskeys + ["gidx"], writes=["xw%d_%d" % (b, j)], dma=True)

        xsT2 = [xsT, cb_.take(8, CAP)]

        def job_T(k):
            b = k % 2
            xs_ = xsT2[k % 2]
            for kc in range(8):
                (tps, tk_) = tbanks[cnts["ti"] % 2]
                cnts["ti"] += 1
                for j in range(CT):
                    A("pe", lambda e, kc=kc, j=j, tps=tps: e.transpose(out=tps[:, j * 128:(j + 1) * 128],
                                                                       in_=xw[b][:, j, kc * 128:(kc + 1) * 128], identity=ident[:]),
                      reads=["xw%d_%d" % (b, j), "ident"], writes=[tk_])
                A("act", lambda e, kc=kc, tps=tps: e.activation(out=xs_[:, kc, :], in_=tps[:, 0:CAP], func=AF.Identity,
                                                                scale=mul2c[:, kc:kc + 1], bias=modc[:, 2, kc:kc + 1]),
                  reads=[tk_, "mul2c", "modc"], writes=["xsT%d_%d" % (k % 2, kc)])

        def job_GU(k):
            b = k % 2
            xs_ = xsT2[k % 2]
            for jc in range(4):
                (gps, gk_) = gbanks[cnts["gi"] % 4]
                (ups, uk_) = gbanks[(cnts["gi"] + 1) % 4]
                cnts["gi"] += 2
                for kc in range(8):
                    A("pe", lambda e, gps=gps, kc=kc, jc=jc: e.matmul(gps[:, 0:CAP], lhsT=Wg[b][:, kc, jc * 128:(jc + 1) * 128], rhs=xs_[:, kc, :],
                                                                     start=(kc == 0), stop=(kc == 7)), reads=WGK[b] + ["xsT%d_%d" % (k % 2, kc)], writes=[gk_])
                for kc in range(8):
                    A("pe", lambda e, ups=ups, kc=kc, jc=jc: e.matmul(ups[:, 0:CAP], lhsT=Wu[b][:, kc, jc * 128:(jc + 1) * 128], rhs=xs_[:, kc, :],
                                                                     start=(kc == 0), stop=(kc == 7)), reads=WUK[b] + ["xsT%d_%d" % (k % 2, kc)], writes=[uk_])
                sb_ = jc % 2
                A("act", lambda e, gps=gps, sb_=sb_: e.activation(out=sgb[sb_][:], in_=gps[:, 0:CAP], func=AF.Silu), reads=[gk_], writes=["sg%d" % sb_])
                A("dve", lambda e, ups=ups, sb_=sb_, jc=jc: e.tensor_tensor(out=actT[:, jc, :], in0=ups[:, 0:CAP], in1=sgb[sb_][:], op=ALU.mult),
                  reads=[uk_, "sg%d" % sb_], writes=["actT"])

        def job_D(k):
            b = k % 2
            for j in range(CT):
                yb = (k * CT + j) % 2
                for cbk in range(2):
                    (dps, dk_) = dbanks[cnts["di"] % 2]
                    cnts["di"] += 1
                    for jc in range(4):
                        A("pe", lambda e, dps=dps, jc=jc, j=j, cbk=cbk: e.matmul(dps, lhsT=actT[:, jc, j * 128:(j + 1) * 128], rhs=Wd[b][:, jc, cbk * 512:(cbk + 1) * 512],
                                                                                start=(jc == 0), stop=(jc == 3)), reads=["actT"] + WDK[b], writes=[dk_])
                    A("dve", lambda e, dps=dps, cbk=cbk, yb=yb: e.tensor_tensor(out=ybuf[yb][:, cbk * 512:(cbk + 1) * 512], in0=dps, in1=g2bc[:, cbk * 512:(cbk + 1) * 512], op=ALU.mult),
                      reads=[dk_, "g2bc"], writes=["ybuf%d" % yb])
                rows = job_rows(k, j, True)
                if k < NEXP:
                    A("pool", lambda e, rows=rows, yb=yb: e.indirect_dma_start(
                        out=ys[:, :], out_offset=bass.IndirectOffsetOnAxis(ap=rows, axis=0), in_=ybuf[yb][:], in_offset=None),
                      reads=["ybuf%d" % yb, "widx"], writes=["ys"], dma=True, dkey="sc_ybuf%d" % yb)
                else:
                    A("pool", lambda e, rows=rows, yb=yb: e.indirect_dma_start(
                        out=ys[:, :], out_offset=bass.IndirectOffsetOnAxis(ap=rows, axis=0), in_=ybuf[yb][:], in_offset=None,
                        bounds_check=breg(e, cfg.XSR - 1), oob_is_err=False),
                      reads=["ybuf%d" % yb, "yidx"], writes=["ys"], dma=True, dkey="sc_ybuf%d" % yb)

        job_gather(0)
        job_gather(1)
        job_T(0)
        for k in range(NJOB):
            job_GU(k)
            if k + 1 < NJOB:
                job_T(k + 1)
            if k + 2 < NJOB:
                job_gather(k + 2)
            job_D(k)
            if k + 2 < NJOB:
                job_load_w(k + 2)

        for tl in range(NTL):
            b = tl % 2
            dma("sp", xmb[b][:], xmid[tl * 128:(tl + 1) * 128, :], ["xmid"], ["xmb%d" % b])
            A("pool", lambda e, tl=tl, b=b: e.indirect_dma_start(out=y1b[b][:], out_offset=None, in_=ys[:, :],
                                                                 in_offset=bass.IndirectOffsetOnAxis(ap=slot1[:, tl:tl + 1], axis=0)),
              reads=["ys", "slot1"], writes=["y1b%d" % b], dma=True)
            A("pool", lambda e, tl=tl, b=b: e.indirect_dma_start(out=y2b[b][:], out_offset=None, in_=ys[:, :],
                                                                 in_offset=bass.IndirectOffsetOnAxis(ap=slot2[:, tl:tl + 1], axis=0)),
              reads=["ys", "slot2"], writes=["y2b"], dma=True)
            A("dve", lambda e, tl=tl, b=b: e.scalar_tensor_tensor(out=xmb[b][:], in0=y1b[b][:], scalar=w1g[:, tl:tl + 1], in1=xmb[b][:], op0=ALU.mult, op1=ALU.add),
              reads=["y1b%d" % b, "w1g", "xmb%d" % b], writes=["xmb%d" % b])
            A("dve", lambda e, tl=tl, b=b: e.scalar_tensor_tensor(out=xmb[b][:], in0=y2b[b][:], scalar=w2g[:, tl:tl + 1], in1=xmb[b][:], op0=ALU.mult, op1=ALU.add),
              reads=["y2b", "w2g", "xmb%d" % b], writes=["xmb%d" % b])
            dma("sp", xo[tl * 128:(tl + 1) * 128, :], xmb[b][:], ["xmb%d" % b], ["xo"], dkey="st_xmb%d" % b)
        p.barrier()


def _colform(v):
    return np.ascontiguousarray(v.reshape(-1, 128).T).astype(np.float32)


def _const_tables(cfg):
    q = np.arange(128)[:, None]
    tabs_idx = np.zeros((128, 5, 128), np.int64)
    mask = np.zeros((128, 5, 128), np.float32)
    for t in range(5):
        k = np.arange(128)[None, :]
        rel = 128 * (4 - t) + q - k
        tabs_idx[:, t, :] = np.clip(rel, -128, 128) + 128
        qc = q // 64
        kc = 2 * (t - 4) + k // 64
        ok = (kc <= qc) & (kc >= qc - 8)
        mask[:, t, :] = ok
    tri = (np.arange(128)[:, None] < np.arange(128)[None, :]).astype(np.float32)
    iot = (np.arange(128)[:, None] + 128 * np.arange(4)[None, :]).astype(np.float32)
    trash = (cfg.TRASH + np.arange(128)[:, None] + 128 * np.arange(max(cfg.NFH, 1))[None, :]).astype(np.float32)
    return tabs_idx, mask, tri, iot, trash


def layer_inputs(cfg, l, xe, cb, first_half, P, li=0):
    tabs_idx, mask, tri, iot, trash = _const_tables(cfg)
    btab = np.ascontiguousarray(P["rel_bias"][:, tabs_idx].transpose(1, 0, 2, 3)).astype(np.float32)
    invc = np.zeros((128, 4, 16), np.float32)
    for g, w in enumerate((2, 4, 8, 16)):
        cnt = np.minimum(np.arange(16) + 1, w) if first_half else np.full(16, w)
        invc[:, g, :] = (1.0 / cnt.astype(np.float64)).astype(np.float32)[None, :]
    hvv = 0.0 if first_half else 1.0
    trash = (cfg.TRASH + np.arange(128)[:, None] + 128 * np.arange(TRC)[None, :]).astype(np.float32)
    trash = np.concatenate([trash, trash + cfg.NFH * 128], axis=1)
    m = {
        "xe": np.ascontiguousarray(xe, dtype=np.float32),
        "cT": _colform(cb),
        "ada_w": P["ada_w"][l], "ada_b": P["ada_b"][l][None, :],
        "n1c": _colform(P["norm1_g"][l]), "n2c": _colform(P["norm2_g"][l]),
        "w_in": P["w_in"][l], "w_out": P["w_out"][l],
        "pool_w": np.ascontiguousarray(P["pool_w"][l].transpose(1, 0, 2)),
        "pscale": _colform(P["pool_scale"][l]),
        "gq": np.ascontiguousarray(np.tile(P["q_norm_g"][l], 2)[:, None]), "gk": np.ascontiguousarray(np.tile(P["k_norm_g"][l], 2)[:, None]),
        "btab": btab, "bmask": mask,
        "wr": np.ascontiguousarray(np.concatenate([P["router_group_w"][l], P["router_expert_w"][l]], axis=1)),
        "br": np.ascontiguousarray(np.tile(np.concatenate([P["router_group_b"][l], P["router_expert_b"][l]])[None, :], (128, 1))),
        "wg": P["moe_w_gate"][l].reshape(NEXP * D, 512), "wu": P["moe_w_up"][l].reshape(NEXP * D, 512), "wd": P["moe_w_down"][l].reshape(NEXP * 512, D),
        "hv": np.full((128, 1), hvv, np.float32), "nhv": np.full((128, 1), 1.0 - hvv, np.float32),
        "invc": invc, "tri": tri, "iot": iot, "trashi": trash,
        "thr": np.tile((cfg.CAP * (np.arange(16) + 1)).astype(np.float32)[None, :], (128, 1)),
        "wv": np.tile(np.arange(32, dtype=np.float32)[None, :], (128, 1)),
        "ev": np.tile(np.arange(32, dtype=np.float32)[None, :], (128, 1)),
        "iot8": (np.arange(128)[:, None] + 128 * np.arange(8)[None, :]).astype(np.float32),
    }
    return {(k + "_%d" % li if k in PERL else k): v for k, v in m.items()}


_NC_CACHE = {}


def kernel(**inputs):
    P = {k: np.asarray(v) for k, v in inputs.items()}
    x = P["x"]
    B, S, _ = x.shape
    cfg0 = Cfg(nkv=4, nfh=4, nm=32, cap=512)
    cfg1 = Cfg(nkv=4, nfh=0, nm=32, cap=512)
    if "nc" not in _NC_CACHE:
        _NC_CACHE["nc"] = build_program([cfg0, cfg1])
    nc = _NC_CACHE["nc"]
    half = S // 2
    in_maps = []
    for c in range(8):
        b, hf = c // 2, c % 2
        main = x[b, hf * half:(hf + 1) * half]
        halo = np.zeros((1024, D), np.float32) if hf == 0 else x[b, half - 1024:half]
        xe = np.concatenate([halo, main], axis=0)
        m = layer_inputs(cfg0, 0, xe, P["c"][b], hf == 0, P, li=0)
        m1 = layer_inputs(cfg1, 1, xe[:128], P["c"][b], hf == 0, P, li=1)
        m.update({k: v for k, v in m1.items() if k.endswith("_1")})
        in_maps.append(m)
    res = run_bass_kernel_spmd(nc, in_maps, core_ids=list(range(8)))
    out = np.empty_like(x)
    for c in range(8):
        b, hf = c // 2, c % 2
        out[b, hf * half:(hf + 1) * half] = res.results[c]["xo"]
    return out
```

```python
from contextlib import ExitStack

import numpy as np
import concourse.bass as bass
import concourse.mybir as mybir
from concourse.bass_utils import run_bass_kernel_spmd

F32 = mybir.dt.float32
BF16 = mybir.dt.bfloat16
I32 = mybir.dt.int32
AF = mybir.ActivationFunctionType
ALU = mybir.AluOpType
AX = mybir.AxisListType

ENGS = ("sp", "act", "dve", "pool", "pe")
PSUM_KEYS = frozenset(["pT", "pZ0", "pZ1", "B3", "B4", "B5", "B6", "B7"])
D = 1024
EPS = 1e-6
NEXP = 32
RT = 8


class Op:
    __slots__ = ("eng", "fn", "dma", "dkey", "deps", "sig", "idx", "tgt", "waits")

    def __init__(self, eng, fn, dma, dkey):
        self.eng = eng
        self.fn = fn
        self.dma = dma
        self.dkey = dkey
        self.deps = []
        self.sig = False
        self.idx = 0
        self.tgt = 0
        self.waits = []


class PB:
    def __init__(self, nc):
        self.nc = nc
        self.ops = []
        self.last_w = {}
        self.readers = {}
        self.dma_cnt = {}

    def add(self, eng, fn, reads=(), writes=(), dma=False, dkey=None):
        if dma and dkey is None:
            dkey = writes[0]
        op = Op(eng, fn, dma, dkey)
        deps = set()
        for k in reads:
            w = self.last_w.get(k)
            if w is not None:
                deps.add(w)
            if k in PSUM_KEYS:
                for r in self.readers.get(k, ()):
                    if r.eng != eng:
                        deps.add(r)
        for k in writes:
            w = self.last_w.get(k)
            if w is not None:
                deps.add(w)
            for r in self.readers.get(k, ()):
                deps.add(r)
        op.deps = list(deps)
        for k in reads:
            self.readers.setdefault(k, []).append(op)
        for k in writes:
            self.last_w[k] = op
            self.readers[k] = []
        if dma:
            self.dma_cnt[dkey] = self.dma_cnt.get(dkey, 0) + 1
            op.tgt = 16 * self.dma_cnt[dkey]
        self.ops.append(op)
        return op

    def barrier(self):
        allkeys = list(set(self.last_w.keys()) | set(self.readers.keys()))
        self.add("sp", lambda e: e.nop(), reads=[], writes=allkeys + ["__bar"])
        for eng in ENGS:
            self.add(eng, lambda e: e.nop(), reads=["__bar"], writes=["__bar_" + eng])
        self.last_w = {k: v for k, v in self.last_w.items() if k.startswith("__bar")}
        self.readers = {k: v for k, v in self.readers.items() if k.startswith("__bar")}

    def emit(self):
        nc = self.nc
        for op in self.ops:
            for d in op.deps:
                if not d.dma:
                    d.sig = True
        cnt = {e: 0 for e in ENGS}
        for op in self.ops:
            if not op.dma and op.sig:
                cnt[op.eng] += 1
                op.idx = cnt[op.eng]
        dkeys = sorted(self.dma_cnt.keys())
        with ExitStack() as st:
            esem = {e: st.enter_context(nc.semaphore("es_" + e)) for e in ENGS}
            dsem = {k: st.enter_context(nc.semaphore("ds%d" % i)) for i, k in enumerate(dkeys)}
            waited = {e: {} for e in ENGS}
            for op in self.ops:
                need = {}
                for d in op.deps:
                    if d.dma:
                        key, val = ("d", d.dkey), d.tgt
                    else:
                        if d.eng == op.eng and op.eng == "pe":
                            continue
                        key, val = ("e", d.eng), d.idx
                    if need.get(key, 0) < val:
                        need[key] = val
                w = waited[op.eng]
                for key, val in need.items():
                    if w.get(key, 0) < val:
                        w[key] = val
                        op.waits.append((dsem[key[1]] if key[0] == "d" else esem[key[1]], val))
            block = st.enter_context(nc.Block())

            def run(engname):
                def body(e):
                    for op in self.ops:
                        if op.eng != engname:
                            continue
                        for (s, v) in op.waits:
                            e.wait_ge(s, v)
                        ins = op.fn(e)
                        if op.dma:
                            ins.then_inc(dsem[op.dkey], 16)
                        elif op.sig:
                            ins.then_inc(esem[engname], 1)
                return body

            block.sync(run("sp"))
            block.scalar(run("act"))
            block.vector(run("dve"))
            block.gpsimd(run("pool"))
            block.tensor(run("pe"))


class Cfg:
    def __init__(self, nkv=4, nfh=0, nm=32, cap=512):
        self.NKV, self.NFH, self.NM, self.CAP = nkv, nfh, nm, cap
        self.NTE = nkv + nfh + nm
        self.NTL = nfh + nm
        self.NST = self.NTE // 4
        self.CT = cap // 128
        self.NTOK = self.NTL * 128
        self.TRASH = 2 * self.NTOK + cap
        self.XSR = self.TRASH + 2 * max(nfh, 1) * 128
        self.NOW = -(-2 * self.NTOK // cap)
        self.NTHR = -(-self.NTOK // cap)
        assert self.NTE % 4 == 0 and nkv % 4 == 0 and nfh % 4 == 0 and cap % 128 == 0


PERL = frozenset(["ada_w", "ada_b", "n1c", "n2c", "n2a", "w_in", "w_out", "pool_w", "pscale", "gq", "gk", "wr", "br", "wg", "wu", "wd"])
TRC = 4


class _Ctx:
    pass


def build_program(cfgs, debug=False):
    nc = bass.Bass("TRN2", target_bir_lowering=False)
    ctx = _Ctx()
    ctx.nc, ctx.memo, ctx.p, ctx.nl = nc, {}, PB(nc), len(cfgs)
    with ExitStack() as st:
        ctx.st = st
        for li, cfg in enumerate(cfgs):
            _emit_layer(ctx, li, cfg, debug)
        ctx.p.emit()
    return nc


def build_layer(cfg, debug=False):
    return build_program([cfg], debug)


def _emit_layer(ctx, li, cfg, debug=False):
    nc, st, memo = ctx.nc, ctx.st, ctx.memo
    last = li == ctx.nl - 1
    NTE, NTL, NST, NKV, NFH, CT, CAP = cfg.NTE, cfg.NTL, cfg.NST, cfg.NKV, cfg.NFH, cfg.CT, cfg.CAP

    def din(name, shape, dt=F32):
        nm = name + ("_%d" % li if name in PERL else "")
        if nm not in memo:
            memo[nm] = nc.dram_tensor(nm, list(shape), dt, kind="ExternalInput").ap()
        return memo[nm]

    def dscr(name, shape, dt, kind="Internal"):
        if name not in memo:
            memo[name] = nc.dram_tensor(name, list(shape), dt, kind=kind).ap()
        return memo[name]

    xe = din("xe", [NTE * 128, D]) if li == 0 else memo["x1_%d" % (li - 1)]
    cT = din("cT", [128, 8])
    ada_w = din("ada_w", [D, 6 * D])
    ada_b = din("ada_b", [1, 6 * D])
    n1c = din("n1c", [128, 8])
    n2c = din("n2c", [128, 8])
    n2a = din("n2a", [128, 8])
    w_in = din("w_in", [D, 2048])
    w_out = din("w_out", [D, D])
    pool_w = din("pool_w", [128, 4, 128])
    pscale = din("pscale", [128, 4])
    gq = din("gq", [128, 1])
    gk = din("gk", [128, 1])
    btab = din("btab", [128, 8, 5, 128])
    bmask = din("bmask", [128, 5, 128])
    wr = din("wr", [D, 36])
    br = din("br", [128, 36])
    wg = din("wg", [NEXP * 128, 8 * 512])
    wu = din("wu", [NEXP * 128, 8 * 512])
    wd = din("wd", [NEXP * 512, D])
    hv = din("hv", [128, 1])
    nhv = din("nhv", [128, 1])
    invc = din("invc", [128, 4, 16])
    tri = din("tri", [128, 128])
    iot = din("iot", [128, 4])
    trashi = din("trashi", [128, 2 * TRC])
    thr = din("thr", [128, 16])
    wv = din("wv", [128, 32])
    ev = din("ev", [128, 32])
    iot8 = din("iot8", [128, 8])
    if last:
        xo = nc.dram_tensor("xo", [NTL * 128, D], F32, kind="ExternalOutput").ap()
    else:
        xo = dscr("x1_%d" % li, [NTL * 128, D], F32)
    xmid = dscr("xmid", [NTL * 128, D], F32, kind="ExternalOutput" if debug else "Internal")
    if debug:
        dbg_logits = nc.dram_tensor("dbg_logits", [128, NTL, 36], F32, kind="ExternalOutput").ap()
        dbg_w = nc.dram_tensor("dbg_w", [128, 2, NTL], F32, kind="ExternalOutput").ap()
        dbg_slot = nc.dram_tensor("dbg_slot", [128, 2, NTL], I32, kind="ExternalOutput").ap()
    xn2s = dscr("xn2s", [NTL * 128, D], BF16)
    xs = dscr("xs", [cfg.XSR, D], BF16)
    ys = dscr("ys", [cfg.XSR, D], F32)

    if True:
        def T(name, shape, dt=F32):
            if name in memo:
                t, shp = memo[name]
                if list(shp) != list(shape):
                    assert len(shp) == len(shape) and shape[1] <= shp[1] and list(shp[2:]) == list(shape[2:]), (name, shp, shape)
                    return t[:, 0:shape[1]]
                return t
            t = st.enter_context(nc.sbuf_tensor(name, list(shape), dt))
            memo[name] = (t, list(shape))
            return t

        def PS(name, shape, dt=F32):
            if name not in memo:
                memo[name] = st.enter_context(nc.psum_tensor(name, list(shape), dt))
            return memo[name]

        ident = T("ident", [128, 128], BF16)
        identf = T("identf", [128, 128])
        ones_bf = T("ones_bf", [128, 128], BF16)
        blk1 = T("blk1", [128, 128], BF16)
        tri_bf = T("tri_bf", [128, 128], BF16)
        onesf = T("onesf", [1, 128])
        epsc = T("epsc", [128, 1])
        eps64 = T("eps64", [128, 1])
        cact = T("cact", [128, 8], BF16)
        ctf = T("ctf", [128, 8])
        n1t = T("n1t", [128, 8])
        n2t = T("n2t", [128, 8])
        n2at = T("n2at", [128, 8])
        modca = T("modca", [128, 2, 8])
        mul2a = T("mul2a", [128, 8])
        wgidx2 = T("wgidx2", [128, 32], I32)
        modc = T("modc", [128, 4, 8])
        mul1c = T("mul1c", [128, 8])
        mul2c = T("mul2c", [128, 8])
        add1b = T("add1b", [128, 8], BF16)
        g2bc = T("g2bc", [128, D])
        bzc = T("bzc", [128, 12])
        bzv = T("bzv", [128, 512])
        bzvm = T("bzvm", [128, 512])
        pw_bf = T("pw_bf", [128, 4, 128], BF16)
        psc = T("psc", [128, 4])
        gqk = T("gqk", [128, 1])
        gkt = T("gkt", [128, 1])
        BT = T("BT", [128, 8, 5, 128], BF16)
        wr_f = T("wr_f", [128, 8, 36])
        wr_bf = T("wr_bf", [128, 8, 36], BF16)
        wr_raw = T("wr_raw", [128, 8, 36], BF16)
        biasR = T("biasR", [128, 36])
        hvt = T("hvt", [128, 1])
        nhvt = T("nhvt", [128, 1])
        invct = T("invct", [128, 4, 16])
        iott = T("iott", [128, 4])
        trt = T("trt", [128, 2 * TRC])
        thrt = T("thrt", [128, 16])
        wvt = T("wvt", [128, 32])
        evt = T("evt", [128, 32])
        iot8t = T("iot8t", [128, 8])
        ssr = T("ssr", [128, 8])
        rst = T("rst", [128, 8])
        logits = T("logits", [128, NTL, 36])
        w1g = T("w1g", [128, NTL])
        w2g = T("w2g", [128, NTL])
        slot1 = T("slot1", [128, NTL], I32)
        slot2 = T("slot2", [128, NTL], I32)
        widx = T("widx", [128, NEXP, CT], I32)

        AF_WORDS = 15104
        AB_WORDS = 58432
        arf = T("arf", [128, AF_WORDS])
        arb = T("arb", [128, AB_WORDS], BF16)

        class Carver:
            def __init__(self, t, n):
                self.t, self.n, self.off = t, n, 0

            def take(self, *shape):
                n = int(np.prod(shape))
                a = self.t[:, self.off:self.off + n]
                self.off += n
                assert self.off <= self.n, (self.off, self.n)
                if len(shape) == 2:
                    return a.rearrange("p (a b) -> p a b", a=shape[0])
                if len(shape) == 3:
                    return a.rearrange("p (a b c) -> p a b c", a=shape[0], b=shape[1])
                return a

        pT = PS("pT", [128, 1024], BF16)
        pZ = PS("pZ", [128, 2, 512])
        pC = PS("B3", [128, 512])
        pS = PS("pS", [128, 1536])
        pV = PS("B7", [128, 512])

        p = ctx.p
        A = p.add

        def dma(eng, out, in_, reads, writes, dkey=None):
            return A(eng, lambda e: e.dma_start(out=out, in_=in_), reads=reads, writes=writes, dma=True, dkey=dkey)

        A("pool", lambda e: e.memset(identf[:], 0.0), writes=["identf"])
        A("pool", lambda e: e.affine_select(out=identf[:], in_=identf[:], pattern=[[-1, 128]], compare_op=ALU.not_equal,
                                            fill=1.0, base=0, channel_multiplier=1), reads=["identf"], writes=["identf"])
        A("dve", lambda e: e.tensor_copy(out=ident[:], in_=identf[:]), reads=["identf"], writes=["ident"])
        A("pool", lambda e: e.memset(ones_bf[:], 1.0), writes=["ones_bf"])
        A("pool", lambda e: e.memset(blk1[:], 0.0), writes=["blk1"])
        A("pool", lambda e: e.memset(blk1[0:64, 0:64], 1.0), reads=["blk1"], writes=["blk1"])
        A("pool", lambda e: e.memset(blk1[64:128, 64:128], 1.0), reads=["blk1"], writes=["blk1"])
        A("pool", lambda e: e.memset(onesf[:], 1.0), writes=["onesf"])
        A("pool", lambda e: e.memset(epsc[:], EPS), writes=["epsc"])
        A("pool", lambda e: e.memset(eps64[:], 64 * EPS), writes=["eps64"])
        for (dst, src, k) in ((ctf, cT, "ctf"), (n1t, n1c, "n1t"), (n2t, n2c, "n2t"), (n2at, n2a, "n2at"), (psc, pscale, "psc"), (gqk, gq, "gqk"),
                              (gkt, gk, "gkt"), (hvt, hv, "hvt"), (nhvt, nhv, "nhvt"), (invct, invc, "invct"),
                              (iott, iot, "iott"), (trt, trashi, "trt"), (thrt, thr, "thrt"), (wvt, wv, "wvt"), (evt, ev, "evt"), (iot8t, iot8, "iot8t"), (biasR, br, "biasR"), (identf, tri, "identf")):
            dma("sp", dst[:], src, [], [k])
        A("dve", lambda e: e.tensor_copy(out=tri_bf[:], in_=identf[:]), reads=["identf"], writes=["tri_bf"])
        A("dve", lambda e: e.tensor_mul(out=gqk[:], in0=gqk[:], in1=gkt[:]), reads=["gqk", "gkt"], writes=["gqk"])
        dma("pool", pw_bf[:], pool_w, [], ["pw_bf"])
        A("act", lambda e: e.activation(out=cact[:], in_=ctf[:], func=AF.Silu), reads=["ctf"], writes=["cact"])

        cf = Carver(arf, AF_WORDS)
        cb_ = Carver(arb, AB_WORDS)
        g1bc = cf.take(D)
        modrow = cf.take(6 * D)[0:1, :]
        adab = cf.take(6 * D)[0:1, :]
        stage = [cb_.take(8, 1536) for _ in range(2)]
        zf = cf.take(D)
        zb = cb_.take(D)
        A("pool", lambda e: e.memset(zf[:], 0.0), writes=["zf"])
        A("pool", lambda e: e.memset(zb[:], 0.0), writes=["zb"])
        for r0 in range(2 * cfg.NM * 128, cfg.TRASH, 128):
            dma("sp", xs[r0:r0 + 128, :], zb[:], ["zb"], ["xs_z%d" % r0], dkey="zinit")
        for r0 in range(cfg.TRASH, cfg.TRASH + 2 * NFH * 128, 128):
            dma("sp", ys[r0:r0 + 128, :], zf[:], ["zf"], ["ys_z%d" % r0], dkey="zinit")
        dma("sp", adab, ada_b, [], ["adab"])
        for g in range(4):
            sb = stage[g % 2]
            dma("pool", sb[:], ada_w[:, g * 1536:(g + 1) * 1536].rearrange("(k p) n -> p k n", p=128), [], ["stage%d" % (g % 2)])
            for cbk in range(3):
                col = g * 1536 + cbk * 512
                bank = cbk % 2
                for kc in range(8):
                    A("pe", lambda e, sb=sb, kc=kc, cbk=cbk, bank=bank: e.matmul(
                        pZ[0:1, bank, :], lhsT=cact[:, kc:kc + 1], rhs=sb[:, kc, cbk * 512:(cbk + 1) * 512],
                        start=(kc == 0), stop=(kc == 7)), reads=["cact", "stage%d" % (g % 2)], writes=["pZ%d" % bank])
                A("dve", lambda e, col=col, bank=bank: e.tensor_tensor(out=modrow[:, col:col + 512], in0=pZ[0:1, bank, :],
                                                                       in1=adab[:, col:col + 512], op=ALU.add),
                  reads=["pZ%d" % bank, "adab"], writes=["modrow"])
        for vi, base in enumerate((0, D, 3 * D, 4 * D)):
            for kc in range(8):
                A("pe", lambda e, vi=vi, base=base, kc=kc: e.matmul(
                    pC[:, vi * 8 + kc: vi * 8 + kc + 1], lhsT=modrow[:, base + kc * 128: base + (kc + 1) * 128],
                    rhs=onesf[:, 0:1], start=True, stop=True), reads=["modrow", "onesf"], writes=["B3"])
        A("dve", lambda e: e.tensor_copy(out=modc[:], in_=pC[:, 0:32].rearrange("p (a b) -> p a b", a=4)), reads=["B3"], writes=["modc"])
        A("dve", lambda e: e.scalar_tensor_tensor(out=mul1c[:], in0=modc[:, 1, :], scalar=1.0, in1=n1t[:], op0=ALU.add, op1=ALU.mult),
          reads=["modc", "n1t"], writes=["mul1c"])
        A("dve", lambda e: e.scalar_tensor_tensor(out=mul2c[:], in0=modc[:, 3, :], scalar=1.0, in1=n2t[:], op0=ALU.add, op1=ALU.mult),
          reads=["modc", "n2t"], writes=["mul2c"])
        A("dve", lambda e: e.tensor_copy(out=add1b[:], in_=modc[:, 0, :]), reads=["modc"], writes=["add1b"])
        for vi, base in enumerate((3 * D, 4 * D)):
            mview = modrow[:, base:base + D].rearrange("o (p k) -> o k p", k=8)
            for kc in range(8):
                A("pe", lambda e, vi=vi, kc=kc, mview=mview: e.matmul(
                    pV[:, vi * 8 + kc: vi * 8 + kc + 1], lhsT=mview[:, kc, :], rhs=onesf[:, 0:1], start=True, stop=True),
                  reads=["modrow", "onesf"], writes=["B7"])
        A("dve", lambda e: e.tensor_copy(out=modca[:], in_=pV[:, 0:16].rearrange("p (a b) -> p a b", a=2)), reads=["B7"], writes=["modca"])
        A("dve", lambda e: e.scalar_tensor_tensor(out=mul2a[:], in0=modca[:, 1, :], scalar=1.0, in1=n2at[:], op0=ALU.add, op1=ALU.mult),
          reads=["modca", "n2at"], writes=["mul2a"])
        for (dst, base, k) in ((g1bc, 2 * D, "g1bc"), (g2bc, 5 * D, "g2bc")):
            for hb in range(2):
                A("pe", lambda e, base=base, hb=hb: e.matmul(pZ[:, hb, :], lhsT=onesf[:, :], rhs=modrow[:, base + hb * 512: base + (hb + 1) * 512],
                                                            start=True, stop=True), reads=["modrow", "onesf"], writes=["pZ%d" % hb])
                A("dve", lambda e, dst=dst, hb=hb: e.tensor_copy(out=dst[:, hb * 512:(hb + 1) * 512], in_=pZ[:, hb, :]),
                  reads=["pZ%d" % hb], writes=[k])
        p.barrier()

        cf = Carver(arf, AF_WORDS)
        cb_ = Carver(arb, AB_WORDS)
        w_in_bf = cb_.take(8, 2048)
        w_out_bf = cb_.take(8, D)
        g1bc = cf.take(D)
        add2rep = cb_.take(8, 128)
        wst = [cf.take(2, 2048) for _ in range(2)]
        wraw = cb_.take(8, 2048)
        bzrow = cf.take(2048)[0:1, :]
        bzrow_b = cb_.take(512)[0:1, :]
        for pc in range(4):
            sbf = wst[pc % 2]
            dma("sp", sbf[:], w_in[pc * 256:(pc + 1) * 256, :].rearrange("(k p) n -> p k n", p=128), [], ["wst%d" % (pc % 2)])
            for kk in range(2):
                kc = pc * 2 + kk
                A("dve", lambda e, sbf=sbf, kk=kk, kc=kc: e.tensor_scalar(out=w_in_bf[:, kc, :], in0=sbf[:, kk, :], scalar1=mul1c[:, kc:kc + 1],
                                                                       scalar2=None, op0=ALU.mult),
                  reads=["wst%d" % (pc % 2), "mul1c"], writes=["w_in_bf"])
                A("act", lambda e, sbf=sbf, kk=kk, kc=kc: e.activation(out=wraw[:, kc, :], in_=sbf[:, kk, :], func=AF.Copy),
                  reads=["wst%d" % (pc % 2)], writes=["wraw"])
        for cbk in range(4):
            for kc in range(8):
                A("pe", lambda e, cbk=cbk, kc=kc: e.matmul(pZ[0:1, cbk % 2, :], lhsT=add1b[:, kc:kc + 1], rhs=wraw[:, kc, cbk * 512:(cbk + 1) * 512],
                                                          start=(kc == 0), stop=(kc == 7)), reads=["add1b", "wraw"], writes=["pZ%d" % (cbk % 2)])
            A("dve", lambda e, cbk=cbk: e.tensor_copy(out=bzrow[:, cbk * 512:(cbk + 1) * 512], in_=pZ[0:1, cbk % 2, :]),
              reads=["pZ%d" % (cbk % 2)], writes=["bzrow"])
        for oc in range(12):
            A("pe", lambda e, oc=oc: e.matmul(pC[:, oc:oc + 1], lhsT=bzrow[:, oc * 128:(oc + 1) * 128], rhs=onesf[:, 0:1], start=True, stop=True),
              reads=["bzrow", "onesf"], writes=["B3"])
        A("dve", lambda e: e.tensor_copy(out=bzc[:], in_=pC[:, 0:12]), reads=["B3"], writes=["bzc"])
        A("pe", lambda e: e.matmul(pZ[:, 0, :], lhsT=onesf[:, :], rhs=bzrow[:, 1536:2048], start=True, stop=True),
          reads=["bzrow", "onesf"], writes=["pZ0"])
        A("dve", lambda e: e.tensor_copy(out=bzv[:], in_=pZ[:, 0, :]), reads=["pZ0"], writes=["bzv"])
        A("dve", lambda e: e.tensor_scalar(out=bzvm[:], in0=bzv[:], scalar1=hvt[:, 0:1], scalar2=None, op0=ALU.mult),
          reads=["bzv", "hvt"], writes=["bzvm"])
        wost = [wst[0][:, :, 0:D], wst[1][:, :, 0:D]]
        for pc in range(4):
            sbf = wost[pc % 2]
            dma("sp", sbf[:], w_out[pc * 256:(pc + 1) * 256, :].rearrange("(k p) n -> p k n", p=128), [], ["wst%d" % (pc % 2)])
            for kk in range(2):
                kc = pc * 2 + kk
                A("dve", lambda e, sbf=sbf, kk=kk, kc=kc: e.tensor_tensor(out=w_out_bf[:, kc, :], in0=sbf[:, kk, :], in1=g1bc[:], op=ALU.mult),
                  reads=["wst%d" % (pc % 2), "g1bc"], writes=["w_out_bf"])
        btf = cf.take(5, 128)
        pen = cf.take(5, 128)
        mk = cf.take(5, 128)
        dma("sp", mk[:], bmask, [], ["mk"])
        A("dve", lambda e: e.tensor_scalar(out=pen[:], in0=mk[:], scalar1=3750.0, scalar2=-3750.0, op0=ALU.mult, op1=ALU.add),
          reads=["mk"], writes=["pen"])
        for h in range(8):
            dma("sp", btf[:], btab[:, h, :, :], [], ["btf"])
            A("dve", lambda e: e.tensor_tensor(out=btf[:], in0=btf[:], in1=mk[:], op=ALU.mult), reads=["btf", "mk"], writes=["btf"])
            A("dve", lambda e, h=h: e.scalar_tensor_tensor(out=BT[:, h, :, :], in0=btf[:], scalar=0.125, in1=pen[:], op0=ALU.mult, op1=ALU.add),
              reads=["btf", "pen"], writes=["BT"])
        dma("sp", wr_f[:], wr.rearrange("(k p) n -> p k n", p=128), [], ["wr_f"])
        A("dve", lambda e: e.tensor_copy(out=wr_raw[:], in_=wr_f[:]), reads=["wr_f"], writes=["wr_raw"])
        for kc in range(8):
            A("dve", lambda e, kc=kc: e.tensor_scalar(out=wr_bf[:, kc, :], in0=wr_f[:, kc, :], scalar1=mul2c[:, kc:kc + 1], scalar2=None, op0=ALU.mult),
              reads=["wr_f", "mul2c"], writes=["wr_bf"])
            A("dve", lambda e, kc=kc: e.tensor_copy(out=add2rep[:, kc, :], in_=modc[:, 2, kc:kc + 1].to_broadcast([128, 128])),
              reads=["modc"], writes=["add2rep"])
        for kc in range(8):
            A("pe", lambda e, kc=kc: e.matmul(pC[:, 0:36], lhsT=add2rep[:, kc, :], rhs=wr_raw[:, kc, :], start=(kc == 0), stop=(kc == 7)),
              reads=["add2rep", "wr_raw"], writes=["B3"])
        A("dve", lambda e: e.tensor_tensor(out=biasR[:], in0=pC[:, 0:36], in1=biasR[:], op=ALU.add), reads=["B3", "biasR"], writes=["biasR"])
        p.barrier()

        cf = Carver(arf, AF_WORDS)
        cb_ = Carver(arb, AB_WORDS)
        w_in_bf = cb_.take(8, 2048)
        w_out_bf = cb_.take(8, D)
        xin = [cf.take(D) for _ in range(2)]
        xr = [cf.take(D) for _ in range(2)]
        xmd = [cf.take(D) for _ in range(2)]
        qf = [cf.take(512) for _ in range(3)]
        rq = [cf.take(512) for _ in range(3)]
        uT = [[cf.take(528) for _ in range(4)] for _ in range(2)]
        ptmp = [cf.take(528) for _ in range(2)]
        rden = cf.take(2, 4)
        xn = [cb_.take(D) for _ in range(2)]
        hT_ = cb_.take(8, 512)
        hT = [hT_, hT_]
        kT = cb_.take(4, RT * 128)
        Vr = cb_.take(RT, 8 * 65).rearrange("p r (h d) -> p r h d", h=8)
        qTm = [cb_.take(4, 512) for _ in range(2)]
        sq = [cb_.take(512) for _ in range(3)]
        pTt = cb_.take(4, 512)
        mixT_ = cb_.take(8, 512)
        mixT = [mixT_, mixT_]
        PTb = [cb_.take(2, 640) for _ in range(2)]
        att = [cb_.take(512) for _ in range(2)]
        xn2 = [cb_.take(D) for _ in range(2)]
        xn2T = [cb_.take(8, 128) for _ in range(2)]

        for b in range(2):
            for g in range(4):
                A("pool", lambda e, b=b, g=g: e.memset(uT[b][g][:, 0:16], 0.0), writes=["uT%d%d" % (b, g)])
        A("pool", lambda e: e.memset(qTm[0][64:128, :, :], 0.0), writes=["qT"])
        A("pool", lambda e: e.memset(qTm[1][0:64, :, :], 0.0), writes=["qT"])

        SSOFF = (0, 640)
        PVR = ((pS, 1280), (pV, 0), (pV, 256))
        HG = ((0, 1, 2), (3, 4, 5), (6, 7))

        def norm_and_transpose(src, srckey, sl, dstT, dstTkeys, dstcols, xnbuf, xnkey, store_to=None, scale_eng="dve", defer=False):
            A("act", lambda e: e.activation(out=xnbuf[:], in_=src, func=AF.Square, accum_out=ssr[:, sl:sl + 1]),
              reads=[srckey], writes=[xnkey, "ssr%d" % sl])
            A("act", lambda e: e.activation(out=rst[:, sl:sl + 1], in_=ssr[:, sl:sl + 1], func=AF.Ln, scale=1.0 / D, bias=epsc[:]),
              reads=["ssr%d" % sl, "epsc"], writes=["rst%d" % sl])
            A("act", lambda e: e.activation(out=rst[:, sl:sl + 1], in_=rst[:, sl:sl + 1], func=AF.Exp, scale=-0.5),
              reads=["rst%d" % sl], writes=["rst%d" % sl])
            if scale_eng == "dve":
                A("dve", lambda e: e.tensor_scalar(out=xnbuf[:], in0=src, scalar1=rst[:, sl:sl + 1], scalar2=None, op0=ALU.mult),
                  reads=[srckey, "rst%d" % sl], writes=[xnkey])
            else:
                A("act", lambda e: e.activation(out=xnbuf[:], in_=src, func=AF.Copy, scale=rst[:, sl:sl + 1]),
                  reads=[srckey, "rst%d" % sl], writes=[xnkey])
            if store_to is not None:
                dma("pool", store_to, xnbuf[:], [xnkey], ["xn2s"], dkey="st_" + xnkey)

            def part_b():
                for kc in range(8):
                    A("pe", lambda e, kc=kc: e.transpose(out=pT[:, kc * 128:(kc + 1) * 128], in_=xnbuf[:, kc * 128:(kc + 1) * 128], identity=ident[:]),
                      reads=[xnkey, "ident"], writes=["pT"])
                A("dve", lambda e: e.tensor_copy(out=dstT[:, :, dstcols], in_=pT[:].rearrange("p (a b) -> p a b", a=8)),
                  reads=["pT"], writes=dstTkeys)
            if defer:
                return part_b
            part_b()

        ZB = [(pZ[:, 0, :], "pZ0"), (pZ[:, 1, :], "pZ1"), (pS[:, 0:512], "B4"), (pS[:, 512:1024], "B5"), (pS[:, 1024:1536], "B6")]
        SB = [(pC, "B3"), (pV, "B7")]
        zcnt = {"z": 0, "s": 0}

        def in_chunk(s, oc, ub, slot0, halo_st, full_st):
            zps, zk = ZB[zcnt["z"] % 5]
            zcnt["z"] += 1
            kslots = ["kT%d" % (slot0 + i) for i in range(4)]
            for kc in range(8):
                A("pe", lambda e, kc=kc: e.matmul(zps, lhsT=w_in_bf[:, kc, oc * 128:(oc + 1) * 128], rhs=hT_[:, kc, :],
                                                  start=(kc == 0), stop=(kc == 7)), reads=["w_in_bf", "hT"], writes=[zk])
            if oc < 4:
                g = oc
                if halo_st:
                    A("dve", lambda e: e.tensor_scalar(out=uT[ub][g][:, 16:528], in0=zps, scalar1=bzc[:, oc:oc + 1],
                                                       scalar2=hvt[:, 0:1], op0=ALU.add, op1=ALU.mult),
                      reads=[zk, "bzc", "hvt"], writes=["uT%d%d" % (ub, g)])
                else:
                    A("dve", lambda e: e.tensor_scalar(out=uT[ub][g][:, 16:528], in0=zps, scalar1=bzc[:, oc:oc + 1],
                                                       scalar2=None, op0=ALU.add),
                      reads=[zk, "bzc"], writes=["uT%d%d" % (ub, g)])
                return None
            isq = oc < 8
            c = (oc - 4) % 4
            tb = oc % 3
            A("dve", lambda e: e.tensor_scalar(out=qf[tb][:], in0=zps, scalar1=bzc[:, oc:oc + 1], scalar2=None, op0=ALU.add),
              reads=[zk, "bzc"], writes=["qf%d" % tb])
            A("act", lambda e: e.activation(out=sq[tb][:], in_=qf[tb][:], func=AF.Square),
              reads=["qf%d" % tb], writes=["sq%d" % tb])
            sps, sk = SB[zcnt["s"] % 2]
            zcnt["s"] += 1

            def part2():
                A("pe", lambda e: e.matmul(sps[:], lhsT=blk1[:], rhs=sq[tb][:], start=True, stop=True), reads=["blk1", "sq%d" % tb], writes=[sk])
                A("act", lambda e: e.activation(out=rq[tb][:], in_=sps[:], func=AF.Ln, bias=eps64[:]), reads=[sk, "eps64"], writes=["rq%d" % tb])
                A("act", lambda e: e.activation(out=rq[tb][:], in_=rq[tb][:], func=AF.Exp, scale=-0.5), reads=["rq%d" % tb], writes=["rq%d" % tb])
                if isq:
                    A("dve", lambda e: e.tensor_tensor(out=qTm[0][0:64, c, :], in0=qf[tb][0:64, :], in1=rq[tb][0:64, :], op=ALU.mult),
                      reads=["qf%d" % tb, "rq%d" % tb], writes=["qT"])
                    A("dve", lambda e: e.tensor_tensor(out=qTm[1][64:128, c, :], in0=qf[tb][64:128, :], in1=rq[tb][64:128, :], op=ALU.mult),
                      reads=["qf%d" % tb, "rq%d" % tb], writes=["qT"])
                else:
                    A("dve", lambda e: e.scalar_tensor_tensor(out=kT[:, c, slot0 * 128:(slot0 + 4) * 128], in0=qf[tb][:], scalar=gqk[:, 0:1],
                                                              in1=rq[tb][:], op0=ALU.mult, op1=ALU.mult),
                      reads=["qf%d" % tb, "rq%d" % tb, "gqk"], writes=kslots)
            return part2

        def v_tile(s, i, halo_st):
            te = 4 * s + i
            sl = te % RT
            zps, zk = ZB[zcnt["z"] % 5]
            zcnt["z"] += 1
            for kc in range(8):
                A("pe", lambda e, kc=kc: e.matmul(zps, lhsT=hT_[:, kc, i * 128:(i + 1) * 128], rhs=w_in_bf[:, kc, 1536:2048],
                                                  start=(kc == 0), stop=(kc == 7)), reads=["w_in_bf", "hT"], writes=[zk])
            zv = zps.rearrange("p (h d) -> p h d", h=8)
            if halo_st:
                A("dve", lambda e: e.scalar_tensor_tensor(out=Vr[:, sl, :, 0:64], in0=zv, scalar=hvt[:, 0:1],
                                                          in1=bzvm[:].rearrange("p (h d) -> p h d", h=8), op0=ALU.mult, op1=ALU.add),
                  reads=[zk, "hvt", "bzvm"], writes=["V%d" % sl])
                A("pool", lambda e: e.tensor_copy(out=Vr[:, sl, :, 64:65], in_=hvt[:, 0:1].unsqueeze(1).to_broadcast([128, 8, 1])),
                  reads=["hvt"], writes=["V%d" % sl])
            else:
                A("dve", lambda e: e.tensor_tensor(out=Vr[:, sl, :, 0:64], in0=zv, in1=bzv[:].rearrange("p (h d) -> p h d", h=8), op=ALU.add),
                  reads=[zk, "bzv"], writes=["V%d" % sl])
                A("pool", lambda e: e.memset(Vr[:, sl, :, 64:65], 1.0), writes=["V%d" % sl])

        def pool_group(g, ub, first_main):
            U = uT[ub][g]
            uk = "uT%d%d" % (ub, g)
            cur, curk = U, uk
            sh = 1
            for stp in range(g + 1):
                dstb = ptmp[stp % 2]
                dk = "ptmp%d" % (stp % 2)
                lo = 2 * sh - 1
                A("pool", lambda e, cur=cur, dstb=dstb, lo=lo, sh=sh: e.tensor_tensor(out=dstb[:, lo:528], in0=cur[:, lo:528], in1=cur[:, lo - sh:528 - sh], op=ALU.add),
                  reads=[curk], writes=[dk])
                cur, curk = dstb, dk
                sh *= 2
            w = 2 ** (g + 1)
            fin = cur
            A("dve", lambda e: e.scalar_tensor_tensor(out=pTt[:, g, :], in0=fin[:, 16:528], scalar=1.0 / w, in1=U[:, 16:528],
                                                      op0=ALU.mult, op1=ALU.subtract),
              reads=[curk, uk], writes=["pTt%d" % g])
            if first_main:
                A("pool", lambda e: e.tensor_tensor(out=fin[:, 0:16], in0=fin[:, 16:32], in1=invct[:, g, :], op=ALU.mult),
                  reads=[curk, "invct"], writes=[curk])
                A("pool", lambda e: e.tensor_tensor(out=pTt[:, g, 0:16], in0=fin[:, 0:16], in1=U[:, 16:32], op=ALU.subtract),
                  reads=[curk, uk], writes=["pTt%d" % g])
            def part_b():
                sps, sk = SB[g % 2]
                A("pe", lambda e: e.matmul(sps[:], lhsT=pw_bf[:, g, :], rhs=pTt[:, g, :], start=True, stop=True), reads=["pw_bf", "pTt%d" % g], writes=[sk])
                A("act", lambda e: e.activation(out=mixT_[:, g, :], in_=sps[:], func=AF.Copy, scale=psc[:, g:g + 1]),
                  reads=[sk, "psc"], writes=["mixT"])
            return part_b

        def attn_pair(te, i, pr, ab):
            c = pr
            pb2 = pr % 2
            PTp = PTb[pb2]
            ptk = "PT%d" % pb2
            for hh in range(2):
                pb = 64 * hh
                h = 2 * pr + hh
                for t in range(4):
                    ksl = (te - 4 + t) % RT
                    A("pe", lambda e, t=t, ksl=ksl, pb=pb, hh=hh: e.matmul(
                        pS[:, hh * 512 + t * 128: hh * 512 + (t + 1) * 128], lhsT=kT[:, c, ksl * 128:(ksl + 1) * 128],
                        rhs=qTm[hh][:, c, i * 128:(i + 1) * 128], start=True, stop=False),
                      reads=["kT%d" % ksl, "qT"], writes=["B%d" % (4 + hh)])
                    A("pe", lambda e, t=t, h=h, hh=hh: e.matmul(pS[:, hh * 512 + t * 128: hh * 512 + (t + 1) * 128], lhsT=BT[:, h, t, :], rhs=ident[:],
                                                                start=False, stop=True), reads=["BT", "ident"], writes=["B%d" % (4 + hh)])
            ksl4 = te % RT
            for hh in range(2):
                pb = 64 * hh
                h = 2 * pr + hh
                A("pe", lambda e, pb=pb, hh=hh: e.matmul(
                    pS[:, 1024 + hh * 128: 1024 + (hh + 1) * 128], lhsT=kT[:, c, ksl4 * 128:(ksl4 + 1) * 128],
                    rhs=qTm[hh][:, c, i * 128:(i + 1) * 128], start=True, stop=False),
                  reads=["kT%d" % ksl4, "qT"], writes=["B6"])
                A("pe", lambda e, h=h, hh=hh: e.matmul(pS[:, 1024 + hh * 128: 1024 + (hh + 1) * 128], lhsT=BT[:, h, 4, :], rhs=ident[:],
                                                       start=False, stop=True), reads=["BT", "ident"], writes=["B6"])
            for hh in range(2):
                A("act", lambda e, hh=hh: e.activation(out=PTp[:, hh, 0:512], in_=pS[:, hh * 512:(hh + 1) * 512], func=AF.Exp, scale=8.0),
                  reads=["B%d" % (4 + hh)], writes=[ptk])
            A("act", lambda e: e.activation(out=PTp[:, :, 512:640], in_=pS[:, 1024:1280].rearrange("p (a b) -> p a b", a=2), func=AF.Exp, scale=8.0),
              reads=["B6"], writes=[ptk])
            for hh in range(2):
                h = 2 * pr + hh
                pvt, pvk = (pV, "B7") if h < 4 else (pC, "B3")
                co = (h % 4) * 65
                for t in range(5):
                    ksl = (te - 4 + t) % RT
                    A("pe", lambda e, t=t, ksl=ksl, hh=hh, h=h, pvt=pvt, co=co: e.matmul(
                        pvt[:, co: co + 65], lhsT=PTp[:, hh, t * 128:(t + 1) * 128], rhs=Vr[:, ksl, h, :],
                        start=(t == 0), stop=(t == 4)), reads=[ptk, "V%d" % ksl], writes=[pvk])

        def attn_norm(hgi, ab):
            pvt, pvk = (pV, "B7") if hgi == 0 else (pC, "B3")
            pvv = pvt[:, 0:260].rearrange("p (h d) -> p h d", h=4)
            A("dve", lambda e: e.tensor_scalar(out=rden[:, hgi, :].unsqueeze(2), in0=pvv[:, :, 64:65], scalar1=1e-30, scalar2=None, op0=ALU.add),
              reads=[pvk], writes=["rden%d" % hgi])
            A("dve", lambda e: e.reciprocal(out=rden[:, hgi, :], in_=rden[:, hgi, :]),
              reads=["rden%d" % hgi], writes=["rden%d" % hgi])
            A("dve", lambda e: e.tensor_tensor(
                out=att[ab][:, hgi * 256:(hgi + 1) * 256].rearrange("p (h d) -> p h d", h=4), in0=pvv[:, :, 0:64],
                in1=rden[:, hgi, :].unsqueeze(2).to_broadcast([128, 4, 64]), op=ALU.mult),
              reads=[pvk, "rden%d" % hgi], writes=["att%d" % ab])

        def attention_tile(s, i):
            te = 4 * s + i
            ab = te % 2
            for pr in range(4):
                attn_pair(te, i, pr, ab)
                if pr % 2 == 1:
                    attn_norm(pr // 2, ab)

        def post_attention(s, i):
            te = 4 * s + i
            tl = te - NKV
            ab = te % 2
            for c in range(4):
                A("pe", lambda e, c=c: e.transpose(out=pT[:, c * 128:(c + 1) * 128], in_=att[ab][:, c * 128:(c + 1) * 128], identity=ident[:]),
                  reads=["att%d" % ab, "ident"], writes=["pT"])
            A("dve", lambda e: e.tensor_copy(out=mixT_[:, 4:8, i * 128:(i + 1) * 128], in_=pT[:, 0:512].rearrange("p (a b) -> p a b", a=4)),
              reads=["pT"], writes=["mixT"])
            for cbk in range(2):
                for kc in range(8):
                    A("pe", lambda e, cbk=cbk, kc=kc: e.matmul(pZ[:, cbk, :], lhsT=mixT_[:, kc, i * 128:(i + 1) * 128], rhs=w_out_bf[:, kc, cbk * 512:(cbk + 1) * 512],
                                                               start=(kc == 0), stop=(kc == 7)), reads=["mixT", "w_out_bf"], writes=["pZ%d" % cbk])
            rb = te % 2
            dma("sp", xr[rb][:], xe[te * 128:(te + 1) * 128, :], [], ["xr%d" % rb])
            A("dve", lambda e: e.tensor_tensor(out=xmd[rb][:], in0=pZ[:].rearrange("p a b -> p (a b)"), in1=xr[rb][:], op=ALU.add),
              reads=["pZ0", "pZ1", "xr%d" % rb], writes=["xmd%d" % rb])
            dma("pool", xmid[tl * 128:(tl + 1) * 128, :], xmd[rb][:], ["xmd%d" % rb], ["xmid"], dkey="st_xmd%d" % rb)

            def norm2_a():
                pb_ = norm_and_transpose(xmd[rb][:], "xmd%d" % rb, te % 8, xn2T[rb], ["xn2T%d" % rb], slice(0, 128), xn2[rb], "xn2%d" % rb,
                                         store_to=xn2s[tl * 128:(tl + 1) * 128, :], scale_eng="act", defer=True)

                def part_b():
                    pb_()
                    for kc in range(8):
                        A("pe", lambda e, kc=kc: e.matmul(pC[:, 0:36], lhsT=xn2T[rb][:, kc, :], rhs=wr_bf[:, kc, :], start=(kc == 0), stop=(kc == 7)),
                          reads=["xn2T%d" % rb, "wr_bf"], writes=["B3"])
                    A("dve", lambda e: e.tensor_tensor(out=logits[:, tl, :], in0=pC[:, 0:36], in1=biasR[:], op=ALU.add),
                      reads=["B3", "biasR"], writes=["logits"])
                return part_b
            return norm2_a

        def tail_copy(ub, g):
            A("pool", lambda e: e.tensor_copy(out=uT[ub][g][:, 0:16], in_=uT[1 - ub][g][:, 512:528]),
              reads=["uT%d%d" % (1 - ub, g)], writes=["uT%d%d" % (ub, g)])

        def norm_tile(s, i, defer=False):
            te = 4 * s + i
            xi = te % 2
            dma("sp", xin[xi][:], xe[te * 128:(te + 1) * 128, :], [], ["xin%d" % xi])
            return norm_and_transpose(xin[xi][:], "xin%d" % xi, te % 8, hT_, ["hT"], slice(i * 128, (i + 1) * 128),
                                      xn[te % 2], "xn%d" % (te % 2), defer=defer)

        q_norm2 = []
        q_b = []

        def do_st(s):
            halo_st = (4 * s) < NKV + NFH
            full_st = (4 * s) >= NKV
            first_main = (4 * s) == NKV + NFH
            if s == 0:
                for i in range(4):
                    norm_tile(0, i)
            ub = s % 2
            if s > 0:
                for g in range(4):
                    tail_copy(ub, g)
            slot0 = (4 * s) % RT
            pend2 = []
            pool_b = []
            for oc in range(12):
                if 4 <= oc < 8 and not full_st:
                    continue
                p2 = in_chunk(s, oc, ub, slot0, halo_st, full_st)
                if oc == 3 and full_st:
                    for g in range(4):
                        pool_b.append(pool_group(g, ub, first_main))
                if len(pend2) > 1:
                    pend2.pop(0)()
                if p2 is not None:
                    pend2.append(p2)
            v_tile(s, 0, halo_st)
            if pend2:
                pend2.pop(0)()
            v_tile(s, 1, halo_st)
            if pend2:
                pend2.pop(0)()
            for i in range(2, 4):
                v_tile(s, i, halo_st)
            for i in range(4):
                nb = norm_tile(s + 1, i, defer=True) if s + 1 < NST else None
                if full_st:
                    if i == 0:
                        for pb2 in pool_b:
                            pb2()
                    attention_tile(s, i)
                    if q_norm2:
                        q_b.append(q_norm2.pop(0)())
                    if len(q_b) > 1:
                        q_b.pop(0)()
                if nb is not None:
                    nb()
                if full_st:
                    q_norm2.append(post_attention(s, i))
            if s == NST - 1:
                while q_norm2:
                    q_b.append(q_norm2.pop(0)())
                while q_b:
                    q_b.pop(0)()

        for s in range(NST):
            do_st(s)
        p.barrier()

        cf = Carver(arf, AF_WORDS)
        cb_ = Carver(arb, AB_WORDS)
        NL = NTL
        R1 = cf.take(NL, 32)
        R2 = cf.take(NL, 32)
        R3 = cf.take(NL, 32)
        R4 = cf.take(NL, 32)
        sm = [cf.take(NL) for _ in range(6)]
        cntb = [cf.take(32) for _ in range(2)]
        startb = cf.take(32)
        widf = cf.take(NEXP, CT)
        ybuf = [cf.take(D) for _ in range(2)]
        sgb = [cf.take(CAP) for _ in range(2)]
        xmb = [cf.take(D) for _ in range(2)]
        y1b = [cf.take(D) for _ in range(2)]
        y2b_ = cf.take(D)
        y2b = [y2b_, y2b_]
        Abf = cb_.take(NL, 32)
        xtl = [cb_.take(D) for _ in range(4)]
        xw = [cb_.take(CT, D) for _ in range(2)]
        xsT = cb_.take(8, CAP)
        actT = cb_.take(4, CAP)
        Wg = [cb_.take(8, 512) for _ in range(2)]
        Wu = [cb_.take(8, 512) for _ in range(2)]
        Wd = [cb_.take(4, D) for _ in range(2)]

        WGK = [["Wg%d_%d" % (b, kc) for kc in range(8)] for b in range(2)]
        WUK = [["Wu%d_%d" % (b, kc) for kc in range(8)] for b in range(2)]
        WDK = [["Wd%d_%d" % (b, jc) for jc in range(4)] for b in range(2)]

        def load_w(e_):
            b = e_ % 2
            dma("pool", Wg[b][:].rearrange("p k n -> p (k n)"), wg[e_ * 128:(e_ + 1) * 128, :], [], WGK[b], dkey="Wg%d" % b)
            dma("pool", Wu[b][:].rearrange("p k n -> p (k n)"), wu[e_ * 128:(e_ + 1) * 128, :], [], WUK[b], dkey="Wu%d" % b)
            dma("pool", Wd[b][:], wd[e_ * 512:(e_ + 1) * 512, :].rearrange("(k p) n -> p k n", p=128), [], WDK[b], dkey="Wd%d" % b)

        load_w(0)
        load_w(1)

        gl = logits[:, :, 0:4]
        el = logits[:, :, 4:36]
        V = lambda e: e
        gmax, gsum, m1, m2, dd, ee = sm
        gone = R1[:, :, 0:4]
        A("dve", lambda e: e.reduce_max(out=gmax[:], in_=gl, axis=AX.X), reads=["logits"], writes=["gmax"])
        A("dve", lambda e: e.tensor_tensor(out=gone, in0=gl, in1=gmax[:].unsqueeze(2).to_broadcast([128, NL, 4]), op=ALU.is_equal),
          reads=["logits", "gmax"], writes=["R1"])
        gex = R2[:, :, 0:4]
        A("dve", lambda e: e.tensor_tensor(out=gex, in0=gl, in1=gmax[:].unsqueeze(2).to_broadcast([128, NL, 4]), op=ALU.subtract),
          reads=["logits", "gmax"], writes=["R2"])
        A("act", lambda e: e.activation(out=gex, in_=gex, func=AF.Exp), reads=["R2"], writes=["R2"])
        A("dve", lambda e: e.reduce_sum(out=gsum[:], in_=gex, axis=AX.X), reads=["R2"], writes=["gsum"])
        A("dve", lambda e: e.reciprocal(out=gsum[:], in_=gsum[:]), reads=["gsum"], writes=["gsum"])
        BIG = 1.0e4
        A("dve", lambda e: e.tensor_scalar(out=gone, in0=gone, scalar1=BIG, scalar2=-BIG, op0=ALU.mult, op1=ALU.add), reads=["R1"], writes=["R1"])
        em = R3
        A("dve", lambda e: e.tensor_tensor(out=em[:].rearrange("p n (g j) -> p n g j", g=4), in0=el.rearrange("p n (g j) -> p n g j", g=4),
                                           in1=gone.unsqueeze(3).to_broadcast([128, NL, 4, 8]), op=ALU.add), reads=["logits", "R1"], writes=["R3"])
        A("dve", lambda e: e.reduce_max(out=m1[:], in_=em[:], axis=AX.X), reads=["R3"], writes=["m1"])
        oh1 = R1
        A("dve", lambda e: e.tensor_tensor(out=oh1[:], in0=em[:], in1=m1[:].unsqueeze(2).to_broadcast([128, NL, 32]), op=ALU.is_equal),
          reads=["R3", "m1"], writes=["R1"])
        em2 = R2
        A("dve", lambda e: e.scalar_tensor_tensor(out=em2[:], in0=oh1[:], scalar=-BIG, in1=em[:], op0=ALU.mult, op1=ALU.add),
          reads=["R1", "R3"], writes=["R2"])
        A("dve", lambda e: e.reduce_max(out=m2[:], in_=em2[:], axis=AX.X), reads=["R2"], writes=["m2"])
        oh2 = R3
        A("dve", lambda e: e.tensor_tensor(out=oh2[:], in0=em2[:], in1=m2[:].unsqueeze(2).to_broadcast([128, NL, 32]), op=ALU.is_equal),
          reads=["R2", "m2"], writes=["R3"])
        A("dve", lambda e: e.tensor_tensor(out=dd[:], in0=m2[:], in1=m1[:], op=ALU.subtract), reads=["m1", "m2"], writes=["dd"])
        A("act", lambda e: e.activation(out=ee[:], in_=dd[:], func=AF.Exp), reads=["dd"], writes=["ee"])
        A("dve", lambda e: e.tensor_scalar(out=dd[:], in0=ee[:], scalar1=1.0, scalar2=None, op0=ALU.add), reads=["ee"], writes=["dd"])
        A("dve", lambda e: e.reciprocal(out=dd[:], in_=dd[:]), reads=["dd"], writes=["dd"])
        A("dve", lambda e: e.tensor_tensor(out=ee[:], in0=ee[:], in1=dd[:], op=ALU.mult), reads=["ee", "dd"], writes=["ee"])
        A("dve", lambda e: e.tensor_tensor(out=w1g[:], in0=dd[:], in1=gsum[:], op=ALU.mult), reads=["dd", "gsum"], writes=["w1g"])
        A("dve", lambda e: e.tensor_tensor(out=w2g[:], in0=ee[:], in1=gsum[:], op=ALU.mult), reads=["ee", "gsum"], writes=["w2g"])
        Asum = R2
        A("dve", lambda e: e.tensor_tensor(out=Asum[:], in0=oh1[:], in1=oh2[:], op=ALU.add), reads=["R1", "R3"], writes=["R2"])
        if NFH > 0:
            A("dve", lambda e: e.tensor_scalar(out=Asum[:, 0:NFH, :], in0=Asum[:, 0:NFH, :], scalar1=hvt[:, 0:1], scalar2=None, op0=ALU.mult),
              reads=["R2", "hvt"], writes=["R2"])
        A("dve", lambda e: e.tensor_copy(out=Abf[:], in_=Asum[:]), reads=["R2"], writes=["Abf"])
        Af = Abf[:].rearrange("p n e -> p (n e)")
        ncol = NL * 32
        banks = [(pZ[:, 0, :], "pZ0"), (pZ[:, 1, :], "pZ1"), (pC[:], "B3"), (pV[:], "B7")]
        assert ncol <= 1536
        Rk = R4[:].rearrange("p n e -> p (n e)")
        Tt = R2[:].rearrange("p n e -> p (n e)")
        for (lhs, lk, dst, dk) in ((tri_bf, "tri_bf", Rk, "R4"), (ones_bf, "ones_bf", Tt, "R2")):
            for c0 in range(0, ncol, 512):
                cw = min(512, ncol - c0)
                A("pe", lambda e, lhs=lhs, c0=c0, cw=cw: e.matmul(pS[:, c0:c0 + cw], lhsT=lhs[:], rhs=Af[:, c0:c0 + cw], start=True, stop=True),
                  reads=[lk, "Abf"], writes=["B4", "B5", "B6"])
            A("dve", lambda e, dst=dst: e.tensor_copy(out=dst, in_=pS[:, 0:ncol]), reads=["B4", "B5", "B6"], writes=[dk])
        A("dve", lambda e: e.memset(cntb[0][:], 0.0), writes=["cnt"])
        for n in range(NL):
            if n > 0:
                A("dve", lambda e, n=n: e.tensor_tensor(out=R4[:, n, :], in0=R4[:, n, :], in1=cntb[0][:], op=ALU.add), reads=["R4", "cnt"], writes=["R4"])
            A("dve", lambda e, n=n: e.tensor_tensor(out=cntb[0][:], in0=cntb[0][:], in1=R2[:, n, :], op=ALU.add), reads=["R2", "cnt"], writes=["cnt"])
        A("dve", lambda e: e.memset(startb[:], 0.0), writes=["startb"])
        for j in range(1, 32):
            A("dve", lambda e, j=j: e.tensor_tensor(out=startb[:, j:j + 1], in0=startb[:, j - 1:j], in1=cntb[0][:, j - 1:j], op=ALU.add),
              reads=["startb", "cnt"], writes=["startb"])
        A("dve", lambda e: e.tensor_tensor(out=R4[:], in0=R4[:], in1=startb[:].unsqueeze(1).to_broadcast([128, NL, 32]), op=ALU.add),
          reads=["R4", "startb"], writes=["R4"])
        for ki, (oh, ohk, sl_i, slk, tmpk) in enumerate(((oh1, "R1", slot1, "slot1", "gmax"), (oh2, "R3", slot2, "slot2", "m1"))):
            tmp = gmax if tmpk == "gmax" else m1
            A("dve", lambda e, oh=oh: e.tensor_tensor(out=oh[:], in0=oh[:], in1=R4[:], op=ALU.mult), reads=[ohk, "R4"], writes=[ohk])
            A("dve", lambda e, oh=oh, tmp=tmp: e.reduce_sum(out=tmp[:], in_=oh[:], axis=AX.X), reads=[ohk], writes=[tmpk])
            if NFH > 0:
                A("dve", lambda e, tmp=tmp: e.tensor_scalar(out=tmp[:, 0:NFH], in0=tmp[:, 0:NFH], scalar1=hvt[:, 0:1], scalar2=None, op0=ALU.mult),
                  reads=[tmpk, "hvt"], writes=[tmpk])
                A("dve", lambda e, tmp=tmp, ki=ki: e.scalar_tensor_tensor(out=tmp[:, 0:NFH], in0=trt[:, ki * TRC: ki * TRC + NFH], scalar=nhvt[:, 0:1], in1=tmp[:, 0:NFH],
                                                                 op0=ALU.mult, op1=ALU.add), reads=[tmpk, "trt", "nhvt"], writes=[tmpk])
            A("dve", lambda e, tmp=tmp, sl_i=sl_i: e.tensor_copy(out=sl_i[:], in_=tmp[:]), reads=[tmpk], writes=[slk])
        A("dve", lambda e: e.tensor_tensor(out=widf[:], in0=startb[:].unsqueeze(2).to_broadcast([128, NEXP, CT]),
                                           in1=iott[:, 0:CT].unsqueeze(1).to_broadcast([128, NEXP, CT]), op=ALU.add),
          reads=["startb", "iott"], writes=["widf"])
        A("dve", lambda e: e.tensor_copy(out=widx[:], in_=widf[:]), reads=["widf"], writes=["widx"])

        if debug:
            dma("sp", dbg_logits, logits[:], ["logits"], ["dbg_logits"])
            dma("sp", dbg_w[:, 0, :], w1g[:], ["w1g"], ["dbg_w1"])
            dma("sp", dbg_w[:, 1, :], w2g[:], ["w2g"], ["dbg_w2"])
            dma("sp", dbg_slot[:, 0, :], slot1[:], ["slot1"], ["dbg_s1"])
            dma("sp", dbg_slot[:, 1, :], slot2[:], ["slot2"], ["dbg_s2"])
        xskeys = []
        for tl in range(NTL):
            b = tl % 4
            dma("sp", xtl[b][:], xn2s[tl * 128:(tl + 1) * 128, :], ["xn2s"], ["xtl%d" % b])
            for k_, (sl_i, slk) in enumerate(((slot1, "slot1"), (slot2, "slot2"))):
                key = "xs_%d_%d" % (tl, k_)
                xskeys.append(key)
                A("pool", lambda e, sl_i=sl_i, tl=tl, b=b: e.indirect_dma_start(
                    out=xs[:, :], out_offset=bass.IndirectOffsetOnAxis(ap=sl_i[:, tl:tl + 1], axis=0), in_=xtl[b][:], in_offset=None),
                  reads=["xtl%d" % b, slk], writes=[key], dma=True, dkey="sc_xtl%d" % b)

        NOW, NTHR = cfg.NOW, cfg.NTHR
        BIGI = 1.0e6
        cnt_ = cntb[0]
        assert NOW <= NL and NTHR <= NL
        gtm = R2[:, 0:NTHR, :].rearrange("p t e -> p (t e)").rearrange("p (e t) -> p e t", e=32)
        nov = cf.take(32)
        cum = cf.take(32)
        indw = R1[:, 0:NOW, :]
        tmpw = R3[:, 0:NOW, :]
        limv = cf.take(32)
        jbase = cf.take(32)
        wsc = [cf.take(NOW) for _ in range(5)]
        gidf = cf.take(NOW, CT)
        yidf = cf.take(NOW, CT)
        mskf = cf.take(NOW, CT)
        wgidf = cf.take(NOW, 8)
        wdidf = cf.take(NOW, 4)
        gidx = T("gidx", [128, NOW, CT], I32)
        yidx = T("yidx", [128, NOW, CT], I32)
        wgidx = T("wgidx", [128, NOW, 8], I32)
        wdidx = T("wdidx", [128, NOW, 4], I32)
        DV = lambda fn, r, w: A("dve", fn, reads=r, writes=w)
        DV(lambda e: e.tensor_tensor(out=gtm[:], in0=cnt_[:].unsqueeze(2).to_broadcast([128, 32, NTHR]),
                                     in1=thrt[:, 0:NTHR].unsqueeze(1).to_broadcast([128, 32, NTHR]), op=ALU.is_gt), ["cnt", "thrt"], ["R2"])
        DV(lambda e: e.reduce_sum(out=nov[:], in_=gtm[:], axis=AX.X), ["R2"], ["nov"])
        DV(lambda e: e.memset(cum[:], 0.0), [], ["cum"])
        for j in range(1, 32):
            DV(lambda e, j=j: e.tensor_tensor(out=cum[:, j:j + 1], in0=cum[:, j - 1:j], in1=nov[:, j - 1:j], op=ALU.add), ["cum", "nov"], ["cum"])
        wvb = wvt[:, 0:NOW].unsqueeze(2).to_broadcast([128, NOW, 32])
        DV(lambda e: e.tensor_tensor(out=indw[:], in0=cum[:].unsqueeze(1).to_broadcast([128, NOW, 32]), in1=wvb, op=ALU.is_le), ["cum", "wvt"], ["R1"])
        DV(lambda e: e.tensor_tensor(out=limv[:], in0=cum[:], in1=nov[:], op=ALU.add), ["cum", "nov"], ["limv"])
        DV(lambda e: e.tensor_tensor(out=tmpw[:], in0=limv[:].unsqueeze(1).to_broadcast([128, NOW, 32]), in1=wvb, op=ALU.is_gt), ["limv", "wvt"], ["R3"])
        DV(lambda e: e.tensor_tensor(out=indw[:], in0=indw[:], in1=tmpw[:], op=ALU.mult), ["R1", "R3"], ["R1"])
        vld, ew, ow, lw, tw = wsc
        DV(lambda e: e.reduce_sum(out=vld[:], in_=indw[:], axis=AX.X), ["R1"], ["vld"])
        DV(lambda e: e.tensor_tensor(out=tmpw[:], in0=indw[:], in1=evt[:].unsqueeze(1).to_broadcast([128, NOW, 32]), op=ALU.mult), ["R1", "evt"], ["R3"])
        DV(lambda e: e.reduce_sum(out=ew[:], in_=tmpw[:], axis=AX.X), ["R3"], ["ew"])
        DV(lambda e: e.tensor_scalar(out=jbase[:], in0=cum[:], scalar1=-float(CAP), scalar2=float(CAP), op0=ALU.mult, op1=ALU.add), ["cum"], ["jbase"])
        DV(lambda e: e.tensor_tensor(out=jbase[:], in0=jbase[:], in1=startb[:], op=ALU.add), ["jbase", "startb"], ["jbase"])
        DV(lambda e: e.tensor_tensor(out=tmpw[:], in0=indw[:], in1=jbase[:].unsqueeze(1).to_broadcast([128, NOW, 32]), op=ALU.mult), ["R1", "jbase"], ["R3"])
        DV(lambda e: e.reduce_sum(out=ow[:], in_=tmpw[:], axis=AX.X), ["R3"], ["ow"])
        DV(lambda e: e.scalar_tensor_tensor(out=ow[:], in0=wvt[:, 0:NOW], scalar=float(CAP), in1=ow[:], op0=ALU.mult, op1=ALU.add), ["ow", "wvt"], ["ow"])
        DV(lambda e: e.tensor_tensor(out=ow[:], in0=ow[:], in1=vld[:], op=ALU.mult), ["ow", "vld"], ["ow"])
        DV(lambda e: e.tensor_tensor(out=limv[:], in0=startb[:], in1=cnt_[:], op=ALU.add), ["startb", "cnt"], ["limv"])
        DV(lambda e: e.tensor_tensor(out=tmpw[:], in0=indw[:], in1=limv[:].unsqueeze(1).to_broadcast([128, NOW, 32]), op=ALU.mult), ["R1", "limv"], ["R3"])
        DV(lambda e: e.reduce_sum(out=lw[:], in_=tmpw[:], axis=AX.X), ["R3"], ["lw"])
        DV(lambda e: e.tensor_scalar(out=tw[:], in0=vld[:], scalar1=-BIGI, scalar2=BIGI, op0=ALU.mult, op1=ALU.add), ["vld"], ["tw"])
        DV(lambda e: e.tensor_tensor(out=gidf[:], in0=ow[:].unsqueeze(2).to_broadcast([128, NOW, CT]),
                                     in1=iott[:, 0:CT].unsqueeze(1).to_broadcast([128, NOW, CT]), op=ALU.add), ["ow", "iott"], ["gidf"])
        DV(lambda e: e.tensor_tensor(out=mskf[:], in0=gidf[:], in1=lw[:].unsqueeze(2).to_broadcast([128, NOW, CT]), op=ALU.is_lt), ["gidf", "lw"], ["mskf"])
        DV(lambda e: e.scalar_tensor_tensor(out=yidf[:], in0=gidf[:], scalar=-BIGI, in1=mskf[:], op0=ALU.add, op1=ALU.mult), ["gidf", "mskf"], ["yidf"])
        DV(lambda e: e.tensor_scalar(out=yidf[:], in0=yidf[:], scalar1=BIGI, scalar2=None, op0=ALU.add), ["yidf"], ["yidf"])
        DV(lambda e: e.tensor_tensor(out=gidf[:], in0=gidf[:], in1=tw[:].unsqueeze(2).to_broadcast([128, NOW, CT]), op=ALU.add), ["gidf", "tw"], ["gidf"])
        DV(lambda e: e.tensor_copy(out=gidx[:], in_=gidf[:]), ["gidf"], ["gidx"])
        DV(lambda e: e.tensor_copy(out=yidx[:], in_=yidf[:]), ["yidf"], ["yidx"])
        DV(lambda e: e.scalar_tensor_tensor(out=ew[:], in0=ew[:], scalar=1024.0, in1=tw[:], op0=ALU.mult, op1=ALU.add), ["ew", "tw"], ["ew"])
        DV(lambda e: e.tensor_tensor(out=wgidf[:], in0=ew[:].unsqueeze(2).to_broadcast([128, NOW, 8]),
                                     in1=iot8t[:].unsqueeze(1).to_broadcast([128, NOW, 8]), op=ALU.add), ["ew", "iot8t"], ["wgidf"])
        DV(lambda e: e.tensor_copy(out=wgidx[:], in_=wgidf[:]), ["wgidf"], ["wgidx"])
        DV(lambda e: e.scalar_tensor_tensor(out=lw[:], in0=ew[:], scalar=0.125, in1=tw[:], op0=ALU.mult, op1=ALU.add), ["ew", "tw"], ["lw"])
        DV(lambda e: e.tensor_scalar(out=lw[:], in0=lw[:], scalar1=iott[:, 0:1], scalar2=None, op0=ALU.add), ["lw", "iott"], ["lw"])
        DV(lambda e: e.tensor_copy(out=wgidx2[:, 0:NOW], in_=lw[:]), ["lw"], ["wgidx2"])
        DV(lambda e: e.scalar_tensor_tensor(out=ew[:], in0=ew[:], scalar=0.5, in1=tw[:], op0=ALU.mult, op1=ALU.add), ["ew", "tw"], ["ew"])
        DV(lambda e: e.tensor_tensor(out=wdidf[:], in0=ew[:].unsqueeze(2).to_broadcast([128, NOW, 4]),
                                     in1=iot8t[:, 0:4].unsqueeze(1).to_broadcast([128, NOW, 4]), op=ALU.add), ["ew", "iot8t"], ["wdidf"])
        DV(lambda e: e.tensor_copy(out=wdidx[:], in_=wdidf[:]), ["wdidf"], ["wdidx"])

        gbanks = [(pZ[:, 0, :], "pZ0"), (pZ[:, 1, :], "pZ1"), (pC[:], "B3"), (pV[:], "B7")]
        dbanks = [(pS[:, 512:1024], "B5"), (pS[:, 1024:1536], "B6")]
        pT2 = pS[:, 0:512].bitcast(BF16)
        tbanks = [(pT, "pT"), (pT2, "B4")]
        cnts = {"gi": 0, "di": 0, "ti": 0}
        NJOB = NEXP + NOW
        wg2, wu2, wd2 = wg, wu, wd

        bregs = memo.setdefault("__bregs", {})

        def breg(e, val):
            if val not in bregs:
                r = e.alloc_register("bc%d" % val)
                e.reg_mov(r, val)
                bregs[val] = r
            return bregs[val]

        def job_rows(k, j, for_y):
            if k < NEXP:
                return widx[:, k, j:j + 1]
            return (yidx if for_y else gidx)[:, k - NEXP, j:j + 1]

        def job_load_w(k):
            b = k % 2
            if k < NEXP:
                load_w(k)
                return
            w = k - NEXP
            og, ou, od = [], [], []
            og.append(A("pool", lambda e: e.indirect_dma_start(out=Wg[b][:].rearrange("p k n -> p (k n)"), out_offset=None, in_=wg2,
                                                               in_offset=bass.IndirectOffsetOnAxis(ap=wgidx2[:, w:w + 1], axis=0),
                                                               bounds_check=breg(e, NEXP * 128 - 1), oob_is_err=False),
                        reads=["wgidx2"], writes=WGK[b], dma=True, dkey="Wg%d" % b))
            ou.append(A("pool", lambda e: e.indirect_dma_start(out=Wu[b][:].rearrange("p k n -> p (k n)"), out_offset=None, in_=wu2,
                                                               in_offset=bass.IndirectOffsetOnAxis(ap=wgidx2[:, w:w + 1], axis=0),
                                                               bounds_check=breg(e, NEXP * 128 - 1), oob_is_err=False),
                        reads=["wgidx2"], writes=WUK[b], dma=True, dkey="Wu%d" % b))
            for jc in range(4):
                od.append(A("pool", lambda e, jc=jc: e.indirect_dma_start(out=Wd[b][:, jc, :], out_offset=None, in_=wd2,
                                                                          in_offset=bass.IndirectOffsetOnAxis(ap=wdidx[:, w, jc:jc + 1], axis=0),
                                                                          bounds_check=breg(e, NEXP * 512 - 1), oob_is_err=False),
                            reads=["wdidx"], writes=[WDK[b][jc]], dma=True, dkey="Wd%d" % b))
            for grp in (og, ou, od):
                for o_ in grp:
                    o_.tgt = grp[-1].tgt

        def job_gather(k):
            b = k % 2
            for j in range(CT):
                rows = job_rows(k, j, False)
                if k < NEXP:
                    A("pool", lambda e, j=j, rows=rows: e.indirect_dma_start(
                        out=xw[b][:, j, :], out_offset=None, in_=xs[:, :], in_offset=bass.IndirectOffsetOnAxis(ap=rows, axis=0)),
                      reads=xskeys + ["widx"], writes=["xw%d_%d" % (b, j)], dma=True)
                else:
                    A("pool", lambda e, j=j, rows=rows: e.indirect_dma_start(
                        out=xw[b][:, j, :], out_offset=None, in_=xs[:, :], in_offset=bass.IndirectOffsetOnAxis(ap=rows, axis=0),
                        bounds_check=breg(e, cfg.XSR - 1), oob_is_err=False),
                      reads=xskeys + ["gidx"], writes=["xw%d_%d" % (b, j)], dma=True)

        xsT2 = [xsT, cb_.take(8, CAP)]

        def job_T(k):
            b = k % 2
            xs_ = xsT2[k % 2]
            for kc in range(8):
                (tps, tk_) = tbanks[cnts["ti"] % 2]
                cnts["ti"] += 1
                for j in range(CT):
                    A("pe", lambda e, kc=kc, j=j, tps=tps: e.transpose(out=tps[:, j * 128:(j + 1) * 128],
                                                                       in_=xw[b][:, j, :].rearrange("s (p k) -> s k p", k=8)[:, kc, :], identity=ident[:]),
                      reads=["xw%d_%d" % (b, j), "ident"], writes=[tk_])
                A("act", lambda e, kc=kc, tps=tps: e.activation(out=xs_[:, kc, :], in_=tps[:, 0:CAP], func=AF.Identity,
                                                                scale=mul2a[:, kc:kc + 1], bias=modca[:, 0, kc:kc + 1]),
                  reads=[tk_, "mul2a", "modca"], writes=["xsT%d_%d" % (k % 2, kc)])

        def job_GU(k):
            b = k % 2
            xs_ = xsT2[k % 2]
            for jc in range(4):
                (gps, gk_) = gbanks[cnts["gi"] % 4]
                (ups, uk_) = gbanks[(cnts["gi"] + 1) % 4]
                cnts["gi"] += 2
                for kc in range(8):
                    A("pe", lambda e, gps=gps, kc=kc, jc=jc: e.matmul(gps[:, 0:CAP], lhsT=Wg[b][:, kc, jc * 128:(jc + 1) * 128], rhs=xs_[:, kc, :],
                                                                     start=(kc == 0), stop=(kc == 7)), reads=WGK[b] + ["xsT%d_%d" % (k % 2, kc)], writes=[gk_])
                for kc in range(8):
                    A("pe", lambda e, ups=ups, kc=kc, jc=jc: e.matmul(ups[:, 0:CAP], lhsT=Wu[b][:, kc, jc * 128:(jc + 1) * 128], rhs=xs_[:, kc, :],
                                                                     start=(kc == 0), stop=(kc == 7)), reads=WUK[b] + ["xsT%d_%d" % (k % 2, kc)], writes=[uk_])
                sb_ = jc % 2
                A("act", lambda e, gps=gps, sb_=sb_: e.activation(out=sgb[sb_][:], in_=gps[:, 0:CAP], func=AF.Silu), reads=[gk_], writes=["sg%d" % sb_])
                A("dve", lambda e, ups=ups, sb_=sb_, jc=jc: e.tensor_tensor(out=actT[:, jc, :], in0=ups[:, 0:CAP], in1=sgb[sb_][:], op=ALU.mult),
                  reads=[uk_, "sg%d" % sb_], writes=["actT"])

        def job_D(k):
            b = k % 2
            for j in range(CT):
                yb = (k * CT + j) % 2
                for cbk in range(2):
                    (dps, dk_) = dbanks[cnts["di"] % 2]
                    cnts["di"] += 1
                    for jc in range(4):
                        A("pe", lambda e, dps=dps, jc=jc, j=j, cbk=cbk: e.matmul(dps, lhsT=actT[:, jc, j * 128:(j + 1) * 128], rhs=Wd[b][:, jc, cbk * 512:(cbk + 1) * 512],
                                                                                start=(jc == 0), stop=(jc == 3)), reads=["actT"] + WDK[b], writes=[dk_])
                    A("dve", lambda e, dps=dps, cbk=cbk, yb=yb: e.tensor_tensor(out=ybuf[yb][:, cbk * 512:(cbk + 1) * 512], in0=dps, in1=g2bc[:, cbk * 512:(cbk + 1) * 512], op=ALU.mult),
                      reads=[dk_, "g2bc"], writes=["ybuf%d" % yb])
                rows = job_rows(k, j, True)
                if k < NEXP:
                    A("pool", lambda e, rows=rows, yb=yb: e.indirect_dma_start(
                        out=ys[:, :], out_offset=bass.IndirectOffsetOnAxis(ap=rows, axis=0), in_=ybuf[yb][:], in_offset=None),
                      reads=["ybuf%d" % yb, "widx"], writes=["ys"], dma=True, dkey="sc_ybuf%d" % yb)
                else:
                    A("pool", lambda e, rows=rows, yb=yb: e.indirect_dma_start(
                        out=ys[:, :], out_offset=bass.IndirectOffsetOnAxis(ap=rows, axis=0), in_=ybuf[yb][:], in_offset=None,
                        bounds_check=breg(e, cfg.XSR - 1), oob_is_err=False),
                      reads=["ybuf%d" % yb, "yidx"], writes=["ys"], dma=True, dkey="sc_ybuf%d" % yb)

        job_gather(0)
        job_gather(1)
        job_T(0)
        for k in range(NJOB):
            job_GU(k)
            if k + 1 < NJOB:
                job_T(k + 1)
            if k + 2 < NJOB:
                job_gather(k + 2)
            job_D(k)
            if k + 2 < NJOB:
                job_load_w(k + 2)

        for tl in range(NTL):
            b = tl % 2
            dma("sp", xmb[b][:], xmid[tl * 128:(tl + 1) * 128, :], ["xmid"], ["xmb%d" % b])
            A("pool", lambda e, tl=tl, b=b: e.indirect_dma_start(out=y1b[b][:], out_offset=None, in_=ys[:, :],
                                                                 in_offset=bass.IndirectOffsetOnAxis(ap=slot1[:, tl:tl + 1], axis=0)),
              reads=["ys", "slot1"], writes=["y1b%d" % b], dma=True)
            A("pool", lambda e, tl=tl, b=b: e.indirect_dma_start(out=y2b[b][:], out_offset=None, in_=ys[:, :],
                                                                 in_offset=bass.IndirectOffsetOnAxis(ap=slot2[:, tl:tl + 1], axis=0)),
              reads=["ys", "slot2"], writes=["y2b"], dma=True)
            A("dve", lambda e, tl=tl, b=b: e.scalar_tensor_tensor(out=xmb[b][:], in0=y1b[b][:], scalar=w1g[:, tl:tl + 1], in1=xmb[b][:], op0=ALU.mult, op1=ALU.add),
              reads=["y1b%d" % b, "w1g", "xmb%d" % b], writes=["xmb%d" % b])
            A("dve", lambda e, tl=tl, b=b: e.scalar_tensor_tensor(out=xmb[b][:], in0=y2b[b][:], scalar=w2g[:, tl:tl + 1], in1=xmb[b][:], op0=ALU.mult, op1=ALU.add),
              reads=["y2b", "w2g", "xmb%d" % b], writes=["xmb%d" % b])
            dma("sp", xo[tl * 128:(tl + 1) * 128, :], xmb[b][:], ["xmb%d" % b], ["xo"], dkey="st_xmb%d" % b)
        p.barrier()


def _colform(v):
    return np.ascontiguousarray(v.reshape(-1, 128).T).astype(np.float32)


def _const_tables(cfg):
    q = np.arange(128)[:, None]
    tabs_idx = np.zeros((128, 5, 128), np.int64)
    mask = np.zeros((128, 5, 128), np.float32)
    for t in range(5):
        k = np.arange(128)[None, :]
        rel = 128 * (4 - t) + q - k
        tabs_idx[:, t, :] = np.clip(rel, -128, 128) + 128
        qc = q // 64
        kc = 2 * (t - 4) + k // 64
        ok = (kc <= qc) & (kc >= qc - 8)
        mask[:, t, :] = ok
    tri = (np.arange(128)[:, None] < np.arange(128)[None, :]).astype(np.float32)
    iot = (np.arange(128)[:, None] + 128 * np.arange(4)[None, :]).astype(np.float32)
    trash = (cfg.TRASH + np.arange(128)[:, None] + 128 * np.arange(max(cfg.NFH, 1))[None, :]).astype(np.float32)
    return tabs_idx, mask, tri, iot, trash


def layer_inputs(cfg, l, xe, cb, first_half, P, li=0):
    tabs_idx, mask, tri, iot, trash = _const_tables(cfg)
    btab = np.ascontiguousarray(P["rel_bias"][:, tabs_idx].transpose(1, 0, 2, 3)).astype(np.float32)
    invc = np.zeros((128, 4, 16), np.float32)
    for g, w in enumerate((2, 4, 8, 16)):
        cnt = np.minimum(np.arange(16) + 1, w) if first_half else np.full(16, w)
        invc[:, g, :] = (1.0 / cnt.astype(np.float64)).astype(np.float32)[None, :]
    hvv = 0.0 if first_half else 1.0
    trash = (cfg.TRASH + np.arange(128)[:, None] + 128 * np.arange(TRC)[None, :]).astype(np.float32)
    trash = np.concatenate([trash, trash + cfg.NFH * 128], axis=1)
    m = {
        "xe": np.ascontiguousarray(xe, dtype=np.float32),
        "cT": _colform(cb),
        "ada_w": P["ada_w"][l], "ada_b": P["ada_b"][l][None, :],
        "n1c": _colform(P["norm1_g"][l]), "n2c": _colform(P["norm2_g"][l]),
        "n2a": np.ascontiguousarray(P["norm2_g"][l].reshape(128, 8)).astype(np.float32),
        "w_in": P["w_in"][l], "w_out": P["w_out"][l],
        "pool_w": np.ascontiguousarray(P["pool_w"][l].transpose(1, 0, 2)),
        "pscale": _colform(P["pool_scale"][l]),
        "gq": np.ascontiguousarray(np.tile(P["q_norm_g"][l], 2)[:, None]), "gk": np.ascontiguousarray(np.tile(P["k_norm_g"][l], 2)[:, None]),
        "btab": btab, "bmask": mask,
        "wr": np.ascontiguousarray(np.concatenate([P["router_group_w"][l], P["router_expert_w"][l]], axis=1)),
        "br": np.ascontiguousarray(np.tile(np.concatenate([P["router_group_b"][l], P["router_expert_b"][l]])[None, :], (128, 1))),
        "wg": P["moe_w_gate"][l].reshape(NEXP * 128, 8 * 512), "wu": P["moe_w_up"][l].reshape(NEXP * 128, 8 * 512), "wd": P["moe_w_down"][l].reshape(NEXP * 512, D),
        "hv": np.full((128, 1), hvv, np.float32), "nhv": np.full((128, 1), 1.0 - hvv, np.float32),
        "invc": invc, "tri": tri, "iot": iot, "trashi": trash,
        "thr": np.tile((cfg.CAP * (np.arange(16) + 1)).astype(np.float32)[None, :], (128, 1)),
        "wv": np.tile(np.arange(32, dtype=np.float32)[None, :], (128, 1)),
        "ev": np.tile(np.arange(32, dtype=np.float32)[None, :], (128, 1)),
        "iot8": (np.arange(128)[:, None] + 128 * np.arange(8)[None, :]).astype(np.float32),
    }
    return {(k + "_%d" % li if k in PERL else k): v for k, v in m.items()}


_NC_CACHE = {}


def kernel(**inputs):
    P = {k: np.asarray(v) for k, v in inputs.items()}
    x = P["x"]
    B, S, _ = x.shape
    cfg0 = Cfg(nkv=4, nfh=4, nm=32, cap=512)
    cfg1 = Cfg(nkv=4, nfh=0, nm=32, cap=512)
    if "nc" not in _NC_CACHE:
        _NC_CACHE["nc"] = build_program([cfg0, cfg1])
    nc = _NC_CACHE["nc"]
    half = S // 2
    in_maps = []
    for c in range(8):
        b, hf = c // 2, c % 2
        main = x[b, hf * half:(hf + 1) * half]
        halo = np.zeros((1024, D), np.float32) if hf == 0 else x[b, half - 1024:half]
        xe = np.concatenate([halo, main], axis=0)
        m = layer_inputs(cfg0, 0, xe, P["c"][b], hf == 0, P, li=0)
        m1 = layer_inputs(cfg1, 1, xe[:128], P["c"][b], hf == 0, P, li=1)
        m.update({k: v for k, v in m1.items() if k.endswith("_1")})
        in_maps.append(m)
    res = run_bass_kernel_spmd(nc, in_maps, core_ids=list(range(8)))
    out = np.empty_like(x)
    for c in range(8):
        b, hf = c // 2, c % 2
        out[b, hf * half:(hf + 1) * half] = res.results[c]["xo"]
    return out
```

```python
from contextlib import ExitStack

import numpy as np
import concourse.bass as bass
import concourse.mybir as mybir
from concourse.bass_utils import run_bass_kernel_spmd

F32 = mybir.dt.float32
BF16 = mybir.dt.bfloat16
I32 = mybir.dt.int32
AF = mybir.ActivationFunctionType
ALU = mybir.AluOpType
AX = mybir.AxisListType

ENGS = ("sp", "act", "dve", "pool", "pe")
PSUM_KEYS = frozenset(["pT", "pZ0", "pZ1", "B3", "B4", "B5", "B6", "B7"])
D = 1024
EPS = 1e-6
NEXP = 32
RT = 8


class Op:
    __slots__ = ("eng", "fn", "dma", "dkey", "deps", "sig", "idx", "tgt", "waits")

    def __init__(self, eng, fn, dma, dkey):
        self.eng = eng
        self.fn = fn
        self.dma = dma
        self.dkey = dkey
        self.deps = []
        self.sig = False
        self.idx = 0
        self.tgt = 0
        self.waits = []


class PB:
    def __init__(self, nc):
        self.nc = nc
        self.ops = []
        self.last_w = {}
        self.readers = {}
        self.dma_cnt = {}

    def add(self, eng, fn, reads=(), writes=(), dma=False, dkey=None):
        if dma and dkey is None:
            dkey = writes[0]
        op = Op(eng, fn, dma, dkey)
        deps = set()
        for k in reads:
            w = self.last_w.get(k)
            if w is not None:
                deps.add(w)
            if k in PSUM_KEYS:
                for r in self.readers.get(k, ()):
                    if r.eng != eng:
                        deps.add(r)
        for k in writes:
            w = self.last_w.get(k)
            if w is not None:
                deps.add(w)
            for r in self.readers.get(k, ()):
                deps.add(r)
        op.deps = list(deps)
        for k in reads:
            self.readers.setdefault(k, []).append(op)
        for k in writes:
            self.last_w[k] = op
            self.readers[k] = []
        if dma:
            self.dma_cnt[dkey] = self.dma_cnt.get(dkey, 0) + 1
            op.tgt = 16 * self.dma_cnt[dkey]
        self.ops.append(op)
        return op

    def barrier(self):
        allkeys = list(set(self.last_w.keys()) | set(self.readers.keys()))
        self.add("sp", lambda e: e.nop(), reads=[], writes=allkeys + ["__bar"])
        for eng in ENGS:
            self.add(eng, lambda e: e.nop(), reads=["__bar"], writes=["__bar_" + eng])
        self.last_w = {k: v for k, v in self.last_w.items() if k.startswith("__bar")}
        self.readers = {k: v for k, v in self.readers.items() if k.startswith("__bar")}

    def emit(self):
        nc = self.nc
        for op in self.ops:
            for d in op.deps:
                if not d.dma:
                    d.sig = True
        cnt = {e: 0 for e in ENGS}
        for op in self.ops:
            if not op.dma and op.sig:
                cnt[op.eng] += 1
                op.idx = cnt[op.eng]
        dkeys = sorted(self.dma_cnt.keys())
        with ExitStack() as st:
            esem = {e: st.enter_context(nc.semaphore("es_" + e)) for e in ENGS}
            dsem = {k: st.enter_context(nc.semaphore("ds%d" % i)) for i, k in enumerate(dkeys)}
            waited = {e: {} for e in ENGS}
            for op in self.ops:
                need = {}
                for d in op.deps:
                    if d.dma:
                        key, val = ("d", d.dkey), d.tgt
                    else:
                        if d.eng == op.eng and op.eng == "pe":
                            continue
                        key, val = ("e", d.eng), d.idx
                    if need.get(key, 0) < val:
                        need[key] = val
                w = waited[op.eng]
                for key, val in need.items():
                    if w.get(key, 0) < val:
                        w[key] = val
                        op.waits.append((dsem[key[1]] if key[0] == "d" else esem[key[1]], val))
            block = st.enter_context(nc.Block())

            def run(engname):
                def body(e):
                    for op in self.ops:
                        if op.eng != engname:
                            continue
                        for (s, v) in op.waits:
                            e.wait_ge(s, v)
                        ins = op.fn(e)
                        if op.dma:
                            ins.then_inc(dsem[op.dkey], 16)
                        elif op.sig:
                            ins.then_inc(esem[engname], 1)
                return body

            block.sync(run("sp"))
            block.scalar(run("act"))
            block.vector(run("dve"))
            block.gpsimd(run("pool"))
            block.tensor(run("pe"))


class Cfg:
    def __init__(self, nkv=4, nfh=0, nm=32, cap=512):
        self.NKV, self.NFH, self.NM, self.CAP = nkv, nfh, nm, cap
        self.NTE = nkv + nfh + nm
        self.NTL = nfh + nm
        self.NST = self.NTE // 4
        self.CT = cap // 128
        self.NTOK = self.NTL * 128
        self.TRASH = 2 * self.NTOK + cap
        self.XSR = self.TRASH + 2 * max(nfh, 1) * 128
        self.NOW = -(-2 * self.NTOK // cap)
        self.NTHR = -(-self.NTOK // cap)
        assert self.NTE % 4 == 0 and nkv % 4 == 0 and nfh % 4 == 0 and cap % 128 == 0


PERL = frozenset(["ada_w", "ada_b", "n1c", "n2c", "n2a", "w_in", "w_out", "pool_w", "pscale", "gq", "gk", "wr", "br", "wg", "wu", "wd"])
TRC = 4


class _Ctx:
    pass


def build_program(cfgs, debug=False):
    nc = bass.Bass("TRN2", target_bir_lowering=False)
    ctx = _Ctx()
    ctx.nc, ctx.memo, ctx.p, ctx.nl = nc, {}, PB(nc), len(cfgs)
    with ExitStack() as st:
        ctx.st = st
        for li, cfg in enumerate(cfgs):
            _emit_layer(ctx, li, cfg, debug)
        ctx.p.emit()
    return nc


def build_layer(cfg, debug=False):
    return build_program([cfg], debug)


def _emit_layer(ctx, li, cfg, debug=False):
    nc, st, memo = ctx.nc, ctx.st, ctx.memo
    last = li == ctx.nl - 1
    NTE, NTL, NST, NKV, NFH, CT, CAP = cfg.NTE, cfg.NTL, cfg.NST, cfg.NKV, cfg.NFH, cfg.CT, cfg.CAP

    def din(name, shape, dt=F32):
        nm = name + ("_%d" % li if name in PERL else "")
        if nm not in memo:
            memo[nm] = nc.dram_tensor(nm, list(shape), dt, kind="ExternalInput").ap()
        return memo[nm]

    def dscr(name, shape, dt, kind="Internal"):
        if name not in memo:
            memo[name] = nc.dram_tensor(name, list(shape), dt, kind=kind).ap()
        return memo[name]

    xe = din("xe", [NTE * 128, D]) if li == 0 else memo["x1_%d" % (li - 1)]
    cT = din("cT", [128, 8])
    ada_w = din("ada_w", [D, 6 * D])
    ada_b = din("ada_b", [1, 6 * D])
    n1c = din("n1c", [128, 8])
    n2c = din("n2c", [128, 8])
    n2a = din("n2a", [128, 8])
    w_in = din("w_in", [D, 2048])
    w_out = din("w_out", [D, D])
    pool_w = din("pool_w", [128, 4, 128])
    pscale = din("pscale", [128, 4])
    gq = din("gq", [128, 1])
    gk = din("gk", [128, 1])
    btab = din("btab", [128, 8, 5, 128])
    bmask = din("bmask", [128, 5, 128])
    wr = din("wr", [D, 36])
    br = din("br", [128, 36])
    wg = din("wg", [NEXP * 128, 8 * 512])
    wu = din("wu", [NEXP * 128, 8 * 512])
    wd = din("wd", [NEXP * 512, D])
    hv = din("hv", [128, 1])
    nhv = din("nhv", [128, 1])
    invc = din("invc", [128, 4, 16])
    tri = din("tri", [128, 128])
    iot = din("iot", [128, 4])
    trashi = din("trashi", [128, 2 * TRC])
    thr = din("thr", [128, 16])
    wv = din("wv", [128, 32])
    ev = din("ev", [128, 32])
    iot8 = din("iot8", [128, 8])
    if last:
        xo = nc.dram_tensor("xo", [NTL * 128, D], F32, kind="ExternalOutput").ap()
    else:
        xo = dscr("x1_%d" % li, [NTL * 128, D], F32)
    xmid = dscr("xmid", [NTL * 128, D], F32, kind="ExternalOutput" if debug else "Internal")
    if debug:
        dbg_logits = nc.dram_tensor("dbg_logits", [128, NTL, 36], F32, kind="ExternalOutput").ap()
        dbg_w = nc.dram_tensor("dbg_w", [128, 2, NTL], F32, kind="ExternalOutput").ap()
        dbg_slot = nc.dram_tensor("dbg_slot", [128, 2, NTL], I32, kind="ExternalOutput").ap()
    xn2s = dscr("xn2s", [NTL * 128, D], BF16)
    xs = dscr("xs", [cfg.XSR, D], BF16)
    ys = dscr("ys", [cfg.XSR, D], F32)

    if True:
        def T(name, shape, dt=F32):
            if name in memo:
                t, shp = memo[name]
                if list(shp) != list(shape):
                    assert len(shp) == len(shape) and shape[1] <= shp[1] and list(shp[2:]) == list(shape[2:]), (name, shp, shape)
                    return t[:, 0:shape[1]]
                return t
            t = st.enter_context(nc.sbuf_tensor(name, list(shape), dt))
            memo[name] = (t, list(shape))
            return t

        def PS(name, shape, dt=F32):
            if name not in memo:
                memo[name] = st.enter_context(nc.psum_tensor(name, list(shape), dt))
            return memo[name]

        ident = T("ident", [128, 128], BF16)
        identf = T("identf", [128, 128])
        ones_bf = T("ones_bf", [128, 128], BF16)
        blk1 = T("blk1", [128, 128], BF16)
        tri_bf = T("tri_bf", [128, 128], BF16)
        onesf = T("onesf", [1, 128])
        epsc = T("epsc", [128, 1])
        eps64 = T("eps64", [128, 1])
        cact = T("cact", [128, 8], BF16)
        ctf = T("ctf", [128, 8])
        n1t = T("n1t", [128, 8])
        n2t = T("n2t", [128, 8])
        n2at = T("n2at", [128, 8])
        modca = T("modca", [128, 2, 8])
        mul2a = T("mul2a", [128, 8])
        wgidx2 = T("wgidx2", [128, 32], I32)
        modc = T("modc", [128, 4, 8])
        mul1c = T("mul1c", [128, 8])
        mul2c = T("mul2c", [128, 8])
        add1b = T("add1b", [128, 8], BF16)
        g2bc = T("g2bc", [128, D])
        bzc = T("bzc", [128, 12])
        bzv = T("bzv", [128, 512])
        bzvm = T("bzvm", [128, 512])
        pw_bf = T("pw_bf", [128, 4, 128], BF16)
        psc = T("psc", [128, 4])
        gqk = T("gqk", [128, 1])
        gkt = T("gkt", [128, 1])
        BT = T("BT", [128, 8, 5, 128], BF16)
        wr_f = T("wr_f", [128, 8, 36])
        wr_bf = T("wr_bf", [128, 8, 36], BF16)
        wr_raw = T("wr_raw", [128, 8, 36], BF16)
        biasR = T("biasR", [128, 36])
        hvt = T("hvt", [128, 1])
        nhvt = T("nhvt", [128, 1])
        invct = T("invct", [128, 4, 16])
        iott = T("iott", [128, 4])
        trt = T("trt", [128, 2 * TRC])
        thrt = T("thrt", [128, 16])
        wvt = T("wvt", [128, 32])
        evt = T("evt", [128, 32])
        iot8t = T("iot8t", [128, 8])
        ssr = T("ssr", [128, 8])
        rst = T("rst", [128, 8])
        logits = T("logits", [128, NTL, 36])
        w1g = T("w1g", [128, NTL])
        w2g = T("w2g", [128, NTL])
        slot1 = T("slot1", [128, NTL], I32)
        slot2 = T("slot2", [128, NTL], I32)
        widx = T("widx", [128, NEXP, CT], I32)

        AF_WORDS = 15104
        AB_WORDS = 58432
        arf = T("arf", [128, AF_WORDS])
        arb = T("arb", [128, AB_WORDS], BF16)

        class Carver:
            def __init__(self, t, n):
                self.t, self.n, self.off = t, n, 0

            def take(self, *shape):
                n = int(np.prod(shape))
                a = self.t[:, self.off:self.off + n]
                self.off += n
                assert self.off <= self.n, (self.off, self.n)
                if len(shape) == 2:
                    return a.rearrange("p (a b) -> p a b", a=shape[0])
                if len(shape) == 3:
                    return a.rearrange("p (a b c) -> p a b c", a=shape[0], b=shape[1])
                return a

        pT = PS("pT", [128, 1024], BF16)
        pZ = PS("pZ", [128, 2, 512])
        pC = PS("B3", [128, 512])
        pS = PS("pS", [128, 1536])
        pV = PS("B7", [128, 512])

        p = ctx.p
        A = p.add

        def dma(eng, out, in_, reads, writes, dkey=None):
            return A(eng, lambda e: e.dma_start(out=out, in_=in_), reads=reads, writes=writes, dma=True, dkey=dkey)

        A("pool", lambda e: e.memset(identf[:], 0.0), writes=["identf"])
        A("pool", lambda e: e.affine_select(out=identf[:], in_=identf[:], pattern=[[-1, 128]], compare_op=ALU.not_equal,
                                            fill=1.0, base=0, channel_multiplier=1), reads=["identf"], writes=["identf"])
        A("dve", lambda e: e.tensor_copy(out=ident[:], in_=identf[:]), reads=["identf"], writes=["ident"])
        A("pool", lambda e: e.memset(ones_bf[:], 1.0), writes=["ones_bf"])
        A("pool", lambda e: e.memset(blk1[:], 0.0), writes=["blk1"])
        A("pool", lambda e: e.memset(blk1[0:64, 0:64], 1.0), reads=["blk1"], writes=["blk1"])
        A("pool", lambda e: e.memset(blk1[64:128, 64:128], 1.0), reads=["blk1"], writes=["blk1"])
        A("pool", lambda e: e.memset(onesf[:], 1.0), writes=["onesf"])
        A("pool", lambda e: e.memset(epsc[:], EPS), writes=["epsc"])
        A("pool", lambda e: e.memset(eps64[:], 64 * EPS), writes=["eps64"])
        for (dst, src, k) in ((ctf, cT, "ctf"), (n1t, n1c, "n1t"), (n2t, n2c, "n2t"), (n2at, n2a, "n2at"), (psc, pscale, "psc"), (gqk, gq, "gqk"),
                              (gkt, gk, "gkt"), (hvt, hv, "hvt"), (nhvt, nhv, "nhvt"), (invct, invc, "invct"),
                              (iott, iot, "iott"), (trt, trashi, "trt"), (thrt, thr, "thrt"), (wvt, wv, "wvt"), (evt, ev, "evt"), (iot8t, iot8, "iot8t"), (biasR, br, "biasR"), (identf, tri, "identf")):
            dma("sp", dst[:], src, [], [k])
        A("dve", lambda e: e.tensor_copy(out=tri_bf[:], in_=identf[:]), reads=["identf"], writes=["tri_bf"])
        A("dve", lambda e: e.tensor_mul(out=gqk[:], in0=gqk[:], in1=gkt[:]), reads=["gqk", "gkt"], writes=["gqk"])
        dma("pool", pw_bf[:], pool_w, [], ["pw_bf"])
        A("act", lambda e: e.activation(out=cact[:], in_=ctf[:], func=AF.Silu), reads=["ctf"], writes=["cact"])

        cf = Carver(arf, AF_WORDS)
        cb_ = Carver(arb, AB_WORDS)
        g1bc = cf.take(D)
        modrow = cf.take(6 * D)[0:1, :]
        adab = cf.take(6 * D)[0:1, :]
        stage = [cb_.take(8, 1536) for _ in range(2)]
        zf = cf.take(D)
        zb = cb_.take(D)
        A("pool", lambda e: e.memset(zf[:], 0.0), writes=["zf"])
        A("pool", lambda e: e.memset(zb[:], 0.0), writes=["zb"])
        for r0 in range(2 * cfg.NM * 128, cfg.TRASH, 128):
            dma("sp", xs[r0:r0 + 128, :], zb[:], ["zb"], ["xs_z%d" % r0], dkey="zinit")
        for r0 in range(cfg.TRASH, cfg.TRASH + 2 * NFH * 128, 128):
            dma("sp", ys[r0:r0 + 128, :], zf[:], ["zf"], ["ys_z%d" % r0], dkey="zinit")
        dma("sp", adab, ada_b, [], ["adab"])
        for g in range(4):
            sb = stage[g % 2]
            dma("pool", sb[:], ada_w[:, g * 1536:(g + 1) * 1536].rearrange("(k p) n -> p k n", p=128), [], ["stage%d" % (g % 2)])
            for cbk in range(3):
                col = g * 1536 + cbk * 512
                bank = cbk % 2
                for kc in range(8):
                    A("pe", lambda e, sb=sb, kc=kc, cbk=cbk, bank=bank: e.matmul(
                        pZ[0:1, bank, :], lhsT=cact[:, kc:kc + 1], rhs=sb[:, kc, cbk * 512:(cbk + 1) * 512],
                        start=(kc == 0), stop=(kc == 7)), reads=["cact", "stage%d" % (g % 2)], writes=["pZ%d" % bank])
                A("dve", lambda e, col=col, bank=bank: e.tensor_tensor(out=modrow[:, col:col + 512], in0=pZ[0:1, bank, :],
                                                                       in1=adab[:, col:col + 512], op=ALU.add),
                  reads=["pZ%d" % bank, "adab"], writes=["modrow"])
        for vi, base in enumerate((0, D, 3 * D, 4 * D)):
            for kc in range(8):
                A("pe", lambda e, vi=vi, base=base, kc=kc: e.matmul(
                    pC[:, vi * 8 + kc: vi * 8 + kc + 1], lhsT=modrow[:, base + kc * 128: base + (kc + 1) * 128],
                    rhs=onesf[:, 0:1], start=True, stop=True), reads=["modrow", "onesf"], writes=["B3"])
        A("dve", lambda e: e.tensor_copy(out=modc[:], in_=pC[:, 0:32].rearrange("p (a b) -> p a b", a=4)), reads=["B3"], writes=["modc"])
        A("dve", lambda e: e.scalar_tensor_tensor(out=mul1c[:], in0=modc[:, 1, :], scalar=1.0, in1=n1t[:], op0=ALU.add, op1=ALU.mult),
          reads=["modc", "n1t"], writes=["mul1c"])
        A("dve", lambda e: e.scalar_tensor_tensor(out=mul2c[:], in0=modc[:, 3, :], scalar=1.0, in1=n2t[:], op0=ALU.add, op1=ALU.mult),
          reads=["modc", "n2t"], writes=["mul2c"])
        A("dve", lambda e: e.tensor_copy(out=add1b[:], in_=modc[:, 0, :]), reads=["modc"], writes=["add1b"])
        for vi, base in enumerate((3 * D, 4 * D)):
            mview = modrow[:, base:base + D].rearrange("o (p k) -> o k p", k=8)
            for kc in range(8):
                A("pe", lambda e, vi=vi, kc=kc, mview=mview: e.matmul(
                    pV[:, vi * 8 + kc: vi * 8 + kc + 1], lhsT=mview[:, kc, :], rhs=onesf[:, 0:1], start=True, stop=True),
                  reads=["modrow", "onesf"], writes=["B7"])
        A("dve", lambda e: e.tensor_copy(out=modca[:], in_=pV[:, 0:16].rearrange("p (a b) -> p a b", a=2)), reads=["B7"], writes=["modca"])
        A("dve", lambda e: e.scalar_tensor_tensor(out=mul2a[:], in0=modca[:, 1, :], scalar=1.0, in1=n2at[:], op0=ALU.add, op1=ALU.mult),
          reads=["modca", "n2at"], writes=["mul2a"])
        for (dst, base, k) in ((g1bc, 2 * D, "g1bc"), (g2bc, 5 * D, "g2bc")):
            for hb in range(2):
                A("pe", lambda e, base=base, hb=hb: e.matmul(pZ[:, hb, :], lhsT=onesf[:, :], rhs=modrow[:, base + hb * 512: base + (hb + 1) * 512],
                                                            start=True, stop=True), reads=["modrow", "onesf"], writes=["pZ%d" % hb])
                A("dve", lambda e, dst=dst, hb=hb: e.tensor_copy(out=dst[:, hb * 512:(hb + 1) * 512], in_=pZ[:, hb, :]),
                  reads=["pZ%d" % hb], writes=[k])
        p.barrier()

        cf = Carver(arf, AF_WORDS)
        cb_ = Carver(arb, AB_WORDS)
        w_in_bf = cb_.take(8, 2048)
        w_out_bf = cb_.take(8, D)
        g1bc = cf.take(D)
        add2rep = cb_.take(8, 128)
        wst = [cf.take(2, 2048) for _ in range(2)]
        wraw = cb_.take(8, 2048)
        bzrow = cf.take(2048)[0:1, :]
        bzrow_b = cb_.take(512)[0:1, :]
        for pc in range(4):
            sbf = wst[pc % 2]
            dma("sp", sbf[:], w_in[pc * 256:(pc + 1) * 256, :].rearrange("(k p) n -> p k n", p=128), [], ["wst%d" % (pc % 2)])
            for kk in range(2):
                kc = pc * 2 + kk
                A("dve", lambda e, sbf=sbf, kk=kk, kc=kc: e.tensor_scalar(out=w_in_bf[:, kc, :], in0=sbf[:, kk, :], scalar1=mul1c[:, kc:kc + 1],
                                                                       scalar2=None, op0=ALU.mult),
                  reads=["wst%d" % (pc % 2), "mul1c"], writes=["w_in_bf"])
                A("act", lambda e, sbf=sbf, kk=kk, kc=kc: e.activation(out=wraw[:, kc, :], in_=sbf[:, kk, :], func=AF.Copy),
                  reads=["wst%d" % (pc % 2)], writes=["wraw"])
        for cbk in range(4):
            for kc in range(8):
                A("pe", lambda e, cbk=cbk, kc=kc: e.matmul(pZ[0:1, cbk % 2, :], lhsT=add1b[:, kc:kc + 1], rhs=wraw[:, kc, cbk * 512:(cbk + 1) * 512],
                                                          start=(kc == 0), stop=(kc == 7)), reads=["add1b", "wraw"], writes=["pZ%d" % (cbk % 2)])
            A("dve", lambda e, cbk=cbk: e.tensor_copy(out=bzrow[:, cbk * 512:(cbk + 1) * 512], in_=pZ[0:1, cbk % 2, :]),
              reads=["pZ%d" % (cbk % 2)], writes=["bzrow"])
        for oc in range(12):
            A("pe", lambda e, oc=oc: e.matmul(pC[:, oc:oc + 1], lhsT=bzrow[:, oc * 128:(oc + 1) * 128], rhs=onesf[:, 0:1], start=True, stop=True),
              reads=["bzrow", "onesf"], writes=["B3"])
        A("dve", lambda e: e.tensor_copy(out=bzc[:], in_=pC[:, 0:12]), reads=["B3"], writes=["bzc"])
        A("pe", lambda e: e.matmul(pZ[:, 0, :], lhsT=onesf[:, :], rhs=bzrow[:, 1536:2048], start=True, stop=True),
          reads=["bzrow", "onesf"], writes=["pZ0"])
        A("dve", lambda e: e.tensor_copy(out=bzv[:], in_=pZ[:, 0, :]), reads=["pZ0"], writes=["bzv"])
        A("dve", lambda e: e.tensor_scalar(out=bzvm[:], in0=bzv[:], scalar1=hvt[:, 0:1], scalar2=None, op0=ALU.mult),
          reads=["bzv", "hvt"], writes=["bzvm"])
        wost = [wst[0][:, :, 0:D], wst[1][:, :, 0:D]]
        for pc in range(4):
            sbf = wost[pc % 2]
            dma("sp", sbf[:], w_out[pc * 256:(pc + 1) * 256, :].rearrange("(k p) n -> p k n", p=128), [], ["wst%d" % (pc % 2)])
            for kk in range(2):
                kc = pc * 2 + kk
                A("dve", lambda e, sbf=sbf, kk=kk, kc=kc: e.tensor_tensor(out=w_out_bf[:, kc, :], in0=sbf[:, kk, :], in1=g1bc[:], op=ALU.mult),
                  reads=["wst%d" % (pc % 2), "g1bc"], writes=["w_out_bf"])
        btf = cf.take(5, 128)
        pen = cf.take(5, 128)
        mk = cf.take(5, 128)
        dma("sp", mk[:], bmask, [], ["mk"])
        A("dve", lambda e: e.tensor_scalar(out=pen[:], in0=mk[:], scalar1=3750.0, scalar2=-3750.0, op0=ALU.mult, op1=ALU.add),
          reads=["mk"], writes=["pen"])
        for h in range(8):
            dma("sp", btf[:], btab[:, h, :, :], [], ["btf"])
            A("dve", lambda e: e.tensor_tensor(out=btf[:], in0=btf[:], in1=mk[:], op=ALU.mult), reads=["btf", "mk"], writes=["btf"])
            A("dve", lambda e, h=h: e.scalar_tensor_tensor(out=BT[:, h, :, :], in0=btf[:], scalar=0.125, in1=pen[:], op0=ALU.mult, op1=ALU.add),
              reads=["btf", "pen"], writes=["BT"])
        dma("sp", wr_f[:], wr.rearrange("(k p) n -> p k n", p=128), [], ["wr_f"])
        A("dve", lambda e: e.tensor_copy(out=wr_raw[:], in_=wr_f[:]), reads=["wr_f"], writes=["wr_raw"])
        for kc in range(8):
            A("dve", lambda e, kc=kc: e.tensor_scalar(out=wr_bf[:, kc, :], in0=wr_f[:, kc, :], scalar1=mul2c[:, kc:kc + 1], scalar2=None, op0=ALU.mult),
              reads=["wr_f", "mul2c"], writes=["wr_bf"])
            A("dve", lambda e, kc=kc: e.tensor_copy(out=add2rep[:, kc, :], in_=modc[:, 2, kc:kc + 1].to_broadcast([128, 128])),
              reads=["modc"], writes=["add2rep"])
        for kc in range(8):
            A("pe", lambda e, kc=kc: e.matmul(pC[:, 0:36], lhsT=add2rep[:, kc, :], rhs=wr_raw[:, kc, :], start=(kc == 0), stop=(kc == 7)),
              reads=["add2rep", "wr_raw"], writes=["B3"])
        A("dve", lambda e: e.tensor_tensor(out=biasR[:], in0=pC[:, 0:36], in1=biasR[:], op=ALU.add), reads=["B3", "biasR"], writes=["biasR"])
        p.barrier()

        cf = Carver(arf, AF_WORDS)
        cb_ = Carver(arb, AB_WORDS)
        w_in_bf = cb_.take(8, 2048)
        w_out_bf = cb_.take(8, D)
        xin = [cf.take(D) for _ in range(2)]
        xr = [cf.take(D) for _ in range(2)]
        xmd = [cf.take(D) for _ in range(2)]
        qf = [cf.take(512) for _ in range(3)]
        rq = [cf.take(512) for _ in range(3)]
        uT = [[cf.take(528) for _ in range(4)] for _ in range(2)]
        ptmp = [cf.take(528) for _ in range(2)]
        rden = cf.take(2, 4)
        xn = [cb_.take(D) for _ in range(2)]
        hT_ = cb_.take(8, 512)
        hT = [hT_, hT_]
        kT = cb_.take(4, RT * 128)
        Vr = cb_.take(RT, 8 * 65).rearrange("p r (h d) -> p r h d", h=8)
        qTm = [cb_.take(4, 512) for _ in range(2)]
        sq = [cb_.take(512) for _ in range(3)]
        pTt = cb_.take(4, 512)
        mixT_ = cb_.take(8, 512)
        mixT = [mixT_, mixT_]
        PTb = [cb_.take(2, 640) for _ in range(2)]
        att = [cb_.take(512) for _ in range(2)]
        xn2 = [cb_.take(D) for _ in range(2)]
        xn2T = [cb_.take(8, 128) for _ in range(2)]

        for b in range(2):
            for g in range(4):
                A("pool", lambda e, b=b, g=g: e.memset(uT[b][g][:, 0:16], 0.0), writes=["uT%d%d" % (b, g)])
        A("pool", lambda e: e.memset(qTm[0][64:128, :, :], 0.0), writes=["qT"])
        A("pool", lambda e: e.memset(qTm[1][0:64, :, :], 0.0), writes=["qT"])

        SSOFF = (0, 640)
        PVR = ((pS, 1280), (pV, 0), (pV, 256))
        HG = ((0, 1, 2), (3, 4, 5), (6, 7))

        def norm_and_transpose(src, srckey, sl, dstT, dstTkeys, dstcols, xnbuf, xnkey, store_to=None, scale_eng="dve", defer=False):
            A("act", lambda e: e.activation(out=xnbuf[:], in_=src, func=AF.Square, accum_out=ssr[:, sl:sl + 1]),
              reads=[srckey], writes=[xnkey, "ssr%d" % sl])
            A("act", lambda e: e.activation(out=rst[:, sl:sl + 1], in_=ssr[:, sl:sl + 1], func=AF.Ln, scale=1.0 / D, bias=epsc[:]),
              reads=["ssr%d" % sl, "epsc"], writes=["rst%d" % sl])
            A("act", lambda e: e.activation(out=rst[:, sl:sl + 1], in_=rst[:, sl:sl + 1], func=AF.Exp, scale=-0.5),
              reads=["rst%d" % sl], writes=["rst%d" % sl])
            if scale_eng == "dve":
                A("dve", lambda e: e.tensor_scalar(out=xnbuf[:], in0=src, scalar1=rst[:, sl:sl + 1], scalar2=None, op0=ALU.mult),
                  reads=[srckey, "rst%d" % sl], writes=[xnkey])
            else:
                A("act", lambda e: e.activation(out=xnbuf[:], in_=src, func=AF.Copy, scale=rst[:, sl:sl + 1]),
                  reads=[srckey, "rst%d" % sl], writes=[xnkey])
            if store_to is not None:
                dma("pool", store_to, xnbuf[:], [xnkey], ["xn2s"], dkey="st_" + xnkey)

            def part_b():
                for kc in range(8):
                    A("pe", lambda e, kc=kc: e.transpose(out=pT[:, kc * 128:(kc + 1) * 128], in_=xnbuf[:, kc * 128:(kc + 1) * 128], identity=ident[:]),
                      reads=[xnkey, "ident"], writes=["pT"])
                A("dve", lambda e: e.tensor_copy(out=dstT[:, :, dstcols], in_=pT[:].rearrange("p (a b) -> p a b", a=8)),
                  reads=["pT"], writes=dstTkeys)
            if defer:
                return part_b
            part_b()

        ZB = [(pZ[:, 0, :], "pZ0"), (pZ[:, 1, :], "pZ1"), (pS[:, 0:512], "B4"), (pS[:, 512:1024], "B5"), (pS[:, 1024:1536], "B6")]
        SB = [(pC, "B3"), (pV, "B7")]
        zcnt = {"z": 0, "s": 0}

        def in_chunk(s, oc, ub, slot0, halo_st, full_st):
            zps, zk = ZB[zcnt["z"] % 5]
            zcnt["z"] += 1
            kslots = ["kT%d" % (slot0 + i) for i in range(4)]
            for kc in range(8):
                A("pe", lambda e, kc=kc: e.matmul(zps, lhsT=w_in_bf[:, kc, oc * 128:(oc + 1) * 128], rhs=hT_[:, kc, :],
                                                  start=(kc == 0), stop=(kc == 7)), reads=["w_in_bf", "hT"], writes=[zk])
            if oc < 4:
                g = oc
                if halo_st:
                    A("dve", lambda e: e.tensor_scalar(out=uT[ub][g][:, 16:528], in0=zps, scalar1=bzc[:, oc:oc + 1],
                                                       scalar2=hvt[:, 0:1], op0=ALU.add, op1=ALU.mult),
                      reads=[zk, "bzc", "hvt"], writes=["uT%d%d" % (ub, g)])
                else:
                    A("dve", lambda e: e.tensor_scalar(out=uT[ub][g][:, 16:528], in0=zps, scalar1=bzc[:, oc:oc + 1],
                                                       scalar2=None, op0=ALU.add),
                      reads=[zk, "bzc"], writes=["uT%d%d" % (ub, g)])
                return None
            isq = oc < 8
            c = (oc - 4) % 4
            tb = oc % 3
            A("dve", lambda e: e.tensor_scalar(out=qf[tb][:], in0=zps, scalar1=bzc[:, oc:oc + 1], scalar2=None, op0=ALU.add),
              reads=[zk, "bzc"], writes=["qf%d" % tb])
            A("act", lambda e: e.activation(out=sq[tb][:], in_=qf[tb][:], func=AF.Square),
              reads=["qf%d" % tb], writes=["sq%d" % tb])
            sps, sk = SB[zcnt["s"] % 2]
            zcnt["s"] += 1

            def part2():
                A("pe", lambda e: e.matmul(sps[:], lhsT=blk1[:], rhs=sq[tb][:], start=True, stop=True), reads=["blk1", "sq%d" % tb], writes=[sk])
                A("act", lambda e: e.activation(out=rq[tb][:], in_=sps[:], func=AF.Ln, bias=eps64[:]), reads=[sk, "eps64"], writes=["rq%d" % tb])
                A("act", lambda e: e.activation(out=rq[tb][:], in_=rq[tb][:], func=AF.Exp, scale=-0.5), reads=["rq%d" % tb], writes=["rq%d" % tb])
                if isq:
                    A("dve", lambda e: e.tensor_tensor(out=qTm[0][0:64, c, :], in0=qf[tb][0:64, :], in1=rq[tb][0:64, :], op=ALU.mult),
                      reads=["qf%d" % tb, "rq%d" % tb], writes=["qT"])
                    A("dve", lambda e: e.tensor_tensor(out=qTm[1][64:128, c, :], in0=qf[tb][64:128, :], in1=rq[tb][64:128, :], op=ALU.mult),
                      reads=["qf%d" % tb, "rq%d" % tb], writes=["qT"])
                else:
                    A("dve", lambda e: e.scalar_tensor_tensor(out=kT[:, c, slot0 * 128:(slot0 + 4) * 128], in0=qf[tb][:], scalar=gqk[:, 0:1],
                                                              in1=rq[tb][:], op0=ALU.mult, op1=ALU.mult),
                      reads=["qf%d" % tb, "rq%d" % tb, "gqk"], writes=kslots)
            return part2

        def v_tile(s, i, halo_st):
            te = 4 * s + i
            sl = te % RT
            zps, zk = ZB[zcnt["z"] % 5]
            zcnt["z"] += 1
            for kc in range(8):
                A("pe", lambda e, kc=kc: e.matmul(zps, lhsT=hT_[:, kc, i * 128:(i + 1) * 128], rhs=w_in_bf[:, kc, 1536:2048],
                                                  start=(kc == 0), stop=(kc == 7)), reads=["w_in_bf", "hT"], writes=[zk])
            zv = zps.rearrange("p (h d) -> p h d", h=8)
            if halo_st:
                A("dve", lambda e: e.scalar_tensor_tensor(out=Vr[:, sl, :, 0:64], in0=zv, scalar=hvt[:, 0:1],
                                                          in1=bzvm[:].rearrange("p (h d) -> p h d", h=8), op0=ALU.mult, op1=ALU.add),
                  reads=[zk, "hvt", "bzvm"], writes=["V%d" % sl])
                A("pool", lambda e: e.tensor_copy(out=Vr[:, sl, :, 64:65], in_=hvt[:, 0:1].unsqueeze(1).to_broadcast([128, 8, 1])),
                  reads=["hvt"], writes=["V%d" % sl])
            else:
                A("dve", lambda e: e.tensor_tensor(out=Vr[:, sl, :, 0:64], in0=zv, in1=bzv[:].rearrange("p (h d) -> p h d", h=8), op=ALU.add),
                  reads=[zk, "bzv"], writes=["V%d" % sl])
                A("pool", lambda e: e.memset(Vr[:, sl, :, 64:65], 1.0), writes=["V%d" % sl])

        def pool_group(g, ub, first_main):
            U = uT[ub][g]
            uk = "uT%d%d" % (ub, g)
            cur, curk = U, uk
            sh = 1
            for stp in range(g + 1):
                dstb = ptmp[stp % 2]
                dk = "ptmp%d" % (stp % 2)
                lo = 2 * sh - 1
                A("pool", lambda e, cur=cur, dstb=dstb, lo=lo, sh=sh: e.tensor_tensor(out=dstb[:, lo:528], in0=cur[:, lo:528], in1=cur[:, lo - sh:528 - sh], op=ALU.add),
                  reads=[curk], writes=[dk])
                cur, curk = dstb, dk
                sh *= 2
            w = 2 ** (g + 1)
            fin = cur
            A("dve", lambda e: e.scalar_tensor_tensor(out=pTt[:, g, :], in0=fin[:, 16:528], scalar=1.0 / w, in1=U[:, 16:528],
                                                      op0=ALU.mult, op1=ALU.subtract),
              reads=[curk, uk], writes=["pTt%d" % g])
            if first_main:
                A("pool", lambda e: e.tensor_tensor(out=fin[:, 0:16], in0=fin[:, 16:32], in1=invct[:, g, :], op=ALU.mult),
                  reads=[curk, "invct"], writes=[curk])
                A("pool", lambda e: e.tensor_tensor(out=pTt[:, g, 0:16], in0=fin[:, 0:16], in1=U[:, 16:32], op=ALU.subtract),
                  reads=[curk, uk], writes=["pTt%d" % g])
            def part_b():
                sps, sk = SB[g % 2]
                A("pe", lambda e: e.matmul(sps[:], lhsT=pw_bf[:, g, :], rhs=pTt[:, g, :], start=True, stop=True), reads=["pw_bf", "pTt%d" % g], writes=[sk])
                A("act", lambda e: e.activation(out=mixT_[:, g, :], in_=sps[:], func=AF.Copy, scale=psc[:, g:g + 1]),
                  reads=[sk, "psc"], writes=["mixT"])
            return part_b

        def attn_pair(te, i, pr, ab):
            c = pr
            pb2 = pr % 2
            PTp = PTb[pb2]
            ptk = "PT%d" % pb2
            for hh in range(2):
                pb = 64 * hh
                h = 2 * pr + hh
                for t in range(4):
                    ksl = (te - 4 + t) % RT
                    A("pe", lambda e, t=t, ksl=ksl, pb=pb, hh=hh: e.matmul(
                        pS[:, hh * 512 + t * 128: hh * 512 + (t + 1) * 128], lhsT=kT[:, c, ksl * 128:(ksl + 1) * 128],
                        rhs=qTm[hh][:, c, i * 128:(i + 1) * 128], start=True, stop=False),
                      reads=["kT%d" % ksl, "qT"], writes=["B%d" % (4 + hh)])
                    A("pe", lambda e, t=t, h=h, hh=hh: e.matmul(pS[:, hh * 512 + t * 128: hh * 512 + (t + 1) * 128], lhsT=BT[:, h, t, :], rhs=ident[:],
                                                                start=False, stop=True), reads=["BT", "ident"], writes=["B%d" % (4 + hh)])
            ksl4 = te % RT
            for hh in range(2):
                pb = 64 * hh
                h = 2 * pr + hh
                A("pe", lambda e, pb=pb, hh=hh: e.matmul(
                    pS[:, 1024 + hh * 128: 1024 + (hh + 1) * 128], lhsT=kT[:, c, ksl4 * 128:(ksl4 + 1) * 128],
                    rhs=qTm[hh][:, c, i * 128:(i + 1) * 128], start=True, stop=False),
                  reads=["kT%d" % ksl4, "qT"], writes=["B6"])
                A("pe", lambda e, h=h, hh=hh: e.matmul(pS[:, 1024 + hh * 128: 1024 + (hh + 1) * 128], lhsT=BT[:, h, 4, :], rhs=ident[:],
                                                       start=False, stop=True), reads=["BT", "ident"], writes=["B6"])
            for hh in range(2):
                A("act", lambda e, hh=hh: e.activation(out=PTp[:, hh, 0:512], in_=pS[:, hh * 512:(hh + 1) * 512], func=AF.Exp, scale=8.0),
                  reads=["B%d" % (4 + hh)], writes=[ptk])
            A("act", lambda e: e.activation(out=PTp[:, :, 512:640], in_=pS[:, 1024:1280].rearrange("p (a b) -> p a b", a=2), func=AF.Exp, scale=8.0),
              reads=["B6"], writes=[ptk])
            for hh in range(2):
                h = 2 * pr + hh
                pvt, pvk = (pV, "B7") if h < 4 else (pC, "B3")
                co = (h % 4) * 65
                for t in range(5):
                    ksl = (te - 4 + t) % RT
                    A("pe", lambda e, t=t, ksl=ksl, hh=hh, h=h, pvt=pvt, co=co: e.matmul(
                        pvt[:, co: co + 65], lhsT=PTp[:, hh, t * 128:(t + 1) * 128], rhs=Vr[:, ksl, h, :],
                        start=(t == 0), stop=(t == 4)), reads=[ptk, "V%d" % ksl], writes=[pvk])

        def attn_norm(hgi, ab):
            pvt, pvk = (pV, "B7") if hgi == 0 else (pC, "B3")
            pvv = pvt[:, 0:260].rearrange("p (h d) -> p h d", h=4)
            A("dve", lambda e: e.tensor_scalar(out=rden[:, hgi, :].unsqueeze(2), in0=pvv[:, :, 64:65], scalar1=1e-30, scalar2=None, op0=ALU.add),
              reads=[pvk], writes=["rden%d" % hgi])
            A("dve", lambda e: e.reciprocal(out=rden[:, hgi, :], in_=rden[:, hgi, :]),
              reads=["rden%d" % hgi], writes=["rden%d" % hgi])
            A("dve", lambda e: e.tensor_tensor(
                out=att[ab][:, hgi * 256:(hgi + 1) * 256].rearrange("p (h d) -> p h d", h=4), in0=pvv[:, :, 0:64],
                in1=rden[:, hgi, :].unsqueeze(2).to_broadcast([128, 4, 64]), op=ALU.mult),
              reads=[pvk, "rden%d" % hgi], writes=["att%d" % ab])

        def attention_tile(s, i):
            te = 4 * s + i
            ab = te % 2
            for pr in range(4):
                attn_pair(te, i, pr, ab)
                if pr % 2 == 1:
                    attn_norm(pr // 2, ab)

        def post_attention(s, i):
            te = 4 * s + i
            tl = te - NKV
            ab = te % 2
            for c in range(4):
                A("pe", lambda e, c=c: e.transpose(out=pT[:, c * 128:(c + 1) * 128], in_=att[ab][:, c * 128:(c + 1) * 128], identity=ident[:]),
                  reads=["att%d" % ab, "ident"], writes=["pT"])
            A("dve", lambda e: e.tensor_copy(out=mixT_[:, 4:8, i * 128:(i + 1) * 128], in_=pT[:, 0:512].rearrange("p (a b) -> p a b", a=4)),
              reads=["pT"], writes=["mixT"])
            for cbk in range(2):
                for kc in range(8):
                    A("pe", lambda e, cbk=cbk, kc=kc: e.matmul(pZ[:, cbk, :], lhsT=mixT_[:, kc, i * 128:(i + 1) * 128], rhs=w_out_bf[:, kc, cbk * 512:(cbk + 1) * 512],
                                                               start=(kc == 0), stop=(kc == 7)), reads=["mixT", "w_out_bf"], writes=["pZ%d" % cbk])
            rb = te % 2
            dma("sp", xr[rb][:], xe[te * 128:(te + 1) * 128, :], [], ["xr%d" % rb])
            A("dve", lambda e: e.tensor_tensor(out=xmd[rb][:], in0=pZ[:].rearrange("p a b -> p (a b)"), in1=xr[rb][:], op=ALU.add),
              reads=["pZ0", "pZ1", "xr%d" % rb], writes=["xmd%d" % rb])
            dma("pool", xmid[tl * 128:(tl + 1) * 128, :], xmd[rb][:], ["xmd%d" % rb], ["xmid"], dkey="st_xmd%d" % rb)

            def norm2_a():
                pb_ = norm_and_transpose(xmd[rb][:], "xmd%d" % rb, te % 8, xn2T[rb], ["xn2T%d" % rb], slice(0, 128), xn2[rb], "xn2%d" % rb,
                                         store_to=xn2s[tl * 128:(tl + 1) * 128, :], scale_eng="act", defer=True)

                def part_b():
                    pb_()
                    for kc in range(8):
                        A("pe", lambda e, kc=kc: e.matmul(pC[:, 0:36], lhsT=xn2T[rb][:, kc, :], rhs=wr_bf[:, kc, :], start=(kc == 0), stop=(kc == 7)),
                          reads=["xn2T%d" % rb, "wr_bf"], writes=["B3"])
                    A("dve", lambda e: e.tensor_tensor(out=logits[:, tl, :], in0=pC[:, 0:36], in1=biasR[:], op=ALU.add),
                      reads=["B3", "biasR"], writes=["logits"])
                return part_b
            return norm2_a

        def tail_copy(ub, g):
            A("pool", lambda e: e.tensor_copy(out=uT[ub][g][:, 0:16], in_=uT[1 - ub][g][:, 512:528]),
              reads=["uT%d%d" % (1 - ub, g)], writes=["uT%d%d" % (ub, g)])

        def norm_tile(s, i, defer=False):
            te = 4 * s + i
            xi = te % 2
            dma("sp", xin[xi][:], xe[te * 128:(te + 1) * 128, :], [], ["xin%d" % xi])
            return norm_and_transpose(xin[xi][:], "xin%d" % xi, te % 8, hT_, ["hT"], slice(i * 128, (i + 1) * 128),
                                      xn[te % 2], "xn%d" % (te % 2), defer=defer)

        q_norm2 = []
        q_b = []

        def do_st(s):
            halo_st = (4 * s) < NKV + NFH
            full_st = (4 * s) >= NKV
            first_main = (4 * s) == NKV + NFH
            if s == 0:
                for i in range(4):
                    norm_tile(0, i)
            ub = s % 2
            if s > 0:
                for g in range(4):
                    tail_copy(ub, g)
            slot0 = (4 * s) % RT
            pend2 = []
            pool_b = []
            for oc in range(12):
                if 4 <= oc < 8 and not full_st:
                    continue
                p2 = in_chunk(s, oc, ub, slot0, halo_st, full_st)
                if oc == 3 and full_st:
                    for g in range(4):
                        pool_b.append(pool_group(g, ub, first_main))
                if len(pend2) > 1:
                    pend2.pop(0)()
                if p2 is not None:
                    pend2.append(p2)
            v_tile(s, 0, halo_st)
            if pend2:
                pend2.pop(0)()
            v_tile(s, 1, halo_st)
            if pend2:
                pend2.pop(0)()
            for i in range(2, 4):
                v_tile(s, i, halo_st)
            for i in range(4):
                nb = norm_tile(s + 1, i, defer=True) if s + 1 < NST else None
                if full_st:
                    if i == 0:
                        for pb2 in pool_b:
                            pb2()
                    attention_tile(s, i)
                    if q_norm2:
                        q_b.append(q_norm2.pop(0)())
                    if len(q_b) > 1:
                        q_b.pop(0)()
                if nb is not None:
                    nb()
                if full_st:
                    q_norm2.append(post_attention(s, i))
            if s == NST - 1:
                while q_norm2:
                    q_b.append(q_norm2.pop(0)())
                while q_b:
                    q_b.pop(0)()

        for s in range(NST):
            do_st(s)
        p.barrier()

        cf = Carver(arf, AF_WORDS)
        cb_ = Carver(arb, AB_WORDS)
        NL = NTL
        R1 = cf.take(NL, 32)
        R2 = cf.take(NL, 32)
        R3 = cf.take(NL, 32)
        R4 = cf.take(NL, 32)
        sm = [cf.take(NL) for _ in range(6)]
        cntb = [cf.take(32) for _ in range(2)]
        startb = cf.take(32)
        widf = cf.take(NEXP, CT)
        ybuf = [cf.take(D) for _ in range(2)]
        sgb = [cf.take(CAP) for _ in range(2)]
        xmb = [cf.take(D) for _ in range(2)]
        y1b = [cf.take(D) for _ in range(2)]
        y2b = [cf.take(D) for _ in range(2)]
        Abf = cb_.take(NL, 32)
        xtl = [cb_.take(D) for _ in range(4)]
        xw = [cb_.take(CT, D) for _ in range(2)]
        xsT = cb_.take(8, CAP)
        actT = cb_.take(4, CAP)
        Wg = [cb_.take(8, 512) for _ in range(2)]
        Wu = [cb_.take(8, 512) for _ in range(2)]
        Wd = [cb_.take(4, D) for _ in range(2)]

        WGK = [["Wg%d_%d" % (b, kc) for kc in range(8)] for b in range(2)]
        WUK = [["Wu%d_%d" % (b, kc) for kc in range(8)] for b in range(2)]
        WDK = [["Wd%d_%d" % (b, jc) for jc in range(4)] for b in range(2)]

        def load_w(e_):
            b = e_ % 2
            dma("pool", Wg[b][:].rearrange("p k n -> p (k n)"), wg[e_ * 128:(e_ + 1) * 128, :], [], WGK[b], dkey="Wg%d" % b)
            dma("pool", Wu[b][:].rearrange("p k n -> p (k n)"), wu[e_ * 128:(e_ + 1) * 128, :], [], WUK[b], dkey="Wu%d" % b)
            dma("pool", Wd[b][:], wd[e_ * 512:(e_ + 1) * 512, :].rearrange("(k p) n -> p k n", p=128), [], WDK[b], dkey="Wd%d" % b)

        load_w(0)
        load_w(1)

        gl = logits[:, :, 0:4]
        el = logits[:, :, 4:36]
        V = lambda e: e
        gmax, gsum, m1, m2, dd, ee = sm
        gone = R1[:, :, 0:4]
        A("dve", lambda e: e.reduce_max(out=gmax[:], in_=gl, axis=AX.X), reads=["logits"], writes=["gmax"])
        A("dve", lambda e: e.tensor_tensor(out=gone, in0=gl, in1=gmax[:].unsqueeze(2).to_broadcast([128, NL, 4]), op=ALU.is_equal),
          reads=["logits", "gmax"], writes=["R1"])
        gex = R2[:, :, 0:4]
        A("dve", lambda e: e.tensor_tensor(out=gex, in0=gl, in1=gmax[:].unsqueeze(2).to_broadcast([128, NL, 4]), op=ALU.subtract),
          reads=["logits", "gmax"], writes=["R2"])
        A("act", lambda e: e.activation(out=gex, in_=gex, func=AF.Exp), reads=["R2"], writes=["R2"])
        A("dve", lambda e: e.reduce_sum(out=gsum[:], in_=gex, axis=AX.X), reads=["R2"], writes=["gsum"])
        A("dve", lambda e: e.reciprocal(out=gsum[:], in_=gsum[:]), reads=["gsum"], writes=["gsum"])
        BIG = 1.0e4
        A("dve", lambda e: e.tensor_scalar(out=gone, in0=gone, scalar1=BIG, scalar2=-BIG, op0=ALU.mult, op1=ALU.add), reads=["R1"], writes=["R1"])
        em = R3
        A("dve", lambda e: e.tensor_tensor(out=em[:].rearrange("p n (g j) -> p n g j", g=4), in0=el.rearrange("p n (g j) -> p n g j", g=4),
                                           in1=gone.unsqueeze(3).to_broadcast([128, NL, 4, 8]), op=ALU.add), reads=["logits", "R1"], writes=["R3"])
        A("dve", lambda e: e.reduce_max(out=m1[:], in_=em[:], axis=AX.X), reads=["R3"], writes=["m1"])
        oh1 = R1
        A("dve", lambda e: e.tensor_tensor(out=oh1[:], in0=em[:], in1=m1[:].unsqueeze(2).to_broadcast([128, NL, 32]), op=ALU.is_equal),
          reads=["R3", "m1"], writes=["R1"])
        em2 = R2
        A("dve", lambda e: e.scalar_tensor_tensor(out=em2[:], in0=oh1[:], scalar=-BIG, in1=em[:], op0=ALU.mult, op1=ALU.add),
          reads=["R1", "R3"], writes=["R2"])
        A("dve", lambda e: e.reduce_max(out=m2[:], in_=em2[:], axis=AX.X), reads=["R2"], writes=["m2"])
        oh2 = R3
        A("dve", lambda e: e.tensor_tensor(out=oh2[:], in0=em2[:], in1=m2[:].unsqueeze(2).to_broadcast([128, NL, 32]), op=ALU.is_equal),
          reads=["R2", "m2"], writes=["R3"])
        A("dve", lambda e: e.tensor_tensor(out=dd[:], in0=m2[:], in1=m1[:], op=ALU.subtract), reads=["m1", "m2"], writes=["dd"])
        A("act", lambda e: e.activation(out=ee[:], in_=dd[:], func=AF.Exp), reads=["dd"], writes=["ee"])
        A("dve", lambda e: e.tensor_scalar(out=dd[:], in0=ee[:], scalar1=1.0, scalar2=None, op0=ALU.add), reads=["ee"], writes=["dd"])
        A("dve", lambda e: e.reciprocal(out=dd[:], in_=dd[:]), reads=["dd"], writes=["dd"])
        A("dve", lambda e: e.tensor_tensor(out=ee[:], in0=ee[:], in1=dd[:], op=ALU.mult), reads=["ee", "dd"], writes=["ee"])
        A("dve", lambda e: e.tensor_tensor(out=w1g[:], in0=dd[:], in1=gsum[:], op=ALU.mult), reads=["dd", "gsum"], writes=["w1g"])
        A("dve", lambda e: e.tensor_tensor(out=w2g[:], in0=ee[:], in1=gsum[:], op=ALU.mult), reads=["ee", "gsum"], writes=["w2g"])
        Asum = R2
        A("dve", lambda e: e.tensor_tensor(out=Asum[:], in0=oh1[:], in1=oh2[:], op=ALU.add), reads=["R1", "R3"], writes=["R2"])
        if NFH > 0:
            A("dve", lambda e: e.tensor_scalar(out=Asum[:, 0:NFH, :], in0=Asum[:, 0:NFH, :], scalar1=hvt[:, 0:1], scalar2=None, op0=ALU.mult),
              reads=["R2", "hvt"], writes=["R2"])
        A("dve", lambda e: e.tensor_copy(out=Abf[:], in_=Asum[:]), reads=["R2"], writes=["Abf"])
        Af = Abf[:].rearrange("p n e -> p (n e)")
        ncol = NL * 32
        banks = [(pZ[:, 0, :], "pZ0"), (pZ[:, 1, :], "pZ1"), (pC[:], "B3"), (pV[:], "B7")]
        assert ncol <= 1536
        Rk = R4[:].rearrange("p n e -> p (n e)")
        Tt = R2[:].rearrange("p n e -> p (n e)")
        for (lhs, lk, dst, dk) in ((tri_bf, "tri_bf", Rk, "R4"), (ones_bf, "ones_bf", Tt, "R2")):
            for c0 in range(0, ncol, 512):
                cw = min(512, ncol - c0)
                A("pe", lambda e, lhs=lhs, c0=c0, cw=cw: e.matmul(pS[:, c0:c0 + cw], lhsT=lhs[:], rhs=Af[:, c0:c0 + cw], start=True, stop=True),
                  reads=[lk, "Abf"], writes=["B4", "B5", "B6"])
            A("dve", lambda e, dst=dst: e.tensor_copy(out=dst, in_=pS[:, 0:ncol]), reads=["B4", "B5", "B6"], writes=[dk])
        A("dve", lambda e: e.memset(cntb[0][:], 0.0), writes=["cnt"])
        for n in range(NL):
            if n > 0:
                A("dve", lambda e, n=n: e.tensor_tensor(out=R4[:, n, :], in0=R4[:, n, :], in1=cntb[0][:], op=ALU.add), reads=["R4", "cnt"], writes=["R4"])
            A("dve", lambda e, n=n: e.tensor_tensor(out=cntb[0][:], in0=cntb[0][:], in1=R2[:, n, :], op=ALU.add), reads=["R2", "cnt"], writes=["cnt"])
        A("dve", lambda e: e.memset(startb[:], 0.0), writes=["startb"])
        for j in range(1, 32):
            A("dve", lambda e, j=j: e.tensor_tensor(out=startb[:, j:j + 1], in0=startb[:, j - 1:j], in1=cntb[0][:, j - 1:j], op=ALU.add),
              reads=["startb", "cnt"], writes=["startb"])
        A("dve", lambda e: e.tensor_tensor(out=R4[:], in0=R4[:], in1=startb[:].unsqueeze(1).to_broadcast([128, NL, 32]), op=ALU.add),
          reads=["R4", "startb"], writes=["R4"])
        for ki, (oh, ohk, sl_i, slk, tmpk) in enumerate(((oh1, "R1", slot1, "slot1", "gmax"), (oh2, "R3", slot2, "slot2", "m1"))):
            tmp = gmax if tmpk == "gmax" else m1
            A("dve", lambda e, oh=oh: e.tensor_tensor(out=oh[:], in0=oh[:], in1=R4[:], op=ALU.mult), reads=[ohk, "R4"], writes=[ohk])
            A("dve", lambda e, oh=oh, tmp=tmp: e.reduce_sum(out=tmp[:], in_=oh[:], axis=AX.X), reads=[ohk], writes=[tmpk])
            if NFH > 0:
                A("dve", lambda e, tmp=tmp: e.tensor_scalar(out=tmp[:, 0:NFH], in0=tmp[:, 0:NFH], scalar1=hvt[:, 0:1], scalar2=None, op0=ALU.mult),
                  reads=[tmpk, "hvt"], writes=[tmpk])
                A("dve", lambda e, tmp=tmp, ki=ki: e.scalar_tensor_tensor(out=tmp[:, 0:NFH], in0=trt[:, ki * TRC: ki * TRC + NFH], scalar=nhvt[:, 0:1], in1=tmp[:, 0:NFH],
                                                                 op0=ALU.mult, op1=ALU.add), reads=[tmpk, "trt", "nhvt"], writes=[tmpk])
            A("dve", lambda e, tmp=tmp, sl_i=sl_i: e.tensor_copy(out=sl_i[:], in_=tmp[:]), reads=[tmpk], writes=[slk])
        A("dve", lambda e: e.tensor_tensor(out=widf[:], in0=startb[:].unsqueeze(2).to_broadcast([128, NEXP, CT]),
                                           in1=iott[:, 0:CT].unsqueeze(1).to_broadcast([128, NEXP, CT]), op=ALU.add),
          reads=["startb", "iott"], writes=["widf"])
        A("dve", lambda e: e.tensor_copy(out=widx[:], in_=widf[:]), reads=["widf"], writes=["widx"])

        if debug:
            dma("sp", dbg_logits, logits[:], ["logits"], ["dbg_logits"])
            dma("sp", dbg_w[:, 0, :], w1g[:], ["w1g"], ["dbg_w1"])
            dma("sp", dbg_w[:, 1, :], w2g[:], ["w2g"], ["dbg_w2"])
            dma("sp", dbg_slot[:, 0, :], slot1[:], ["slot1"], ["dbg_s1"])
            dma("sp", dbg_slot[:, 1, :], slot2[:], ["slot2"], ["dbg_s2"])
        xskeys = []
        for tl in range(NTL):
            b = tl % 4
            dma("sp", xtl[b][:], xn2s[tl * 128:(tl + 1) * 128, :], ["xn2s"], ["xtl%d" % b])
            for k_, (sl_i, slk) in enumerate(((slot1, "slot1"), (slot2, "slot2"))):
                key = "xs_%d_%d" % (tl, k_)
                xskeys.append(key)
                A("pool", lambda e, sl_i=sl_i, tl=tl, b=b: e.indirect_dma_start(
                    out=xs[:, :], out_offset=bass.IndirectOffsetOnAxis(ap=sl_i[:, tl:tl + 1], axis=0), in_=xtl[b][:], in_offset=None),
                  reads=["xtl%d" % b, slk], writes=[key], dma=True, dkey="sc_xtl%d" % b)

        NOW, NTHR = cfg.NOW, cfg.NTHR
        BIGI = 1.0e6
        cnt_ = cntb[0]
        assert NOW <= NL and NTHR <= NL
        gtm = R2[:, 0:NTHR, :].rearrange("p t e -> p (t e)").rearrange("p (e t) -> p e t", e=32)
        nov = cf.take(32)
        cum = cf.take(32)
        indw = R1[:, 0:NOW, :]
        tmpw = R3[:, 0:NOW, :]
        limv = cf.take(32)
        jbase = cf.take(32)
        wsc = [cf.take(NOW) for _ in range(5)]
        gidf = cf.take(NOW, CT)
        yidf = cf.take(NOW, CT)
        mskf = cf.take(NOW, CT)
        wgidf = cf.take(NOW, 8)
        wdidf = cf.take(NOW, 4)
        gidx = T("gidx", [128, NOW, CT], I32)
        yidx = T("yidx", [128, NOW, CT], I32)
        wgidx = T("wgidx", [128, NOW, 8], I32)
        wdidx = T("wdidx", [128, NOW, 4], I32)
        DV = lambda fn, r, w: A("dve", fn, reads=r, writes=w)
        DV(lambda e: e.tensor_tensor(out=gtm[:], in0=cnt_[:].unsqueeze(2).to_broadcast([128, 32, NTHR]),
                                     in1=thrt[:, 0:NTHR].unsqueeze(1).to_broadcast([128, 32, NTHR]), op=ALU.is_gt), ["cnt", "thrt"], ["R2"])
        DV(lambda e: e.reduce_sum(out=nov[:], in_=gtm[:], axis=AX.X), ["R2"], ["nov"])
        DV(lambda e: e.memset(cum[:], 0.0), [], ["cum"])
        for j in range(1, 32):
            DV(lambda e, j=j: e.tensor_tensor(out=cum[:, j:j + 1], in0=cum[:, j - 1:j], in1=nov[:, j - 1:j], op=ALU.add), ["cum", "nov"], ["cum"])
        wvb = wvt[:, 0:NOW].unsqueeze(2).to_broadcast([128, NOW, 32])
        DV(lambda e: e.tensor_tensor(out=indw[:], in0=cum[:].unsqueeze(1).to_broadcast([128, NOW, 32]), in1=wvb, op=ALU.is_le), ["cum", "wvt"], ["R1"])
        DV(lambda e: e.tensor_tensor(out=limv[:], in0=cum[:], in1=nov[:], op=ALU.add), ["cum", "nov"], ["limv"])
        DV(lambda e: e.tensor_tensor(out=tmpw[:], in0=limv[:].unsqueeze(1).to_broadcast([128, NOW, 32]), in1=wvb, op=ALU.is_gt), ["limv", "wvt"], ["R3"])
        DV(lambda e: e.tensor_tensor(out=indw[:], in0=indw[:], in1=tmpw[:], op=ALU.mult), ["R1", "R3"], ["R1"])
        vld, ew, ow, lw, tw = wsc
        DV(lambda e: e.reduce_sum(out=vld[:], in_=indw[:], axis=AX.X), ["R1"], ["vld"])
        DV(lambda e: e.tensor_tensor(out=tmpw[:], in0=indw[:], in1=evt[:].unsqueeze(1).to_broadcast([128, NOW, 32]), op=ALU.mult), ["R1", "evt"], ["R3"])
        DV(lambda e: e.reduce_sum(out=ew[:], in_=tmpw[:], axis=AX.X), ["R3"], ["ew"])
        DV(lambda e: e.tensor_scalar(out=jbase[:], in0=cum[:], scalar1=-float(CAP), scalar2=float(CAP), op0=ALU.mult, op1=ALU.add), ["cum"], ["jbase"])
        DV(lambda e: e.tensor_tensor(out=jbase[:], in0=jbase[:], in1=startb[:], op=ALU.add), ["jbase", "startb"], ["jbase"])
        DV(lambda e: e.tensor_tensor(out=tmpw[:], in0=indw[:], in1=jbase[:].unsqueeze(1).to_broadcast([128, NOW, 32]), op=ALU.mult), ["R1", "jbase"], ["R3"])
        DV(lambda e: e.reduce_sum(out=ow[:], in_=tmpw[:], axis=AX.X), ["R3"], ["ow"])
        DV(lambda e: e.scalar_tensor_tensor(out=ow[:], in0=wvt[:, 0:NOW], scalar=float(CAP), in1=ow[:], op0=ALU.mult, op1=ALU.add), ["ow", "wvt"], ["ow"])
        DV(lambda e: e.tensor_tensor(out=ow[:], in0=ow[:], in1=vld[:], op=ALU.mult), ["ow", "vld"], ["ow"])
        DV(lambda e: e.tensor_tensor(out=limv[:], in0=startb[:], in1=cnt_[:], op=ALU.add), ["startb", "cnt"], ["limv"])
        DV(lambda e: e.tensor_tensor(out=tmpw[:], in0=indw[:], in1=limv[:].unsqueeze(1).to_broadcast([128, NOW, 32]), op=ALU.mult), ["R1", "limv"], ["R3"])
        DV(lambda e: e.reduce_sum(out=lw[:], in_=tmpw[:], axis=AX.X), ["R3"], ["lw"])
        DV(lambda e: e.tensor_scalar(out=tw[:], in0=vld[:], scalar1=-BIGI, scalar2=BIGI, op0=ALU.mult, op1=ALU.add), ["vld"], ["tw"])
        DV(lambda e: e.tensor_tensor(out=gidf[:], in0=ow[:].unsqueeze(2).to_broadcast([128, NOW, CT]),
                                     in1=iott[:, 0:CT].unsqueeze(1).to_broadcast([128, NOW, CT]), op=ALU.add), ["ow", "iott"], ["gidf"])
        DV(lambda e: e.tensor_tensor(out=mskf[:], in0=gidf[:], in1=lw[:].unsqueeze(2).to_broadcast([128, NOW, CT]), op=ALU.is_lt), ["gidf", "lw"], ["mskf"])
        DV(lambda e: e.scalar_tensor_tensor(out=yidf[:], in0=gidf[:], scalar=-BIGI, in1=mskf[:], op0=ALU.add, op1=ALU.mult), ["gidf", "mskf"], ["yidf"])
        DV(lambda e: e.tensor_scalar(out=yidf[:], in0=yidf[:], scalar1=BIGI, scalar2=None, op0=ALU.add), ["yidf"], ["yidf"])
        DV(lambda e: e.tensor_tensor(out=gidf[:], in0=gidf[:], in1=tw[:].unsqueeze(2).to_broadcast([128, NOW, CT]), op=ALU.add), ["gidf", "tw"], ["gidf"])
        DV(lambda e: e.tensor_copy(out=gidx[:], in_=gidf[:]), ["gidf"], ["gidx"])
        DV(lambda e: e.tensor_copy(out=yidx[:], in_=yidf[:]), ["yidf"], ["yidx"])
        DV(lambda e: e.scalar_tensor_tensor(out=ew[:], in0=ew[:], scalar=1024.0, in1=tw[:], op0=ALU.mult, op1=ALU.add), ["ew", "tw"], ["ew"])
        DV(lambda e: e.tensor_tensor(out=wgidf[:], in0=ew[:].unsqueeze(2).to_broadcast([128, NOW, 8]),
                                     in1=iot8t[:].unsqueeze(1).to_broadcast([128, NOW, 8]), op=ALU.add), ["ew", "iot8t"], ["wgidf"])
        DV(lambda e: e.tensor_copy(out=wgidx[:], in_=wgidf[:]), ["wgidf"], ["wgidx"])
        DV(lambda e: e.scalar_tensor_tensor(out=lw[:], in0=ew[:], scalar=0.125, in1=tw[:], op0=ALU.mult, op1=ALU.add), ["ew", "tw"], ["lw"])
        DV(lambda e: e.tensor_scalar(out=lw[:], in0=lw[:], scalar1=iott[:, 0:1], scalar2=None, op0=ALU.add), ["lw", "iott"], ["lw"])
        DV(lambda e: e.tensor_copy(out=wgidx2[:, 0:NOW], in_=lw[:]), ["lw"], ["wgidx2"])
        DV(lambda e: e.scalar_tensor_tensor(out=ew[:], in0=ew[:], scalar=0.5, in1=tw[:], op0=ALU.mult, op1=ALU.add), ["ew", "tw"], ["ew"])
        DV(lambda e: e.tensor_tensor(out=wdidf[:], in0=ew[:].unsqueeze(2).to_broadcast([128, NOW, 4]),
                                     in1=iot8t[:, 0:4].unsqueeze(1).to_broadcast([128, NOW, 4]), op=ALU.add), ["ew", "iot8t"], ["wdidf"])
        DV(lambda e: e.tensor_copy(out=wdidx[:], in_=wdidf[:]), ["wdidf"], ["wdidx"])

        gbanks = [(pZ[:, 0, :], "pZ0"), (pZ[:, 1, :], "pZ1"), (pC[:], "B3"), (pV[:], "B7")]
        dbanks = [(pS[:, 512:1024], "B5"), (pS[:, 1024:1536], "B6")]
        pT2 = pS[:, 0:512].bitcast(BF16)
        tbanks = [(pT, "pT"), (pT2, "B4")]
        cnts = {"gi": 0, "di": 0, "ti": 0}
        NJOB = NEXP + NOW
        wg2, wu2, wd2 = wg, wu, wd

        bregs = memo.setdefault("__bregs", {})

        def breg(e, val):
            if val not in bregs:
                r = e.alloc_register("bc%d" % val)
                e.reg_mov(r, val)
                bregs[val] = r
            return bregs[val]

        def job_rows(k, j, for_y):
            if k < NEXP:
                return widx[:, k, j:j + 1]
            return (yidx if for_y else gidx)[:, k - NEXP, j:j + 1]

        def job_load_w(k):
            b = k % 2
            if k < NEXP:
                load_w(k)
                return
            w = k - NEXP
            og, ou, od = [], [], []
            og.append(A("pool", lambda e: e.indirect_dma_start(out=Wg[b][:].rearrange("p k n -> p (k n)"), out_offset=None, in_=wg2,
                                                               in_offset=bass.IndirectOffsetOnAxis(ap=wgidx2[:, w:w + 1], axis=0),
                                                               bounds_check=breg(e, NEXP * 128 - 1), oob_is_err=False),
                        reads=["wgidx2"], writes=WGK[b], dma=True, dkey="Wg%d" % b))
            ou.append(A("pool", lambda e: e.indirect_dma_start(out=Wu[b][:].rearrange("p k n -> p (k n)"), out_offset=None, in_=wu2,
                                                               in_offset=bass.IndirectOffsetOnAxis(ap=wgidx2[:, w:w + 1], axis=0),
                                                               bounds_check=breg(e, NEXP * 128 - 1), oob_is_err=False),
                        reads=["wgidx2"], writes=WUK[b], dma=True, dkey="Wu%d" % b))
            for jc in range(4):
                od.append(A("pool", lambda e, jc=jc: e.indirect_dma_start(out=Wd[b][:, jc, :], out_offset=None, in_=wd2,
                                                                          in_offset=bass.IndirectOffsetOnAxis(ap=wdidx[:, w, jc:jc + 1], axis=0),
                                                                          bounds_check=breg(e, NEXP * 512 - 1), oob_is_err=False),
                            reads=["wdidx"], writes=[WDK[b][jc]], dma=True, dkey="Wd%d" % b))
            for grp in (og, ou, od):
                for o_ in grp:
                    o_.tgt = grp[-1].tgt

        def job_gather(k):
            b = k % 2
            for j in range(CT):
                rows = job_rows(k, j, False)
                if k < NEXP:
                    A("pool", lambda e, j=j, rows=rows: e.indirect_dma_start(
                        out=xw[b][:, j, :], out_offset=None, in_=xs[:, :], in_offset=bass.IndirectOffsetOnAxis(ap=rows, axis=0)),
                      reads=xskeys + ["widx"], writes=["xw%d_%d" % (b, j)], dma=True)
                else:
                    A("pool", lambda e, j=j, rows=rows: e.indirect_dma_start(
                        out=xw[b][:, j, :], out_offset=None, in_=xs[:, :], in_offset=bass.IndirectOffsetOnAxis(ap=rows, axis=0),
                        bounds_check=breg(e, cfg.XSR - 1), oob_is_err=False),
                      reads=xskeys + ["gidx"], writes=["xw%d_%d" % (b, j)], dma=True)

        xsT2 = [xsT, cb_.take(8, CAP)]

        def job_T(k):
            b = k % 2
            xs_ = xsT2[k % 2]
            for kc in range(8):
                (tps, tk_) = tbanks[cnts["ti"] % 2]
                cnts["ti"] += 1
                for j in range(CT):
                    A("pe", lambda e, kc=kc, j=j, tps=tps: e.transpose(out=tps[:, j * 128:(j + 1) * 128],
                                                                       in_=xw[b][:, j, :].rearrange("s (p k) -> s k p", k=8)[:, kc, :], identity=ident[:]),
                      reads=["xw%d_%d" % (b, j), "ident"], writes=[tk_])
                A("act", lambda e, kc=kc, tps=tps: e.activation(out=xs_[:, kc, :], in_=tps[:, 0:CAP], func=AF.Identity,
                                                                scale=mul2a[:, kc:kc + 1], bias=modca[:, 0, kc:kc + 1]),
                  reads=[tk_, "mul2a", "modca"], writes=["xsT%d_%d" % (k % 2, kc)])

        def job_GU(k):
            b = k % 2
            xs_ = xsT2[k % 2]
            for jc in range(4):
                (gps, gk_) = gbanks[cnts["gi"] % 4]
                (ups, uk_) = gbanks[(cnts["gi"] + 1) % 4]
                cnts["gi"] += 2
                for kc in range(8):
                    A("pe", lambda e, gps=gps, kc=kc, jc=jc: e.matmul(gps[:, 0:CAP], lhsT=Wg[b][:, kc, jc * 128:(jc + 1) * 128], rhs=xs_[:, kc, :],
                                                                     start=(kc == 0), stop=(kc == 7)), reads=WGK[b] + ["xsT%d_%d" % (k % 2, kc)], writes=[gk_])
                for kc in range(8):
                    A("pe", lambda e, ups=ups, kc=kc, jc=jc: e.matmul(ups[:, 0:CAP], lhsT=Wu[b][:, kc, jc * 128:(jc + 1) * 128], rhs=xs_[:, kc, :],
                                                                     start=(kc == 0), stop=(kc == 7)), reads=WUK[b] + ["xsT%d_%d" % (k % 2, kc)], writes=[uk_])
                sb_ = jc % 2
                A("act", lambda e, gps=gps, sb_=sb_: e.activation(out=sgb[sb_][:], in_=gps[:, 0:CAP], func=AF.Silu), reads=[gk_], writes=["sg%d" % sb_])
                A("dve", lambda e, ups=ups, sb_=sb_, jc=jc: e.tensor_tensor(out=actT[:, jc, :], in0=ups[:, 0:CAP], in1=sgb[sb_][:], op=ALU.mult),
                  reads=[uk_, "sg%d" % sb_], writes=["actT"])

        def job_D(k):
            b = k % 2
            for j in range(CT):
                yb = (k * CT + j) % 2
                for cbk in range(2):
                    (dps, dk_) = dbanks[cnts["di"] % 2]
                    cnts["di"] += 1
                    for jc in range(4):
                        A("pe", lambda e, dps=dps, jc=jc, j=j, cbk=cbk: e.matmul(dps, lhsT=actT[:, jc, j * 128:(j + 1) * 128], rhs=Wd[b][:, jc, cbk * 512:(cbk + 1) * 512],
                                                                                start=(jc == 0), stop=(jc == 3)), reads=["actT"] + WDK[b], writes=[dk_])
                    A("dve", lambda e, dps=dps, cbk=cbk, yb=yb: e.tensor_tensor(out=ybuf[yb][:, cbk * 512:(cbk + 1) * 512], in0=dps, in1=g2bc[:, cbk * 512:(cbk + 1) * 512], op=ALU.mult),
                      reads=[dk_, "g2bc"], writes=["ybuf%d" % yb])
                rows = job_rows(k, j, True)
                if k < NEXP:
                    A("pool", lambda e, rows=rows, yb=yb: e.indirect_dma_start(
                        out=ys[:, :], out_offset=bass.IndirectOffsetOnAxis(ap=rows, axis=0), in_=ybuf[yb][:], in_offset=None),
                      reads=["ybuf%d" % yb, "widx"], writes=["ys"], dma=True, dkey="sc_ybuf%d" % yb)
                else:
                    A("pool", lambda e, rows=rows, yb=yb: e.indirect_dma_start(
                        out=ys[:, :], out_offset=bass.IndirectOffsetOnAxis(ap=rows, axis=0), in_=ybuf[yb][:], in_offset=None,
                        bounds_check=breg(e, cfg.XSR - 1), oob_is_err=False),
                      reads=["ybuf%d" % yb, "yidx"], writes=["ys"], dma=True, dkey="sc_ybuf%d" % yb)

        job_gather(0)
        job_gather(1)
        job_T(0)
        for k in range(NJOB):
            job_GU(k)
            if k + 1 < NJOB:
                job_T(k + 1)
            if k + 2 < NJOB:
                job_gather(k + 2)
            job_D(k)
            if k + 2 < NJOB:
                job_load_w(k + 2)

        for tl in range(NTL):
            b = tl % 2
            dma("sp", xmb[b][:], xmid[tl * 128:(tl + 1) * 128, :], ["xmid"], ["xmb%d" % b])
            A("pool", lambda e, tl=tl, b=b: e.indirect_dma_start(out=y1b[b][:], out_offset=None, in_=ys[:, :],
                                                                 in_offset=bass.IndirectOffsetOnAxis(ap=slot1[:, tl:tl + 1], axis=0)),
              reads=["ys", "slot1"], writes=["y1b%d" % b], dma=True)
            A("pool", lambda e, tl=tl, b=b: e.indirect_dma_start(out=y2b[b][:], out_offset=None, in_=ys[:, :],
                                                                 in_offset=bass.IndirectOffsetOnAxis(ap=slot2[:, tl:tl + 1], axis=0)),
              reads=["ys", "slot2"], writes=["y2b%d" % b], dma=True)
            A("dve", lambda e, tl=tl, b=b: e.scalar_tensor_tensor(out=xmb[b][:], in0=y1b[b][:], scalar=w1g[:, tl:tl + 1], in1=xmb[b][:], op0=ALU.mult, op1=ALU.add),
              reads=["y1b%d" % b, "w1g", "xmb%d" % b], writes=["xmb%d" % b])
            A("dve", lambda e, tl=tl, b=b: e.scalar_tensor_tensor(out=xmb[b][:], in0=y2b[b][:], scalar=w2g[:, tl:tl + 1], in1=xmb[b][:], op0=ALU.mult, op1=ALU.add),
              reads=["y2b%d" % b, "w2g", "xmb%d" % b], writes=["xmb%d" % b])
            dma("act", xo[tl * 128:(tl + 1) * 128, :], xmb[b][:], ["xmb%d" % b], ["xo_%d" % tl], dkey="st_xmb%d" % b)
        p.barrier()


def _colform(v):
    return np.ascontiguousarray(v.reshape(-1, 128).T).astype(np.float32)


def _const_tables(cfg):
    q = np.arange(128)[:, None]
    tabs_idx = np.zeros((128, 5, 128), np.int64)
    mask = np.zeros((128, 5, 128), np.float32)
    for t in range(5):
        k = np.arange(128)[None, :]
        rel = 128 * (4 - t) + q - k
        tabs_idx[:, t, :] = np.clip(rel, -128, 128) + 128
        qc = q // 64
        kc = 2 * (t - 4) + k // 64
        ok = (kc <= qc) & (kc >= qc - 8)
        mask[:, t, :] = ok
    tri = (np.arange(128)[:, None] < np.arange(128)[None, :]).astype(np.float32)
    iot = (np.arange(128)[:, None] + 128 * np.arange(4)[None, :]).astype(np.float32)
    trash = (cfg.TRASH + np.arange(128)[:, None] + 128 * np.arange(max(cfg.NFH, 1))[None, :]).astype(np.float32)
    return tabs_idx, mask, tri, iot, trash


def layer_inputs(cfg, l, xe, cb, first_half, P, li=0):
    tabs_idx, mask, tri, iot, trash = _const_tables(cfg)
    btab = np.ascontiguousarray(P["rel_bias"][:, tabs_idx].transpose(1, 0, 2, 3)).astype(np.float32)
    invc = np.zeros((128, 4, 16), np.float32)
    for g, w in enumerate((2, 4, 8, 16)):
        cnt = np.minimum(np.arange(16) + 1, w) if first_half else np.full(16, w)
        invc[:, g, :] = (1.0 / cnt.astype(np.float64)).astype(np.float32)[None, :]
    hvv = 0.0 if first_half else 1.0
    trash = (cfg.TRASH + np.arange(128)[:, None] + 128 * np.arange(TRC)[None, :]).astype(np.float32)
    trash = np.concatenate([trash, trash + cfg.NFH * 128], axis=1)
    m = {
        "xe": np.ascontiguousarray(xe, dtype=np.float32),
        "cT": _colform(cb),
        "ada_w": P["ada_w"][l], "ada_b": P["ada_b"][l][None, :],
        "n1c": _colform(P["norm1_g"][l]), "n2c": _colform(P["norm2_g"][l]),
        "n2a": np.ascontiguousarray(P["norm2_g"][l].reshape(128, 8)).astype(np.float32),
        "w_in": P["w_in"][l], "w_out": P["w_out"][l],
        "pool_w": np.ascontiguousarray(P["pool_w"][l].transpose(1, 0, 2)),
        "pscale": _colform(P["pool_scale"][l]),
        "gq": np.ascontiguousarray(np.tile(P["q_norm_g"][l], 2)[:, None]), "gk": np.ascontiguousarray(np.tile(P["k_norm_g"][l], 2)[:, None]),
        "btab": btab, "bmask": mask,
        "wr": np.ascontiguousarray(np.concatenate([P["router_group_w"][l], P["router_expert_w"][l]], axis=1)),
        "br": np.ascontiguousarray(np.tile(np.concatenate([P["router_group_b"][l], P["router_expert_b"][l]])[None, :], (128, 1))),
        "wg": P["moe_w_gate"][l].reshape(NEXP * 128, 8 * 512), "wu": P["moe_w_up"][l].reshape(NEXP * 128, 8 * 512), "wd": P["moe_w_down"][l].reshape(NEXP * 512, D),
        "hv": np.full((128, 1), hvv, np.float32), "nhv": np.full((128, 1), 1.0 - hvv, np.float32),
        "invc": invc, "tri": tri, "iot": iot, "trashi": trash,
        "thr": np.tile((cfg.CAP * (np.arange(16) + 1)).astype(np.float32)[None, :], (128, 1)),
        "wv": np.tile(np.arange(32, dtype=np.float32)[None, :], (128, 1)),
        "ev": np.tile(np.arange(32, dtype=np.float32)[None, :], (128, 1)),
        "iot8": (np.arange(128)[:, None] + 128 * np.arange(8)[None, :]).astype(np.float32),
    }
    return {(k + "_%d" % li if k in PERL else k): v for k, v in m.items()}


_NC_CACHE = {}


def kernel(**inputs):
    P = {k: np.asarray(v) for k, v in inputs.items()}
    x = P["x"]
    B, S, _ = x.shape
    cfg0 = Cfg(nkv=4, nfh=4, nm=32, cap=512)
    cfg1 = Cfg(nkv=4, nfh=0, nm=32, cap=512)
    if "nc" not in _NC_CACHE:
        _NC_CACHE["nc"] = build_program([cfg0, cfg1])
    nc = _NC_CACHE["nc"]
    half = S // 2
    in_maps = []
    for c in range(8):
        b, hf = c // 2, c % 2
        main = x[b, hf * half:(hf + 1) * half]
        halo = np.zeros((1024, D), np.float32) if hf == 0 else x[b, half - 1024:half]
        xe = np.concatenate([halo, main], axis=0)
        m = layer_inputs(cfg0, 0, xe, P["c"][b], hf == 0, P, li=0)
        m1 = layer_inputs(cfg1, 1, xe[:128], P["c"][b], hf == 0, P, li=1)
        m.update({k: v for k, v in m1.items() if k.endswith("_1")})
        in_maps.append(m)
    res = run_bass_kernel_spmd(nc, in_maps, core_ids=list(range(8)))
    out = np.empty_like(x)
    for c in range(8):
        b, hf = c // 2, c % 2
        out[b, hf * half:(hf + 1) * half] = res.results[c]["xo"]
    return out
```

```python
from contextlib import ExitStack

import numpy as np
import concourse.bass as bass
import concourse.mybir as mybir
from concourse.bass_utils import run_bass_kernel_spmd

F32 = mybir.dt.float32
BF16 = mybir.dt.bfloat16
I32 = mybir.dt.int32
AF = mybir.ActivationFunctionType
ALU = mybir.AluOpType
AX = mybir.AxisListType

ENGS = ("sp", "act", "dve", "pool", "pe")
PSUM_KEYS = frozenset(["pT", "pZ0", "pZ1", "B3", "B4", "B5", "B6", "B7"])
D = 1024
EPS = 1e-6
NEXP = 32
RT = 8


class Op:
    __slots__ = ("eng", "fn", "dma", "dkey", "deps", "sig", "idx", "tgt", "waits")

    def __init__(self, eng, fn, dma, dkey):
        self.eng = eng
        self.fn = fn
        self.dma = dma
        self.dkey = dkey
        self.deps = []
        self.sig = False
        self.idx = 0
        self.tgt = 0
        self.waits = []


class PB:
    def __init__(self, nc):
        self.nc = nc
        self.ops = []
        self.last_w = {}
        self.readers = {}
        self.dma_cnt = {}

    def add(self, eng, fn, reads=(), writes=(), dma=False, dkey=None):
        if dma and dkey is None:
            dkey = writes[0]
        op = Op(eng, fn, dma, dkey)
        deps = set()
        for k in reads:
            w = self.last_w.get(k)
            if w is not None:
                deps.add(w)
            if k in PSUM_KEYS:
                for r in self.readers.get(k, ()):
                    if r.eng != eng:
                        deps.add(r)
        for k in writes:
            w = self.last_w.get(k)
            if w is not None:
                deps.add(w)
            for r in self.readers.get(k, ()):
                deps.add(r)
        op.deps = list(deps)
        for k in reads:
            self.readers.setdefault(k, []).append(op)
        for k in writes:
            self.last_w[k] = op
            self.readers[k] = []
        if dma:
            self.dma_cnt[dkey] = self.dma_cnt.get(dkey, 0) + 1
            op.tgt = 16 * self.dma_cnt[dkey]
        self.ops.append(op)
        return op

    def barrier(self):
        allkeys = list(set(self.last_w.keys()) | set(self.readers.keys()))
        self.add("sp", lambda e: e.nop(), reads=[], writes=allkeys + ["__bar"])
        for eng in ENGS:
            self.add(eng, lambda e: e.nop(), reads=["__bar"], writes=["__bar_" + eng])
        self.last_w = {k: v for k, v in self.last_w.items() if k.startswith("__bar")}
        self.readers = {k: v for k, v in self.readers.items() if k.startswith("__bar")}

    def emit(self):
        nc = self.nc
        for op in self.ops:
            for d in op.deps:
                if not d.dma:
                    d.sig = True
        cnt = {e: 0 for e in ENGS}
        for op in self.ops:
            if not op.dma and op.sig:
                cnt[op.eng] += 1
                op.idx = cnt[op.eng]
        dkeys = sorted(self.dma_cnt.keys())
        with ExitStack() as st:
            esem = {e: st.enter_context(nc.semaphore("es_" + e)) for e in ENGS}
            dsem = {k: st.enter_context(nc.semaphore("ds%d" % i)) for i, k in enumerate(dkeys)}
            waited = {e: {} for e in ENGS}
            for op in self.ops:
                need = {}
                for d in op.deps:
                    if d.dma:
                        key, val = ("d", d.dkey), d.tgt
                    else:
                        if d.eng == op.eng and op.eng == "pe":
                            continue
                        key, val = ("e", d.eng), d.idx
                    if need.get(key, 0) < val:
                        need[key] = val
                w = waited[op.eng]
                for key, val in need.items():
                    if w.get(key, 0) < val:
                        w[key] = val
                        op.waits.append((dsem[key[1]] if key[0] == "d" else esem[key[1]], val))
            block = st.enter_context(nc.Block())

            def run(engname):
                def body(e):
                    for op in self.ops:
                        if op.eng != engname:
                            continue
                        for (s, v) in op.waits:
                            e.wait_ge(s, v)
                        ins = op.fn(e)
                        if op.dma:
                            ins.then_inc(dsem[op.dkey], 16)
                        elif op.sig:
                            ins.then_inc(esem[engname], 1)
                return body

            block.sync(run("sp"))
            block.scalar(run("act"))
            block.vector(run("dve"))
            block.gpsimd(run("pool"))
            block.tensor(run("pe"))


class Cfg:
    def __init__(self, nkv=4, nfh=0, nm=32, cap=512):
        self.NKV, self.NFH, self.NM, self.CAP = nkv, nfh, nm, cap
        self.NTE = nkv + nfh + nm
        self.NTL = nfh + nm
        self.NST = self.NTE // 4
        self.CT = cap // 128
        self.NTOK = self.NTL * 128
        self.TRASH = 2 * self.NTOK + cap
        self.XSR = self.TRASH + 2 * max(nfh, 1) * 128
        self.NOW = (2 * self.NTOK - 1) // cap
        self.NTHR = -(-self.NTOK // cap)
        assert self.NTE % 4 == 0 and nkv % 4 == 0 and nfh % 4 == 0 and cap % 128 == 0


PERL = frozenset(["ada_w", "ada_b", "n1c", "n2c", "n2a", "w_in", "w_out", "pool_w", "pscale", "gq", "gk", "wr", "br", "wg", "wu", "wd"])
TRC = 4


class _Ctx:
    pass


def build_program(cfgs, debug=False):
    nc = bass.Bass("TRN2", target_bir_lowering=False)
    ctx = _Ctx()
    ctx.nc, ctx.memo, ctx.p, ctx.nl = nc, {}, PB(nc), len(cfgs)
    with ExitStack() as st:
        ctx.st = st
        for li, cfg in enumerate(cfgs):
            _emit_layer(ctx, li, cfg, debug)
        ctx.p.emit()
    return nc


def build_layer(cfg, debug=False):
    return build_program([cfg], debug)


def _emit_layer(ctx, li, cfg, debug=False):
    nc, st, memo = ctx.nc, ctx.st, ctx.memo
    last = li == ctx.nl - 1
    NTE, NTL, NST, NKV, NFH, CT, CAP = cfg.NTE, cfg.NTL, cfg.NST, cfg.NKV, cfg.NFH, cfg.CT, cfg.CAP

    def din(name, shape, dt=F32):
        nm = name + ("_%d" % li if name in PERL else "")
        if nm not in memo:
            memo[nm] = nc.dram_tensor(nm, list(shape), dt, kind="ExternalInput").ap()
        return memo[nm]

    def dscr(name, shape, dt, kind="Internal"):
        if name not in memo:
            memo[name] = nc.dram_tensor(name, list(shape), dt, kind=kind).ap()
        return memo[name]

    xe = din("xe", [NTE * 128, D]) if li == 0 else memo["x1_%d" % (li - 1)]
    cT = din("cT", [128, 8])
    ada_w = din("ada_w", [D, 6 * D])
    ada_b = din("ada_b", [1, 6 * D])
    n1c = din("n1c", [128, 8])
    n2c = din("n2c", [128, 8])
    n2a = din("n2a", [128, 8])
    w_in = din("w_in", [D, 2048])
    w_out = din("w_out", [D, D])
    pool_w = din("pool_w", [128, 4, 128])
    pscale = din("pscale", [128, 4])
    gq = din("gq", [128, 1])
    gk = din("gk", [128, 1])
    btab = din("btab", [128, 8, 5, 128])
    bmask = din("bmask", [128, 5, 128])
    wr = din("wr", [D, 36])
    br = din("br", [128, 36])
    wg = din("wg", [NEXP * 128, 8 * 512])
    wu = din("wu", [NEXP * 128, 8 * 512])
    wd = din("wd", [NEXP * 512, D])
    hv = din("hv", [128, 1])
    nhv = din("nhv", [128, 1])
    invc = din("invc", [128, 4, 16])
    tri = din("tri", [128, 128])
    iot = din("iot", [128, 4])
    trashi = din("trashi", [128, 2 * TRC])
    thr = din("thr", [128, 16])
    wv = din("wv", [128, 32])
    ev = din("ev", [128, 32])
    iot8 = din("iot8", [128, 8])
    if last:
        xo = nc.dram_tensor("xo", [NTL * 128, D], F32, kind="ExternalOutput").ap()
    else:
        xo = dscr("x1_%d" % li, [NTL * 128, D], F32)
    xmid = dscr("xmid", [NTL * 128, D], F32, kind="ExternalOutput" if debug else "Internal")
    if debug:
        dbg_logits = nc.dram_tensor("dbg_logits", [128, NTL, 36], F32, kind="ExternalOutput").ap()
        dbg_w = nc.dram_tensor("dbg_w", [128, 2, NTL], F32, kind="ExternalOutput").ap()
        dbg_slot = nc.dram_tensor("dbg_slot", [128, 2, NTL], I32, kind="ExternalOutput").ap()
    xn2s = dscr("xn2s", [NTL * 128, D], BF16)
    xs = dscr("xs", [cfg.XSR, D], BF16)
    ys = dscr("ys", [cfg.XSR, D], F32)

    if True:
        def T(name, shape, dt=F32):
            if name in memo:
                t, shp = memo[name]
                if list(shp) != list(shape):
                    assert len(shp) == len(shape) and shape[1] <= shp[1] and list(shp[2:]) == list(shape[2:]), (name, shp, shape)
                    return t[:, 0:shape[1]]
                return t
            t = st.enter_context(nc.sbuf_tensor(name, list(shape), dt))
            memo[name] = (t, list(shape))
            return t

        def PS(name, shape, dt=F32):
            if name not in memo:
                memo[name] = st.enter_context(nc.psum_tensor(name, list(shape), dt))
            return memo[name]

        ident = T("ident", [128, 128], BF16)
        identf = T("identf", [128, 128])
        ones_bf = T("ones_bf", [128, 128], BF16)
        blk1 = T("blk1", [128, 128], BF16)
        tri_bf = T("tri_bf", [128, 128], BF16)
        onesf = T("onesf", [1, 128])
        epsc = T("epsc", [128, 1])
        eps64 = T("eps64", [128, 1])
        cact = T("cact", [128, 8], BF16)
        ctf = T("ctf", [128, 8])
        n1t = T("n1t", [128, 8])
        n2t = T("n2t", [128, 8])
        n2at = T("n2at", [128, 8])
        modca = T("modca", [128, 2, 8])
        mul2a = T("mul2a", [128, 8])
        wgidx2 = T("wgidx2", [128, 32], I32)
        modc = T("modc", [128, 4, 8])
        mul1c = T("mul1c", [128, 8])
        mul2c = T("mul2c", [128, 8])
        add1b = T("add1b", [128, 8], BF16)
        g2bc = T("g2bc", [128, D])
        bzc = T("bzc", [128, 12])
        bzv = T("bzv", [128, 512])
        bzvm = T("bzvm", [128, 512])
        pw_bf = T("pw_bf", [128, 4, 128], BF16)
        psc = T("psc", [128, 4])
        gqk = T("gqk", [128, 1])
        gkt = T("gkt", [128, 1])
        BT = T("BT", [128, 8, 5, 128], BF16)
        wr_f = T("wr_f", [128, 8, 36])
        wr_bf = T("wr_bf", [128, 8, 36], BF16)
        wr_raw = T("wr_raw", [128, 8, 36], BF16)
        biasR = T("biasR", [128, 36])
        hvt = T("hvt", [128, 1])
        nhvt = T("nhvt", [128, 1])
        invct = T("invct", [128, 4, 16])
        iott = T("iott", [128, 4])
        trt = T("trt", [128, 2 * TRC])
        thrt = T("thrt", [128, 16])
        wvt = T("wvt", [128, 32])
        evt = T("evt", [128, 32])
        iot8t = T("iot8t", [128, 8])
        ssr = T("ssr", [128, 8])
        rst = T("rst", [128, 8])
        logits = T("logits", [128, NTL, 36])
        w1g = T("w1g", [128, NTL])
        w2g = T("w2g", [128, NTL])
        slot1 = T("slot1", [128, NTL], I32)
        slot2 = T("slot2", [128, NTL], I32)
        widx = T("widx", [128, NEXP, CT], I32)

        AF_WORDS = 15104
        AB_WORDS = 58432
        arf = T("arf", [128, AF_WORDS])
        arb = T("arb", [128, AB_WORDS], BF16)

        class Carver:
            def __init__(self, t, n):
                self.t, self.n, self.off = t, n, 0

            def take(self, *shape):
                n = int(np.prod(shape))
                a = self.t[:, self.off:self.off + n]
                self.off += n
                assert self.off <= self.n, (self.off, self.n)
                if len(shape) == 2:
                    return a.rearrange("p (a b) -> p a b", a=shape[0])
                if len(shape) == 3:
                    return a.rearrange("p (a b c) -> p a b c", a=shape[0], b=shape[1])
                return a

        pT = PS("pT", [128, 1024], BF16)
        pZ = PS("pZ", [128, 2, 512])
        pC = PS("B3", [128, 512])
        pS = PS("pS", [128, 1536])
        pV = PS("B7", [128, 512])

        p = ctx.p
        A = p.add

        def dma(eng, out, in_, reads, writes, dkey=None):
            return A(eng, lambda e: e.dma_start(out=out, in_=in_), reads=reads, writes=writes, dma=True, dkey=dkey)

        A("pool", lambda e: e.memset(identf[:], 0.0), writes=["identf"])
        A("pool", lambda e: e.affine_select(out=identf[:], in_=identf[:], pattern=[[-1, 128]], compare_op=ALU.not_equal,
                                            fill=1.0, base=0, channel_multiplier=1), reads=["identf"], writes=["identf"])
        A("dve", lambda e: e.tensor_copy(out=ident[:], in_=identf[:]), reads=["identf"], writes=["ident"])
        A("pool", lambda e: e.memset(ones_bf[:], 1.0), writes=["ones_bf"])
        A("pool", lambda e: e.memset(blk1[:], 0.0), writes=["blk1"])
        A("pool", lambda e: e.memset(blk1[0:64, 0:64], 1.0), reads=["blk1"], writes=["blk1"])
        A("pool", lambda e: e.memset(blk1[64:128, 64:128], 1.0), reads=["blk1"], writes=["blk1"])
        A("pool", lambda e: e.memset(onesf[:], 1.0), writes=["onesf"])
        A("pool", lambda e: e.memset(epsc[:], EPS), writes=["epsc"])
        A("pool", lambda e: e.memset(eps64[:], 64 * EPS), writes=["eps64"])
        for (dst, src, k) in ((ctf, cT, "ctf"), (n1t, n1c, "n1t"), (n2t, n2c, "n2t"), (n2at, n2a, "n2at"), (psc, pscale, "psc"), (gqk, gq, "gqk"),
                              (gkt, gk, "gkt"), (hvt, hv, "hvt"), (nhvt, nhv, "nhvt"), (invct, invc, "invct"),
                              (iott, iot, "iott"), (trt, trashi, "trt"), (thrt, thr, "thrt"), (wvt, wv, "wvt"), (evt, ev, "evt"), (iot8t, iot8, "iot8t"), (biasR, br, "biasR"), (identf, tri, "identf")):
            dma("sp", dst[:], src, [], [k])
        A("dve", lambda e: e.tensor_copy(out=tri_bf[:], in_=identf[:]), reads=["identf"], writes=["tri_bf"])
        A("dve", lambda e: e.tensor_mul(out=gqk[:], in0=gqk[:], in1=gkt[:]), reads=["gqk", "gkt"], writes=["gqk"])
        dma("pool", pw_bf[:], pool_w, [], ["pw_bf"])
        A("act", lambda e: e.activation(out=cact[:], in_=ctf[:], func=AF.Silu), reads=["ctf"], writes=["cact"])

        cf = Carver(arf, AF_WORDS)
        cb_ = Carver(arb, AB_WORDS)
        g1bc = cf.take(D)
        modrow = cf.take(6 * D)[0:1, :]
        adab = cf.take(6 * D)[0:1, :]
        stage = [cb_.take(8, 1536) for _ in range(2)]
        zf = cf.take(D)
        zb = cb_.take(D)
        A("pool", lambda e: e.memset(zf[:], 0.0), writes=["zf"])
        A("pool", lambda e: e.memset(zb[:], 0.0), writes=["zb"])
        for r0 in range(2 * cfg.NM * 128, cfg.TRASH, 128):
            dma("sp", xs[r0:r0 + 128, :], zb[:], ["zb"], ["xs_z%d" % r0], dkey="zinit")
        for r0 in range(cfg.TRASH, cfg.TRASH + 2 * NFH * 128, 128):
            dma("sp", ys[r0:r0 + 128, :], zf[:], ["zf"], ["ys_z%d" % r0], dkey="zinit")
        dma("sp", adab, ada_b, [], ["adab"])
        for g in range(4):
            sb = stage[g % 2]
            dma("pool", sb[:], ada_w[:, g * 1536:(g + 1) * 1536].rearrange("(k p) n -> p k n", p=128), [], ["stage%d" % (g % 2)])
            for cbk in range(3):
                col = g * 1536 + cbk * 512
                bank = cbk % 2
                for kc in range(8):
                    A("pe", lambda e, sb=sb, kc=kc, cbk=cbk, bank=bank: e.matmul(
                        pZ[0:1, bank, :], lhsT=cact[:, kc:kc + 1], rhs=sb[:, kc, cbk * 512:(cbk + 1) * 512],
                        start=(kc == 0), stop=(kc == 7)), reads=["cact", "stage%d" % (g % 2)], writes=["pZ%d" % bank])
                A("dve", lambda e, col=col, bank=bank: e.tensor_tensor(out=modrow[:, col:col + 512], in0=pZ[0:1, bank, :],
                                                                       in1=adab[:, col:col + 512], op=ALU.add),
                  reads=["pZ%d" % bank, "adab"], writes=["modrow"])
        for vi, base in enumerate((0, D, 3 * D, 4 * D)):
            for kc in range(8):
                A("pe", lambda e, vi=vi, base=base, kc=kc: e.matmul(
                    pC[:, vi * 8 + kc: vi * 8 + kc + 1], lhsT=modrow[:, base + kc * 128: base + (kc + 1) * 128],
                    rhs=onesf[:, 0:1], start=True, stop=True), reads=["modrow", "onesf"], writes=["B3"])
        A("dve", lambda e: e.tensor_copy(out=modc[:], in_=pC[:, 0:32].rearrange("p (a b) -> p a b", a=4)), reads=["B3"], writes=["modc"])
        A("dve", lambda e: e.scalar_tensor_tensor(out=mul1c[:], in0=modc[:, 1, :], scalar=1.0, in1=n1t[:], op0=ALU.add, op1=ALU.mult),
          reads=["modc", "n1t"], writes=["mul1c"])
        A("dve", lambda e: e.scalar_tensor_tensor(out=mul2c[:], in0=modc[:, 3, :], scalar=1.0, in1=n2t[:], op0=ALU.add, op1=ALU.mult),
          reads=["modc", "n2t"], writes=["mul2c"])
        A("dve", lambda e: e.tensor_copy(out=add1b[:], in_=modc[:, 0, :]), reads=["modc"], writes=["add1b"])
        for vi, base in enumerate((3 * D, 4 * D)):
            mview = modrow[:, base:base + D].rearrange("o (p k) -> o k p", k=8)
            for kc in range(8):
                A("pe", lambda e, vi=vi, kc=kc, mview=mview: e.matmul(
                    pV[:, vi * 8 + kc: vi * 8 + kc + 1], lhsT=mview[:, kc, :], rhs=onesf[:, 0:1], start=True, stop=True),
                  reads=["modrow", "onesf"], writes=["B7"])
        A("dve", lambda e: e.tensor_copy(out=modca[:], in_=pV[:, 0:16].rearrange("p (a b) -> p a b", a=2)), reads=["B7"], writes=["modca"])
        A("dve", lambda e: e.scalar_tensor_tensor(out=mul2a[:], in0=modca[:, 1, :], scalar=1.0, in1=n2at[:], op0=ALU.add, op1=ALU.mult),
          reads=["modca", "n2at"], writes=["mul2a"])
        for (dst, base, k) in ((g1bc, 2 * D, "g1bc"), (g2bc, 5 * D, "g2bc")):
            for hb in range(2):
                A("pe", lambda e, base=base, hb=hb: e.matmul(pZ[:, hb, :], lhsT=onesf[:, :], rhs=modrow[:, base + hb * 512: base + (hb + 1) * 512],
                                                            start=True, stop=True), reads=["modrow", "onesf"], writes=["pZ%d" % hb])
                A("dve", lambda e, dst=dst, hb=hb: e.tensor_copy(out=dst[:, hb * 512:(hb + 1) * 512], in_=pZ[:, hb, :]),
                  reads=["pZ%d" % hb], writes=[k])
        p.barrier()

        cf = Carver(arf, AF_WORDS)
        cb_ = Carver(arb, AB_WORDS)
        w_in_bf = cb_.take(8, 2048)
        w_out_bf = cb_.take(8, D)
        g1bc = cf.take(D)
        add2rep = cb_.take(8, 128)
        wst = [cf.take(2, 2048) for _ in range(2)]
        wraw = cb_.take(8, 2048)
        bzrow = cf.take(2048)[0:1, :]
        bzrow_b = cb_.take(512)[0:1, :]
        for pc in range(4):
            sbf = wst[pc % 2]
            dma("sp", sbf[:], w_in[pc * 256:(pc + 1) * 256, :].rearrange("(k p) n -> p k n", p=128), [], ["wst%d" % (pc % 2)])
            for kk in range(2):
                kc = pc * 2 + kk
                A("dve", lambda e, sbf=sbf, kk=kk, kc=kc: e.tensor_scalar(out=w_in_bf[:, kc, :], in0=sbf[:, kk, :], scalar1=mul1c[:, kc:kc + 1],
                                                                       scalar2=None, op0=ALU.mult),
                  reads=["wst%d" % (pc % 2), "mul1c"], writes=["w_in_bf"])
                A("act", lambda e, sbf=sbf, kk=kk, kc=kc: e.activation(out=wraw[:, kc, :], in_=sbf[:, kk, :], func=AF.Copy),
                  reads=["wst%d" % (pc % 2)], writes=["wraw"])
        for cbk in range(4):
            for kc in range(8):
                A("pe", lambda e, cbk=cbk, kc=kc: e.matmul(pZ[0:1, cbk % 2, :], lhsT=add1b[:, kc:kc + 1], rhs=wraw[:, kc, cbk * 512:(cbk + 1) * 512],
                                                          start=(kc == 0), stop=(kc == 7)), reads=["add1b", "wraw"], writes=["pZ%d" % (cbk % 2)])
            A("dve", lambda e, cbk=cbk: e.tensor_copy(out=bzrow[:, cbk * 512:(cbk + 1) * 512], in_=pZ[0:1, cbk % 2, :]),
              reads=["pZ%d" % (cbk % 2)], writes=["bzrow"])
        for oc in range(12):
            A("pe", lambda e, oc=oc: e.matmul(pC[:, oc:oc + 1], lhsT=bzrow[:, oc * 128:(oc + 1) * 128], rhs=onesf[:, 0:1], start=True, stop=True),
              reads=["bzrow", "onesf"], writes=["B3"])
        A("dve", lambda e: e.tensor_copy(out=bzc[:], in_=pC[:, 0:12]), reads=["B3"], writes=["bzc"])
        A("pe", lambda e: e.matmul(pZ[:, 0, :], lhsT=onesf[:, :], rhs=bzrow[:, 1536:2048], start=True, stop=True),
          reads=["bzrow", "onesf"], writes=["pZ0"])
        A("dve", lambda e: e.tensor_copy(out=bzv[:], in_=pZ[:, 0, :]), reads=["pZ0"], writes=["bzv"])
        A("dve", lambda e: e.tensor_scalar(out=bzvm[:], in0=bzv[:], scalar1=hvt[:, 0:1], scalar2=None, op0=ALU.mult),
          reads=["bzv", "hvt"], writes=["bzvm"])
        wost = [wst[0][:, :, 0:D], wst[1][:, :, 0:D]]
        for pc in range(4):
            sbf = wost[pc % 2]
            dma("sp", sbf[:], w_out[pc * 256:(pc + 1) * 256, :].rearrange("(k p) n -> p k n", p=128), [], ["wst%d" % (pc % 2)])
            for kk in range(2):
                kc = pc * 2 + kk
                A("dve", lambda e, sbf=sbf, kk=kk, kc=kc: e.tensor_tensor(out=w_out_bf[:, kc, :], in0=sbf[:, kk, :], in1=g1bc[:], op=ALU.mult),
                  reads=["wst%d" % (pc % 2), "g1bc"], writes=["w_out_bf"])
        btf = cf.take(5, 128)
        pen = cf.take(5, 128)
        mk = cf.take(5, 128)
        dma("sp", mk[:], bmask, [], ["mk"])
        A("dve", lambda e: e.tensor_scalar(out=pen[:], in0=mk[:], scalar1=3750.0, scalar2=-3750.0, op0=ALU.mult, op1=ALU.add),
          reads=["mk"], writes=["pen"])
        for h in range(8):
            dma("sp", btf[:], btab[:, h, :, :], [], ["btf"])
            A("dve", lambda e: e.tensor_tensor(out=btf[:], in0=btf[:], in1=mk[:], op=ALU.mult), reads=["btf", "mk"], writes=["btf"])
            A("dve", lambda e, h=h: e.scalar_tensor_tensor(out=BT[:, h, :, :], in0=btf[:], scalar=0.125, in1=pen[:], op0=ALU.mult, op1=ALU.add),
              reads=["btf", "pen"], writes=["BT"])
        dma("sp", wr_f[:], wr.rearrange("(k p) n -> p k n", p=128), [], ["wr_f"])
        A("dve", lambda e: e.tensor_copy(out=wr_raw[:], in_=wr_f[:]), reads=["wr_f"], writes=["wr_raw"])
        for kc in range(8):
            A("dve", lambda e, kc=kc: e.tensor_scalar(out=wr_bf[:, kc, :], in0=wr_f[:, kc, :], scalar1=mul2c[:, kc:kc + 1], scalar2=None, op0=ALU.mult),
              reads=["wr_f", "mul2c"], writes=["wr_bf"])
            A("dve", lambda e, kc=kc: e.tensor_copy(out=add2rep[:, kc, :], in_=modc[:, 2, kc:kc + 1].to_broadcast([128, 128])),
              reads=["modc"], writes=["add2rep"])
        for kc in range(8):
            A("pe", lambda e, kc=kc: e.matmul(pC[:, 0:36], lhsT=add2rep[:, kc, :], rhs=wr_raw[:, kc, :], start=(kc == 0), stop=(kc == 7)),
              reads=["add2rep", "wr_raw"], writes=["B3"])
        A("dve", lambda e: e.tensor_tensor(out=biasR[:], in0=pC[:, 0:36], in1=biasR[:], op=ALU.add), reads=["B3", "biasR"], writes=["biasR"])
        p.barrier()

        cf = Carver(arf, AF_WORDS)
        cb_ = Carver(arb, AB_WORDS)
        w_in_bf = cb_.take(8, 2048)
        w_out_bf = cb_.take(8, D)
        xin = [cf.take(D) for _ in range(2)]
        xr = [cf.take(D) for _ in range(2)]
        xmd = [cf.take(D) for _ in range(2)]
        qf = [cf.take(512) for _ in range(3)]
        rq = [cf.take(512) for _ in range(3)]
        uT = [[cf.take(528) for _ in range(4)] for _ in range(2)]
        ptmp = [cf.take(528) for _ in range(2)]
        rden = cf.take(2, 4)
        xn = [cb_.take(D) for _ in range(2)]
        hT_ = cb_.take(8, 512)
        hT = [hT_, hT_]
        kT = cb_.take(4, RT * 128)
        Vr = cb_.take(RT, 8 * 65).rearrange("p r (h d) -> p r h d", h=8)
        qTm = [cb_.take(4, 512) for _ in range(2)]
        sq = [cb_.take(512) for _ in range(3)]
        pTt = cb_.take(4, 512)
        mixT_ = cb_.take(8, 512)
        mixT = [mixT_, mixT_]
        PTb = [cb_.take(2, 640) for _ in range(2)]
        att = [cb_.take(512) for _ in range(2)]
        xn2 = [cb_.take(D) for _ in range(2)]
        xn2T = [cb_.take(8, 128) for _ in range(2)]

        for b in range(2):
            for g in range(4):
                A("pool", lambda e, b=b, g=g: e.memset(uT[b][g][:, 0:16], 0.0), writes=["uT%d%d" % (b, g)])
        A("pool", lambda e: e.memset(qTm[0][64:128, :, :], 0.0), writes=["qT"])
        A("pool", lambda e: e.memset(qTm[1][0:64, :, :], 0.0), writes=["qT"])

        SSOFF = (0, 640)
        PVR = ((pS, 1280), (pV, 0), (pV, 256))
        HG = ((0, 1, 2), (3, 4, 5), (6, 7))

        def norm_and_transpose(src, srckey, sl, dstT, dstTkeys, dstcols, xnbuf, xnkey, store_to=None, scale_eng="dve", defer=False):
            A("act", lambda e: e.activation(out=xnbuf[:], in_=src, func=AF.Square, accum_out=ssr[:, sl:sl + 1]),
              reads=[srckey], writes=[xnkey, "ssr%d" % sl])
            A("act", lambda e: e.activation(out=rst[:, sl:sl + 1], in_=ssr[:, sl:sl + 1], func=AF.Ln, scale=1.0 / D, bias=epsc[:]),
              reads=["ssr%d" % sl, "epsc"], writes=["rst%d" % sl])
            A("act", lambda e: e.activation(out=rst[:, sl:sl + 1], in_=rst[:, sl:sl + 1], func=AF.Exp, scale=-0.5),
              reads=["rst%d" % sl], writes=["rst%d" % sl])
            if scale_eng == "dve":
                A("dve", lambda e: e.tensor_scalar(out=xnbuf[:], in0=src, scalar1=rst[:, sl:sl + 1], scalar2=None, op0=ALU.mult),
                  reads=[srckey, "rst%d" % sl], writes=[xnkey])
            else:
                A("act", lambda e: e.activation(out=xnbuf[:], in_=src, func=AF.Copy, scale=rst[:, sl:sl + 1]),
                  reads=[srckey, "rst%d" % sl], writes=[xnkey])
            if store_to is not None:
                dma("pool", store_to, xnbuf[:], [xnkey], ["xn2s"], dkey="st_" + xnkey)

            def part_b():
                for kc in range(8):
                    A("pe", lambda e, kc=kc: e.transpose(out=pT[:, kc * 128:(kc + 1) * 128], in_=xnbuf[:, kc * 128:(kc + 1) * 128], identity=ident[:]),
                      reads=[xnkey, "ident"], writes=["pT"])
                A("dve", lambda e: e.tensor_copy(out=dstT[:, :, dstcols], in_=pT[:].rearrange("p (a b) -> p a b", a=8)),
                  reads=["pT"], writes=dstTkeys)
            if defer:
                return part_b
            part_b()

        ZB = [(pZ[:, 0, :], "pZ0"), (pZ[:, 1, :], "pZ1"), (pS[:, 0:512], "B4"), (pS[:, 512:1024], "B5"), (pS[:, 1024:1536], "B6")]
        SB = [(pC, "B3"), (pV, "B7")]
        zcnt = {"z": 0, "s": 0}

        def in_chunk(s, oc, ub, slot0, halo_st, full_st):
            zps, zk = ZB[zcnt["z"] % 5]
            zcnt["z"] += 1
            kslots = ["kT%d" % (slot0 + i) for i in range(4)]
            for kc in range(8):
                A("pe", lambda e, kc=kc: e.matmul(zps, lhsT=w_in_bf[:, kc, oc * 128:(oc + 1) * 128], rhs=hT_[:, kc, :],
                                                  start=(kc == 0), stop=(kc == 7)), reads=["w_in_bf", "hT"], writes=[zk])
            if oc < 4:
                g = oc
                if halo_st:
                    A("dve", lambda e: e.tensor_scalar(out=uT[ub][g][:, 16:528], in0=zps, scalar1=bzc[:, oc:oc + 1],
                                                       scalar2=hvt[:, 0:1], op0=ALU.add, op1=ALU.mult),
                      reads=[zk, "bzc", "hvt"], writes=["uT%d%d" % (ub, g)])
                else:
                    A("dve", lambda e: e.tensor_scalar(out=uT[ub][g][:, 16:528], in0=zps, scalar1=bzc[:, oc:oc + 1],
                                                       scalar2=None, op0=ALU.add),
                      reads=[zk, "bzc"], writes=["uT%d%d" % (ub, g)])
                return None
            isq = oc < 8
            c = (oc - 4) % 4
            tb = oc % 3
            A("dve", lambda e: e.tensor_scalar(out=qf[tb][:], in0=zps, scalar1=bzc[:, oc:oc + 1], scalar2=None, op0=ALU.add),
              reads=[zk, "bzc"], writes=["qf%d" % tb])
            A("act", lambda e: e.activation(out=sq[tb][:], in_=qf[tb][:], func=AF.Square),
              reads=["qf%d" % tb], writes=["sq%d" % tb])
            sps, sk = SB[zcnt["s"] % 2]
            zcnt["s"] += 1

            def part2():
                A("pe", lambda e: e.matmul(sps[:], lhsT=blk1[:], rhs=sq[tb][:], start=True, stop=True), reads=["blk1", "sq%d" % tb], writes=[sk])
                A("act", lambda e: e.activation(out=rq[tb][:], in_=sps[:], func=AF.Ln, bias=eps64[:]), reads=[sk, "eps64"], writes=["rq%d" % tb])
                A("act", lambda e: e.activation(out=rq[tb][:], in_=rq[tb][:], func=AF.Exp, scale=-0.5), reads=["rq%d" % tb], writes=["rq%d" % tb])
                if isq:
                    A("dve", lambda e: e.tensor_tensor(out=qTm[0][0:64, c, :], in0=qf[tb][0:64, :], in1=rq[tb][0:64, :], op=ALU.mult),
                      reads=["qf%d" % tb, "rq%d" % tb], writes=["qT"])
                    A("dve", lambda e: e.tensor_tensor(out=qTm[1][64:128, c, :], in0=qf[tb][64:128, :], in1=rq[tb][64:128, :], op=ALU.mult),
                      reads=["qf%d" % tb, "rq%d" % tb], writes=["qT"])
                else:
                    A("dve", lambda e: e.scalar_tensor_tensor(out=kT[:, c, slot0 * 128:(slot0 + 4) * 128], in0=qf[tb][:], scalar=gqk[:, 0:1],
                                                              in1=rq[tb][:], op0=ALU.mult, op1=ALU.mult),
                      reads=["qf%d" % tb, "rq%d" % tb, "gqk"], writes=kslots)
            return part2

        def v_tile(s, i, halo_st):
            te = 4 * s + i
            sl = te % RT
            zps, zk = ZB[zcnt["z"] % 5]
            zcnt["z"] += 1
            for kc in range(8):
                A("pe", lambda e, kc=kc: e.matmul(zps, lhsT=hT_[:, kc, i * 128:(i + 1) * 128], rhs=w_in_bf[:, kc, 1536:2048],
                                                  start=(kc == 0), stop=(kc == 7)), reads=["w_in_bf", "hT"], writes=[zk])
            zv = zps.rearrange("p (h d) -> p h d", h=8)
            if halo_st:
                A("dve", lambda e: e.scalar_tensor_tensor(out=Vr[:, sl, :, 0:64], in0=zv, scalar=hvt[:, 0:1],
                                                          in1=bzvm[:].rearrange("p (h d) -> p h d", h=8), op0=ALU.mult, op1=ALU.add),
                  reads=[zk, "hvt", "bzvm"], writes=["V%d" % sl])
                A("pool", lambda e: e.tensor_copy(out=Vr[:, sl, :, 64:65], in_=hvt[:, 0:1].unsqueeze(1).to_broadcast([128, 8, 1])),
                  reads=["hvt"], writes=["V%d" % sl])
            else:
                A("dve", lambda e: e.tensor_tensor(out=Vr[:, sl, :, 0:64], in0=zv, in1=bzv[:].rearrange("p (h d) -> p h d", h=8), op=ALU.add),
                  reads=[zk, "bzv"], writes=["V%d" % sl])
                A("pool", lambda e: e.memset(Vr[:, sl, :, 64:65], 1.0), writes=["V%d" % sl])

        def pool_group(g, ub, first_main):
            U = uT[ub][g]
            uk = "uT%d%d" % (ub, g)
            cur, curk = U, uk
            sh = 1
            for stp in range(g + 1):
                dstb = ptmp[stp % 2]
                dk = "ptmp%d" % (stp % 2)
                lo = 2 * sh - 1
                A("pool", lambda e, cur=cur, dstb=dstb, lo=lo, sh=sh: e.tensor_tensor(out=dstb[:, lo:528], in0=cur[:, lo:528], in1=cur[:, lo - sh:528 - sh], op=ALU.add),
                  reads=[curk], writes=[dk])
                cur, curk = dstb, dk
                sh *= 2
            w = 2 ** (g + 1)
            fin = cur
            A("dve", lambda e: e.scalar_tensor_tensor(out=pTt[:, g, :], in0=fin[:, 16:528], scalar=1.0 / w, in1=U[:, 16:528],
                                                      op0=ALU.mult, op1=ALU.subtract),
              reads=[curk, uk], writes=["pTt%d" % g])
            if first_main:
                A("pool", lambda e: e.tensor_tensor(out=fin[:, 0:16], in0=fin[:, 16:32], in1=invct[:, g, :], op=ALU.mult),
                  reads=[curk, "invct"], writes=[curk])
                A("pool", lambda e: e.tensor_tensor(out=pTt[:, g, 0:16], in0=fin[:, 0:16], in1=U[:, 16:32], op=ALU.subtract),
                  reads=[curk, uk], writes=["pTt%d" % g])
            def part_b():
                sps, sk = SB[g % 2]
                A("pe", lambda e: e.matmul(sps[:], lhsT=pw_bf[:, g, :], rhs=pTt[:, g, :], start=True, stop=True), reads=["pw_bf", "pTt%d" % g], writes=[sk])
                A("act", lambda e: e.activation(out=mixT_[:, g, :], in_=sps[:], func=AF.Copy, scale=psc[:, g:g + 1]),
                  reads=[sk, "psc"], writes=["mixT"])
            return part_b

        def attn_pair(te, i, pr, ab):
            c = pr
            pb2 = pr % 2
            PTp = PTb[pb2]
            ptk = "PT%d" % pb2
            for hh in range(2):
                pb = 64 * hh
                h = 2 * pr + hh
                for t in range(4):
                    ksl = (te - 4 + t) % RT
                    A("pe", lambda e, t=t, ksl=ksl, pb=pb, hh=hh: e.matmul(
                        pS[:, hh * 512 + t * 128: hh * 512 + (t + 1) * 128], lhsT=kT[:, c, ksl * 128:(ksl + 1) * 128],
                        rhs=qTm[hh][:, c, i * 128:(i + 1) * 128], start=True, stop=False),
                      reads=["kT%d" % ksl, "qT"], writes=["B%d" % (4 + hh)])
                    A("pe", lambda e, t=t, h=h, hh=hh: e.matmul(pS[:, hh * 512 + t * 128: hh * 512 + (t + 1) * 128], lhsT=BT[:, h, t, :], rhs=ident[:],
                                                                start=False, stop=True), reads=["BT", "ident"], writes=["B%d" % (4 + hh)])
            ksl4 = te % RT
            for hh in range(2):
                pb = 64 * hh
                h = 2 * pr + hh
                A("pe", lambda e, pb=pb, hh=hh: e.matmul(
                    pS[:, 1024 + hh * 128: 1024 + (hh + 1) * 128], lhsT=kT[:, c, ksl4 * 128:(ksl4 + 1) * 128],
                    rhs=qTm[hh][:, c, i * 128:(i + 1) * 128], start=True, stop=False),
                  reads=["kT%d" % ksl4, "qT"], writes=["B6"])
                A("pe", lambda e, h=h, hh=hh: e.matmul(pS[:, 1024 + hh * 128: 1024 + (hh + 1) * 128], lhsT=BT[:, h, 4, :], rhs=ident[:],
                                                       start=False, stop=True), reads=["BT", "ident"], writes=["B6"])
            for hh in range(2):
                A("act", lambda e, hh=hh: e.activation(out=PTp[:, hh, 0:512], in_=pS[:, hh * 512:(hh + 1) * 512], func=AF.Exp, scale=8.0),
                  reads=["B%d" % (4 + hh)], writes=[ptk])
            A("act", lambda e: e.activation(out=PTp[:, :, 512:640], in_=pS[:, 1024:1280].rearrange("p (a b) -> p a b", a=2), func=AF.Exp, scale=8.0),
              reads=["B6"], writes=[ptk])
            for hh in range(2):
                h = 2 * pr + hh
                pvt, pvk = (pV, "B7") if h < 4 else (pC, "B3")
                co = (h % 4) * 65
                for t in range(5):
                    ksl = (te - 4 + t) % RT
                    A("pe", lambda e, t=t, ksl=ksl, hh=hh, h=h, pvt=pvt, co=co: e.matmul(
                        pvt[:, co: co + 65], lhsT=PTp[:, hh, t * 128:(t + 1) * 128], rhs=Vr[:, ksl, h, :],
                        start=(t == 0), stop=(t == 4)), reads=[ptk, "V%d" % ksl], writes=[pvk])

        def attn_norm(hgi, ab):
            pvt, pvk = (pV, "B7") if hgi == 0 else (pC, "B3")
            pvv = pvt[:, 0:260].rearrange("p (h d) -> p h d", h=4)
            A("dve", lambda e: e.tensor_scalar(out=rden[:, hgi, :].unsqueeze(2), in0=pvv[:, :, 64:65], scalar1=1e-30, scalar2=None, op0=ALU.add),
              reads=[pvk], writes=["rden%d" % hgi])
            A("dve", lambda e: e.reciprocal(out=rden[:, hgi, :], in_=rden[:, hgi, :]),
              reads=["rden%d" % hgi], writes=["rden%d" % hgi])
            A("dve", lambda e: e.tensor_tensor(
                out=att[ab][:, hgi * 256:(hgi + 1) * 256].rearrange("p (h d) -> p h d", h=4), in0=pvv[:, :, 0:64],
                in1=rden[:, hgi, :].unsqueeze(2).to_broadcast([128, 4, 64]), op=ALU.mult),
              reads=[pvk, "rden%d" % hgi], writes=["att%d" % ab])

        def attention_tile(s, i):
            te = 4 * s + i
            ab = te % 2
            for pr in range(4):
                attn_pair(te, i, pr, ab)
                if pr % 2 == 1:
                    attn_norm(pr // 2, ab)

        def post_attention(s, i):
            te = 4 * s + i
            tl = te - NKV
            ab = te % 2
            for c in range(4):
                A("pe", lambda e, c=c: e.transpose(out=pT[:, c * 128:(c + 1) * 128], in_=att[ab][:, c * 128:(c + 1) * 128], identity=ident[:]),
                  reads=["att%d" % ab, "ident"], writes=["pT"])
            A("dve", lambda e: e.tensor_copy(out=mixT_[:, 4:8, i * 128:(i + 1) * 128], in_=pT[:, 0:512].rearrange("p (a b) -> p a b", a=4)),
              reads=["pT"], writes=["mixT"])
            for cbk in range(2):
                for kc in range(8):
                    A("pe", lambda e, cbk=cbk, kc=kc: e.matmul(pZ[:, cbk, :], lhsT=mixT_[:, kc, i * 128:(i + 1) * 128], rhs=w_out_bf[:, kc, cbk * 512:(cbk + 1) * 512],
                                                               start=(kc == 0), stop=(kc == 7)), reads=["mixT", "w_out_bf"], writes=["pZ%d" % cbk])
            rb = te % 2
            dma("sp", xr[rb][:], xe[te * 128:(te + 1) * 128, :], [], ["xr%d" % rb])
            A("dve", lambda e: e.tensor_tensor(out=xmd[rb][:], in0=pZ[:].rearrange("p a b -> p (a b)"), in1=xr[rb][:], op=ALU.add),
              reads=["pZ0", "pZ1", "xr%d" % rb], writes=["xmd%d" % rb])
            dma("pool", xmid[tl * 128:(tl + 1) * 128, :], xmd[rb][:], ["xmd%d" % rb], ["xmid"], dkey="st_xmd%d" % rb)

            def norm2_a():
                pb_ = norm_and_transpose(xmd[rb][:], "xmd%d" % rb, te % 8, xn2T[rb], ["xn2T%d" % rb], slice(0, 128), xn2[rb], "xn2%d" % rb,
                                         store_to=xn2s[tl * 128:(tl + 1) * 128, :], scale_eng="act", defer=True)

                def part_b():
                    pb_()
                    for kc in range(8):
                        A("pe", lambda e, kc=kc: e.matmul(pC[:, 0:36], lhsT=xn2T[rb][:, kc, :], rhs=wr_bf[:, kc, :], start=(kc == 0), stop=(kc == 7)),
                          reads=["xn2T%d" % rb, "wr_bf"], writes=["B3"])
                    A("dve", lambda e: e.tensor_tensor(out=logits[:, tl, :], in0=pC[:, 0:36], in1=biasR[:], op=ALU.add),
                      reads=["B3", "biasR"], writes=["logits"])
                return part_b
            return norm2_a

        def tail_copy(ub, g):
            A("pool", lambda e: e.tensor_copy(out=uT[ub][g][:, 0:16], in_=uT[1 - ub][g][:, 512:528]),
              reads=["uT%d%d" % (1 - ub, g)], writes=["uT%d%d" % (ub, g)])

        def norm_tile(s, i, defer=False):
            te = 4 * s + i
            xi = te % 2
            dma("sp", xin[xi][:], xe[te * 128:(te + 1) * 128, :], [], ["xin%d" % xi])
            return norm_and_transpose(xin[xi][:], "xin%d" % xi, te % 8, hT_, ["hT"], slice(i * 128, (i + 1) * 128),
                                      xn[te % 2], "xn%d" % (te % 2), defer=defer)

        q_norm2 = []
        q_b = []

        def do_st(s):
            halo_st = (4 * s) < NKV + NFH
            full_st = (4 * s) >= NKV
            first_main = (4 * s) == NKV + NFH
            if s == 0:
                for i in range(4):
                    norm_tile(0, i)
            ub = s % 2
            if s > 0:
                for g in range(4):
                    tail_copy(ub, g)
            slot0 = (4 * s) % RT
            pend2 = []
            pool_b = []
            for oc in range(12):
                if 4 <= oc < 8 and not full_st:
                    continue
                p2 = in_chunk(s, oc, ub, slot0, halo_st, full_st)
                if oc == 3 and full_st:
                    for g in range(4):
                        pool_b.append(pool_group(g, ub, first_main))
                if len(pend2) > 1:
                    pend2.pop(0)()
                if p2 is not None:
                    pend2.append(p2)
            v_tile(s, 0, halo_st)
            if pend2:
                pend2.pop(0)()
            v_tile(s, 1, halo_st)
            if pend2:
                pend2.pop(0)()
            for i in range(2, 4):
                v_tile(s, i, halo_st)
            for i in range(4):
                nb = norm_tile(s + 1, i, defer=True) if s + 1 < NST else None
                if full_st:
                    if i == 0:
                        for pb2 in pool_b:
                            pb2()
                    attention_tile(s, i)
                    if q_norm2:
                        q_b.append(q_norm2.pop(0)())
                    if len(q_b) > 1:
                        q_b.pop(0)()
                if nb is not None:
                    nb()
                if full_st:
                    q_norm2.append(post_attention(s, i))
            if s == NST - 1:
                while q_norm2:
                    q_b.append(q_norm2.pop(0)())
                while q_b:
                    q_b.pop(0)()

        for s in range(NST):
            do_st(s)
        p.barrier()

        cf = Carver(arf, AF_WORDS)
        cb_ = Carver(arb, AB_WORDS)
        NL = NTL
        R1 = cf.take(NL, 32)
        R2 = cf.take(NL, 32)
        R3 = cf.take(NL, 32)
        R4 = cf.take(NL, 32)
        sm = [cf.take(NL) for _ in range(6)]
        cntb = [cf.take(32) for _ in range(2)]
        startb = cf.take(32)
        widf = cf.take(NEXP, CT)
        ybuf = [cf.take(D) for _ in range(2)]
        sgb = [cf.take(CAP) for _ in range(2)]
        xmb = [cf.take(D) for _ in range(2)]
        y1b = [cf.take(D) for _ in range(2)]
        y2b = [cf.take(D) for _ in range(2)]
        Abf = cb_.take(NL, 32)
        xtl = [cb_.take(D) for _ in range(4)]
        xw = [cb_.take(CT, D) for _ in range(2)]
        xsT = cb_.take(8, CAP)
        actT = cb_.take(4, CAP)
        Wg = [cb_.take(8, 512) for _ in range(2)]
        Wu = [cb_.take(8, 512) for _ in range(2)]
        Wd = [cb_.take(4, D) for _ in range(2)]

        WGK = [["Wg%d_%d" % (b, kc) for kc in range(8)] for b in range(2)]
        WUK = [["Wu%d_%d" % (b, kc) for kc in range(8)] for b in range(2)]
        WDK = [["Wd%d_%d" % (b, jc) for jc in range(4)] for b in range(2)]

        def load_w(e_):
            b = e_ % 2
            dma("pool", Wg[b][:].rearrange("p k n -> p (k n)"), wg[e_ * 128:(e_ + 1) * 128, :], [], WGK[b], dkey="Wg%d" % b)
            dma("pool", Wu[b][:].rearrange("p k n -> p (k n)"), wu[e_ * 128:(e_ + 1) * 128, :], [], WUK[b], dkey="Wu%d" % b)
            dma("pool", Wd[b][:], wd[e_ * 512:(e_ + 1) * 512, :].rearrange("(k p) n -> p k n", p=128), [], WDK[b], dkey="Wd%d" % b)

        load_w(0)
        load_w(1)

        gl = logits[:, :, 0:4]
        el = logits[:, :, 4:36]
        V = lambda e: e
        gmax, gsum, m1, m2, dd, ee = sm
        gone = R1[:, :, 0:4]
        A("dve", lambda e: e.reduce_max(out=gmax[:], in_=gl, axis=AX.X), reads=["logits"], writes=["gmax"])
        A("dve", lambda e: e.tensor_tensor(out=gone, in0=gl, in1=gmax[:].unsqueeze(2).to_broadcast([128, NL, 4]), op=ALU.is_equal),
          reads=["logits", "gmax"], writes=["R1"])
        gex = R2[:, :, 0:4]
        A("dve", lambda e: e.tensor_tensor(out=gex, in0=gl, in1=gmax[:].unsqueeze(2).to_broadcast([128, NL, 4]), op=ALU.subtract),
          reads=["logits", "gmax"], writes=["R2"])
        A("act", lambda e: e.activation(out=gex, in_=gex, func=AF.Exp), reads=["R2"], writes=["R2"])
        A("dve", lambda e: e.reduce_sum(out=gsum[:], in_=gex, axis=AX.X), reads=["R2"], writes=["gsum"])
        A("dve", lambda e: e.reciprocal(out=gsum[:], in_=gsum[:]), reads=["gsum"], writes=["gsum"])
        BIG = 1.0e4
        A("dve", lambda e: e.tensor_scalar(out=gone, in0=gone, scalar1=BIG, scalar2=-BIG, op0=ALU.mult, op1=ALU.add), reads=["R1"], writes=["R1"])
        em = R3
        A("dve", lambda e: e.tensor_tensor(out=em[:].rearrange("p n (g j) -> p n g j", g=4), in0=el.rearrange("p n (g j) -> p n g j", g=4),
                                           in1=gone.unsqueeze(3).to_broadcast([128, NL, 4, 8]), op=ALU.add), reads=["logits", "R1"], writes=["R3"])
        A("dve", lambda e: e.reduce_max(out=m1[:], in_=em[:], axis=AX.X), reads=["R3"], writes=["m1"])
        oh1 = R1
        A("dve", lambda e: e.tensor_tensor(out=oh1[:], in0=em[:], in1=m1[:].unsqueeze(2).to_broadcast([128, NL, 32]), op=ALU.is_equal),
          reads=["R3", "m1"], writes=["R1"])
        em2 = R2
        A("dve", lambda e: e.scalar_tensor_tensor(out=em2[:], in0=oh1[:], scalar=-BIG, in1=em[:], op0=ALU.mult, op1=ALU.add),
          reads=["R1", "R3"], writes=["R2"])
        A("dve", lambda e: e.reduce_max(out=m2[:], in_=em2[:], axis=AX.X), reads=["R2"], writes=["m2"])
        oh2 = R3
        A("dve", lambda e: e.tensor_tensor(out=oh2[:], in0=em2[:], in1=m2[:].unsqueeze(2).to_broadcast([128, NL, 32]), op=ALU.is_equal),
          reads=["R2", "m2"], writes=["R3"])
        A("dve", lambda e: e.tensor_tensor(out=dd[:], in0=m2[:], in1=m1[:], op=ALU.subtract), reads=["m1", "m2"], writes=["dd"])
        A("act", lambda e: e.activation(out=ee[:], in_=dd[:], func=AF.Exp), reads=["dd"], writes=["ee"])
        A("dve", lambda e: e.tensor_scalar(out=dd[:], in0=ee[:], scalar1=1.0, scalar2=None, op0=ALU.add), reads=["ee"], writes=["dd"])
        A("dve", lambda e: e.reciprocal(out=dd[:], in_=dd[:]), reads=["dd"], writes=["dd"])
        A("dve", lambda e: e.tensor_tensor(out=ee[:], in0=ee[:], in1=dd[:], op=ALU.mult), reads=["ee", "dd"], writes=["ee"])
        A("dve", lambda e: e.tensor_tensor(out=w1g[:], in0=dd[:], in1=gsum[:], op=ALU.mult), reads=["dd", "gsum"], writes=["w1g"])
        A("dve", lambda e: e.tensor_tensor(out=w2g[:], in0=ee[:], in1=gsum[:], op=ALU.mult), reads=["ee", "gsum"], writes=["w2g"])
        Asum = R2
        A("dve", lambda e: e.tensor_tensor(out=Asum[:], in0=oh1[:], in1=oh2[:], op=ALU.add), reads=["R1", "R3"], writes=["R2"])
        if NFH > 0:
            A("dve", lambda e: e.tensor_scalar(out=Asum[:, 0:NFH, :], in0=Asum[:, 0:NFH, :], scalar1=hvt[:, 0:1], scalar2=None, op0=ALU.mult),
              reads=["R2", "hvt"], writes=["R2"])
        A("dve", lambda e: e.tensor_copy(out=Abf[:], in_=Asum[:]), reads=["R2"], writes=["Abf"])
        Af = Abf[:].rearrange("p n e -> p (n e)")
        ncol = NL * 32
        banks = [(pZ[:, 0, :], "pZ0"), (pZ[:, 1, :], "pZ1"), (pC[:], "B3"), (pV[:], "B7")]
        assert ncol <= 1536
        Rk = R4[:].rearrange("p n e -> p (n e)")
        Tt = R2[:].rearrange("p n e -> p (n e)")
        for (lhs, lk, dst, dk) in ((tri_bf, "tri_bf", Rk, "R4"), (ones_bf, "ones_bf", Tt, "R2")):
            for c0 in range(0, ncol, 512):
                cw = min(512, ncol - c0)
                A("pe", lambda e, lhs=lhs, c0=c0, cw=cw: e.matmul(pS[:, c0:c0 + cw], lhsT=lhs[:], rhs=Af[:, c0:c0 + cw], start=True, stop=True),
                  reads=[lk, "Abf"], writes=["B4", "B5", "B6"])
            A("dve", lambda e, dst=dst: e.tensor_copy(out=dst, in_=pS[:, 0:ncol]), reads=["B4", "B5", "B6"], writes=[dk])
        A("dve", lambda e: e.memset(cntb[0][:], 0.0), writes=["cnt"])
        for n in range(NL):
            if n > 0:
                A("dve", lambda e, n=n: e.tensor_tensor(out=R4[:, n, :], in0=R4[:, n, :], in1=cntb[0][:], op=ALU.add), reads=["R4", "cnt"], writes=["R4"])
            A("dve", lambda e, n=n: e.tensor_tensor(out=cntb[0][:], in0=cntb[0][:], in1=R2[:, n, :], op=ALU.add), reads=["R2", "cnt"], writes=["cnt"])
        A("dve", lambda e: e.memset(startb[:], 0.0), writes=["startb"])
        for j in range(1, 32):
            A("dve", lambda e, j=j: e.tensor_tensor(out=startb[:, j:j + 1], in0=startb[:, j - 1:j], in1=cntb[0][:, j - 1:j], op=ALU.add),
              reads=["startb", "cnt"], writes=["startb"])
        A("dve", lambda e: e.tensor_tensor(out=R4[:], in0=R4[:], in1=startb[:].unsqueeze(1).to_broadcast([128, NL, 32]), op=ALU.add),
          reads=["R4", "startb"], writes=["R4"])
        for ki, (oh, ohk, sl_i, slk, tmpk) in enumerate(((oh1, "R1", slot1, "slot1", "gmax"), (oh2, "R3", slot2, "slot2", "m1"))):
            tmp = gmax if tmpk == "gmax" else m1
            A("dve", lambda e, oh=oh: e.tensor_tensor(out=oh[:], in0=oh[:], in1=R4[:], op=ALU.mult), reads=[ohk, "R4"], writes=[ohk])
            A("dve", lambda e, oh=oh, tmp=tmp: e.reduce_sum(out=tmp[:], in_=oh[:], axis=AX.X), reads=[ohk], writes=[tmpk])
            if NFH > 0:
                A("dve", lambda e, tmp=tmp: e.tensor_scalar(out=tmp[:, 0:NFH], in0=tmp[:, 0:NFH], scalar1=hvt[:, 0:1], scalar2=None, op0=ALU.mult),
                  reads=[tmpk, "hvt"], writes=[tmpk])
                A("dve", lambda e, tmp=tmp, ki=ki: e.scalar_tensor_tensor(out=tmp[:, 0:NFH], in0=trt[:, ki * TRC: ki * TRC + NFH], scalar=nhvt[:, 0:1], in1=tmp[:, 0:NFH],
                                                                 op0=ALU.mult, op1=ALU.add), reads=[tmpk, "trt", "nhvt"], writes=[tmpk])
            A("dve", lambda e, tmp=tmp, sl_i=sl_i: e.tensor_copy(out=sl_i[:], in_=tmp[:]), reads=[tmpk], writes=[slk])
        A("dve", lambda e: e.tensor_tensor(out=widf[:], in0=startb[:].unsqueeze(2).to_broadcast([128, NEXP, CT]),
                                           in1=iott[:, 0:CT].unsqueeze(1).to_broadcast([128, NEXP, CT]), op=ALU.add),
          reads=["startb", "iott"], writes=["widf"])
        A("dve", lambda e: e.tensor_copy(out=widx[:], in_=widf[:]), reads=["widf"], writes=["widx"])

        if debug:
            dma("sp", dbg_logits, logits[:], ["logits"], ["dbg_logits"])
            dma("sp", dbg_w[:, 0, :], w1g[:], ["w1g"], ["dbg_w1"])
            dma("sp", dbg_w[:, 1, :], w2g[:], ["w2g"], ["dbg_w2"])
            dma("sp", dbg_slot[:, 0, :], slot1[:], ["slot1"], ["dbg_s1"])
            dma("sp", dbg_slot[:, 1, :], slot2[:], ["slot2"], ["dbg_s2"])
        xskeys = []
        for tl in range(NTL):
            b = tl % 4
            dma("sp", xtl[b][:], xn2s[tl * 128:(tl + 1) * 128, :], ["xn2s"], ["xtl%d" % b])
            for k_, (sl_i, slk) in enumerate(((slot1, "slot1"), (slot2, "slot2"))):
                key = "xs_%d_%d" % (tl, k_)
                xskeys.append(key)
                A("pool", lambda e, sl_i=sl_i, tl=tl, b=b: e.indirect_dma_start(
                    out=xs[:, :], out_offset=bass.IndirectOffsetOnAxis(ap=sl_i[:, tl:tl + 1], axis=0), in_=xtl[b][:], in_offset=None),
                  reads=["xtl%d" % b, slk], writes=[key], dma=True, dkey="sc_xtl%d" % b)

        NOW, NTHR = cfg.NOW, cfg.NTHR
        BIGI = 1.0e6
        cnt_ = cntb[0]
        assert NOW <= NL and NTHR <= NL
        gtm = R2[:, 0:NTHR, :].rearrange("p t e -> p (t e)").rearrange("p (e t) -> p e t", e=32)
        nov = cf.take(32)
        cum = cf.take(32)
        indw = R1[:, 0:NOW, :]
        tmpw = R3[:, 0:NOW, :]
        limv = cf.take(32)
        jbase = cf.take(32)
        wsc = [cf.take(NOW) for _ in range(5)]
        gidf = cf.take(NOW, CT)
        yidf = cf.take(NOW, CT)
        mskf = cf.take(NOW, CT)
        wgidf = cf.take(NOW, 8)
        wdidf = cf.take(NOW, 4)
        gidx = T("gidx", [128, NOW, CT], I32)
        yidx = T("yidx", [128, NOW, CT], I32)
        wgidx = T("wgidx", [128, NOW, 8], I32)
        wdidx = T("wdidx", [128, NOW, 4], I32)
        DV = lambda fn, r, w: A("dve", fn, reads=r, writes=w)
        DV(lambda e: e.tensor_tensor(out=gtm[:], in0=cnt_[:].unsqueeze(2).to_broadcast([128, 32, NTHR]),
                                     in1=thrt[:, 0:NTHR].unsqueeze(1).to_broadcast([128, 32, NTHR]), op=ALU.is_gt), ["cnt", "thrt"], ["R2"])
        DV(lambda e: e.reduce_sum(out=nov[:], in_=gtm[:], axis=AX.X), ["R2"], ["nov"])
        DV(lambda e: e.memset(cum[:], 0.0), [], ["cum"])
        for j in range(1, 32):
            DV(lambda e, j=j: e.tensor_tensor(out=cum[:, j:j + 1], in0=cum[:, j - 1:j], in1=nov[:, j - 1:j], op=ALU.add), ["cum", "nov"], ["cum"])
        wvb = wvt[:, 0:NOW].unsqueeze(2).to_broadcast([128, NOW, 32])
        DV(lambda e: e.tensor_tensor(out=indw[:], in0=cum[:].unsqueeze(1).to_broadcast([128, NOW, 32]), in1=wvb, op=ALU.is_le), ["cum", "wvt"], ["R1"])
        DV(lambda e: e.tensor_tensor(out=limv[:], in0=cum[:], in1=nov[:], op=ALU.add), ["cum", "nov"], ["limv"])
        DV(lambda e: e.tensor_tensor(out=tmpw[:], in0=limv[:].unsqueeze(1).to_broadcast([128, NOW, 32]), in1=wvb, op=ALU.is_gt), ["limv", "wvt"], ["R3"])
        DV(lambda e: e.tensor_tensor(out=indw[:], in0=indw[:], in1=tmpw[:], op=ALU.mult), ["R1", "R3"], ["R1"])
        vld, ew, ow, lw, tw = wsc
        DV(lambda e: e.reduce_sum(out=vld[:], in_=indw[:], axis=AX.X), ["R1"], ["vld"])
        DV(lambda e: e.tensor_tensor(out=tmpw[:], in0=indw[:], in1=evt[:].unsqueeze(1).to_broadcast([128, NOW, 32]), op=ALU.mult), ["R1", "evt"], ["R3"])
        DV(lambda e: e.reduce_sum(out=ew[:], in_=tmpw[:], axis=AX.X), ["R3"], ["ew"])
        DV(lambda e: e.tensor_scalar(out=jbase[:], in0=cum[:], scalar1=-float(CAP), scalar2=float(CAP), op0=ALU.mult, op1=ALU.add), ["cum"], ["jbase"])
        DV(lambda e: e.tensor_tensor(out=jbase[:], in0=jbase[:], in1=startb[:], op=ALU.add), ["jbase", "startb"], ["jbase"])
        DV(lambda e: e.tensor_tensor(out=tmpw[:], in0=indw[:], in1=jbase[:].unsqueeze(1).to_broadcast([128, NOW, 32]), op=ALU.mult), ["R1", "jbase"], ["R3"])
        DV(lambda e: e.reduce_sum(out=ow[:], in_=tmpw[:], axis=AX.X), ["R3"], ["ow"])
        DV(lambda e: e.scalar_tensor_tensor(out=ow[:], in0=wvt[:, 0:NOW], scalar=float(CAP), in1=ow[:], op0=ALU.mult, op1=ALU.add), ["ow", "wvt"], ["ow"])
        DV(lambda e: e.tensor_tensor(out=ow[:], in0=ow[:], in1=vld[:], op=ALU.mult), ["ow", "vld"], ["ow"])
        DV(lambda e: e.tensor_tensor(out=limv[:], in0=startb[:], in1=cnt_[:], op=ALU.add), ["startb", "cnt"], ["limv"])
        DV(lambda e: e.tensor_tensor(out=tmpw[:], in0=indw[:], in1=limv[:].unsqueeze(1).to_broadcast([128, NOW, 32]), op=ALU.mult), ["R1", "limv"], ["R3"])
        DV(lambda e: e.reduce_sum(out=lw[:], in_=tmpw[:], axis=AX.X), ["R3"], ["lw"])
        DV(lambda e: e.tensor_scalar(out=tw[:], in0=vld[:], scalar1=-BIGI, scalar2=BIGI, op0=ALU.mult, op1=ALU.add), ["vld"], ["tw"])
        DV(lambda e: e.tensor_tensor(out=gidf[:], in0=ow[:].unsqueeze(2).to_broadcast([128, NOW, CT]),
                                     in1=iott[:, 0:CT].unsqueeze(1).to_broadcast([128, NOW, CT]), op=ALU.add), ["ow", "iott"], ["gidf"])
        DV(lambda e: e.tensor_tensor(out=mskf[:], in0=gidf[:], in1=lw[:].unsqueeze(2).to_broadcast([128, NOW, CT]), op=ALU.is_lt), ["gidf", "lw"], ["mskf"])
        DV(lambda e: e.scalar_tensor_tensor(out=yidf[:], in0=gidf[:], scalar=-BIGI, in1=mskf[:], op0=ALU.add, op1=ALU.mult), ["gidf", "mskf"], ["yidf"])
        DV(lambda e: e.tensor_scalar(out=yidf[:], in0=yidf[:], scalar1=BIGI, scalar2=None, op0=ALU.add), ["yidf"], ["yidf"])
        DV(lambda e: e.tensor_tensor(out=gidf[:], in0=gidf[:], in1=tw[:].unsqueeze(2).to_broadcast([128, NOW, CT]), op=ALU.add), ["gidf", "tw"], ["gidf"])
        DV(lambda e: e.tensor_copy(out=gidx[:], in_=gidf[:]), ["gidf"], ["gidx"])
        DV(lambda e: e.tensor_copy(out=yidx[:], in_=yidf[:]), ["yidf"], ["yidx"])
        DV(lambda e: e.scalar_tensor_tensor(out=ew[:], in0=ew[:], scalar=1024.0, in1=tw[:], op0=ALU.mult, op1=ALU.add), ["ew", "tw"], ["ew"])
        DV(lambda e: e.tensor_tensor(out=wgidf[:], in0=ew[:].unsqueeze(2).to_broadcast([128, NOW, 8]),
                                     in1=iot8t[:].unsqueeze(1).to_broadcast([128, NOW, 8]), op=ALU.add), ["ew", "iot8t"], ["wgidf"])
        DV(lambda e: e.tensor_copy(out=wgidx[:], in_=wgidf[:]), ["wgidf"], ["wgidx"])
        DV(lambda e: e.scalar_tensor_tensor(out=lw[:], in0=ew[:], scalar=0.125, in1=tw[:], op0=ALU.mult, op1=ALU.add), ["ew", "tw"], ["lw"])
        DV(lambda e: e.tensor_scalar(out=lw[:], in0=lw[:], scalar1=iott[:, 0:1], scalar2=None, op0=ALU.add), ["lw", "iott"], ["lw"])
        DV(lambda e: e.tensor_copy(out=wgidx2[:, 0:NOW], in_=lw[:]), ["lw"], ["wgidx2"])
        DV(lambda e: e.scalar_tensor_tensor(out=ew[:], in0=ew[:], scalar=0.5, in1=tw[:], op0=ALU.mult, op1=ALU.add), ["ew", "tw"], ["ew"])
        DV(lambda e: e.tensor_tensor(out=wdidf[:], in0=ew[:].unsqueeze(2).to_broadcast([128, NOW, 4]),
                                     in1=iot8t[:, 0:4].unsqueeze(1).to_broadcast([128, NOW, 4]), op=ALU.add), ["ew", "iot8t"], ["wdidf"])
        DV(lambda e: e.tensor_copy(out=wdidx[:], in_=wdidf[:]), ["wdidf"], ["wdidx"])

        gbanks = [(pZ[:, 0, :], "pZ0"), (pZ[:, 1, :], "pZ1"), (pC[:], "B3"), (pV[:], "B7")]
        dbanks = [(pS[:, 512:1024], "B5"), (pS[:, 1024:1536], "B6")]
        pT2 = pS[:, 0:512].bitcast(BF16)
        tbanks = [(pT, "pT"), (pT2, "B4")]
        cnts = {"gi": 0, "di": 0, "ti": 0}
        NJOB = NEXP + NOW
        wg2, wu2, wd2 = wg, wu, wd

        bregs = memo.setdefault("__bregs", {})

        def breg(e, val):
            if val not in bregs:
                r = e.alloc_register("bc%d" % val)
                e.reg_mov(r, val)
                bregs[val] = r
            return bregs[val]

        def job_rows(k, j, for_y):
            if k < NEXP:
                return widx[:, k, j:j + 1]
            return (yidx if for_y else gidx)[:, k - NEXP, j:j + 1]

        def job_load_w(k):
            b = k % 2
            if k < NEXP:
                load_w(k)
                return
            w = k - NEXP
            og, ou, od = [], [], []
            og.append(A("pool", lambda e: e.indirect_dma_start(out=Wg[b][:].rearrange("p k n -> p (k n)"), out_offset=None, in_=wg2,
                                                               in_offset=bass.IndirectOffsetOnAxis(ap=wgidx2[:, w:w + 1], axis=0),
                                                               bounds_check=breg(e, NEXP * 128 - 1), oob_is_err=False),
                        reads=["wgidx2"], writes=WGK[b], dma=True, dkey="Wg%d" % b))
            ou.append(A("pool", lambda e: e.indirect_dma_start(out=Wu[b][:].rearrange("p k n -> p (k n)"), out_offset=None, in_=wu2,
                                                               in_offset=bass.IndirectOffsetOnAxis(ap=wgidx2[:, w:w + 1], axis=0),
                                                               bounds_check=breg(e, NEXP * 128 - 1), oob_is_err=False),
                        reads=["wgidx2"], writes=WUK[b], dma=True, dkey="Wu%d" % b))
            for jc in range(4):
                od.append(A("pool", lambda e, jc=jc: e.indirect_dma_start(out=Wd[b][:, jc, :], out_offset=None, in_=wd2,
                                                                          in_offset=bass.IndirectOffsetOnAxis(ap=wdidx[:, w, jc:jc + 1], axis=0),
                                                                          bounds_check=breg(e, NEXP * 512 - 1), oob_is_err=False),
                            reads=["wdidx"], writes=[WDK[b][jc]], dma=True, dkey="Wd%d" % b))
            for grp in (og, ou, od):
                for o_ in grp:
                    o_.tgt = grp[-1].tgt

        def job_gather(k):
            b = k % 2
            for j in range(CT):
                rows = job_rows(k, j, False)
                if k < NEXP:
                    A("pool", lambda e, j=j, rows=rows: e.indirect_dma_start(
                        out=xw[b][:, j, :], out_offset=None, in_=xs[:, :], in_offset=bass.IndirectOffsetOnAxis(ap=rows, axis=0)),
                      reads=xskeys + ["widx"], writes=["xw%d_%d" % (b, j)], dma=True)
                else:
                    A("pool", lambda e, j=j, rows=rows: e.indirect_dma_start(
                        out=xw[b][:, j, :], out_offset=None, in_=xs[:, :], in_offset=bass.IndirectOffsetOnAxis(ap=rows, axis=0),
                        bounds_check=breg(e, cfg.XSR - 1), oob_is_err=False),
                      reads=xskeys + ["gidx"], writes=["xw%d_%d" % (b, j)], dma=True)

        xsT2 = [xsT, cb_.take(8, CAP)]
        y1b = y1b + [cb_.take(2 * D).bitcast(F32)]
        y2b = y2b + [cb_.take(2 * D).bitcast(F32)]
        NYB = 3

        def job_T(k):
            b = k % 2
            xs_ = xsT2[k % 2]
            for kc in range(8):
                (tps, tk_) = tbanks[cnts["ti"] % 2]
                cnts["ti"] += 1
                for j in range(CT):
                    A("pe", lambda e, kc=kc, j=j, tps=tps: e.transpose(out=tps[:, j * 128:(j + 1) * 128],
                                                                       in_=xw[b][:, j, :].rearrange("s (p k) -> s k p", k=8)[:, kc, :], identity=ident[:]),
                      reads=["xw%d_%d" % (b, j), "ident"], writes=[tk_])
                A("act", lambda e, kc=kc, tps=tps: e.activation(out=xs_[:, kc, :], in_=tps[:, 0:CAP], func=AF.Identity,
                                                                scale=mul2a[:, kc:kc + 1], bias=modca[:, 0, kc:kc + 1]),
                  reads=[tk_, "mul2a", "modca"], writes=["xsT%d_%d" % (k % 2, kc)])

        def job_GU(k):
            b = k % 2
            xs_ = xsT2[k % 2]
            for jc in range(4):
                (gps, gk_) = gbanks[cnts["gi"] % 4]
                (ups, uk_) = gbanks[(cnts["gi"] + 1) % 4]
                cnts["gi"] += 2
                for kc in range(8):
                    A("pe", lambda e, gps=gps, kc=kc, jc=jc: e.matmul(gps[:, 0:CAP], lhsT=Wg[b][:, kc, jc * 128:(jc + 1) * 128], rhs=xs_[:, kc, :],
                                                                     start=(kc == 0), stop=(kc == 7)), reads=WGK[b] + ["xsT%d_%d" % (k % 2, kc)], writes=[gk_])
                for kc in range(8):
                    A("pe", lambda e, ups=ups, kc=kc, jc=jc: e.matmul(ups[:, 0:CAP], lhsT=Wu[b][:, kc, jc * 128:(jc + 1) * 128], rhs=xs_[:, kc, :],
                                                                     start=(kc == 0), stop=(kc == 7)), reads=WUK[b] + ["xsT%d_%d" % (k % 2, kc)], writes=[uk_])
                sb_ = jc % 2
                A("act", lambda e, gps=gps, sb_=sb_: e.activation(out=sgb[sb_][:], in_=gps[:, 0:CAP], func=AF.Silu), reads=[gk_], writes=["sg%d" % sb_])
                A("dve", lambda e, ups=ups, sb_=sb_, jc=jc: e.tensor_tensor(out=actT[:, jc, :], in0=ups[:, 0:CAP], in1=sgb[sb_][:], op=ALU.mult),
                  reads=[uk_, "sg%d" % sb_], writes=["actT"])

        def job_D(k):
            b = k % 2
            for j in range(CT):
                yb = (k * CT + j) % 2
                for cbk in range(2):
                    (dps, dk_) = dbanks[cnts["di"] % 2]
                    cnts["di"] += 1
                    for jc in range(4):
                        A("pe", lambda e, dps=dps, jc=jc, j=j, cbk=cbk: e.matmul(dps, lhsT=actT[:, jc, j * 128:(j + 1) * 128], rhs=Wd[b][:, jc, cbk * 512:(cbk + 1) * 512],
                                                                                start=(jc == 0), stop=(jc == 3)), reads=["actT"] + WDK[b], writes=[dk_])
                    A("dve", lambda e, dps=dps, cbk=cbk, yb=yb: e.tensor_tensor(out=ybuf[yb][:, cbk * 512:(cbk + 1) * 512], in0=dps, in1=g2bc[:, cbk * 512:(cbk + 1) * 512], op=ALU.mult),
                      reads=[dk_, "g2bc"], writes=["ybuf%d" % yb])
                rows = job_rows(k, j, True)
                if k < NEXP:
                    A("pool", lambda e, rows=rows, yb=yb: e.indirect_dma_start(
                        out=ys[:, :], out_offset=bass.IndirectOffsetOnAxis(ap=rows, axis=0), in_=ybuf[yb][:], in_offset=None),
                      reads=["ybuf%d" % yb, "widx"], writes=["ys"], dma=True, dkey="sc_ybuf%d" % yb)
                else:
                    A("pool", lambda e, rows=rows, yb=yb: e.indirect_dma_start(
                        out=ys[:, :], out_offset=bass.IndirectOffsetOnAxis(ap=rows, axis=0), in_=ybuf[yb][:], in_offset=None,
                        bounds_check=breg(e, cfg.XSR - 1), oob_is_err=False),
                      reads=["ybuf%d" % yb, "yidx"], writes=["ys"], dma=True, dkey="sc_ybuf%d" % yb)

        job_gather(0)
        job_gather(1)
        job_T(0)
        for k in range(NJOB):
            job_GU(k)
            if k + 1 < NJOB:
                job_T(k + 1)
            if k + 2 < NJOB:
                job_gather(k + 2)
            job_D(k)
            if k + 2 < NJOB:
                job_load_w(k + 2)

        for tl in range(NTL):
            b = tl % 2
            yb3 = tl % NYB
            dma("sp", xmb[b][:], xmid[tl * 128:(tl + 1) * 128, :], ["xmid"], ["xmb%d" % b])
            A("pool", lambda e, tl=tl, b=b, yb3=yb3: e.indirect_dma_start(out=y1b[yb3][:], out_offset=None, in_=ys[:, :],
                                                                 in_offset=bass.IndirectOffsetOnAxis(ap=slot1[:, tl:tl + 1], axis=0)),
              reads=["ys", "slot1"], writes=["y1b%d" % yb3], dma=True)
            A("pool", lambda e, tl=tl, b=b, yb3=yb3: e.indirect_dma_start(out=y2b[yb3][:], out_offset=None, in_=ys[:, :],
                                                                 in_offset=bass.IndirectOffsetOnAxis(ap=slot2[:, tl:tl + 1], axis=0)),
              reads=["ys", "slot2"], writes=["y2b%d" % yb3], dma=True)
            A("dve", lambda e, tl=tl, b=b, yb3=yb3: e.scalar_tensor_tensor(out=xmb[b][:], in0=y1b[yb3][:], scalar=w1g[:, tl:tl + 1], in1=xmb[b][:], op0=ALU.mult, op1=ALU.add),
              reads=["y1b%d" % yb3, "w1g", "xmb%d" % b], writes=["xmb%d" % b])
            A("dve", lambda e, tl=tl, b=b, yb3=yb3: e.scalar_tensor_tensor(out=xmb[b][:], in0=y2b[yb3][:], scalar=w2g[:, tl:tl + 1], in1=xmb[b][:], op0=ALU.mult, op1=ALU.add),
              reads=["y2b%d" % yb3, "w2g", "xmb%d" % b], writes=["xmb%d" % b])
            dma("act", xo[tl * 128:(tl + 1) * 128, :], xmb[b][:], ["xmb%d" % b], ["xo_%d" % tl], dkey="st_xmb%d" % b)
        p.barrier()


def _colform(v):
    return np.ascontiguousarray(v.reshape(-1, 128).T).astype(np.float32)


def _const_tables(cfg):
    q = np.arange(128)[:, None]
    tabs_idx = np.zeros((128, 5, 128), np.int64)
    mask = np.zeros((128, 5, 128), np.float32)
    for t in range(5):
        k = np.arange(128)[None, :]
        rel = 128 * (4 - t) + q - k
        tabs_idx[:, t, :] = np.clip(rel, -128, 128) + 128
        qc = q // 64
        kc = 2 * (t - 4) + k // 64
        ok = (kc <= qc) & (kc >= qc - 8)
        mask[:, t, :] = ok
    tri = (np.arange(128)[:, None] < np.arange(128)[None, :]).astype(np.float32)
    iot = (np.arange(128)[:, None] + 128 * np.arange(4)[None, :]).astype(np.float32)
    trash = (cfg.TRASH + np.arange(128)[:, None] + 128 * np.arange(max(cfg.NFH, 1))[None, :]).astype(np.float32)
    return tabs_idx, mask, tri, iot, trash


def layer_inputs(cfg, l, xe, cb, first_half, P, li=0):
    tabs_idx, mask, tri, iot, trash = _const_tables(cfg)
    btab = np.ascontiguousarray(P["rel_bias"][:, tabs_idx].transpose(1, 0, 2, 3)).astype(np.float32)
    invc = np.zeros((128, 4, 16), np.float32)
    for g, w in enumerate((2, 4, 8, 16)):
        cnt = np.minimum(np.arange(16) + 1, w) if first_half else np.full(16, w)
        invc[:, g, :] = (1.0 / cnt.astype(np.float64)).astype(np.float32)[None, :]
    hvv = 0.0 if first_half else 1.0
    trash = (cfg.TRASH + np.arange(128)[:, None] + 128 * np.arange(TRC)[None, :]).astype(np.float32)
    trash = np.concatenate([trash, trash + cfg.NFH * 128], axis=1)
    m = {
        "xe": np.ascontiguousarray(xe, dtype=np.float32),
        "cT": _colform(cb),
        "ada_w": P["ada_w"][l], "ada_b": P["ada_b"][l][None, :],
        "n1c": _colform(P["norm1_g"][l]), "n2c": _colform(P["norm2_g"][l]),
        "n2a": np.ascontiguousarray(P["norm2_g"][l].reshape(128, 8)).astype(np.float32),
        "w_in": P["w_in"][l], "w_out": P["w_out"][l],
        "pool_w": np.ascontiguousarray(P["pool_w"][l].transpose(1, 0, 2)),
        "pscale": _colform(P["pool_scale"][l]),
        "gq": np.ascontiguousarray(np.tile(P["q_norm_g"][l], 2)[:, None]), "gk": np.ascontiguousarray(np.tile(P["k_norm_g"][l], 2)[:, None]),
        "btab": btab, "bmask": mask,
        "wr": np.ascontiguousarray(np.concatenate([P["router_group_w"][l], P["router_expert_w"][l]], axis=1)),
        "br": np.ascontiguousarray(np.tile(np.concatenate([P["router_group_b"][l], P["router_expert_b"][l]])[None, :], (128, 1))),
        "wg": P["moe_w_gate"][l].reshape(NEXP * 128, 8 * 512), "wu": P["moe_w_up"][l].reshape(NEXP * 128, 8 * 512), "wd": P["moe_w_down"][l].reshape(NEXP * 512, D),
        "hv": np.full((128, 1), hvv, np.float32), "nhv": np.full((128, 1), 1.0 - hvv, np.float32),
        "invc": invc, "tri": tri, "iot": iot, "trashi": trash,
        "thr": np.tile((cfg.CAP * (np.arange(16) + 1)).astype(np.float32)[None, :], (128, 1)),
        "wv": np.tile(np.arange(32, dtype=np.float32)[None, :], (128, 1)),
        "ev": np.tile(np.arange(32, dtype=np.float32)[None, :], (128, 1)),
        "iot8": (np.arange(128)[:, None] + 128 * np.arange(8)[None, :]).astype(np.float32),
    }
    return {(k + "_%d" % li if k in PERL else k): v for k, v in m.items()}


_NC_CACHE = {}


def kernel(**inputs):
    P = {k: np.asarray(v) for k, v in inputs.items()}
    x = P["x"]
    B, S, _ = x.shape
    cfg0 = Cfg(nkv=4, nfh=4, nm=32, cap=512)
    cfg1 = Cfg(nkv=4, nfh=0, nm=32, cap=512)
    if "nc" not in _NC_CACHE:
        _NC_CACHE["nc"] = build_program([cfg0, cfg1])
    nc = _NC_CACHE["nc"]
    half = S // 2
    in_maps = []
    for c in range(8):
        b, hf = c // 2, c % 2
        main = x[b, hf * half:(hf + 1) * half]
        halo = np.zeros((1024, D), np.float32) if hf == 0 else x[b, half - 1024:half]
        xe = np.concatenate([halo, main], axis=0)
        m = layer_inputs(cfg0, 0, xe, P["c"][b], hf == 0, P, li=0)
        m1 = layer_inputs(cfg1, 1, xe[:128], P["c"][b], hf == 0, P, li=1)
        m.update({k: v for k, v in m1.items() if k.endswith("_1")})
        in_maps.append(m)
    res = run_bass_kernel_spmd(nc, in_maps, core_ids=list(range(8)))
    out = np.empty_like(x)
    for c in range(8):
        b, hf = c // 2, c % 2
        out[b, hf * half:(hf + 1) * half] = res.results[c]["xo"]
    return out
```
